# Optimizing a Trainium2 kernel written in Bass

```python
import math
import jax
import jax.numpy as jnp
from jax import lax
import numpy as np

D_MODEL = 2048
BATCH = 1
SEQ = 8192
DEPTH = 1

NORM_EPS = 1e-6

ATT_HEAD_DIM = 128
ATT_HEADS = D_MODEL // ATT_HEAD_DIM
ATT_WIDTH = ATT_HEADS * ATT_HEAD_DIM
ATT_PATTERNS = ((128, 1), (512, 4), (2048, 16))
ATT_GROUPS = len(ATT_PATTERNS)
ALIBI_MAX_BIAS = 8.0

GDN_HEAD_DIM = 128
GDN_QK_HEADS = D_MODEL // 128
GDN_V_HEADS = 2 * GDN_QK_HEADS
GDN_QK_WIDTH = GDN_QK_HEADS * GDN_HEAD_DIM
GDN_V_WIDTH = GDN_V_HEADS * GDN_HEAD_DIM
GDN_CONV_CH = 2 * GDN_QK_WIDTH + GDN_V_WIDTH
GDN_CONV_WIDTH = 5
GDN_CHUNK = 64

MOE_GROUPS = 8
MOE_EXPERTS_PER_GROUP = 8
MOE_EXPERTS = MOE_GROUPS * MOE_EXPERTS_PER_GROUP
MOE_TOP_K = 2
MOE_FF = D_MODEL // 4
MOE_BLOCK = 128

IN_SPLITS = (ATT_GROUPS * ATT_WIDTH,
             ATT_GROUPS * ATT_WIDTH,
             ATT_WIDTH,
             GDN_QK_WIDTH,
             GDN_QK_WIDTH,
             GDN_V_WIDTH,
             GDN_V_WIDTH,
             2 * GDN_V_HEADS,
             2 * GDN_V_HEADS,
             D_MODEL,
             D_MODEL)
IN_WIDTH = sum(IN_SPLITS)
IN_OFFSETS = tuple(int(v) for v in np.cumsum(IN_SPLITS[:-1]))

kernel_name = 'hybrid_dilated_attn_gdn_hmoe_encoder'


def rms_norm(x, gain):
    xf = x.astype(jnp.float32)
    return xf * lax.rsqrt(jnp.mean(xf * xf, axis=-1, keepdims=True) + NORM_EPS) * gain.astype(jnp.float32)


def l2_norm(x):
    return x * lax.rsqrt(jnp.sum(x * x, axis=-1, keepdims=True) + NORM_EPS)


def alibi_slopes():
    n = ATT_GROUPS * ATT_HEADS
    s = jnp.exp2(-ALIBI_MAX_BIAS * jnp.arange(1, n + 1, dtype=jnp.float32) / n)
    return s.reshape(ATT_GROUPS, ATT_HEADS)


def dilated_window_attention(q, k, v, slopes, window, dilation):
    b, s, h, c = q.shape
    half = window // (2 * dilation)
    blk = half
    sub_len = s // dilation
    n_blk = -(-sub_len // blk)
    pad = n_blk * blk - sub_len

    def to_blocks(a):
        cc = a.shape[-1]
        a = a.reshape(b, sub_len, dilation, h, cc).transpose(0, 2, 1, 3, 4)
        a = jnp.pad(a, ((0, 0), (0, 0), (0, pad), (0, 0), (0, 0)))
        return a.reshape(b, dilation, n_blk, blk, h, cc)

    def from_blocks(a):
        cc = a.shape[-1]
        a = a.reshape(b, dilation, n_blk * blk, h, cc)[:, :, :sub_len]
        return a.transpose(0, 2, 1, 3, 4).reshape(b, s, h, cc)

    def with_neighbours(a):
        a = jnp.pad(a, ((0, 0), (0, 0), (1, 1), (0, 0), (0, 0), (0, 0)))
        return jnp.concatenate([a[:, :, :-2], a[:, :, 1:-1], a[:, :, 2:]], axis=3)

    qb = to_blocks(q)
    kb = with_neighbours(to_blocks(k))
    vb = with_neighbours(to_blocks(v))
    q_pos = jnp.arange(n_blk * blk).reshape(n_blk, blk)
    k_pos = (jnp.arange(n_blk)[:, None] - 1) * blk + jnp.arange(3 * blk)[None, :]
    rel = k_pos[:, None, :] - q_pos[:, :, None]
    valid = (jnp.abs(rel) <= half) & (k_pos[:, None, :] >= 0) & (k_pos[:, None, :] < sub_len)
    dist = (jnp.abs(rel) * dilation).astype(jnp.float32)
    scores = jnp.einsum('brnqhc,brnkhc->brhnqk', qb, kb) * (c ** -0.5)
    scores = scores - slopes[:, None, None, None] * dist
    scores = jnp.where(valid, scores, -1e30)
    m = jnp.max(scores, axis=-1, keepdims=True)
    p = jnp.exp(scores - m)
    l = jnp.sum(p, axis=-1, keepdims=True)
    out = jnp.einsum('brhnqk,brnkhc->brnqhc', p / l, vb)
    lse = jnp.transpose((m + jnp.log(l))[..., 0], (0, 1, 3, 4, 2))
    return from_blocks(out), from_blocks(lse[..., None])[..., 0]


def dilated_attention_mixer(q_all, k_all, v, q_gain, k_gain):
    b, s = v.shape[:2]
    q_all = rms_norm(q_all, q_gain[:, None, :])
    k_all = rms_norm(k_all, k_gain[:, None, :])
    v = v.astype(jnp.float32)
    slopes = alibi_slopes()
    outs, lses = [], []
    for g, (window, dilation) in enumerate(ATT_PATTERNS):
        o, lse = dilated_window_attention(q_all[:, :, g], k_all[:, :, g], v, slopes[g], window, dilation)
        outs.append(o)
        lses.append(lse)
    weights = jax.nn.softmax(jnp.stack(lses, axis=0), axis=0)
    out = jnp.sum(weights[..., None] * jnp.stack(outs, axis=0), axis=0)
    return out.reshape(b, s, ATT_WIDTH)


def centred_depthwise_conv(x, w):
    width, ch = w.shape
    return lax.conv_general_dilated(
        x, w[:, None, :].astype(x.dtype), window_strides=(1,),
        padding=[((width - 1) // 2, width // 2)],
        dimension_numbers=('NWC', 'WIO', 'NWC'), feature_group_count=ch)


def chunk_gated_delta_rule(q, k, v, g, beta):
    b, s, h, dk = k.shape
    dv = v.shape[-1]
    c = GDN_CHUNK
    n = s // c

    def chunked(t):
        t = t.reshape((b, n, c, h) + t.shape[3:])
        return jnp.moveaxis(t, 3, 1)

    q = chunked(q * (dk ** -0.5))
    k = chunked(k)
    v = chunked(v)
    g = jnp.cumsum(chunked(g), axis=-1)
    beta = chunked(beta)
    tri = jnp.tril(jnp.ones((c, c), dtype=bool))
    strict = jnp.tril(jnp.ones((c, c), dtype=bool), -1)
    diff = g[..., :, None] - g[..., None, :]
    decay = jnp.where(tri, jnp.exp(jnp.where(tri, diff, 0.0)), 0.0)
    k_beta = k * beta[..., None]
    lower = jnp.where(strict, jnp.einsum('bhnid,bhnjd->bhnij', k_beta, k) * decay, 0.0)
    eye = jnp.eye(c, dtype=jnp.float32)
    rhs = jnp.concatenate([v * beta[..., None], k_beta * jnp.exp(g)[..., None]], axis=-1)
    sol = lax.linalg.triangular_solve(jnp.broadcast_to(lower + eye, lower.shape), rhs,
                                      left_side=True, lower=True)
    u, w = sol[..., :dv], sol[..., dv:]
    intra = jnp.where(tri, jnp.einsum('bhnid,bhnjd->bhnij', q, k) * decay, 0.0)
    q_dec = q * jnp.exp(g)[..., None]
    k_tail = k * jnp.exp(g[..., -1:] - g)[..., None]
    chunk_decay = jnp.exp(g[..., -1])

    def step(state, inp):
        u_n, w_n, q_n, k_n, a_n, d_n = inp
        v_new = u_n - jnp.einsum('bhik,bhkv->bhiv', w_n, state)
        o_n = jnp.einsum('bhik,bhkv->bhiv', q_n, state) + jnp.einsum('bhij,bhjv->bhiv', a_n, v_new)
        state = state * d_n[..., None, None] + jnp.einsum('bhik,bhiv->bhkv', k_n, v_new)
        return state, o_n

    xs = tuple(jnp.moveaxis(t, 2, 0) for t in (u, w, q_dec, k_tail, intra, chunk_decay))
    state0 = jnp.zeros((b, h, dk, dv), jnp.float32)
    _, o = lax.scan(step, state0, xs)
    return jnp.transpose(o, (1, 0, 3, 2, 4)).reshape(b, s, h, dv)


def gated_deltanet_mixer(q, k, v, z, a, beta_logit, conv_w, a_log, dt_bias, norm_gain):
    b, s = q.shape[:2]
    f32 = jnp.float32
    qkv = jnp.concatenate([q, k, v], axis=-1).astype(f32)
    qkv = jax.nn.silu(centred_depthwise_conv(qkv, conv_w.astype(f32)))
    q, k, v = jnp.split(qkv, (GDN_QK_WIDTH, 2 * GDN_QK_WIDTH), axis=-1)
    rep = GDN_V_HEADS // GDN_QK_HEADS
    q = jnp.repeat(l2_norm(q.reshape(b, s, GDN_QK_HEADS, GDN_HEAD_DIM)), rep, axis=2)
    k = jnp.repeat(l2_norm(k.reshape(b, s, GDN_QK_HEADS, GDN_HEAD_DIM)), rep, axis=2)
    v = v.reshape(b, s, GDN_V_HEADS, GDN_HEAD_DIM)
    beta = jax.nn.sigmoid(beta_logit.astype(f32))
    g = -jnp.exp(a_log.astype(f32)) * jax.nn.softplus(a.astype(f32) + dt_bias.astype(f32))
    o_fwd = chunk_gated_delta_rule(q, k, v, g[:, :, 0], beta[:, :, 0])
    flip = lambda t: jnp.flip(t, axis=1)
    o_bwd = flip(chunk_gated_delta_rule(flip(q), flip(k), flip(v), flip(g[:, :, 1]), flip(beta[:, :, 1])))
    o = rms_norm(o_fwd + o_bwd, norm_gain) * jax.nn.silu(z.reshape(b, s, GDN_V_HEADS, GDN_HEAD_DIM).astype(f32))
    return o.reshape(b, s, GDN_V_WIDTH)


def hierarchical_moe(h, w_group_router, b_group_router, w_expert_router, b_expert_router, w_gate, w_up, w_down):
    b, s, d = h.shape
    t = b * s
    hf = h.reshape(t, d)
    group_prob = jax.nn.softmax((hf @ w_group_router).astype(jnp.float32) + b_group_router.astype(jnp.float32), axis=-1)
    group_w, group_id = lax.top_k(group_prob, 1)
    expert_logits = ((hf @ w_expert_router).astype(jnp.float32) + b_expert_router.astype(jnp.float32))
    expert_logits = expert_logits.reshape(t, MOE_GROUPS, MOE_EXPERTS_PER_GROUP)
    in_group = jnp.take_along_axis(expert_logits, group_id[:, :, None], axis=1)[:, 0]
    local_w, local_id = lax.top_k(jax.nn.softmax(in_group, axis=-1), MOE_TOP_K)
    local_w = local_w / jnp.sum(local_w, axis=-1, keepdims=True)
    expert_id = group_id * MOE_EXPERTS_PER_GROUP + local_id
    weight = group_w * local_w

    n_assign = t * MOE_TOP_K
    n_blocks = -(-n_assign // MOE_BLOCK) + MOE_EXPERTS
    n_slots = n_blocks * MOE_BLOCK
    flat_e = expert_id.reshape(n_assign)
    flat_w = weight.reshape(n_assign)
    flat_t = jnp.repeat(jnp.arange(t, dtype=jnp.int32), MOE_TOP_K)
    order = jnp.argsort(flat_e)
    se, st, sw = flat_e[order], flat_t[order], flat_w[order]
    counts = jax.ops.segment_sum(jnp.ones((n_assign,), jnp.int32), flat_e, num_segments=MOE_EXPERTS)
    starts = jnp.cumsum(counts) - counts
    padded = (counts + MOE_BLOCK - 1) // MOE_BLOCK * MOE_BLOCK
    padded_ends = jnp.cumsum(padded)
    padded_starts = padded_ends - padded
    dest = padded_starts[se] + jnp.arange(n_assign, dtype=jnp.int32) - starts[se]
    slot_tok = jnp.full((n_slots,), t, jnp.int32).at[dest].set(st)
    slot_w = jnp.zeros((n_slots,), jnp.float32).at[dest].set(sw)
    block_expert = jnp.clip(jnp.searchsorted(padded_ends, jnp.arange(n_blocks) * MOE_BLOCK, side='right'),
                            0, MOE_EXPERTS - 1)
    xs = jnp.concatenate([hf, jnp.zeros((1, d), hf.dtype)], axis=0)[slot_tok].reshape(n_blocks, MOE_BLOCK, d)

    def expert_block(args):
        xb, e = args
        return (jax.nn.silu(xb @ w_gate[e]) * (xb @ w_up[e])) @ w_down[e]

    y = lax.map(expert_block, (xs, block_expert)).reshape(n_slots, d)
    out = jax.ops.segment_sum(y * slot_w[:, None], slot_tok, num_segments=t + 1)[:t]
    return out.reshape(b, s, d)


def setup_inputs(seed: int = 0) -> dict:
    key = jax.random.key(seed)
    ks = jax.random.split(key, 20)
    f32 = jnp.float32
    L = DEPTH

    def normal(k, shape, fan_in):
        return jax.random.normal(k, shape, f32) * (fan_in ** -0.5)

    def gain(k, shape):
        return 1.0 + 0.02 * jax.random.normal(k, shape, f32)

    dt = jnp.exp(jax.random.uniform(ks[7], (L, 2, GDN_V_HEADS), f32, math.log(1e-3), math.log(1e-1)))
    return {
        'x': jax.random.normal(ks[0], (BATCH, SEQ, D_MODEL), f32),
        'norm1_gain': gain(ks[1], (L, D_MODEL)),
        'w_in': normal(ks[2], (L, D_MODEL, IN_WIDTH), D_MODEL),
        'q_norm_gain': gain(ks[3], (L, ATT_GROUPS, ATT_HEAD_DIM)),
        'k_norm_gain': gain(ks[4], (L, ATT_GROUPS, ATT_HEAD_DIM)),
        'gdn_conv_w': normal(ks[5], (L, GDN_CONV_WIDTH, GDN_CONV_CH), GDN_CONV_WIDTH),
        'gdn_a_log': jnp.log(jax.random.uniform(ks[6], (L, 2, GDN_V_HEADS), f32, 1.0, 16.0)),
        'gdn_dt_bias': dt + jnp.log(-jnp.expm1(-dt)),
        'gdn_norm_gain': gain(ks[8], (L, GDN_HEAD_DIM)),
        'w_branch_att': normal(ks[9], (L, ATT_WIDTH, D_MODEL), ATT_WIDTH),
        'w_branch_gdn': normal(ks[10], (L, GDN_V_WIDTH, D_MODEL), GDN_V_WIDTH),
        'w_out': normal(ks[11], (L, D_MODEL, D_MODEL), D_MODEL),
        'norm2_gain': gain(ks[12], (L, D_MODEL)),
        'w_group_router': normal(ks[13], (L, D_MODEL, MOE_GROUPS), D_MODEL),
        'b_group_router': 0.01 * jax.random.normal(ks[14], (L, MOE_GROUPS), f32),
        'w_expert_router': normal(ks[15], (L, D_MODEL, MOE_EXPERTS), D_MODEL),
        'b_expert_router': 0.01 * jax.random.normal(ks[16], (L, MOE_EXPERTS), f32),
        'w_gate': normal(ks[17], (L, MOE_EXPERTS, D_MODEL, MOE_FF), D_MODEL),
        'w_up': normal(ks[18], (L, MOE_EXPERTS, D_MODEL, MOE_FF), D_MODEL),
        'w_down': normal(ks[19], (L, MOE_EXPERTS, MOE_FF, D_MODEL), MOE_FF),
    }


def reference(x, norm1_gain, w_in, q_norm_gain, k_norm_gain, gdn_conv_w, gdn_a_log, gdn_dt_bias,
              gdn_norm_gain, w_branch_att, w_branch_gdn, w_out, norm2_gain, w_group_router,
              b_group_router, w_expert_router, b_expert_router, w_gate, w_up, w_down):
    dt = x.dtype
    b, s = x.shape[:2]
    for i in range(DEPTH):
        h = rms_norm(x, norm1_gain[i]).astype(dt)
        proj = h @ w_in[i]
        (q_att, k_att, v_att, q_gdn, k_gdn, v_gdn, z_gdn, a_gdn, b_gdn,
         gate_att, gate_gdn) = jnp.split(proj, IN_OFFSETS, axis=-1)
        y_att = dilated_attention_mixer(
            q_att.reshape(b, s, ATT_GROUPS, ATT_HEADS, ATT_HEAD_DIM),
            k_att.reshape(b, s, ATT_GROUPS, ATT_HEADS, ATT_HEAD_DIM),
            v_att.reshape(b, s, ATT_HEADS, ATT_HEAD_DIM),
            q_norm_gain[i], k_norm_gain[i]).astype(dt)
        y_gdn = gated_deltanet_mixer(
            q_gdn, k_gdn, v_gdn, z_gdn,
            a_gdn.reshape(b, s, 2, GDN_V_HEADS), b_gdn.reshape(b, s, 2, GDN_V_HEADS),
            gdn_conv_w[i], gdn_a_log[i], gdn_dt_bias[i], gdn_norm_gain[i]).astype(dt)
        merged = (jax.nn.sigmoid(gate_att) * (y_att @ w_branch_att[i])
                  + jax.nn.sigmoid(gate_gdn) * (y_gdn @ w_branch_gdn[i]))
        x = x + merged @ w_out[i]
        h2 = rms_norm(x, norm2_gain[i]).astype(dt)
        x = x + hierarchical_moe(h2, w_group_router[i], b_group_router[i], w_expert_router[i],
                                 b_expert_router[i], w_gate[i], w_up[i], w_down[i]).astype(dt)
    return x
```

```python
import contextlib
import numpy as np
import concourse.bass as bass
import concourse.mybir as mybir
from concourse.bass_utils import run_bass_kernel_spmd

F32 = mybir.dt.float32
BF16 = mybir.dt.bfloat16
AF = mybir.ActivationFunctionType
ALU = mybir.AluOpType
AX = mybir.AxisListType

D = 2048
S = 8192
NCORES = 8
NCH = D // 128
EPS = 1e-6

ENG_NAMES = ("pe", "dve", "act", "pool", "sp")


class Sched:
    def __init__(self, nc, stack):
        self.nc = nc
        self.stack = stack
        self.q = {k: [] for k in ENG_NAMES}
        self.sems = {}
        self.count = {}
        self.waited = {k: {} for k in ENG_NAMES}
        self.last_w = {}
        self.readers = {}

    def _sem(self, key):
        if key not in self.sems:
            name = "s%d" % len(self.sems)
            self.sems[key] = self.stack.enter_context(self.nc.semaphore(name))
            self.count[key] = 0
        return self.sems[key]

    def _deps(self, eng, reads, writes):
        deps = []
        for r in reads:
            if r in self.last_w:
                deps.append(self.last_w[r])
        for w in writes:
            if w in self.last_w:
                deps.append(self.last_w[w])
            deps.extend(self.readers.get(w, ()))
        waits = []
        for (key, val, src) in deps:
            if src == "pe" and eng == "pe":
                continue
            if self.waited[eng].get(key, 0) >= val:
                continue
            self.waited[eng][key] = val
            waits.append((key, val))
        return waits

    def _commit(self, tok, reads, writes):
        for w in writes:
            self.last_w[w] = tok
            self.readers[w] = []
        for r in reads:
            self.readers.setdefault(r, []).append(tok)

    def op(self, eng, fn, reads=(), writes=()):
        waits = self._deps(eng, reads, writes)
        key = ("e", eng)
        sem = self._sem(key)
        self.count[key] += 1
        tok = (key, self.count[key], eng)
        wl = [(self._sem(k), v) for (k, v) in waits]

        def emit(e, fn=fn, wl=wl, sem=sem):
            for (s, v) in wl:
                e.wait_ge(s, v)
            fn(e).then_inc(sem, 1)
        self.q[eng].append(emit)
        self._commit(tok, reads, writes)

    def dma(self, eng, out, in_, sem_key, reads=(), writes=(), **kw):
        waits = self._deps(eng, reads, writes)
        key = ("d", sem_key)
        sem = self._sem(key)
        self.count[key] += 16
        tok = (key, self.count[key], "dma")
        wl = [(self._sem(k), v) for (k, v) in waits]

        def emit(e, wl=wl, sem=sem):
            for (s, v) in wl:
                e.wait_ge(s, v)
            e.dma_start(out=out, in_=in_, **kw).then_inc(sem, 16)
        self.q[eng].append(emit)
        self._commit(tok, reads, writes)

    def final_wait(self, eng, resources):
        waits = self._deps(eng, resources, ())
        wl = [(self._sem(k), v) for (k, v) in waits]

        def emit(e, wl=wl):
            for (s, v) in wl:
                e.wait_ge(s, v)
        self.q[eng].append(emit)

    def run(self):
        nc = self.nc
        with nc.Block() as block:
            @block.tensor
            def _(e):
                for f in self.q["pe"]:
                    f(e)

            @block.vector
            def _(e):
                for f in self.q["dve"]:
                    f(e)

            @block.scalar
            def _(e):
                for f in self.q["act"]:
                    f(e)

            @block.gpsimd
            def _(e):
                for f in self.q["pool"]:
                    f(e)

            @block.sync
            def _(e):
                for f in self.q["sp"]:
                    f(e)


def emit_stage1(nc, xT, w, gain, pT, n_cols, n_tok, tok_blk=256):
    assert n_cols % 128 == 0 and n_tok % tok_blk == 0
    ncc = n_cols // 128
    ntt = n_tok // tok_blk
    xT_v = xT.rearrange("(c p) t -> p c t", p=128)
    w_v = w.rearrange("(c p) n -> p c n", p=128)
    with contextlib.ExitStack() as st:
        sb = lambda name, shape, dt: st.enter_context(nc.sbuf_tensor(name, shape, dt))
        ps = lambda name, shape, dt: st.enter_context(nc.psum_tensor(name, shape, dt))
        wb = sb("wb", [128, NCH, n_cols], BF16)
        g_sb = sb("g_sb", [128, NCH], F32)
        ones = sb("ones1", [128, 128], BF16)
        xf = [sb("xf%d" % i, [128, NCH, tok_blk], F32) for i in range(2)]
        xb = [sb("xb%d" % i, [128, NCH, tok_blk], BF16) for i in range(2)]
        xsq = [sb("xsq%d" % i, [128, NCH, tok_blk], BF16) for i in range(1)] * 2
        rstd = [sb("rstd%d" % i, [128, tok_blk], F32) for i in range(2)]
        ob = [sb("ob%d" % i, [128, tok_blk], BF16) for i in range(4)]
        ps_ss = ps("ps_ss", [128, tok_blk], F32)
        ps_o = [ps("ps_o%d" % i, [128, tok_blk], F32) for i in range(4)]
        sc = Sched(nc, st)

        sc.op("pool", lambda e: e.memset(ones[:, :], 1.0), writes=["ones"])
        sc.dma("sp", g_sb[:, :], gain[:, :], "g_sb", writes=["g_sb"])
        for c in range(NCH):
            sc.dma("pool", wb[:, c, :], w_v[:, c, :], "wb", writes=[("wb", c), "wb_all"])
        for c in range(NCH):
            sc.op("dve", lambda e, c=c: e.tensor_scalar(wb[:, c, :], wb[:, c, :], g_sb[:, c:c + 1], None, ALU.mult),
                  reads=[("wb", c), "wb_all", "g_sb"], writes=[("wb", c)])
        for t in range(ntt):
            b = t % 2
            tsl = slice(t * tok_blk, (t + 1) * tok_blk)
            sc.dma("sp", xf[b][:, :, :], xT_v[:, :, tsl], ("xf", b), writes=[("xf", b)])
            sc.op("act", lambda e, b=b: e.activation(out=xsq[b][:, :, :], in_=xf[b][:, :, :], func=AF.Square),
                  reads=[("xf", b)], writes=["xsq"])
            sc.op("dve", lambda e, b=b: e.tensor_copy(xb[b][:, :, :], xf[b][:, :, :]),
                  reads=[("xf", b)], writes=[("xb", b)])
            for c in range(NCH):
                sc.op("pe", lambda e, b=b, c=c: e.matmul(ps_ss[:, :], ones[:, :], xsq[b][:, c, :],
                                                         start=(c == 0), stop=(c == NCH - 1)),
                      reads=["xsq", "ones"], writes=["ps_ss"])
            sc.op("dve", lambda e, b=b: e.tensor_scalar(rstd[b][:, :], ps_ss[:, :], 1.0 / D, EPS, ALU.mult, ALU.add),
                  reads=["ps_ss"], writes=[("rstd", b)])
            sc.op("act", lambda e, b=b: e.activation(out=rstd[b][:, :], in_=rstd[b][:, :], func=AF.Sqrt),
                  reads=[("rstd", b)], writes=[("rstd", b)])
            sc.op("dve", lambda e, b=b: e.reciprocal(rstd[b][:, :], rstd[b][:, :]),
                  reads=[("rstd", b)], writes=[("rstd", b)])
            for j in range(ncc):
                k = (t * ncc + j) % 4
                for c in range(NCH):
                    sc.op("pe", lambda e, b=b, c=c, j=j, k=k: e.matmul(
                        ps_o[k][:, :], wb[:, c, j * 128:(j + 1) * 128], xb[b][:, c, :],
                        start=(c == 0), stop=(c == NCH - 1)),
                        reads=[("xb", b), ("wb", c)], writes=[("ps_o", k)])
                sc.op("dve", lambda e, b=b, k=k: e.tensor_tensor(ob[k][:, :], ps_o[k][:, :], rstd[b][:, :], ALU.mult),
                      reads=[("ps_o", k), ("rstd", b)], writes=[("ob", k)])
                sc.dma("sp", pT[j * 128:(j + 1) * 128, tsl], ob[k][:, :], ("ob", k),
                       reads=[("ob", k)], writes=["pT_out"])
        sc.final_wait("sp", [("ob", k) for k in range(4)] + ["pT_out"])
        for k in range(4):
            key = ("d", ("ob", k))
            if key in sc.sems:
                sc.q["sp"].append(lambda e, s=sc.sems[key], v=sc.count[key]: e.wait_ge(s, v))
        sc.run()


def build_stage1(n_cols, n_tok, tok_blk=256):
    nc = bass.Bass("TRN2", target_bir_lowering=False)
    xT = nc.dram_tensor("xT", [D, n_tok], F32, kind="ExternalInput").ap()
    w = nc.dram_tensor("w", [D, n_cols], F32, kind="ExternalInput").ap()
    gain = nc.dram_tensor("gain", [128, NCH], F32, kind="ExternalInput").ap()
    pT = nc.dram_tensor("pT", [n_cols, n_tok], BF16, kind="ExternalOutput").ap()
    emit_stage1(nc, xT, w, gain, pT, n_cols, n_tok, tok_blk)
    return nc


ATT_PATTERNS = ((128, 1), (512, 4), (2048, 16))
HALF = 64


def alibi_slopes_np():
    n = 48
    return np.exp2(-8.0 * np.arange(1, n + 1, dtype=np.float64) / n).reshape(3, 16)


def att_bias_consts(head_slot):
    out = np.zeros((3, 128, 2, 128), np.float32)
    i = np.arange(128)[:, None]
    j = np.arange(128)[None, :]
    sl = alibi_slopes_np()
    for g, (_, d) in enumerate(ATT_PATTERNS):
        for h, off in enumerate((-64, 64)):
            rel = i - j + off
            b = -sl[g, head_slot] * np.abs(rel) * d
            out[g, :, h, :] = np.where(np.abs(rel) <= HALF, b, -1e30)
    return out


def emit_att(nc, q_of, k_of, vT, qg, kg, bias, ident_d, yT, n_tok, tag):
    QT = 512
    PIECE = min(2048, n_tok)
    DMAX = 16
    with contextlib.ExitStack() as st:
        sb = lambda name, shape, dt: st.enter_context(nc.sbuf_tensor(name + tag, shape, dt))
        ps = lambda name, shape, dt: st.enter_context(nc.psum_tensor(name + tag, shape, dt))
        sc = Sched(nc, st)
        ones = sb("a_ones", [128, 128], BF16)
        ident = sb("a_ident", [128, 128], BF16)
        g_q = sb("a_gq", [128, 3], F32)
        g_k = sb("a_gk", [128, 3], F32)
        gmax = sb("a_gmax", [128, 8], F32)
        gabs = sb("a_gabs", [128, 8], BF16)
        gT = sb("a_gT", [8, 128], BF16)
        gred = sb("a_gred", [8, 2], BF16)
        negB = sb("a_negB", [128, 1], F32)
        bias_sb = sb("a_bias", [128, 3, 2, 128], F32)
        raw = sb("a_raw", [128, PIECE], BF16)
        sq = sb("a_sq", [128, PIECE], BF16)
        rs = sb("a_rs", [128, PIECE], F32)
        vT_sb = sb("a_vT", [128, n_tok], BF16)
        accO = sb("a_accO", [128, n_tok], F32)
        accL = sb("a_accL", [128, n_tok], F32)
        yb = sb("a_yb", [128, PIECE], BF16)
        qn = sb("a_qn", [128, n_tok], BF16)
        kn = sb("a_kn", [128, n_tok + 128 * DMAX], BF16)
        vg = sb("a_vg", [128, n_tok + 128 * DMAX], BF16)
        sS = sb("a_sS", [128, 4, 2, 128], F32)
        pP = sb("a_pP", [128, 4, 2, 128], BF16)
        ps_n = ps("a_psn", [128, 512], F32)
        ps_t = ps("a_pst", [128, 128], BF16)
        ps_s = ps("a_pss", [128, 4, 2, 128], F32)
        ps_O = ps("a_psO", [128, QT], F32)
        ps_L = ps("a_psL", [128, QT], F32)
        ps_g = ps("a_psg", [128, 128], F32)

        sc.op("pool", lambda e: e.memset(ones[:, :], 1.0), writes=["ones"])
        sc.dma("sp", ident[:, :], ident_d[:, :], "c_ident", writes=["ident"])
        sc.dma("sp", g_q[:, :], qg[:, :], "c_gq", writes=["g_q"])
        sc.dma("sp", g_k[:, :], kg[:, :], "c_gk", writes=["g_k"])
        sc.dma("sp", bias_sb[:, :, :, :], bias.rearrange("g p h j -> p g h j"), "c_bias", writes=["bias"])
        sc.dma("sp", vT_sb[:, :], vT, "c_v", writes=["vT"])

        def bcast_absmax(gain_sb, col, gtag):
            sc.op("dve", lambda e: e.tensor_scalar(gmax[:, 0:3], gain_sb[:, :], -1.0, None, ALU.mult),
                  reads=[gtag, "gmax"], writes=["gmax"])
            sc.op("dve", lambda e: e.tensor_tensor(gmax[:, 0:3], gmax[:, 0:3], gain_sb[:, :], ALU.max),
                  reads=[gtag, "gmax"], writes=["gmax"])
            sc.op("dve", lambda e: e.reduce_max(gmax[:, 4:5], gmax[:, 0:3], AX.X), reads=["gmax"], writes=["gmax"])
            sc.op("dve", lambda e: e.tensor_copy(gabs[:, 0:1], gmax[:, 4:5]), reads=["gmax"], writes=["gabs"])
            sc.op("pe", lambda e: e.transpose(ps_t[0:1, :], gabs[:, 0:1], ident[:, :]), reads=["gabs", "ident"], writes=["ps_t"])
            sc.op("dve", lambda e: e.tensor_copy(gT[0:1, :], ps_t[0:1, :]), reads=["ps_t"], writes=["gT"])
            sc.op("dve", lambda e: e.reduce_max(gred[0:1, 0:1], gT[0:1, :], AX.X), reads=["gT"], writes=["gred"])
            sc.op("pe", lambda e: e.matmul(ps_g[:, col:col + 1], ones[0:1, :], gred[0:1, 0:1], start=True, stop=True),
                  reads=["gred", "ones"], writes=["ps_g"])
            sc.op("dve", lambda e: e.tensor_copy(gmax[:, 5 + col:6 + col], ps_g[:, col:col + 1]), reads=["ps_g", "gmax"], writes=["gmax"])
        sc.op("pool", lambda e: e.memset(gmax[:, :], 0.0), writes=["gmax"])
        bcast_absmax(g_q, 0, "g_q")
        bcast_absmax(g_k, 1, "g_k")
        sc.op("dve", lambda e: e.scalar_tensor_tensor(negB[:, :], gmax[:, 5:6], -1.02 * float(np.sqrt(128.0)),
                                                      gmax[:, 6:7], ALU.mult, ALU.mult),
              reads=["gmax"], writes=["negB"])

        def norm_into(src, gain_sb, g, dst, dst_is_k, extra_scale):
            d = ATT_PATTERNS[g][1]
            L = n_tok // d
            name = "kn" if dst_is_k else "qn"
            if dst_is_k:
                sc.op("pool", lambda e: e.memset(dst[:, :], 0.0), writes=[name])
            for T0 in range(0, n_tok, PIECE):
                sc.dma("sp", raw[:, :], src[:, T0:T0 + PIECE], "a_raw", writes=["raw"])
                sc.op("act", lambda e: e.activation(out=sq[:, :], in_=raw[:, :], func=AF.Square), reads=["raw"], writes=["sq"])
                for t0 in range(0, PIECE, 512):
                    sc.op("pe", lambda e, t0=t0: e.matmul(ps_n[:, :], ones[:, :], sq[:, t0:t0 + 512], start=True, stop=True),
                          reads=["sq", "ones"], writes=["ps_n"])
                    sc.op("dve", lambda e, t0=t0: e.tensor_scalar(rs[:, t0:t0 + 512], ps_n[:, :], 1.0 / 128, EPS, ALU.mult, ALU.add),
                          reads=["ps_n"], writes=["rs"])
                sc.op("act", lambda e: e.activation(out=rs[:, :], in_=rs[:, :], func=AF.Sqrt), reads=["rs"], writes=["rs"])
                sc.op("dve", lambda e: e.reciprocal(rs[:, :], rs[:, :]), reads=["rs"], writes=["rs"])
                if extra_scale != 1.0:
                    sc.op("dve", lambda e: e.tensor_scalar(rs[:, :], rs[:, :], extra_scale, None, ALU.mult), reads=["rs"], writes=["rs"])
                l0, l1 = T0 // d, (T0 + PIECE) // d
                if dst_is_k:
                    out_ap = dst[:, 0:d * (L + 128)].rearrange("p (r l) -> p r l", r=d)[:, :, HALF + l0:HALF + l1]
                else:
                    out_ap = dst[:, 0:n_tok].rearrange("p (r l) -> p r l", r=d)[:, :, l0:l1]
                in_raw = raw[:, :].rearrange("p (l r) -> p r l", r=d)
                in_rs = rs[:, :].rearrange("p (l r) -> p r l", r=d)
                sc.op("dve", lambda e, out_ap=out_ap, in_raw=in_raw, in_rs=in_rs: e.scalar_tensor_tensor(
                    out_ap, in_raw, gain_sb[:, g:g + 1], in_rs, ALU.mult, ALU.mult),
                    reads=["raw", "rs", "g_q", "g_k", name], writes=[name])

        first = True
        for g in range(3):
            d = ATT_PATTERNS[g][1]
            L = n_tok // d
            nb = L // 128 + 1
            Lp = L + 128
            norm_into(q_of(g), g_q, g, qn, False, float(128.0 ** -0.5))
            norm_into(k_of(g), g_k, g, kn, True, 1.0)
            sc.op("pool", lambda e: e.memset(vg[:, :], 0.0), writes=["vg"])
            v_res = vT_sb[:, :].rearrange("p (l r) -> p r l", r=d)
            for r in range(d):
                for b in range(nb):
                    lo, hi = max(128 * b - HALF, 0), min(128 * b + HALF, L)
                    p0 = lo - (128 * b - HALF)
                    n = hi - lo
                    col = (r * nb + b) * 128
                    sc.op("pe", lambda e, vr=v_res, r=r, lo=lo, hi=hi, n=n: e.transpose(ps_t[0:n, :], vr[:, r, lo:hi], ident[:, :]),
                          reads=["vT", "ident"], writes=["ps_t"])
                    sc.op("act", lambda e, p0=p0, n=n, col=col: e.copy(vg[p0:p0 + n, col:col + 128], ps_t[0:n, :]),
                          reads=["ps_t", "vg"], writes=["vg"])
            accO_v = accO[:, :].rearrange("p (l r) -> p r l", r=d)
            accL_v = accL[:, :].rearrange("p (l r) -> p r l", r=d)
            for r in range(d):
                for q0 in range(0, L, QT):
                    nt = min(QT, L - q0) // 128
                    for a in range(nt):
                        qa = q0 // 128 + a
                        for h in range(2):
                            kcol = r * Lp + 128 * (qa + h)
                            sc.op("pe", lambda e, a=a, h=h, kcol=kcol, r=r, qa=qa, L=L: e.matmul(
                                ps_s[:, a, h, :], kn[:, kcol:kcol + 128], qn[:, r * L + 128 * qa:r * L + 128 * qa + 128],
                                start=True, stop=True), reads=["kn", "qn"], writes=["ps_s"])
                    sc.op("dve", lambda e, g=g, nt=nt: e.tensor_tensor(
                        sS[:, 0:nt, :, :], ps_s[:, 0:nt, :, :],
                        bias_sb[:, g, :, :].unsqueeze(1).to_broadcast([128, nt, 2, 128]),
                        ALU.add), reads=["ps_s", "bias"], writes=["sS"])
                    sc.op("act", lambda e, nt=nt: e.activation(out=pP[:, 0:nt, :, :], in_=sS[:, 0:nt, :, :], func=AF.Exp, bias=negB[:, 0:1]),
                          reads=["sS", "negB"], writes=["pP"])
                    if q0 == 0:
                        sc.op("pool", lambda e: e.memset(pP[0:64, 0, 0, :], 0.0), reads=["pP"], writes=["pP"])
                    if q0 + 128 * nt == L:
                        sc.op("pool", lambda e, nt=nt: e.memset(pP[64:128, nt - 1, 1, :], 0.0), reads=["pP"], writes=["pP"])
                    for a in range(nt):
                        qa = q0 // 128 + a
                        for h in range(2):
                            col = (r * nb + qa + h) * 128
                            sc.op("pe", lambda e, a=a, h=h, col=col: e.matmul(
                                ps_O[:, 128 * a:128 * a + 128], vg[:, col:col + 128], pP[:, a, h, :],
                                start=(h == 0), stop=(h == 1)), reads=["vg", "pP"], writes=["ps_O"])
                            sc.op("pe", lambda e, a=a, h=h: e.matmul(
                                ps_L[:, 128 * a:128 * a + 128], ones[:, :], pP[:, a, h, :],
                                start=(h == 0), stop=(h == 1)), reads=["ones", "pP"], writes=["ps_L"])
                    n = 128 * nt
                    if first:
                        sc.op("dve", lambda e, av=accO_v, r=r, q0=q0, n=n: e.tensor_copy(av[:, r, q0:q0 + n], ps_O[:, 0:n]),
                              reads=["ps_O"], writes=["accO"])
                        sc.op("act", lambda e, av=accL_v, r=r, q0=q0, n=n: e.copy(av[:, r, q0:q0 + n], ps_L[:, 0:n]),
                              reads=["ps_L"], writes=["accL"])
                    else:
                        sc.op("dve", lambda e, av=accO_v, r=r, q0=q0, n=n: e.tensor_tensor(av[:, r, q0:q0 + n], av[:, r, q0:q0 + n], ps_O[:, 0:n], ALU.add),
                              reads=["ps_O", "accO"], writes=["accO"])
                        sc.op("dve", lambda e, av=accL_v, r=r, q0=q0, n=n: e.tensor_tensor(av[:, r, q0:q0 + n], av[:, r, q0:q0 + n], ps_L[:, 0:n], ALU.add),
                              reads=["ps_L", "accL"], writes=["accL"])
            first = False
        sc.op("dve", lambda e: e.reciprocal(accL[:, :], accL[:, :]), reads=["accL"], writes=["accL"])
        for T0 in range(0, n_tok, PIECE):
            sc.op("dve", lambda e, T0=T0: e.tensor_tensor(yb[:, :], accO[:, T0:T0 + PIECE], accL[:, T0:T0 + PIECE], ALU.mult),
                  reads=["accO", "accL", "yb"], writes=["yb"])
            sc.dma("sp", yT[:, T0:T0 + PIECE], yb[:, :], "a_yb", reads=["yb"], writes=["y_out"])
        sc.final_wait("sp", ["y_out", "yb"])
        sc.run()


def build_att(n_tok):
    nc = bass.Bass("TRN2", target_bir_lowering=False)
    qT = nc.dram_tensor("qT", [3, 128, n_tok], BF16, kind="ExternalInput").ap()
    kT = nc.dram_tensor("kT", [3, 128, n_tok], BF16, kind="ExternalInput").ap()
    vT = nc.dram_tensor("vT", [128, n_tok], BF16, kind="ExternalInput").ap()
    qg = nc.dram_tensor("qg", [128, 3], F32, kind="ExternalInput").ap()
    kg = nc.dram_tensor("kg", [128, 3], F32, kind="ExternalInput").ap()
    bias = nc.dram_tensor("bias", [3, 128, 2, 128], F32, kind="ExternalInput").ap()
    ident_d = nc.dram_tensor("ident", [128, 128], BF16, kind="ExternalInput").ap()
    yT = nc.dram_tensor("yT", [128, n_tok], BF16, kind="ExternalOutput").ap()
    emit_att(nc, lambda g: qT[g], lambda g: kT[g], vT[:, :], qg, kg, bias, ident_d, yT, n_tok, "")
    return nc


def gdn_consts():
    i = np.arange(128)[:, None]; j = np.arange(128)[None, :]
    same = (i // 64) == (j // 64)
    c = {}
    c["ident_f"] = np.eye(128, dtype=np.float32)
    c["ones_f"] = np.ones((128, 128), np.float32)
    c["mstrict"] = np.stack([(same & (i > j)), (same & (i < j))]).astype(np.float32)
    c["mincl"] = np.stack([(same & (i >= j)), (same & (i <= j))]).astype(np.float32)
    r = i; m = j
    c["cum"] = np.stack([(same & (r <= m)), (same & (r >= m))]).astype(np.float32)
    lastf = np.where(np.arange(128) < 64, 63, 127)[None, :]; lastb = np.where(np.arange(128) < 64, 0, 64)[None, :]
    c["sel"] = np.stack([(r == lastf), (r == lastb)]).astype(np.float32)
    selh = np.zeros((2, 2, 128, 128), np.float32)
    for dr in range(2):
        for hf in range(2):
            selh[dr, hf, (63 + 64 * hf) if dr == 0 else 64 * hf, :] = 1.0
    c["selh"] = selh
    return c


GDN_IN = (("qraw", [2, 128, None], BF16), ("kraw", [2, 128, None], BF16), ("vraw", [4, 128, None], BF16), ("zraw", [4, 128, None], BF16),
          ("abr", [16, None], BF16))
GDN_PAR = (("cw", [128, 8, 5], F32), ("alog", [128, 8], F32), ("dtb", [128, 8], F32), ("ng", [128, 128], F32),
           ("ident_f", [128, 128], F32), ("ident_b", [128, 128], BF16), ("ones_f", [128, 128], F32),
           ("mstrict", [2, 128, 128], F32), ("mincl", [2, 128, 128], F32), ("cum", [2, 128, 128], F32), ("sel", [2, 128, 128], F32),
           ("selh", [2, 2, 128, 128], F32))


def build_gdn(n_tok):
    nc = bass.Bass("TRN2", target_bir_lowering=False)
    aps = {}
    for (name, shape, dt) in GDN_IN + GDN_PAR:
        aps[name] = nc.dram_tensor(name, [n_tok if v is None else v for v in shape], dt, kind="ExternalInput").ap()
    yg = nc.dram_tensor("yg", [n_tok, 512], BF16, kind="ExternalOutput").ap()
    ofw = nc.dram_tensor("ofw_scr", [4, n_tok, 128], F32).ap()
    emit_gdn(nc, aps, yg, ofw, n_tok)
    return nc


def emit_gdn(nc, aps, yg, ofw, n_tok):
    TB = 512; NP = 4
    nblk = n_tok // TB
    qraw, kraw, vraw, zraw, abr = (aps[k] for k in ("qraw", "kraw", "vraw", "zraw", "abr"))
    cw, alog, dtb, ngd = (aps[k] for k in ("cw", "alog", "dtb", "ng"))
    c_ident_f, c_ident_b, c_ones_f = aps["ident_f"], aps["ident_b"], aps["ones_f"]
    c_mstrict, c_mincl, c_cum, c_sel, c_selh = (aps[k] for k in ("mstrict", "mincl", "cum", "sel", "selh"))
    with contextlib.ExitStack() as st:
        sb = lambda name, shape, dt: st.enter_context(nc.sbuf_tensor(name, shape, dt))
        ps = lambda name, shape, dt: st.enter_context(nc.psum_tensor(name, shape, dt))
        sc = Sched(nc, st)
        V = lambda fn, r, w: sc.op("dve", fn, reads=r, writes=w)
        A = lambda fn, r, w: sc.op("act", fn, reads=r, writes=w)
        G = lambda fn, r, w: sc.op("pool", fn, reads=r, writes=w)
        T = lambda fn, r, w: sc.op("pe", fn, reads=r, writes=w)
        identf = sb("identf", [128, 128], F32); identb = sb("identb", [128, 128], BF16); onesf = sb("onesf", [128, 128], F32)
        mstr = sb("mstr", [128, 2, 128], F32); minc = sb("minc", [128, 2, 128], F32)
        cum = sb("cum_sb", [128, 2, 128], F32); sel = sb("sel_sb", [128, 2, 128], F32); selh = sb("selh_sb", [128, 4, 128], F32)
        cw_sb = sb("cw_sb", [128, 8, 5], F32); nega = sb("nega", [128, 8], F32); dtb_sb = sb("dtb_sb", [128, 8], F32)
        ng_sb = sb("ng_sb", [128, 128], F32)
        for (t_, d_, nm) in ((identf, c_ident_f, "identf"), (identb, c_ident_b, "identb"), (onesf, c_ones_f, "onesf"),
                             (nega, alog, "nega"), (dtb_sb, dtb, "dtb"), (ng_sb, ngd, "ng"), (cw_sb, cw, "cw")):
            sc.dma("sp", t_[:], d_[:] if len(d_.shape) == 2 else d_[:, :, :], "c_" + nm, writes=[nm])
        sc.dma("sp", mstr[:, :, :], c_mstrict.rearrange("r p j -> p r j"), "c_mstr", writes=["mstr"])
        sc.dma("sp", minc[:, :, :], c_mincl.rearrange("r p j -> p r j"), "c_minc", writes=["minc"])
        sc.dma("sp", cum[:, :, :], c_cum.rearrange("r p j -> p r j"), "c_cum", writes=["cum"])
        sc.dma("sp", sel[:, :, :], c_sel.rearrange("r p j -> p r j"), "c_sel", writes=["sel"])
        sc.dma("sp", selh[:, :, :], c_selh.rearrange("r h p j -> p (r h) j"), "c_selh", writes=["selh"])
        A(lambda e: e.activation(out=nega[:, :], in_=nega[:, :], func=AF.Exp), ["nega"], ["nega"])
        V(lambda e: e.tensor_scalar(nega[:, :], nega[:, :], -1.0, None, ALU.mult), ["nega"], ["nega"])

        raw = sb("raw_g", [128, TB + 4], BF16); rawf = sb("rawf", [128, TB + 4], F32)
        acc = sb("acc_g", [128, TB], F32); sil = sb("sil_g", [128, TB], F32); sq = sb("sq_g", [128, TB], F32); rn = sb("rn_g", [128, TB], F32)
        fm = [sb("fm%d" % i, [128, TB], BF16) for i in range(8)]
        tm = [sb("tm%d" % i, [128, NP, 128], BF16) for i in range(8)]
        zt = [sb("zt%d" % i, [128, NP, 128], F32) for i in range(4)]
        KK = [sb("KK%d" % i, [128, NP, 128], F32) for i in range(2)]; QK = [sb("QK%d" % i, [128, NP, 128], F32) for i in range(2)]
        ab_fm = sb("ab_fm", [16, TB], BF16); ab_tm = sb("ab_tm", [128, NP, 16], F32)
        gt = {n: sb("gt_" + n, [128, NP, 4], F32) for n in ("x", "nx", "ax", "e", "l", "g", "beta", "nbeta", "G", "eG", "gl", "tail", "bG", "d0", "d1")}
        diagG = sb("diagG", [128, NP, 128], F32); dec = sb("dec", [128, NP, 128], F32); t1 = sb("t1", [128, NP, 128], F32)
        XA = [sb("XA%d" % i, [128, NP, 384], F32) for i in range(2)]; Bb = [sb("Bb%d" % i, [128, NP, 128], F32) for i in range(2)]
        intra = sb("intra", [128, NP, 128], BF16); qd = sb("qd", [128, NP, 128], BF16)
        scan = [[{n: sb("sc%d_%d_%s" % (bb, vh, n), [128, NP, 128], BF16) for n in ("u", "wT", "qdT", "inT", "kt")} for vh in range(4)] for bb in range(2)]
        dS = [[sb("dS%d_%d" % (bb, hf), [128, NP, 4], F32) for hf in range(2)] for bb in range(2)]
        oblk = [sb("oblk%d" % vh, [128, NP, 128], F32) for vh in range(4)]
        ofl = sb("ofl", [128, NP, 128], F32); junk = sb("junk", [128, 128], F32); ss = sb("ss_g", [128, NP], F32); yb = sb("yb", [128, NP, 128], BF16)
        Sst = [sb("S%d" % vh, [128, 128], F32) for vh in range(4)]; Sbf = [sb("Sbf%d" % vh, [128, 128], BF16) for vh in range(4)]
        vnew = [sb("vnew%d" % vh, [128, 128], BF16) for vh in range(4)]
        psM = ps("psM", [128, NP, 128], F32); psXA = ps("psXA", [128, 2, 512], F32); psA = ps("psA", [128, NP, 128], F32); psB = ps("psB", [128, NP, 128], F32)
        psT = ps("psT", [128, NP, 128], BF16)
        psg = psB[:, 0, :]
        pss = [ps("pss%d" % i, [128, 3, 128], F32) for i in range(2)]
        bc = lambda ap: ap.unsqueeze(2).to_broadcast([128, NP, 128])
        bcp = lambda ap: ap.unsqueeze(1).to_broadcast([128, NP, 128])

        def prep_block(dr, blk, bb):
            t0 = blk * TB
            lo, hi = max(t0 - 2, 0), min(t0 + TB + 2, n_tok)
            srcs = [(qraw, 0), (qraw, 1), (kraw, 0), (kraw, 1), (vraw, 0), (vraw, 1), (vraw, 2), (vraw, 3)]
            for ti, (src, idx) in enumerate(srcs):
                G(lambda e: e.memset(raw[:, :], 0.0), [], ["raw"])
                sc.dma("sp", raw[:, lo - (t0 - 2):hi - (t0 - 2)], src[idx, :, lo:hi], "raw_g", writes=["raw"])
                V(lambda e: e.tensor_copy(rawf[:, :], raw[:, :]), ["raw"], ["rawf"])
                V(lambda e, ti=ti: e.tensor_scalar(acc[:, :], rawf[:, 0:TB], cw_sb[:, ti, 0:1], None, ALU.mult), ["rawf", "cw"], ["acc"])
                for j in range(1, 5):
                    V(lambda e, ti=ti, j=j: e.scalar_tensor_tensor(acc[:, :], rawf[:, j:j + TB], cw_sb[:, ti, j:j + 1], acc[:, :], ALU.mult, ALU.add),
                      ["rawf", "cw", "acc"], ["acc"])
                if ti >= 4:
                    A(lambda e, ti=ti: e.activation(out=fm[ti][:, :], in_=acc[:, :], func=AF.Silu), ["acc"], [("fm", ti)])
                else:
                    A(lambda e: e.activation(out=sil[:, :], in_=acc[:, :], func=AF.Silu), ["acc"], ["sil"])
                    V(lambda e: e.tensor_tensor(sq[:, :], sil[:, :], sil[:, :], ALU.mult), ["sil"], ["sq"])
                    T(lambda e: e.matmul(psXA[:, 0, :], onesf[:, :], sq[:, :], start=True, stop=True), ["sq", "onesf"], ["psXA"])
                    V(lambda e: e.tensor_scalar(rn[:, :], psXA[:, 0, :], EPS, None, ALU.add), ["psXA"], ["rn"])
                    A(lambda e: e.activation(out=rn[:, :], in_=rn[:, :], func=AF.Sqrt), ["rn"], ["rn"])
                    V(lambda e: e.reciprocal(rn[:, :], rn[:, :]), ["rn"], ["rn"])
                    scl = float(128.0 ** -0.5) if ti < 2 else 1.0
                    V(lambda e, ti=ti, scl=scl: e.scalar_tensor_tensor(fm[ti][:, :], sil[:, :], scl, rn[:, :], ALU.mult, ALU.mult), ["sil", "rn"], [("fm", ti)])
                for p in range(NP):
                    T(lambda e, ti=ti, p=p: e.transpose(psT[:, p, :], fm[ti][:, 128 * p:128 * p + 128], identb[:, :]), [("fm", ti), "identb"], ["psT"])
                A(lambda e, ti=ti: e.copy(tm[ti][:, :, :], psT[:, :, :]), ["psT"], [("tm", ti)])
                yield
            sc.dma("sp", ab_fm[:, :], abr[:, t0:t0 + TB], "ab_fm", writes=["ab_fm"])
            for p in range(NP):
                T(lambda e, p=p: e.transpose(psT[:, p, 0:16], ab_fm[:, 128 * p:128 * p + 128], identb[0:16, 0:16]), ["ab_fm", "identb"], ["psT"])
            V(lambda e: e.tensor_copy(ab_tm[:, :, :], psT[:, :, 0:16]), ["psT"], ["ab_tm"])
            g_ = gt
            row = lambda t_, dr=dr: t_[:, dr * 4:dr * 4 + 4].unsqueeze(1).to_broadcast([128, NP, 4])
            V(lambda e: e.tensor_tensor(g_["x"][:], ab_tm[:, :, dr * 4:dr * 4 + 4], row(dtb_sb), ALU.add), ["ab_tm", "dtb"], ["g_x"])
            V(lambda e: e.tensor_scalar(g_["nx"][:], g_["x"][:], -1.0, None, ALU.mult), ["g_x"], ["g_nx"])
            V(lambda e: e.tensor_tensor(g_["ax"][:], g_["x"][:], g_["nx"][:], ALU.max), ["g_x", "g_nx"], ["g_ax"])
            A(lambda e: e.activation(out=g_["e"][:], in_=g_["ax"][:], func=AF.Exp, scale=-1.0), ["g_ax"], ["g_e"])
            V(lambda e: e.tensor_scalar(g_["e"][:], g_["e"][:], 1.0, None, ALU.add), ["g_e"], ["g_e"])
            A(lambda e: e.activation(out=g_["l"][:], in_=g_["e"][:], func=AF.Ln), ["g_e"], ["g_l"])
            V(lambda e: e.scalar_tensor_tensor(g_["g"][:], g_["x"][:], 0.0, g_["l"][:], ALU.max, ALU.add), ["g_x", "g_l"], ["g_g"])
            V(lambda e: e.tensor_tensor(g_["g"][:], g_["g"][:], row(nega), ALU.mult), ["g_g", "nega"], ["g_g"])
            A(lambda e: e.activation(out=g_["beta"][:], in_=ab_tm[:, :, 8 + dr * 4:12 + dr * 4], func=AF.Sigmoid), ["ab_tm"], ["g_beta"])
            V(lambda e: e.tensor_scalar(g_["nbeta"][:], g_["beta"][:], -1.0, None, ALU.mult), ["g_beta"], ["g_nbeta"])
            T(lambda e: e.matmul(psg[:, 0:16], cum[:, dr, :], g_["g"][:].rearrange("p a b -> p (a b)"), start=True, stop=True), ["g_g", "cum"], [("psB", 0)])
            V(lambda e: e.tensor_copy(g_["G"][:].rearrange("p a b -> p (a b)"), psg[:, 0:16]), [("psB", 0)], ["g_G"])
            A(lambda e: e.activation(out=g_["eG"][:], in_=g_["G"][:], func=AF.Exp), ["g_G"], ["g_eG"])
            T(lambda e: e.matmul(psg[:, 16:32], sel[:, dr, :], g_["G"][:].rearrange("p a b -> p (a b)"), start=True, stop=True), ["g_G", "sel"], [("psB", 0)])
            V(lambda e: e.tensor_tensor(g_["gl"][:].rearrange("p a b -> p (a b)"), psg[:, 16:32], g_["G"][:].rearrange("p a b -> p (a b)"), ALU.subtract), [("psB", 0), "g_G"], ["g_gl"])
            A(lambda e: e.activation(out=g_["tail"][:], in_=g_["gl"][:], func=AF.Exp), ["g_gl"], ["g_tail"])
            V(lambda e: e.tensor_tensor(g_["bG"][:], g_["beta"][:], g_["eG"][:], ALU.mult), ["g_beta", "g_eG"], ["g_bG"])
            for hf in range(2):
                T(lambda e, hf=hf: e.matmul(psg[:, 32 + 16 * hf:48 + 16 * hf], selh[:, dr * 2 + hf, :], g_["G"][:].rearrange("p a b -> p (a b)"), start=True, stop=True),
                  ["g_G", "selh"], [("psB", 0)])
                A(lambda e, hf=hf: e.activation(out=dS[bb][hf][:].rearrange("p a b -> p (a b)"), in_=psg[:, 32 + 16 * hf:48 + 16 * hf], func=AF.Exp), [("psB", 0)], [("dS", bb)])
            for qh in range(2):
                for p in range(NP):
                    cs = slice(128 * p, 128 * p + 128)
                    T(lambda e, qh=qh, p=p, cs=cs: e.matmul(psA[:, p, :], fm[2 + qh][:, cs], fm[2 + qh][:, cs], start=True, stop=True), [("fm", 2 + qh)], ["psA"])
                    T(lambda e, qh=qh, p=p, cs=cs: e.matmul(psB[:, p, :], fm[qh][:, cs], fm[2 + qh][:, cs], start=True, stop=True), [("fm", qh), ("fm", 2 + qh)], [("psB", 0), ("psB", 1)])
                V(lambda e, qh=qh: e.tensor_copy(KK[qh][:], psA[:]), ["psA"], [("KK", qh)])
                A(lambda e, qh=qh: e.copy(QK[qh][:], psB[:]), [("psB", 0), ("psB", 1)], [("QK", qh)])
                yield
            for vh in range(4):
                qh = vh // 2
                sb_ = scan[bb][vh]
                Gc = g_["G"][:, :, vh]
                V(lambda e, Gc=Gc: e.tensor_tensor(diagG[:], bcp(identf[:, :]), bc(Gc), ALU.mult), ["identf", "g_G"], ["diagG"])
                for p in range(NP):
                    T(lambda e, p=p: e.matmul(psM[:, p, :], onesf[:, :], diagG[:, p, :], start=True, stop=True), ["diagG", "onesf"], ["psM"])
                V(lambda e, Gc=Gc: e.tensor_tensor(dec[:], bc(Gc), psM[:], ALU.subtract), ["psM", "g_G"], ["dec"])
                V(lambda e: e.tensor_scalar_min(dec[:], dec[:], 0.0), ["dec"], ["dec"])
                A(lambda e: e.activation(out=dec[:], in_=dec[:], func=AF.Exp), ["dec"], ["dec"])
                V(lambda e, qh=qh: e.tensor_tensor(t1[:], dec[:], KK[qh][:], ALU.mult), ["dec", ("KK", qh)], ["t1"])
                V(lambda e, vh=vh: e.tensor_tensor(t1[:], t1[:], bc(g_["nbeta"][:, :, vh]), ALU.mult), ["t1", "g_nbeta"], ["t1"])
                V(lambda e: e.tensor_tensor(XA[0][:, :, 256:384], t1[:], bcp(mstr[:, dr, :]), ALU.mult), ["t1", "mstr", ("XA", 0, 0), ("XA", 0, 1)], [("XA", 0, 0), ("XA", 0, 1)])
                G(lambda e, qh=qh: e.tensor_tensor(t1[:], dec[:], QK[qh][:], ALU.mult), ["dec", ("QK", qh), "t1"], ["t1"])
                G(lambda e: e.tensor_tensor(intra[:], t1[:], bcp(minc[:, dr, :]), ALU.mult), ["t1", "minc"], ["intra"])
                for p in range(NP):
                    T(lambda e, p=p: e.transpose(psM[:, p, :], XA[0][:, p, 256:384], identf[:, :]), [("XA", 0, p // 2), "identf"], ["psM"])
                    T(lambda e, p=p: e.transpose(psT[:, p, :], intra[:, p, :], identb[:, :]), ["intra", "identb"], ["psT"])
                V(lambda e: e.tensor_copy(Bb[0][:], psM[:]), ["psM", ("Bb", 0, 0), ("Bb", 0, 1)], [("Bb", 0, 0), ("Bb", 0, 1)])
                A(lambda e, sb_=sb_: e.copy(sb_["inT"][:], psT[:]), ["psT"], [("scan", bb, vh)])
                yield
                V(lambda e, vh=vh: e.tensor_tensor(XA[0][:, :, 0:128], tm[4 + vh][:], bc(g_["beta"][:, :, vh]), ALU.mult),
                  [("tm", 4 + vh), "g_beta", ("XA", 0, 0), ("XA", 0, 1)], [("XA", 0, 0), ("XA", 0, 1)])
                V(lambda e, vh=vh, qh=qh: e.tensor_tensor(XA[0][:, :, 128:256], tm[2 + qh][:], bc(g_["bG"][:, :, vh]), ALU.mult),
                  [("tm", 2 + qh), "g_bG", ("XA", 0, 0), ("XA", 0, 1)], [("XA", 0, 0), ("XA", 0, 1)])
                for k in range(6):
                    par, nxt = k % 2, (k + 1) % 2
                    ncol = 384 if k < 5 else 256
                    for hp in range(2):
                        prs = slice(2 * hp, 2 * hp + 2)
                        for pl in range(2):
                            p = 2 * hp + pl
                            T(lambda e, p=p, pl=pl, par=par, ncol=ncol: e.matmul(psXA[:, pl, 0:ncol], Bb[par][:, p, :], XA[par][:, p, 0:ncol], start=True, stop=True),
                              [("Bb", par, hp), ("XA", par, hp)], ["psXA"])
                            if k < 5:
                                T(lambda e, p=p, par=par: e.matmul(psB[:, p, :], XA[par][:, p, 256:384], Bb[par][:, p, :], start=True, stop=True),
                                  [("Bb", par, hp), ("XA", par, hp)], [("psB", hp)])
                        V(lambda e, prs=prs, par=par, nxt=nxt: e.tensor_tensor(XA[nxt][:, prs, 0:256], XA[par][:, prs, 0:256], psXA[:, :, 0:256], ALU.add),
                          ["psXA", ("XA", par, hp), ("XA", nxt, hp)], [("XA", nxt, hp)])
                        if k < 5:
                            A(lambda e, prs=prs, nxt=nxt: e.copy(XA[nxt][:, prs, 256:384], psXA[:, :, 256:384]), ["psXA", ("XA", nxt, hp)], [("XA", nxt, hp)])
                            A(lambda e, prs=prs, nxt=nxt: e.copy(Bb[nxt][:, prs, :], psB[:, prs, :]), [("psB", hp), ("Bb", nxt, hp)], [("Bb", nxt, hp)])
                        yield
                A(lambda e, sb_=sb_: e.copy(sb_["u"][:], XA[0][:, :, 0:128]), [("XA", 0, 0), ("XA", 0, 1)], [("scan", bb, vh)])
                V(lambda e: e.tensor_copy(qd[:], XA[0][:, :, 128:256]), [("XA", 0, 0), ("XA", 0, 1), "qd"], ["qd"])
                for p in range(NP):
                    T(lambda e, p=p: e.transpose(psT[:, p, :], qd[:, p, :], identb[:, :]), ["qd", "identb"], ["psT"])
                V(lambda e, sb_=sb_: e.tensor_copy(sb_["wT"][:], psT[:]), ["psT", ("scan", bb, vh)], [("scan", bb, vh)])
                V(lambda e, vh=vh, qh=qh: e.tensor_tensor(qd[:], tm[qh][:], bc(g_["eG"][:, :, vh]), ALU.mult), [("tm", qh), "g_eG"], ["qd"])
                for p in range(NP):
                    T(lambda e, p=p: e.transpose(psT[:, p, :], qd[:, p, :], identb[:, :]), ["qd", "identb"], ["psT"])
                A(lambda e, sb_=sb_: e.copy(sb_["qdT"][:], psT[:]), ["psT", ("scan", bb, vh)], [("scan", bb, vh)])
                G(lambda e, sb_=sb_, vh=vh, qh=qh: e.tensor_tensor(sb_["kt"][:], tm[2 + qh][:], bc(g_["tail"][:, :, vh]), ALU.mult),
                  [("tm", 2 + qh), "g_tail", ("scan", bb, vh)], [("scan", bb, vh)])

        def scan_block(dr, blk, bb):
            order = [(p, hf) for p in range(NP) for hf in range(2)]
            if dr == 1:
                order = order[::-1]
            for si, (p, hf) in enumerate(order):
                rows = slice(64 * hf, 64 * hf + 64)
                for vh in range(4):
                    sb_ = scan[bb][vh]
                    pp = pss[vh % 2]
                    T(lambda e, sb_=sb_, p=p, vh=vh, pp=pp: e.matmul(pp[:, 0, :], sb_["wT"][:, p, :], Sbf[vh][:, :], start=True, stop=True),
                      [("scan", bb, vh), ("Sbf", vh)], [("pss", vh % 2)])
                    V(lambda e, sb_=sb_, p=p, vh=vh, pp=pp, rows=rows: e.tensor_tensor(vnew[vh][rows, :], sb_["u"][rows, p, :], pp[rows, 0, :], ALU.subtract),
                      [("pss", vh % 2), ("scan", bb, vh)], [("vnew", vh)])
                    T(lambda e, sb_=sb_, p=p, vh=vh, pp=pp: e.matmul(pp[:, 1, :], sb_["qdT"][:, p, :], Sbf[vh][:, :], start=True, stop=False),
                      [("scan", bb, vh), ("Sbf", vh)], [("pss", vh % 2)])
                    T(lambda e, sb_=sb_, p=p, vh=vh, pp=pp, rows=rows: e.matmul(pp[:, 1, :], sb_["inT"][rows, p, :], vnew[vh][rows, :], start=False, stop=True),
                      [("scan", bb, vh), ("vnew", vh)], [("pss", vh % 2)])
                    A(lambda e, p=p, vh=vh, pp=pp, rows=rows: e.copy(oblk[vh][rows, p, :], pp[rows, 1, :]), [("pss", vh % 2)], [("oblk", vh)])
                    T(lambda e, sb_=sb_, p=p, vh=vh, pp=pp, rows=rows: e.matmul(pp[:, 2, :], sb_["kt"][rows, p, :], vnew[vh][rows, :], start=True, stop=True),
                      [("scan", bb, vh), ("vnew", vh)], [("pss", vh % 2)])
                    V(lambda e, p=p, vh=vh, pp=pp, hf=hf: e.scalar_tensor_tensor(Sst[vh][:, :], Sst[vh][:, :], dS[bb][hf][:, p, vh:vh + 1], pp[:, 2, :], ALU.mult, ALU.add),
                      [("pss", vh % 2), ("dS", bb), ("S", vh)], [("S", vh)])
                    A(lambda e, vh=vh: e.copy(Sbf[vh][:, :], Sst[vh][:, :]), [("S", vh)], [("Sbf", vh)])
                    yield

        def finish_block(dr, blk):
            t0 = blk * TB
            for vh in range(4):
                dst = ofw[vh, t0:t0 + TB, :].rearrange("(p i) d -> i p d", i=128)
                if dr == 0:
                    sc.dma("sp", dst, oblk[vh][:, :, :], ("oblk", vh), reads=[("oblk", vh)], writes=[("ofw", vh, blk)])
                    continue
                sc.dma("sp", ofl[:, :, :], dst, "ofl", reads=[("ofw", vh, blk)], writes=["ofl"])
                V(lambda e, vh=vh: e.tensor_tensor(ofl[:], ofl[:], oblk[vh][:], ALU.add), ["ofl", ("oblk", vh)], ["ofl"])
                for p in range(NP):
                    A(lambda e, p=p: e.activation(out=junk[:, :], in_=ofl[:, p, :], func=AF.Square, accum_out=ss[:, p:p + 1]), ["ofl", "junk", "ss"], ["junk", "ss"])
                V(lambda e: e.tensor_scalar(ss[:, :], ss[:, :], 1.0 / 128, EPS, ALU.mult, ALU.add), ["ss"], ["ss"])
                A(lambda e: e.activation(out=ss[:, :], in_=ss[:, :], func=AF.Sqrt), ["ss"], ["ss"])
                V(lambda e: e.reciprocal(ss[:, :], ss[:, :]), ["ss"], ["ss"])
                V(lambda e: e.tensor_tensor(ofl[:], ofl[:], bc(ss[:, :]), ALU.mult), ["ofl", "ss"], ["ofl"])
                V(lambda e: e.tensor_tensor(ofl[:], ofl[:], bcp(ng_sb[:, :]), ALU.mult), ["ofl", "ng"], ["ofl"])
                sc.dma("sp", raw[:, 0:TB], zraw[vh, :, t0:t0 + TB], "raw_g", writes=["raw"])
                for p in range(NP):
                    T(lambda e, p=p: e.transpose(psT[:, p, :], raw[:, 128 * p:128 * p + 128], identb[:, :]), ["raw", "identb"], ["psT"])
                A(lambda e, vh=vh: e.activation(out=zt[vh][:], in_=psT[:], func=AF.Silu), ["psT"], [("zt", vh)])
                V(lambda e, vh=vh: e.tensor_tensor(yb[:], ofl[:], zt[vh][:], ALU.mult), ["ofl", ("zt", vh)], ["yb"])
                sc.dma("sp", yg[t0:t0 + TB, 128 * vh:128 * vh + 128].rearrange("(p i) d -> i p d", i=128), yb[:, :, :], "yb", reads=["yb"], writes=["yg_out"])

        for dr in range(2):
            for vh in range(4):
                G(lambda e, vh=vh: e.memset(Sst[vh][:, :], 0.0), [], [("S", vh)])
                G(lambda e, vh=vh: e.memset(Sbf[vh][:, :], 0.0), [], [("Sbf", vh)])
            blocks = list(range(nblk)) if dr == 0 else list(range(nblk))[::-1]
            for _ in prep_block(dr, blocks[0], 0):
                pass
            for bi, blk in enumerate(blocks):
                bb = bi % 2
                gs = scan_block(dr, blk, bb)
                gp = prep_block(dr, blocks[bi + 1], (bi + 1) % 2) if bi + 1 < len(blocks) else iter(())
                alive_s = alive_p = True
                while alive_s or alive_p:
                    if alive_s:
                        alive_s = next(gs, "end") != "end"
                    for _ in range(3):
                        if alive_p:
                            alive_p = next(gp, "end") != "end"
                finish_block(dr, blk)
        sc.final_wait("sp", ["yg_out", "yb"])
        sc.run()


L1_CHUNKS = 27
OFF_QA, OFF_KA, OFF_VA = 0, 6144, 12288
OFF_QG, OFF_KG, OFF_VG, OFF_ZG, OFF_A, OFF_B, OFF_GA, OFF_GB = 14336, 16384, 18432, 22528, 26624, 26688, 26752, 28800


def w1_cols(c):
    cols = []
    r = np.arange(128)
    for s_ in range(2):
        hs = 2 * c + s_
        for g in range(3):
            cols.append(OFF_QA + g * 2048 + hs * 128 + r)
        for g in range(3):
            cols.append(OFF_KA + g * 2048 + hs * 128 + r)
        cols.append(OFF_VA + hs * 128 + r)
    for qh in range(2):
        cols.append(OFF_QG + (2 * c + qh) * 128 + r)
    for qh in range(2):
        cols.append(OFF_KG + (2 * c + qh) * 128 + r)
    for vh in range(4):
        cols.append(OFF_VG + (4 * c + vh) * 128 + r)
    for vh in range(4):
        cols.append(OFF_ZG + (4 * c + vh) * 128 + r)
    ab = np.full(128, -1)
    for dr in range(2):
        for vh in range(4):
            ab[dr * 4 + vh] = OFF_A + dr * 32 + 4 * c + vh
            ab[8 + dr * 4 + vh] = OFF_B + dr * 32 + 4 * c + vh
    cols.append(ab)
    return np.concatenate(cols)


def build_launch1(n_tok):
    nc = bass.Bass("TRN2", target_bir_lowering=False)
    di = lambda name, shape, dt: nc.dram_tensor(name, shape, dt, kind="ExternalInput").ap()
    n_cols = L1_CHUNKS * 128
    xT = di("xT", [D, n_tok], F32); w1 = di("w1", [D, n_cols], F32); gain1 = di("gain1", [128, NCH], F32)
    qg = di("qg", [128, 3], F32); kg = di("kg", [128, 3], F32); bias = di("att_bias", [2, 3, 128, 2, 128], F32)
    aps = {}
    for (name, shape, dt) in GDN_PAR:
        aps[name] = di(name, shape, dt)
    yatt = nc.dram_tensor("yatt", [2, 128, n_tok], BF16, kind="ExternalOutput").ap()
    yg = nc.dram_tensor("yg", [n_tok, 512], BF16, kind="ExternalOutput").ap()
    pT = nc.dram_tensor("pT_scr", [n_cols, n_tok], BF16).ap()
    ofw = nc.dram_tensor("ofw_scr", [4, n_tok, 128], F32).ap()
    emit_stage1(nc, xT, w1, gain1, pT, n_cols, n_tok)
    for s_ in range(2):
        base = 7 * s_ * 128
        emit_att(nc, lambda g, base=base: pT[base + 128 * g:base + 128 * g + 128, :],
                 lambda g, base=base: pT[base + 128 * (3 + g):base + 128 * (4 + g), :],
                 pT[base + 768:base + 896, :], qg, kg, bias[s_], aps["ident_b"], yatt[s_], n_tok, "_s%d" % s_)
    g0 = 14 * 128
    aps["qraw"] = pT[g0:g0 + 256, :].rearrange("(i p) t -> i p t", p=128)
    aps["kraw"] = pT[g0 + 256:g0 + 512, :].rearrange("(i p) t -> i p t", p=128)
    aps["vraw"] = pT[g0 + 512:g0 + 1024, :].rearrange("(i p) t -> i p t", p=128)
    aps["zraw"] = pT[g0 + 1024:g0 + 1536, :].rearrange("(i p) t -> i p t", p=128)
    aps["abr"] = pT[g0 + 1536:g0 + 1552, :]
    emit_gdn(nc, aps, yg, ofw, n_tok)
    return nc


def launch1_inputs(inp, c, n_tok=S):
    import ml_dtypes
    f = np.float32
    cols = w1_cols(c)
    w_in = inp["w_in"][0]
    w1 = np.zeros((D, cols.size), f)
    m = cols >= 0
    w1[:, m] = w_in[:, cols[m]]
    C = gdn_consts()
    cwf = inp["gdn_conv_w"][0]
    chans = [np.arange(128) + (2 * c + qh) * 128 for qh in range(2)] + [2048 + np.arange(128) + (2 * c + qh) * 128 for qh in range(2)] \
        + [4096 + np.arange(128) + (4 * c + vh) * 128 for vh in range(4)]
    cw = np.stack([cwf[:, ch].T for ch in chans], axis=1)
    sel8 = lambda a: np.concatenate([a[0, 4 * c:4 * c + 4], a[1, 4 * c:4 * c + 4]])
    d = {
        "xT": np.ascontiguousarray(inp["x"][0, :n_tok].T), "w1": w1,
        "gain1": np.ascontiguousarray(inp["norm1_gain"][0].reshape(NCH, 128).T),
        "qg": np.ascontiguousarray(inp["q_norm_gain"][0].T), "kg": np.ascontiguousarray(inp["k_norm_gain"][0].T),
        "att_bias": np.stack([att_bias_consts(2 * c), att_bias_consts(2 * c + 1)]),
        "cw": np.ascontiguousarray(cw.astype(f)),
        "alog": np.ascontiguousarray(np.broadcast_to(sel8(inp["gdn_a_log"][0])[None, :], (128, 8)).astype(f)),
        "dtb": np.ascontiguousarray(np.broadcast_to(sel8(inp["gdn_dt_bias"][0])[None, :], (128, 8)).astype(f)),
        "ng": np.ascontiguousarray(np.broadcast_to(inp["gdn_norm_gain"][0][None, :], (128, 128)).astype(f)),
        "ident_f": C["ident_f"], "ident_b": C["ident_f"].astype(ml_dtypes.bfloat16), "ones_f": C["ones_f"],
        "mstrict": C["mstrict"], "mincl": C["mincl"], "cum": C["cum"], "sel": C["sel"], "selh": C["selh"],
    }
    return d


NT = 1024
NEXP = 64


def emit_l2a(nc, a, x2tm, h2b_d, wt_d):
    HT = 512
    xT_v = a["xT"].rearrange("(c p) t -> p c t", p=128)
    yA_v = a["yAT"].rearrange("(c p) t -> p c t", p=128)
    yB_v = a["yBT"].rearrange("(c p) t -> p c t", p=128)
    wgate_v = a["wgate"].rearrange("(c p) n -> p c n", p=128)
    wA_v = a["wA"].rearrange("(c p) n -> p c n", p=128)
    wB_v = a["wB"].rearrange("(c p) n -> p c n", p=128)
    wo_v = a["wo"].rearrange("(c p) n -> p c n", p=128)
    wr_v = a["wr"].rearrange("(c p) n -> p c n", p=128)
    h2b_v = h2b_d.rearrange("(c p) t -> p c t", p=128)
    with contextlib.ExitStack() as st:
        sb = lambda name, shape, dt: st.enter_context(nc.sbuf_tensor(name, shape, dt))
        ps = lambda name, shape, dt: st.enter_context(nc.psum_tensor(name, shape, dt))
        sc = Sched(nc, st)
        V = lambda fn, r, w: sc.op("dve", fn, reads=r, writes=w)
        A = lambda fn, r, w: sc.op("act", fn, reads=r, writes=w)
        G = lambda fn, r, w: sc.op("pool", fn, reads=r, writes=w)
        T = lambda fn, r, w: sc.op("pe", fn, reads=r, writes=w)
        ones = sb("b_ones", [128, 128], BF16); identf = sb("b_identf", [128, 128], F32)
        g1 = sb("b_g1", [128, NCH], F32); g2 = sb("b_g2", [128, NCH], F32)
        wr_sb = sb("b_wr", [128, NCH, 72], F32); br_sb = sb("b_br", [128, 72], F32)
        xf = sb("b_xf", [128, NCH, HT], F32); hb = sb("b_hb", [128, NCH, HT], BF16); xsq = sb("b_xsq", [128, NCH, HT], BF16)
        yA = sb("b_yA", [128, NCH, HT], BF16); bufY = sb("b_bufY", [128, 32 * HT * 2 // 4], F32)
        yB = bufY[:, :].bitcast(BF16).rearrange("p (c t) -> p c t", t=HT)
        stg = bufY[:, :].rearrange("p (k d) -> p k d", d=D)
        h2f = bufY[:, :].rearrange("p (c t) -> p c t", t=HT)
        merged = sb("b_merged", [128, NCH, HT], BF16)
        rstd = sb("b_rstd", [128, HT], F32)
        wgA = [sb("b_wgA%d" % i, [128, NCH, 128], BF16) for i in range(2)]; wgB = [sb("b_wgB%d" % i, [128, NCH, 128], BF16) for i in range(2)]
        wa = [sb("b_wa%d" % i, [128, NCH, 128], BF16) for i in range(2)]; wb_ = [sb("b_wb%d" % i, [128, 32, 128], BF16) for i in range(2)]
        wo = [sb("b_wo%d" % i, [128, NCH, 128], BF16) for i in range(2)]
        tA = sb("b_tA", [128, HT], F32); tB = sb("b_tB", [128, HT], F32); sA = sb("b_sA", [128, HT], F32); sBt = sb("b_sB", [128, HT], F32)
        rt = {n: sb("b_r_" + n, [128, 72], F32) for n in ("lg", "ge", "oh", "tmp", "ig", "m1k", "in2", "m2k", "wl")}
        rs_ = {n: sb("b_s_" + n, [128, 1], F32) for n in ("gmax", "ngmax", "gs", "gw", "m1", "m2", "d12", "w1", "w2")}
        wt_st = sb("b_wtst", [128, 4, 64], F32)
        ps_ss = ps("b_psss", [128, HT], F32)
        psGA = ps("b_psGA", [128, HT], F32); psGB = ps("b_psGB", [128, HT], F32); psMA = ps("b_psMA", [128, HT], F32); psMB = ps("b_psMB", [128, HT], F32)
        psT = ps("b_psT", [128, 4, 128], F32); psR = ps("b_psR", [128, 128], F32)

        G(lambda e: e.memset(ones[:, :], 1.0), [], ["ones"])
        sc.dma("sp", identf[:, :], a["ident_f"][:, :], "c1", writes=["identf"])
        sc.dma("sp", g1[:, :], a["gain1"][:, :], "c2", writes=["g1"])
        sc.dma("sp", g2[:, :], a["gain2"][:, :], "c3", writes=["g2"])
        sc.dma("sp", wr_sb[:, :, :], wr_v, "c4", writes=["wr"])
        sc.dma("sp", br_sb[:, :], a["br"][:, :], "c5", writes=["br"])

        def rms(tag):
            A(lambda e: e.activation(out=xsq[:, :, :], in_=xf[:, :, :], func=AF.Square), ["xf"], ["xsq"])
            for c in range(NCH):
                T(lambda e, c=c: e.matmul(ps_ss[:, :], ones[:, :], xsq[:, c, :], start=(c == 0), stop=(c == NCH - 1)), ["xsq", "ones"], ["ps_ss"])
            V(lambda e: e.tensor_scalar(rstd[:, :], ps_ss[:, :], 1.0 / D, EPS, ALU.mult, ALU.add), ["ps_ss"], ["rstd"])
            A(lambda e: e.activation(out=rstd[:, :], in_=rstd[:, :], func=AF.Sqrt), ["rstd"], ["rstd"])
            V(lambda e: e.reciprocal(rstd[:, :], rstd[:, :]), ["rstd"], ["rstd"])

        for hf in range(NT // HT):
            hs = slice(hf * HT, (hf + 1) * HT)
            sc.dma("sp", xf[:, :, :], xT_v[:, :, hs], "l_xf", writes=["xf"])
            sc.dma("sp", yA[:, :, :], yA_v[:, :, hs], "l_yA", writes=["yA"])
            sc.dma("sp", yB, yB_v[:, :, hs], "l_bufY", writes=["bufY"])
            rms("n1")
            for c in range(NCH):
                V(lambda e, c=c: e.tensor_scalar(hb[:, c, :], xf[:, c, :], g1[:, c:c + 1], None, ALU.mult), ["xf", "g1"], ["hb"])
            for j in range(NCH):
                jb = j % 2
                js = slice(j * 128, (j + 1) * 128)
                j2 = slice(2048 + j * 128, 2048 + (j + 1) * 128)
                sc.dma("pool", wgA[jb][:, :, :], wgate_v[:, :, js], ("wgA", jb), writes=[("wgA", jb)])
                sc.dma("pool", wgB[jb][:, :, :], wgate_v[:, :, j2], ("wgB", jb), writes=[("wgB", jb)])
                sc.dma("pool", wa[jb][:, :, :], wA_v[:, :, js], ("wa", jb), writes=[("wa", jb)])
                sc.dma("pool", wb_[jb][:, :, :], wB_v[:, :, js], ("wb", jb), writes=[("wb", jb)])
                for c in range(NCH):
                    T(lambda e, c=c, jb=jb: e.matmul(psGA[:, :], wgA[jb][:, c, :], hb[:, c, :], start=(c == 0), stop=(c == NCH - 1)), [("wgA", jb), "hb"], ["psGA"])
                for c in range(NCH):
                    T(lambda e, c=c, jb=jb: e.matmul(psGB[:, :], wgB[jb][:, c, :], hb[:, c, :], start=(c == 0), stop=(c == NCH - 1)), [("wgB", jb), "hb"], ["psGB"])
                for c in range(NCH):
                    T(lambda e, c=c, jb=jb: e.matmul(psMA[:, :], wa[jb][:, c, :], yA[:, c, :], start=(c == 0), stop=(c == NCH - 1)), [("wa", jb), "yA"], ["psMA"])
                for c in range(32):
                    T(lambda e, c=c, jb=jb: e.matmul(psMB[:, :], wb_[jb][:, c, :], yB[:, c, :], start=(c == 0), stop=(c == 31)), [("wb", jb), "bufY"], ["psMB"])
                V(lambda e: e.tensor_tensor(tA[:, :], psGA[:, :], rstd[:, :], ALU.mult), ["psGA", "rstd"], ["tA"])
                A(lambda e: e.activation(out=sA[:, :], in_=tA[:, :], func=AF.Sigmoid), ["tA"], ["sA"])
                V(lambda e: e.tensor_tensor(tB[:, :], psGB[:, :], rstd[:, :], ALU.mult), ["psGB", "rstd"], ["tB"])
                A(lambda e: e.activation(out=sBt[:, :], in_=tB[:, :], func=AF.Sigmoid), ["tB"], ["sB"])
                V(lambda e: e.tensor_tensor(tA[:, :], sA[:, :], psMA[:, :], ALU.mult), ["sA", "psMA", "tA"], ["tA"])
                V(lambda e: e.tensor_tensor(tB[:, :], sBt[:, :], psMB[:, :], ALU.mult), ["sB", "psMB", "tB"], ["tB"])
                V(lambda e, j=j: e.tensor_tensor(merged[:, j, :], tA[:, :], tB[:, :], ALU.add), ["tA", "tB"], ["merged"])
            for dch in range(NCH):
                db = dch % 2
                sc.dma("pool", wo[db][:, :, :], wo_v[:, :, dch * 128:(dch + 1) * 128], ("wo", db), writes=[("wo", db)])
                for c in range(NCH):
                    T(lambda e, c=c, db=db: e.matmul(psGA[:, :], wo[db][:, c, :], merged[:, c, :], start=(c == 0), stop=(c == NCH - 1)), [("wo", db), "merged"], ["psGA"])
                V(lambda e, dch=dch: e.tensor_tensor(xf[:, dch, :], xf[:, dch, :], psGA[:, :], ALU.add), ["psGA", "xf"], ["xf"])
            for k in range(4):
                for c in range(NCH):
                    T(lambda e, c=c, k=k: e.transpose(psT[:, c % 4, :], xf[:, c, k * 128:(k + 1) * 128], identf[:, :]), ["xf", "identf"], ["psT"])
                    if c % 4 == 3:
                        V(lambda e, c=c, k=k: e.tensor_copy(stg[:, k, (c - 3) * 128:(c + 1) * 128], psT[:, :, :].rearrange("p a b -> p (a b)")),
                          ["psT", "bufY"], ["bufY"])
            sc.dma("sp", x2tm[hs, :].rearrange("(k p) d -> p k d", p=128), stg, "s_bufY", reads=["bufY"], writes=["x2tm"])
            rms("n2")
            for c in range(NCH):
                V(lambda e, c=c: e.scalar_tensor_tensor(h2f[:, c, :], xf[:, c, :], g2[:, c:c + 1], rstd[:, :], ALU.mult, ALU.mult),
                  ["xf", "g2", "rstd", "bufY"], ["bufY"])
            A(lambda e: e.copy(hb[:, :, :], h2f), ["bufY", "hb"], ["hb"])
            sc.dma("sp", h2b_v[:, :, hs], hb[:, :, :], "s_h2b", reads=["hb"], writes=["h2b_d"])
            for k in range(4):
                for c in range(NCH):
                    T(lambda e, c=c, k=k: e.matmul(psR[:, 0:72], h2f[:, c, k * 128:(k + 1) * 128], wr_sb[:, c, :], start=(c == 0), stop=(c == NCH - 1)),
                      ["bufY", "wr"], ["psR"])
                r = rt; s_ = rs_
                V(lambda e: e.tensor_tensor(r["lg"][:, :], psR[:, 0:72], br_sb[:, :], ALU.add), ["psR", "br"], ["r_lg"])
                V(lambda e: e.reduce_max(s_["gmax"][:, :], r["lg"][:, 0:8], AX.X), ["r_lg"], ["s_gmax"])
                V(lambda e: e.tensor_scalar(s_["ngmax"][:, :], s_["gmax"][:, :], -1.0, None, ALU.mult), ["s_gmax"], ["s_ngmax"])
                A(lambda e: e.activation(out=r["ge"][:, 0:8], in_=r["lg"][:, 0:8], func=AF.Exp, bias=s_["ngmax"][:, 0:1]), ["r_lg", "s_ngmax"], ["r_ge"])
                V(lambda e: e.reduce_sum(s_["gs"][:, :], r["ge"][:, 0:8], AX.X), ["r_ge"], ["s_gs"])
                V(lambda e: e.reciprocal(s_["gw"][:, :], s_["gs"][:, :]), ["s_gs"], ["s_gw"])
                V(lambda e: e.tensor_scalar(r["oh"][:, 0:8], r["lg"][:, 0:8], s_["gmax"][:, 0:1], None, ALU.is_equal), ["r_lg", "s_gmax"], ["r_oh"])
                le = r["lg"][:, 8:72].rearrange("p (g j) -> p g j", j=8)
                V(lambda e, le=le: e.tensor_tensor(r["tmp"][:, 0:64].rearrange("p (g j) -> p g j", j=8), le,
                                                   r["oh"][:, 0:8].unsqueeze(2).to_broadcast([128, 8, 8]), ALU.mult), ["r_lg", "r_oh"], ["r_tmp"])
                V(lambda e: e.reduce_sum(r["ig"][:, 0:8], r["tmp"][:, 0:64].rearrange("p (g j) -> p j g", j=8), AX.X), ["r_tmp"], ["r_ig"])
                V(lambda e: e.reduce_max(s_["m1"][:, :], r["ig"][:, 0:8], AX.X), ["r_ig"], ["s_m1"])
                V(lambda e: e.tensor_scalar(r["m1k"][:, 0:8], r["ig"][:, 0:8], s_["m1"][:, 0:1], None, ALU.is_equal), ["r_ig", "s_m1"], ["r_m1k"])
                V(lambda e: e.scalar_tensor_tensor(r["in2"][:, 0:8], r["m1k"][:, 0:8], -1e30, r["ig"][:, 0:8], ALU.mult, ALU.add), ["r_m1k", "r_ig"], ["r_in2"])
                V(lambda e: e.reduce_max(s_["m2"][:, :], r["in2"][:, 0:8], AX.X), ["r_in2"], ["s_m2"])
                V(lambda e: e.tensor_scalar(r["m2k"][:, 0:8], r["in2"][:, 0:8], s_["m2"][:, 0:1], None, ALU.is_equal), ["r_in2", "s_m2"], ["r_m2k"])
                V(lambda e: e.tensor_tensor(s_["d12"][:, :], s_["m1"][:, :], s_["m2"][:, :], ALU.subtract), ["s_m1", "s_m2"], ["s_d12"])
                A(lambda e: e.activation(out=s_["w1"][:, :], in_=s_["d12"][:, :], func=AF.Sigmoid), ["s_d12"], ["s_w1"])
                A(lambda e: e.activation(out=s_["w2"][:, :], in_=s_["d12"][:, :], func=AF.Sigmoid, scale=-1.0), ["s_d12"], ["s_w2"])
                V(lambda e: e.tensor_scalar(r["wl"][:, 0:8], r["m1k"][:, 0:8], s_["w1"][:, 0:1], None, ALU.mult), ["r_m1k", "s_w1"], ["r_wl"])
                V(lambda e: e.scalar_tensor_tensor(r["wl"][:, 0:8], r["m2k"][:, 0:8], s_["w2"][:, 0:1], r["wl"][:, 0:8], ALU.mult, ALU.add), ["r_m2k", "s_w2", "r_wl"], ["r_wl"])
                V(lambda e: e.tensor_scalar(r["wl"][:, 0:8], r["wl"][:, 0:8], s_["gw"][:, 0:1], None, ALU.mult), ["r_wl", "s_gw"], ["r_wl"])
                V(lambda e, k=k: e.tensor_tensor(wt_st[:, k, :].rearrange("p (g j) -> p g j", j=8), r["oh"][:, 0:8].unsqueeze(2).to_broadcast([128, 8, 8]),
                                                 r["wl"][:, 0:8].unsqueeze(1).to_broadcast([128, 8, 8]), ALU.mult), ["r_oh", "r_wl", "wt_st"], ["wt_st"])
            sc.dma("sp", wt_d[hs, :].rearrange("(k p) e -> p k e", p=128), wt_st[:, :, :], "s_wt", reads=["wt_st"], writes=["wt_d"])
        sc.final_wait("sp", ["x2tm", "h2b_d", "wt_d", "bufY", "hb", "wt_st"])
        sc.run()


def emit_l2b(nc, a, x2tm, h2b_d, wt_d, out, n_exp=NEXP):
    HT = 512
    with contextlib.ExitStack() as st:
        sb = lambda name, shape, dt: st.enter_context(nc.sbuf_tensor(name, shape, dt))
        ps = lambda name, shape, dt: st.enter_context(nc.psum_tensor(name, shape, dt))
        sc = Sched(nc, st)
        V = lambda fn, r, w: sc.op("dve", fn, reads=r, writes=w)
        A = lambda fn, r, w: sc.op("act", fn, reads=r, writes=w)
        T = lambda fn, r, w: sc.op("pe", fn, reads=r, writes=w)
        acc = sb("m_acc", [128, 8, D], F32); h2b = sb("m_h2b", [128, NCH, NT], BF16); wt = sb("m_wt", [128, 8, 64], F32)
        wg = [sb("m_wg%d" % i, [128, NCH, 512], BF16) for i in range(2)]
        wu2 = [sb("m_wu%d" % i, [128, NCH, 512], BF16) for i in range(2)]; wd2 = [sb("m_wd%d" % i, [128, 4, D], BF16) for i in range(2)]
        act = sb("m_act", [128, 4, NT], BF16); slb = [sb("m_slb%d" % i, [128, HT], BF16) for i in range(2)]
        psG = [ps("m_psG%d" % i, [128, HT], F32) for i in range(2)]; psU = [ps("m_psU%d" % i, [128, HT], F32) for i in range(2)]
        psD = [ps("m_psD%d" % i, [128, 512], F32) for i in range(2)]
        sc.dma("sp", acc[:, :, :], x2tm.rearrange("(k p) d -> p k d", p=128), "l_acc", writes=["acc"])
        sc.dma("sp", h2b[:, :, :], h2b_d.rearrange("(c p) t -> p c t", p=128), "l_h2b", writes=["h2b"])
        sc.dma("sp", wt[:, :, :], wt_d.rearrange("(k p) e -> p k e", p=128), "l_wt", writes=["wt"])
        n = 0
        for e_ in range(n_exp):
            eb = e_ % 2
            sc.dma("pool", wg[eb][:, :, :], a["w_gate"][e_].rearrange("(c p) f -> p c f", p=128), ("wg", eb), writes=[("wg", eb)])
            wu, wd = wu2[eb], wd2[eb]
            sc.dma("pool", wu[:, :, :], a["w_up"][e_].rearrange("(c p) f -> p c f", p=128), ("wu", eb), writes=[("wu", eb)])
            sc.dma("pool", wd[:, :, :], a["w_down"][e_].rearrange("(c p) d -> p c d", p=128), ("wd", eb), writes=[("wd", eb)])
            for hf in range(NT // HT):
                hs = slice(hf * HT, (hf + 1) * HT)
                for fch in range(4):
                    pb = n % 2; n += 1
                    fs = slice(fch * 128, (fch + 1) * 128)
                    for c in range(NCH):
                        T(lambda e, c=c, eb=eb, fs=fs, hs=hs, pb=pb: e.matmul(psG[pb][:, :], wg[eb][:, c, fs], h2b[:, c, hs], start=(c == 0), stop=(c == NCH - 1)),
                          [("wg", eb), "h2b"], [("psG", pb)])
                    for c in range(NCH):
                        T(lambda e, wu=wu, c=c, fs=fs, hs=hs, pb=pb: e.matmul(psU[pb][:, :], wu[:, c, fs], h2b[:, c, hs], start=(c == 0), stop=(c == NCH - 1)),
                          [("wu", eb), "h2b"], [("psU", pb)])
                    A(lambda e, pb=pb: e.activation(out=slb[pb][:, :], in_=psG[pb][:, :], func=AF.Silu), [("psG", pb)], [("slb", pb)])
                    V(lambda e, pb=pb, fch=fch, hs=hs: e.tensor_tensor(act[:, fch, hs], slb[pb][:, :], psU[pb][:, :], ALU.mult), [("slb", pb), ("psU", pb)], ["act"])
            for k in range(8):
                for db in range(4):
                    pd = (k * 4 + db) % 2
                    ds_ = slice(db * 512, (db + 1) * 512)
                    for fch in range(4):
                        T(lambda e, wd=wd, k=k, fch=fch, ds_=ds_, pd=pd: e.matmul(psD[pd][:, :], act[:, fch, k * 128:(k + 1) * 128], wd[:, fch, ds_], start=(fch == 0), stop=(fch == 3)),
                          ["act", ("wd", eb)], [("psD", pd)])
                    V(lambda e, k=k, ds_=ds_, pd=pd, e_=e_: e.scalar_tensor_tensor(acc[:, k, ds_], psD[pd][:, :], wt[:, k, e_:e_ + 1], acc[:, k, ds_], ALU.mult, ALU.add),
                      [("psD", pd), "wt", "acc"], ["acc"])
        sc.dma("sp", out.rearrange("(k p) d -> p k d", p=128), acc[:, :, :], "s_out", reads=["acc"], writes=["out"])
        sc.final_wait("sp", ["out"])
        sc.run()


L2_IN = (("xT", [D, NT], F32), ("yAT", [2048, NT], BF16), ("yBT", [4096, NT], BF16), ("gain1", [128, NCH], F32), ("gain2", [128, NCH], F32),
         ("wgate", [D, 4096], F32), ("wA", [2048, D], F32), ("wB", [4096, D], F32), ("wo", [D, D], F32), ("wr", [D, 72], F32), ("br", [128, 72], F32),
         ("ident_f", [128, 128], F32), ("w_gate", [NEXP, D, 512], F32), ("w_up", [NEXP, D, 512], F32), ("w_down", [NEXP, 512, D], F32))


def build_launch2(n_exp=NEXP):
    nc = bass.Bass("TRN2", target_bir_lowering=False)
    a = {name: nc.dram_tensor(name, shape, dt, kind="ExternalInput").ap() for (name, shape, dt) in L2_IN}
    out = nc.dram_tensor("out", [NT, D], F32, kind="ExternalOutput").ap()
    x2tm = nc.dram_tensor("x2tm_scr", [NT, D], F32).ap()
    h2b_d = nc.dram_tensor("h2b_scr", [D, NT], BF16).ap()
    wt_d = nc.dram_tensor("wt_scr", [NT, 64], F32).ap()
    emit_l2a(nc, a, x2tm, h2b_d, wt_d)
    emit_l2b(nc, a, x2tm, h2b_d, wt_d, out, n_exp)
    return nc


def launch2_inputs(inp, c, yAT, yBT, shared):
    ts = slice(c * NT, (c + 1) * NT)
    d = dict(shared)
    d["xT"] = np.ascontiguousarray(inp["x"][0, ts].T)
    d["yAT"] = np.ascontiguousarray(yAT[:, ts]); d["yBT"] = np.ascontiguousarray(yBT[:, ts])
    return d


def launch2_shared(inp):
    f = np.float32
    return {
        "gain1": np.ascontiguousarray(inp["norm1_gain"][0].reshape(NCH, 128).T), "gain2": np.ascontiguousarray(inp["norm2_gain"][0].reshape(NCH, 128).T),
        "wgate": np.ascontiguousarray(inp["w_in"][0][:, OFF_GA:OFF_GA + 4096]), "wA": inp["w_branch_att"][0], "wB": inp["w_branch_gdn"][0], "wo": inp["w_out"][0],
        "wr": np.ascontiguousarray(np.concatenate([inp["w_group_router"][0], inp["w_expert_router"][0]], axis=1)),
        "br": np.ascontiguousarray(np.broadcast_to(np.concatenate([inp["b_group_router"][0], inp["b_expert_router"][0]])[None, :], (128, 72)).astype(f)),
        "ident_f": np.eye(128, dtype=f), "w_gate": inp["w_gate"][0], "w_up": inp["w_up"][0], "w_down": inp["w_down"][0],
    }


def kernel(**inputs):
    inp = {k: np.asarray(v) for k, v in inputs.items()}
    cores = list(range(NCORES))
    nc1 = build_launch1(S)
    res1 = run_bass_kernel_spmd(nc1, [launch1_inputs(inp, c) for c in cores], core_ids=cores).results
    yAT = np.concatenate([np.asarray(res1[c]["yatt"]).reshape(256, S) for c in cores], axis=0)
    yBT = np.concatenate([np.ascontiguousarray(np.asarray(res1[c]["yg"]).T) for c in cores], axis=0)
    del res1
    nc2 = build_launch2()
    shared = launch2_shared(inp)
    res2 = run_bass_kernel_spmd(nc2, [launch2_inputs(inp, c, yAT, yBT, shared) for c in cores], core_ids=cores).results
    out = np.concatenate([np.asarray(res2[c]["out"]) for c in cores], axis=0)
    return out.reshape(1, S, D).astype(np.float32)
```

```python
import contextlib
import numpy as np
import concourse.bass as bass
import concourse.mybir as mybir
from concourse.bass_utils import run_bass_kernel_spmd

F32 = mybir.dt.float32
BF16 = mybir.dt.bfloat16
AF = mybir.ActivationFunctionType
ALU = mybir.AluOpType
AX = mybir.AxisListType

D = 2048
S = 8192
NCORES = 8
NCH = D // 128
EPS = 1e-6

ENG_NAMES = ("pe", "dve", "act", "pool", "sp")


class Sched:
    def __init__(self, nc, stack):
        self.nc = nc
        self.stack = stack
        self.q = {k: [] for k in ENG_NAMES}
        self.sems = {}
        self.count = {}
        self.waited = {k: {} for k in ENG_NAMES}
        self.last_w = {}
        self.readers = {}

    def _sem(self, key):
        if key not in self.sems:
            name = "s%d" % len(self.sems)
            self.sems[key] = self.stack.enter_context(self.nc.semaphore(name))
            self.count[key] = 0
        return self.sems[key]

    def _deps(self, eng, reads, writes):
        deps = []
        for r in reads:
            if r in self.last_w:
                deps.append(self.last_w[r])
        for w in writes:
            if w in self.last_w:
                deps.append(self.last_w[w])
            deps.extend(self.readers.get(w, ()))
        waits = []
        for (key, val, src) in deps:
            if src == "pe" and eng == "pe":
                continue
            if self.waited[eng].get(key, 0) >= val:
                continue
            self.waited[eng][key] = val
            waits.append((key, val))
        return waits

    def _commit(self, tok, reads, writes):
        for w in writes:
            self.last_w[w] = tok
            self.readers[w] = []
        for r in reads:
            self.readers.setdefault(r, []).append(tok)

    def op(self, eng, fn, reads=(), writes=()):
        waits = self._deps(eng, reads, writes)
        key = ("e", eng)
        sem = self._sem(key)
        self.count[key] += 1
        tok = (key, self.count[key], eng)
        wl = [(self._sem(k), v) for (k, v) in waits]

        def emit(e, fn=fn, wl=wl, sem=sem):
            for (s, v) in wl:
                e.wait_ge(s, v)
            fn(e).then_inc(sem, 1)
        self.q[eng].append(emit)
        self._commit(tok, reads, writes)

    def dma(self, eng, out, in_, sem_key, reads=(), writes=(), **kw):
        waits = self._deps(eng, reads, writes)
        key = ("d", sem_key)
        sem = self._sem(key)
        self.count[key] += 16
        tok = (key, self.count[key], "dma")
        wl = [(self._sem(k), v) for (k, v) in waits]

        def emit(e, wl=wl, sem=sem):
            for (s, v) in wl:
                e.wait_ge(s, v)
            e.dma_start(out=out, in_=in_, **kw).then_inc(sem, 16)
        self.q[eng].append(emit)
        self._commit(tok, reads, writes)

    def final_wait(self, eng, resources):
        waits = self._deps(eng, resources, ())
        wl = [(self._sem(k), v) for (k, v) in waits]

        def emit(e, wl=wl):
            for (s, v) in wl:
                e.wait_ge(s, v)
        self.q[eng].append(emit)

    def run(self):
        nc = self.nc
        with nc.Block() as block:
            @block.tensor
            def _(e):
                for f in self.q["pe"]:
                    f(e)

            @block.vector
            def _(e):
                for f in self.q["dve"]:
                    f(e)

            @block.scalar
            def _(e):
                for f in self.q["act"]:
                    f(e)

            @block.gpsimd
            def _(e):
                for f in self.q["pool"]:
                    f(e)

            @block.sync
            def _(e):
                for f in self.q["sp"]:
                    f(e)


def emit_stage1(nc, xT, w, gain, pT, n_cols, n_tok, tok_blk=256):
    assert n_cols % 128 == 0 and n_tok % tok_blk == 0
    ncc = n_cols // 128
    ntt = n_tok // tok_blk
    xT_v = xT.rearrange("(c p) t -> p c t", p=128)
    w_v = w.rearrange("(c p) n -> p c n", p=128)
    with contextlib.ExitStack() as st:
        sb = lambda name, shape, dt: st.enter_context(nc.sbuf_tensor(name, shape, dt))
        ps = lambda name, shape, dt: st.enter_context(nc.psum_tensor(name, shape, dt))
        wb = sb("wb", [128, NCH, n_cols], BF16)
        g_sb = sb("g_sb", [128, NCH], F32)
        ones = sb("ones1", [128, 128], BF16)
        xf = [sb("xf%d" % i, [128, NCH, tok_blk], F32) for i in range(2)]
        xb = [sb("xb%d" % i, [128, NCH, tok_blk], BF16) for i in range(2)]
        xsq = [sb("xsq%d" % i, [128, NCH, tok_blk], BF16) for i in range(1)] * 2
        rstd = [sb("rstd%d" % i, [128, tok_blk], F32) for i in range(2)]
        ob = [sb("ob%d" % i, [128, tok_blk], BF16) for i in range(4)]
        ps_ss = ps("ps_ss", [128, tok_blk], F32)
        ps_o = [ps("ps_o%d" % i, [128, tok_blk], F32) for i in range(4)]
        sc = Sched(nc, st)

        sc.op("pool", lambda e: e.memset(ones[:, :], 1.0), writes=["ones"])
        sc.dma("sp", g_sb[:, :], gain[:, :], "g_sb", writes=["g_sb"])
        for c in range(NCH):
            sc.dma("pool", wb[:, c, :], w_v[:, c, :], "wb", writes=[("wb", c), "wb_all"])
        for c in range(NCH):
            sc.op("dve", lambda e, c=c: e.tensor_scalar(wb[:, c, :], wb[:, c, :], g_sb[:, c:c + 1], None, ALU.mult),
                  reads=[("wb", c), "wb_all", "g_sb"], writes=[("wb", c)])
        for t in range(ntt):
            b = t % 2
            tsl = slice(t * tok_blk, (t + 1) * tok_blk)
            sc.dma("sp", xf[b][:, :, :], xT_v[:, :, tsl], ("xf", b), writes=[("xf", b)])
            sc.op("act", lambda e, b=b: e.activation(out=xsq[b][:, :, :], in_=xf[b][:, :, :], func=AF.Square),
                  reads=[("xf", b)], writes=["xsq"])
            sc.op("dve", lambda e, b=b: e.tensor_copy(xb[b][:, :, :], xf[b][:, :, :]),
                  reads=[("xf", b)], writes=[("xb", b)])
            for c in range(NCH):
                sc.op("pe", lambda e, b=b, c=c: e.matmul(ps_ss[:, :], ones[:, :], xsq[b][:, c, :],
                                                         start=(c == 0), stop=(c == NCH - 1)),
                      reads=["xsq", "ones"], writes=["ps_ss"])
            sc.op("dve", lambda e, b=b: e.tensor_scalar(rstd[b][:, :], ps_ss[:, :], 1.0 / D, EPS, ALU.mult, ALU.add),
                  reads=["ps_ss"], writes=[("rstd", b)])
            sc.op("act", lambda e, b=b: e.activation(out=rstd[b][:, :], in_=rstd[b][:, :], func=AF.Sqrt),
                  reads=[("rstd", b)], writes=[("rstd", b)])
            sc.op("dve", lambda e, b=b: e.reciprocal(rstd[b][:, :], rstd[b][:, :]),
                  reads=[("rstd", b)], writes=[("rstd", b)])
            for j in range(ncc):
                k = (t * ncc + j) % 4
                for c in range(NCH):
                    sc.op("pe", lambda e, b=b, c=c, j=j, k=k: e.matmul(
                        ps_o[k][:, :], wb[:, c, j * 128:(j + 1) * 128], xb[b][:, c, :],
                        start=(c == 0), stop=(c == NCH - 1)),
                        reads=[("xb", b), ("wb", c)], writes=[("ps_o", k)])
                sc.op("dve", lambda e, b=b, k=k: e.tensor_tensor(ob[k][:, :], ps_o[k][:, :], rstd[b][:, :], ALU.mult),
                      reads=[("ps_o", k), ("rstd", b)], writes=[("ob", k)])
                sc.dma("sp", pT[j * 128:(j + 1) * 128, tsl], ob[k][:, :], ("ob", k),
                       reads=[("ob", k)], writes=["pT_out"])
        sc.final_wait("sp", [("ob", k) for k in range(4)] + ["pT_out"])
        for k in range(4):
            key = ("d", ("ob", k))
            if key in sc.sems:
                sc.q["sp"].append(lambda e, s=sc.sems[key], v=sc.count[key]: e.wait_ge(s, v))
        sc.run()


def build_stage1(n_cols, n_tok, tok_blk=256):
    nc = bass.Bass("TRN2", target_bir_lowering=False)
    xT = nc.dram_tensor("xT", [D, n_tok], F32, kind="ExternalInput").ap()
    w = nc.dram_tensor("w", [D, n_cols], F32, kind="ExternalInput").ap()
    gain = nc.dram_tensor("gain", [128, NCH], F32, kind="ExternalInput").ap()
    pT = nc.dram_tensor("pT", [n_cols, n_tok], BF16, kind="ExternalOutput").ap()
    emit_stage1(nc, xT, w, gain, pT, n_cols, n_tok, tok_blk)
    return nc


ATT_PATTERNS = ((128, 1), (512, 4), (2048, 16))
HALF = 64


def alibi_slopes_np():
    n = 48
    return np.exp2(-8.0 * np.arange(1, n + 1, dtype=np.float64) / n).reshape(3, 16)


def att_bias_consts(head_slot):
    out = np.zeros((3, 128, 2, 128), np.float32)
    i = np.arange(128)[:, None]
    j = np.arange(128)[None, :]
    sl = alibi_slopes_np()
    for g, (_, d) in enumerate(ATT_PATTERNS):
        for h, off in enumerate((-64, 64)):
            rel = i - j + off
            b = -sl[g, head_slot] * np.abs(rel) * d
            out[g, :, h, :] = np.where(np.abs(rel) <= HALF, b, -1e30)
    return out


def emit_att(nc, q_of, k_of, vT, qg, kg, bias, ident_d, yT, n_tok, tag):
    QT = 512
    PIECE = min(2048, n_tok)
    DMAX = 16
    with contextlib.ExitStack() as st:
        sb = lambda name, shape, dt: st.enter_context(nc.sbuf_tensor(name + tag, shape, dt))
        ps = lambda name, shape, dt: st.enter_context(nc.psum_tensor(name + tag, shape, dt))
        sc = Sched(nc, st)
        ones = sb("a_ones", [128, 128], BF16)
        ident = sb("a_ident", [128, 128], BF16)
        g_q = sb("a_gq", [128, 3], F32)
        g_k = sb("a_gk", [128, 3], F32)
        gmax = sb("a_gmax", [128, 8], F32)
        gabs = sb("a_gabs", [128, 8], BF16)
        gT = sb("a_gT", [8, 128], BF16)
        gred = sb("a_gred", [8, 2], BF16)
        negB = sb("a_negB", [128, 1], F32)
        bias_sb = sb("a_bias", [128, 3, 2, 128], F32)
        raw = sb("a_raw", [128, PIECE], BF16)
        sq = sb("a_sq", [128, PIECE], BF16)
        rs = sb("a_rs", [128, PIECE], F32)
        vT_sb = sb("a_vT", [128, n_tok], BF16)
        accO = sb("a_accO", [128, n_tok], F32)
        accL = sb("a_accL", [128, n_tok], F32)
        yb = sb("a_yb", [128, PIECE], BF16)
        qn = sb("a_qn", [128, n_tok], BF16)
        kn = sb("a_kn", [128, n_tok + 128 * DMAX], BF16)
        vg = sb("a_vg", [128, n_tok + 128 * DMAX], BF16)
        sS = sb("a_sS", [128, 4, 2, 128], F32)
        pP = sb("a_pP", [128, 4, 2, 128], BF16)
        ps_n = ps("a_psn", [128, 512], F32)
        ps_t = ps("a_pst", [128, 128], BF16)
        ps_s = ps("a_pss", [128, 4, 2, 128], F32)
        ps_O = ps("a_psO", [128, QT], F32)
        ps_L = ps("a_psL", [128, QT], F32)
        ps_g = ps("a_psg", [128, 128], F32)

        sc.op("pool", lambda e: e.memset(ones[:, :], 1.0), writes=["ones"])
        sc.dma("sp", ident[:, :], ident_d[:, :], "c_ident", writes=["ident"])
        sc.dma("sp", g_q[:, :], qg[:, :], "c_gq", writes=["g_q"])
        sc.dma("sp", g_k[:, :], kg[:, :], "c_gk", writes=["g_k"])
        sc.dma("sp", bias_sb[:, :, :, :], bias.rearrange("g p h j -> p g h j"), "c_bias", writes=["bias"])
        sc.dma("sp", vT_sb[:, :], vT, "c_v", writes=["vT"])

        def bcast_absmax(gain_sb, col, gtag):
            sc.op("dve", lambda e: e.tensor_scalar(gmax[:, 0:3], gain_sb[:, :], -1.0, None, ALU.mult),
                  reads=[gtag, "gmax"], writes=["gmax"])
            sc.op("dve", lambda e: e.tensor_tensor(gmax[:, 0:3], gmax[:, 0:3], gain_sb[:, :], ALU.max),
                  reads=[gtag, "gmax"], writes=["gmax"])
            sc.op("dve", lambda e: e.reduce_max(gmax[:, 4:5], gmax[:, 0:3], AX.X), reads=["gmax"], writes=["gmax"])
            sc.op("dve", lambda e: e.tensor_copy(gabs[:, 0:1], gmax[:, 4:5]), reads=["gmax"], writes=["gabs"])
            sc.op("pe", lambda e: e.transpose(ps_t[0:1, :], gabs[:, 0:1], ident[:, :]), reads=["gabs", "ident"], writes=["ps_t"])
            sc.op("dve", lambda e: e.tensor_copy(gT[0:1, :], ps_t[0:1, :]), reads=["ps_t"], writes=["gT"])
            sc.op("dve", lambda e: e.reduce_max(gred[0:1, 0:1], gT[0:1, :], AX.X), reads=["gT"], writes=["gred"])
            sc.op("pe", lambda e: e.matmul(ps_g[:, col:col + 1], ones[0:1, :], gred[0:1, 0:1], start=True, stop=True),
                  reads=["gred", "ones"], writes=["ps_g"])
            sc.op("dve", lambda e: e.tensor_copy(gmax[:, 5 + col:6 + col], ps_g[:, col:col + 1]), reads=["ps_g", "gmax"], writes=["gmax"])
        sc.op("pool", lambda e: e.memset(gmax[:, :], 0.0), writes=["gmax"])
        bcast_absmax(g_q, 0, "g_q")
        bcast_absmax(g_k, 1, "g_k")
        sc.op("dve", lambda e: e.scalar_tensor_tensor(negB[:, :], gmax[:, 5:6], -1.02 * float(np.sqrt(128.0)),
                                                      gmax[:, 6:7], ALU.mult, ALU.mult),
              reads=["gmax"], writes=["negB"])

        def norm_into(src, gain_sb, g, dst, dst_is_k, extra_scale):
            d = ATT_PATTERNS[g][1]
            L = n_tok // d
            name = "kn" if dst_is_k else "qn"
            if dst_is_k:
                sc.op("pool", lambda e: e.memset(dst[:, :], 0.0), writes=[name])
            for T0 in range(0, n_tok, PIECE):
                sc.dma("sp", raw[:, :], src[:, T0:T0 + PIECE], "a_raw", writes=["raw"])
                sc.op("act", lambda e: e.activation(out=sq[:, :], in_=raw[:, :], func=AF.Square), reads=["raw"], writes=["sq"])
                for t0 in range(0, PIECE, 512):
                    sc.op("pe", lambda e, t0=t0: e.matmul(ps_n[:, :], ones[:, :], sq[:, t0:t0 + 512], start=True, stop=True),
                          reads=["sq", "ones"], writes=["ps_n"])
                    sc.op("dve", lambda e, t0=t0: e.tensor_scalar(rs[:, t0:t0 + 512], ps_n[:, :], 1.0 / 128, EPS, ALU.mult, ALU.add),
                          reads=["ps_n"], writes=["rs"])
                sc.op("act", lambda e: e.activation(out=rs[:, :], in_=rs[:, :], func=AF.Sqrt), reads=["rs"], writes=["rs"])
                sc.op("dve", lambda e: e.reciprocal(rs[:, :], rs[:, :]), reads=["rs"], writes=["rs"])
                if extra_scale != 1.0:
                    sc.op("dve", lambda e: e.tensor_scalar(rs[:, :], rs[:, :], extra_scale, None, ALU.mult), reads=["rs"], writes=["rs"])
                l0, l1 = T0 // d, (T0 + PIECE) // d
                if dst_is_k:
                    out_ap = dst[:, 0:d * (L + 128)].rearrange("p (r l) -> p r l", r=d)[:, :, HALF + l0:HALF + l1]
                else:
                    out_ap = dst[:, 0:n_tok].rearrange("p (r l) -> p r l", r=d)[:, :, l0:l1]
                in_raw = raw[:, :].rearrange("p (l r) -> p r l", r=d)
                in_rs = rs[:, :].rearrange("p (l r) -> p r l", r=d)
                sc.op("dve", lambda e, out_ap=out_ap, in_raw=in_raw, in_rs=in_rs: e.scalar_tensor_tensor(
                    out_ap, in_raw, gain_sb[:, g:g + 1], in_rs, ALU.mult, ALU.mult),
                    reads=["raw", "rs", "g_q", "g_k", name], writes=[name])

        first = True
        for g in range(3):
            d = ATT_PATTERNS[g][1]
            L = n_tok // d
            nb = L // 128 + 1
            Lp = L + 128
            norm_into(q_of(g), g_q, g, qn, False, float(128.0 ** -0.5))
            norm_into(k_of(g), g_k, g, kn, True, 1.0)
            sc.op("pool", lambda e: e.memset(vg[:, :], 0.0), writes=["vg"])
            v_res = vT_sb[:, :].rearrange("p (l r) -> p r l", r=d)
            for r in range(d):
                for b in range(nb):
                    lo, hi = max(128 * b - HALF, 0), min(128 * b + HALF, L)
                    p0 = lo - (128 * b - HALF)
                    n = hi - lo
                    col = (r * nb + b) * 128
                    sc.op("pe", lambda e, vr=v_res, r=r, lo=lo, hi=hi, n=n: e.transpose(ps_t[0:n, :], vr[:, r, lo:hi], ident[:, :]),
                          reads=["vT", "ident"], writes=["ps_t"])
                    sc.op("act", lambda e, p0=p0, n=n, col=col: e.copy(vg[p0:p0 + n, col:col + 128], ps_t[0:n, :]),
                          reads=["ps_t", "vg"], writes=["vg"])
            accO_v = accO[:, :].rearrange("p (l r) -> p r l", r=d)
            accL_v = accL[:, :].rearrange("p (l r) -> p r l", r=d)
            for r in range(d):
                for q0 in range(0, L, QT):
                    nt = min(QT, L - q0) // 128
                    for a in range(nt):
                        qa = q0 // 128 + a
                        for h in range(2):
                            kcol = r * Lp + 128 * (qa + h)
                            sc.op("pe", lambda e, a=a, h=h, kcol=kcol, r=r, qa=qa, L=L: e.matmul(
                                ps_s[:, a, h, :], kn[:, kcol:kcol + 128], qn[:, r * L + 128 * qa:r * L + 128 * qa + 128],
                                start=True, stop=True), reads=["kn", "qn"], writes=["ps_s"])
                    sc.op("dve", lambda e, g=g, nt=nt: e.tensor_tensor(
                        sS[:, 0:nt, :, :], ps_s[:, 0:nt, :, :],
                        bias_sb[:, g, :, :].unsqueeze(1).to_broadcast([128, nt, 2, 128]),
                        ALU.add), reads=["ps_s", "bias"], writes=["sS"])
                    sc.op("act", lambda e, nt=nt: e.activation(out=pP[:, 0:nt, :, :], in_=sS[:, 0:nt, :, :], func=AF.Exp, bias=negB[:, 0:1]),
                          reads=["sS", "negB"], writes=["pP"])
                    if q0 == 0:
                        sc.op("pool", lambda e: e.memset(pP[0:64, 0, 0, :], 0.0), reads=["pP"], writes=["pP"])
                    if q0 + 128 * nt == L:
                        sc.op("pool", lambda e, nt=nt: e.memset(pP[64:128, nt - 1, 1, :], 0.0), reads=["pP"], writes=["pP"])
                    for a in range(nt):
                        qa = q0 // 128 + a
                        for h in range(2):
                            col = (r * nb + qa + h) * 128
                            sc.op("pe", lambda e, a=a, h=h, col=col: e.matmul(
                                ps_O[:, 128 * a:128 * a + 128], vg[:, col:col + 128], pP[:, a, h, :],
                                start=(h == 0), stop=(h == 1)), reads=["vg", "pP"], writes=["ps_O"])
                            sc.op("pe", lambda e, a=a, h=h: e.matmul(
                                ps_L[:, 128 * a:128 * a + 128], ones[:, :], pP[:, a, h, :],
                                start=(h == 0), stop=(h == 1)), reads=["ones", "pP"], writes=["ps_L"])
                    n = 128 * nt
                    if first:
                        sc.op("dve", lambda e, av=accO_v, r=r, q0=q0, n=n: e.tensor_copy(av[:, r, q0:q0 + n], ps_O[:, 0:n]),
                              reads=["ps_O"], writes=["accO"])
                        sc.op("act", lambda e, av=accL_v, r=r, q0=q0, n=n: e.copy(av[:, r, q0:q0 + n], ps_L[:, 0:n]),
                              reads=["ps_L"], writes=["accL"])
                    else:
                        sc.op("dve", lambda e, av=accO_v, r=r, q0=q0, n=n: e.tensor_tensor(av[:, r, q0:q0 + n], av[:, r, q0:q0 + n], ps_O[:, 0:n], ALU.add),
                              reads=["ps_O", "accO"], writes=["accO"])
                        sc.op("dve", lambda e, av=accL_v, r=r, q0=q0, n=n: e.tensor_tensor(av[:, r, q0:q0 + n], av[:, r, q0:q0 + n], ps_L[:, 0:n], ALU.add),
                              reads=["ps_L", "accL"], writes=["accL"])
            first = False
        sc.op("dve", lambda e: e.reciprocal(accL[:, :], accL[:, :]), reads=["accL"], writes=["accL"])
        for T0 in range(0, n_tok, PIECE):
            sc.op("dve", lambda e, T0=T0: e.tensor_tensor(yb[:, :], accO[:, T0:T0 + PIECE], accL[:, T0:T0 + PIECE], ALU.mult),
                  reads=["accO", "accL", "yb"], writes=["yb"])
            sc.dma("sp", yT[:, T0:T0 + PIECE], yb[:, :], "a_yb", reads=["yb"], writes=["y_out"])
        sc.final_wait("sp", ["y_out", "yb"])
        sc.run()


def build_att(n_tok):
    nc = bass.Bass("TRN2", target_bir_lowering=False)
    qT = nc.dram_tensor("qT", [3, 128, n_tok], BF16, kind="ExternalInput").ap()
    kT = nc.dram_tensor("kT", [3, 128, n_tok], BF16, kind="ExternalInput").ap()
    vT = nc.dram_tensor("vT", [128, n_tok], BF16, kind="ExternalInput").ap()
    qg = nc.dram_tensor("qg", [128, 3], F32, kind="ExternalInput").ap()
    kg = nc.dram_tensor("kg", [128, 3], F32, kind="ExternalInput").ap()
    bias = nc.dram_tensor("bias", [3, 128, 2, 128], F32, kind="ExternalInput").ap()
    ident_d = nc.dram_tensor("ident", [128, 128], BF16, kind="ExternalInput").ap()
    yT = nc.dram_tensor("yT", [128, n_tok], BF16, kind="ExternalOutput").ap()
    emit_att(nc, lambda g: qT[g], lambda g: kT[g], vT[:, :], qg, kg, bias, ident_d, yT, n_tok, "")
    return nc


def gdn_consts():
    i = np.arange(128)[:, None]; j = np.arange(128)[None, :]
    same = (i // 64) == (j // 64)
    c = {}
    c["ident_f"] = np.eye(128, dtype=np.float32)
    c["ones_f"] = np.ones((128, 128), np.float32)
    c["mstrict"] = np.stack([(same & (i > j)), (same & (i < j))]).astype(np.float32)
    c["mincl"] = np.stack([(same & (i >= j)), (same & (i <= j))]).astype(np.float32)
    r = i; m = j
    c["cum"] = np.stack([(same & (r <= m)), (same & (r >= m))]).astype(np.float32)
    lastf = np.where(np.arange(128) < 64, 63, 127)[None, :]; lastb = np.where(np.arange(128) < 64, 0, 64)[None, :]
    c["sel"] = np.stack([(r == lastf), (r == lastb)]).astype(np.float32)
    selh = np.zeros((2, 2, 128, 128), np.float32)
    for dr in range(2):
        for hf in range(2):
            selh[dr, hf, (63 + 64 * hf) if dr == 0 else 64 * hf, :] = 1.0
    c["selh"] = selh
    return c


GDN_IN = (("qraw", [2, 128, None], BF16), ("kraw", [2, 128, None], BF16), ("vraw", [4, 128, None], BF16), ("zraw", [4, 128, None], BF16),
          ("abr", [16, None], BF16))
GDN_PAR = (("cw", [128, 8, 5], F32), ("alog", [128, 8], F32), ("dtb", [128, 8], F32), ("ng", [128, 128], F32),
           ("ident_f", [128, 128], F32), ("ident_b", [128, 128], BF16), ("ones_f", [128, 128], F32),
           ("mstrict", [2, 128, 128], F32), ("mincl", [2, 128, 128], F32), ("cum", [2, 128, 128], F32), ("sel", [2, 128, 128], F32),
           ("selh", [2, 2, 128, 128], F32))


def build_gdn(n_tok):
    nc = bass.Bass("TRN2", target_bir_lowering=False)
    aps = {}
    for (name, shape, dt) in GDN_IN + GDN_PAR:
        aps[name] = nc.dram_tensor(name, [n_tok if v is None else v for v in shape], dt, kind="ExternalInput").ap()
    yg = nc.dram_tensor("yg", [n_tok, 512], BF16, kind="ExternalOutput").ap()
    ofw = nc.dram_tensor("ofw_scr", [4, n_tok, 128], F32).ap()
    emit_gdn(nc, aps, yg, ofw, n_tok)
    return nc


def emit_gdn(nc, aps, yg, ofw, n_tok):
    TB = 512; NP = 4
    nblk = n_tok // TB
    qraw, kraw, vraw, zraw, abr = (aps[k] for k in ("qraw", "kraw", "vraw", "zraw", "abr"))
    cw, alog, dtb, ngd = (aps[k] for k in ("cw", "alog", "dtb", "ng"))
    c_ident_f, c_ident_b, c_ones_f = aps["ident_f"], aps["ident_b"], aps["ones_f"]
    c_mstrict, c_mincl, c_cum, c_sel, c_selh = (aps[k] for k in ("mstrict", "mincl", "cum", "sel", "selh"))
    with contextlib.ExitStack() as st:
        sb = lambda name, shape, dt: st.enter_context(nc.sbuf_tensor(name, shape, dt))
        ps = lambda name, shape, dt: st.enter_context(nc.psum_tensor(name, shape, dt))
        sc = Sched(nc, st)
        V = lambda fn, r, w: sc.op("dve", fn, reads=r, writes=w)
        A = lambda fn, r, w: sc.op("act", fn, reads=r, writes=w)
        G = lambda fn, r, w: sc.op("pool", fn, reads=r, writes=w)
        T = lambda fn, r, w: sc.op("pe", fn, reads=r, writes=w)
        identf = sb("identf", [128, 128], F32); identb = sb("identb", [128, 128], BF16); onesf = sb("onesf", [128, 128], F32)
        mstr = sb("mstr", [128, 2, 128], F32); minc = sb("minc", [128, 2, 128], F32)
        cum = sb("cum_sb", [128, 2, 128], F32); sel = sb("sel_sb", [128, 2, 128], F32); selh = sb("selh_sb", [128, 4, 128], F32)
        cw_sb = sb("cw_sb", [128, 8, 5], F32); nega = sb("nega", [128, 8], F32); dtb_sb = sb("dtb_sb", [128, 8], F32)
        ng_sb = sb("ng_sb", [128, 128], F32)
        for (t_, d_, nm) in ((identf, c_ident_f, "identf"), (identb, c_ident_b, "identb"), (onesf, c_ones_f, "onesf"),
                             (nega, alog, "nega"), (dtb_sb, dtb, "dtb"), (ng_sb, ngd, "ng"), (cw_sb, cw, "cw")):
            sc.dma("sp", t_[:], d_[:] if len(d_.shape) == 2 else d_[:, :, :], "c_" + nm, writes=[nm])
        sc.dma("sp", mstr[:, :, :], c_mstrict.rearrange("r p j -> p r j"), "c_mstr", writes=["mstr"])
        sc.dma("sp", minc[:, :, :], c_mincl.rearrange("r p j -> p r j"), "c_minc", writes=["minc"])
        sc.dma("sp", cum[:, :, :], c_cum.rearrange("r p j -> p r j"), "c_cum", writes=["cum"])
        sc.dma("sp", sel[:, :, :], c_sel.rearrange("r p j -> p r j"), "c_sel", writes=["sel"])
        sc.dma("sp", selh[:, :, :], c_selh.rearrange("r h p j -> p (r h) j"), "c_selh", writes=["selh"])
        A(lambda e: e.activation(out=nega[:, :], in_=nega[:, :], func=AF.Exp), ["nega"], ["nega"])
        V(lambda e: e.tensor_scalar(nega[:, :], nega[:, :], -1.0, None, ALU.mult), ["nega"], ["nega"])

        raw = sb("raw_g", [128, TB + 4], BF16); rawf = sb("rawf", [128, TB + 4], F32)
        acc = sb("acc_g", [128, TB], F32); sil = sb("sil_g", [128, TB], F32); sq = sb("sq_g", [128, TB], F32); rn = sb("rn_g", [128, TB], F32)
        fm = [sb("fm%d" % i, [128, TB], BF16) for i in range(8)]
        tm = [sb("tm%d" % i, [128, NP, 128], BF16) for i in range(8)]
        zt = [sb("zt%d" % i, [128, NP, 128], F32) for i in range(4)]
        KK = [sb("KK%d" % i, [128, NP, 128], F32) for i in range(2)]; QK = [sb("QK%d" % i, [128, NP, 128], F32) for i in range(2)]
        ab_fm = sb("ab_fm", [16, TB], BF16); ab_tm = sb("ab_tm", [128, NP, 16], F32)
        gt = {n: sb("gt_" + n, [128, NP, 4], F32) for n in ("x", "nx", "ax", "e", "l", "g", "beta", "nbeta", "G", "eG", "gl", "tail", "bG", "d0", "d1")}
        diagG = sb("diagG", [128, NP, 128], F32); dec = sb("dec", [128, NP, 128], F32); t1 = sb("t1", [128, NP, 128], F32)
        Ab = [sb("Ab%d" % i, [128, NP, 128], F32) for i in range(2)]; Bb = [sb("Bb%d" % i, [128, NP, 128], F32) for i in range(2)]
        intra = sb("intra", [128, NP, 128], BF16); X = sb("X_g", [128, NP, 256], F32); qd = sb("qd", [128, NP, 128], BF16)
        scan = [[{n: sb("sc%d_%d_%s" % (bb, vh, n), [128, NP, 128], BF16) for n in ("u", "wT", "qdT", "inT", "kt")} for vh in range(4)] for bb in range(2)]
        dS = [[sb("dS%d_%d" % (bb, hf), [128, NP, 4], F32) for hf in range(2)] for bb in range(2)]
        oblk = [sb("oblk%d" % vh, [128, NP, 128], F32) for vh in range(4)]
        ofl = sb("ofl", [128, NP, 128], F32); junk = sb("junk", [128, 128], F32); ss = sb("ss_g", [128, NP], F32); yb = sb("yb", [128, NP, 128], BF16)
        Sst = [sb("S%d" % vh, [128, 128], F32) for vh in range(4)]; Sbf = [sb("Sbf%d" % vh, [128, 128], BF16) for vh in range(4)]
        vnew = [sb("vnew%d" % vh, [128, 128], BF16) for vh in range(4)]
        psM = ps("psM", [128, NP, 128], F32); psX = ps("psX", [128, NP, 256], F32); psA = ps("psA", [128, NP, 128], F32); psB = ps("psB", [128, NP, 128], F32)
        psT = ps("psT", [128, NP, 128], BF16)
        psg = psB[:, 0, :]
        pss = [ps("pss%d" % i, [128, 3, 128], F32) for i in range(2)]
        bc = lambda ap: ap.unsqueeze(2).to_broadcast([128, NP, 128])
        bcp = lambda ap: ap.unsqueeze(1).to_broadcast([128, NP, 128])

        def prep_block(dr, blk, bb):
            t0 = blk * TB
            lo, hi = max(t0 - 2, 0), min(t0 + TB + 2, n_tok)
            srcs = [(qraw, 0), (qraw, 1), (kraw, 0), (kraw, 1), (vraw, 0), (vraw, 1), (vraw, 2), (vraw, 3)]
            for ti, (src, idx) in enumerate(srcs):
                G(lambda e: e.memset(raw[:, :], 0.0), [], ["raw"])
                sc.dma("sp", raw[:, lo - (t0 - 2):hi - (t0 - 2)], src[idx, :, lo:hi], "raw_g", writes=["raw"])
                V(lambda e: e.tensor_copy(rawf[:, :], raw[:, :]), ["raw"], ["rawf"])
                V(lambda e, ti=ti: e.tensor_scalar(acc[:, :], rawf[:, 0:TB], cw_sb[:, ti, 0:1], None, ALU.mult), ["rawf", "cw"], ["acc"])
                for j in range(1, 5):
                    V(lambda e, ti=ti, j=j: e.scalar_tensor_tensor(acc[:, :], rawf[:, j:j + TB], cw_sb[:, ti, j:j + 1], acc[:, :], ALU.mult, ALU.add),
                      ["rawf", "cw", "acc"], ["acc"])
                if ti >= 4:
                    A(lambda e, ti=ti: e.activation(out=fm[ti][:, :], in_=acc[:, :], func=AF.Silu), ["acc"], [("fm", ti)])
                else:
                    A(lambda e: e.activation(out=sil[:, :], in_=acc[:, :], func=AF.Silu), ["acc"], ["sil"])
                    V(lambda e: e.tensor_tensor(sq[:, :], sil[:, :], sil[:, :], ALU.mult), ["sil"], ["sq"])
                    T(lambda e: e.matmul(psX[:, 0:2, :], onesf[:, :], sq[:, :], start=True, stop=True), ["sq", "onesf"], [("psX", 0)])
                    V(lambda e: e.tensor_scalar(rn[:, :], psX[:, 0:2, :], EPS, None, ALU.add), [("psX", 0)], ["rn"])
                    A(lambda e: e.activation(out=rn[:, :], in_=rn[:, :], func=AF.Sqrt), ["rn"], ["rn"])
                    V(lambda e: e.reciprocal(rn[:, :], rn[:, :]), ["rn"], ["rn"])
                    scl = float(128.0 ** -0.5) if ti < 2 else 1.0
                    V(lambda e, ti=ti, scl=scl: e.scalar_tensor_tensor(fm[ti][:, :], sil[:, :], scl, rn[:, :], ALU.mult, ALU.mult), ["sil", "rn"], [("fm", ti)])
                for p in range(NP):
                    T(lambda e, ti=ti, p=p: e.transpose(psT[:, p, :], fm[ti][:, 128 * p:128 * p + 128], identb[:, :]), [("fm", ti), "identb"], ["psT"])
                A(lambda e, ti=ti: e.copy(tm[ti][:, :, :], psT[:, :, :]), ["psT"], [("tm", ti)])
            sc.dma("sp", ab_fm[:, :], abr[:, t0:t0 + TB], "ab_fm", writes=["ab_fm"])
            for p in range(NP):
                T(lambda e, p=p: e.transpose(psT[:, p, 0:16], ab_fm[:, 128 * p:128 * p + 128], identb[0:16, 0:16]), ["ab_fm", "identb"], ["psT"])
            V(lambda e: e.tensor_copy(ab_tm[:, :, :], psT[:, :, 0:16]), ["psT"], ["ab_tm"])
            g_ = gt
            row = lambda t_, dr=dr: t_[:, dr * 4:dr * 4 + 4].unsqueeze(1).to_broadcast([128, NP, 4])
            V(lambda e: e.tensor_tensor(g_["x"][:], ab_tm[:, :, dr * 4:dr * 4 + 4], row(dtb_sb), ALU.add), ["ab_tm", "dtb"], ["g_x"])
            V(lambda e: e.tensor_scalar(g_["nx"][:], g_["x"][:], -1.0, None, ALU.mult), ["g_x"], ["g_nx"])
            V(lambda e: e.tensor_tensor(g_["ax"][:], g_["x"][:], g_["nx"][:], ALU.max), ["g_x", "g_nx"], ["g_ax"])
            A(lambda e: e.activation(out=g_["e"][:], in_=g_["ax"][:], func=AF.Exp, scale=-1.0), ["g_ax"], ["g_e"])
            V(lambda e: e.tensor_scalar(g_["e"][:], g_["e"][:], 1.0, None, ALU.add), ["g_e"], ["g_e"])
            A(lambda e: e.activation(out=g_["l"][:], in_=g_["e"][:], func=AF.Ln), ["g_e"], ["g_l"])
            V(lambda e: e.scalar_tensor_tensor(g_["g"][:], g_["x"][:], 0.0, g_["l"][:], ALU.max, ALU.add), ["g_x", "g_l"], ["g_g"])
            V(lambda e: e.tensor_tensor(g_["g"][:], g_["g"][:], row(nega), ALU.mult), ["g_g", "nega"], ["g_g"])
            A(lambda e: e.activation(out=g_["beta"][:], in_=ab_tm[:, :, 8 + dr * 4:12 + dr * 4], func=AF.Sigmoid), ["ab_tm"], ["g_beta"])
            V(lambda e: e.tensor_scalar(g_["nbeta"][:], g_["beta"][:], -1.0, None, ALU.mult), ["g_beta"], ["g_nbeta"])
            T(lambda e: e.matmul(psg[:, 0:16], cum[:, dr, :], g_["g"][:].rearrange("p a b -> p (a b)"), start=True, stop=True), ["g_g", "cum"], [("psB", 0)])
            V(lambda e: e.tensor_copy(g_["G"][:].rearrange("p a b -> p (a b)"), psg[:, 0:16]), [("psB", 0)], ["g_G"])
            A(lambda e: e.activation(out=g_["eG"][:], in_=g_["G"][:], func=AF.Exp), ["g_G"], ["g_eG"])
            T(lambda e: e.matmul(psg[:, 16:32], sel[:, dr, :], g_["G"][:].rearrange("p a b -> p (a b)"), start=True, stop=True), ["g_G", "sel"], [("psB", 0)])
            V(lambda e: e.tensor_tensor(g_["gl"][:].rearrange("p a b -> p (a b)"), psg[:, 16:32], g_["G"][:].rearrange("p a b -> p (a b)"), ALU.subtract), [("psB", 0), "g_G"], ["g_gl"])
            A(lambda e: e.activation(out=g_["tail"][:], in_=g_["gl"][:], func=AF.Exp), ["g_gl"], ["g_tail"])
            V(lambda e: e.tensor_tensor(g_["bG"][:], g_["beta"][:], g_["eG"][:], ALU.mult), ["g_beta", "g_eG"], ["g_bG"])
            for hf in range(2):
                T(lambda e, hf=hf: e.matmul(psg[:, 32 + 16 * hf:48 + 16 * hf], selh[:, dr * 2 + hf, :], g_["G"][:].rearrange("p a b -> p (a b)"), start=True, stop=True),
                  ["g_G", "selh"], [("psB", 0)])
                A(lambda e, hf=hf: e.activation(out=dS[bb][hf][:].rearrange("p a b -> p (a b)"), in_=psg[:, 32 + 16 * hf:48 + 16 * hf], func=AF.Exp), [("psB", 0)], [("dS", bb)])
            for qh in range(2):
                for p in range(NP):
                    cs = slice(128 * p, 128 * p + 128)
                    T(lambda e, qh=qh, p=p, cs=cs: e.matmul(psA[:, p, :], fm[2 + qh][:, cs], fm[2 + qh][:, cs], start=True, stop=True), [("fm", 2 + qh)], [("psA", 0), ("psA", 1)])
                    T(lambda e, qh=qh, p=p, cs=cs: e.matmul(psB[:, p, :], fm[qh][:, cs], fm[2 + qh][:, cs], start=True, stop=True), [("fm", qh), ("fm", 2 + qh)], [("psB", 0), ("psB", 1)])
                V(lambda e, qh=qh: e.tensor_copy(KK[qh][:], psA[:]), [("psA", 0), ("psA", 1)], [("KK", qh)])
                A(lambda e, qh=qh: e.copy(QK[qh][:], psB[:]), [("psB", 0), ("psB", 1)], [("QK", qh)])
            for vh in range(4):
                qh = vh // 2
                sb_ = scan[bb][vh]
                Gc = g_["G"][:, :, vh]
                V(lambda e, Gc=Gc: e.tensor_tensor(diagG[:], bcp(identf[:, :]), bc(Gc), ALU.mult), ["identf", "g_G"], ["diagG"])
                for p in range(NP):
                    T(lambda e, p=p: e.matmul(psM[:, p, :], onesf[:, :], diagG[:, p, :], start=True, stop=True), ["diagG", "onesf"], ["psM"])
                V(lambda e, Gc=Gc: e.tensor_tensor(dec[:], bc(Gc), psM[:], ALU.subtract), ["psM", "g_G"], ["dec"])
                V(lambda e: e.tensor_scalar_min(dec[:], dec[:], 0.0), ["dec"], ["dec"])
                A(lambda e: e.activation(out=dec[:], in_=dec[:], func=AF.Exp), ["dec"], ["dec"])
                V(lambda e, qh=qh: e.tensor_tensor(t1[:], dec[:], KK[qh][:], ALU.mult), ["dec", ("KK", qh)], ["t1"])
                V(lambda e, vh=vh: e.tensor_tensor(t1[:], t1[:], bc(g_["nbeta"][:, :, vh]), ALU.mult), ["t1", "g_nbeta"], ["t1"])
                V(lambda e: e.tensor_tensor(Ab[0][:], t1[:], bcp(mstr[:, dr, :]), ALU.mult), ["t1", "mstr"], [("Ab", 0, 0), ("Ab", 0, 1)])
                G(lambda e, qh=qh: e.tensor_tensor(t1[:], dec[:], QK[qh][:], ALU.mult), ["dec", ("QK", qh), "t1"], ["t1"])
                G(lambda e: e.tensor_tensor(intra[:], t1[:], bcp(minc[:, dr, :]), ALU.mult), ["t1", "minc"], ["intra"])
                for p in range(NP):
                    T(lambda e, p=p: e.transpose(psM[:, p, :], Ab[0][:, p, :], identf[:, :]), [("Ab", 0, 0), ("Ab", 0, 1), "identf"], ["psM"])
                    T(lambda e, p=p: e.transpose(psT[:, p, :], intra[:, p, :], identb[:, :]), ["intra", "identb"], ["psT"])
                V(lambda e: e.tensor_copy(Bb[0][:], psM[:]), ["psM"], [("Bb", 0, 0), ("Bb", 0, 1)])
                A(lambda e, sb_=sb_: e.copy(sb_["inT"][:], psT[:]), ["psT"], [("scan", bb, vh)])
                V(lambda e, vh=vh: e.tensor_tensor(X[:, :, 0:128], tm[4 + vh][:], bc(g_["beta"][:, :, vh]), ALU.mult), [("tm", 4 + vh), "g_beta"], [("X", 0), ("X", 1)])
                V(lambda e, vh=vh, qh=qh: e.tensor_tensor(X[:, :, 128:256], tm[2 + qh][:], bc(g_["bG"][:, :, vh]), ALU.mult), [("tm", 2 + qh), "g_bG", ("X", 0), ("X", 1)], [("X", 0), ("X", 1)])
                for k in range(6):
                    a_, b_ = Ab[k % 2], Bb[k % 2]
                    an, bn = Ab[(k + 1) % 2], Bb[(k + 1) % 2]
                    for hp in range(2):
                        for p in (2 * hp, 2 * hp + 1):
                            T(lambda e, p=p, b_=b_: e.matmul(psX[:, p, :], b_[:, p, :], X[:, p, :], start=True, stop=True), [("Bb", k % 2, hp), ("X", hp)], [("psX", hp)])
                        if k < 5:
                            for p in (2 * hp, 2 * hp + 1):
                                T(lambda e, p=p, a_=a_, b_=b_: e.matmul(psA[:, p, :], b_[:, p, :], a_[:, p, :], start=True, stop=True), [("Ab", k % 2, hp), ("Bb", k % 2, hp)], [("psA", hp)])
                                T(lambda e, p=p, a_=a_, b_=b_: e.matmul(psB[:, p, :], a_[:, p, :], b_[:, p, :], start=True, stop=True), [("Ab", k % 2, hp), ("Bb", k % 2, hp)], [("psB", hp)])
                    for hp in range(2):
                        prs = slice(2 * hp, 2 * hp + 2)
                        V(lambda e, prs=prs: e.tensor_tensor(X[:, prs, :], X[:, prs, :], psX[:, prs, :], ALU.add), [("psX", hp), ("X", hp)], [("X", hp)])
                        if k < 5:
                            A(lambda e, an=an, prs=prs: e.copy(an[:, prs, :], psA[:, prs, :]), [("psA", hp)], [("Ab", (k + 1) % 2, hp)])
                            (A if hp == 0 else V)(lambda e, bn=bn, prs=prs, hp=hp: (e.copy if hp == 0 else e.tensor_copy)(bn[:, prs, :], psB[:, prs, :]), [("psB", hp)], [("Bb", (k + 1) % 2, hp)])
                A(lambda e, sb_=sb_: e.copy(sb_["u"][:], X[:, :, 0:128]), [("X", 0), ("X", 1)], [("scan", bb, vh)])
                for p in range(NP):
                    T(lambda e, p=p: e.transpose(psM[:, p, :], X[:, p, 128:256], identf[:, :]), [("X", p // 2), "identf"], ["psM"])
                V(lambda e, sb_=sb_: e.tensor_copy(sb_["wT"][:], psM[:]), ["psM", ("scan", bb, vh)], [("scan", bb, vh)])
                V(lambda e, vh=vh, qh=qh: e.tensor_tensor(qd[:], tm[qh][:], bc(g_["eG"][:, :, vh]), ALU.mult), [("tm", qh), "g_eG"], ["qd"])
                for p in range(NP):
                    T(lambda e, p=p: e.transpose(psT[:, p, :], qd[:, p, :], identb[:, :]), ["qd", "identb"], ["psT"])
                A(lambda e, sb_=sb_: e.copy(sb_["qdT"][:], psT[:]), ["psT", ("scan", bb, vh)], [("scan", bb, vh)])
                G(lambda e, sb_=sb_, vh=vh, qh=qh: e.tensor_tensor(sb_["kt"][:], tm[2 + qh][:], bc(g_["tail"][:, :, vh]), ALU.mult),
                  [("tm", 2 + qh), "g_tail", ("scan", bb, vh)], [("scan", bb, vh)])

        def scan_block(dr, blk, bb):
            order = [(p, hf) for p in range(NP) for hf in range(2)]
            if dr == 1:
                order = order[::-1]
            for si, (p, hf) in enumerate(order):
                rows = slice(64 * hf, 64 * hf + 64)
                for vh in range(4):
                    sb_ = scan[bb][vh]
                    pp = pss[vh % 2]
                    T(lambda e, sb_=sb_, p=p, vh=vh, pp=pp: e.matmul(pp[:, 0, :], sb_["wT"][:, p, :], Sbf[vh][:, :], start=True, stop=True),
                      [("scan", bb, vh), ("Sbf", vh)], [("pss", vh % 2)])
                    V(lambda e, sb_=sb_, p=p, vh=vh, pp=pp, rows=rows: e.tensor_tensor(vnew[vh][rows, :], sb_["u"][rows, p, :], pp[rows, 0, :], ALU.subtract),
                      [("pss", vh % 2), ("scan", bb, vh)], [("vnew", vh)])
                    T(lambda e, sb_=sb_, p=p, vh=vh, pp=pp: e.matmul(pp[:, 1, :], sb_["qdT"][:, p, :], Sbf[vh][:, :], start=True, stop=False),
                      [("scan", bb, vh), ("Sbf", vh)], [("pss", vh % 2)])
                    T(lambda e, sb_=sb_, p=p, vh=vh, pp=pp, rows=rows: e.matmul(pp[:, 1, :], sb_["inT"][rows, p, :], vnew[vh][rows, :], start=False, stop=True),
                      [("scan", bb, vh), ("vnew", vh)], [("pss", vh % 2)])
                    A(lambda e, p=p, vh=vh, pp=pp, rows=rows: e.copy(oblk[vh][rows, p, :], pp[rows, 1, :]), [("pss", vh % 2)], [("oblk", vh)])
                    T(lambda e, sb_=sb_, p=p, vh=vh, pp=pp, rows=rows: e.matmul(pp[:, 2, :], sb_["kt"][rows, p, :], vnew[vh][rows, :], start=True, stop=True),
                      [("scan", bb, vh), ("vnew", vh)], [("pss", vh % 2)])
                    V(lambda e, p=p, vh=vh, pp=pp, hf=hf: e.scalar_tensor_tensor(Sst[vh][:, :], Sst[vh][:, :], dS[bb][hf][:, p, vh:vh + 1], pp[:, 2, :], ALU.mult, ALU.add),
                      [("pss", vh % 2), ("dS", bb), ("S", vh)], [("S", vh)])
                    A(lambda e, vh=vh: e.copy(Sbf[vh][:, :], Sst[vh][:, :]), [("S", vh)], [("Sbf", vh)])

        def finish_block(dr, blk):
            t0 = blk * TB
            for vh in range(4):
                dst = ofw[vh, t0:t0 + TB, :].rearrange("(p i) d -> i p d", i=128)
                if dr == 0:
                    sc.dma("sp", dst, oblk[vh][:, :, :], ("oblk", vh), reads=[("oblk", vh)], writes=[("ofw", vh, blk)])
                    continue
                sc.dma("sp", ofl[:, :, :], dst, "ofl", reads=[("ofw", vh, blk)], writes=["ofl"])
                V(lambda e, vh=vh: e.tensor_tensor(ofl[:], ofl[:], oblk[vh][:], ALU.add), ["ofl", ("oblk", vh)], ["ofl"])
                for p in range(NP):
                    A(lambda e, p=p: e.activation(out=junk[:, :], in_=ofl[:, p, :], func=AF.Square, accum_out=ss[:, p:p + 1]), ["ofl", "junk", "ss"], ["junk", "ss"])
                V(lambda e: e.tensor_scalar(ss[:, :], ss[:, :], 1.0 / 128, EPS, ALU.mult, ALU.add), ["ss"], ["ss"])
                A(lambda e: e.activation(out=ss[:, :], in_=ss[:, :], func=AF.Sqrt), ["ss"], ["ss"])
                V(lambda e: e.reciprocal(ss[:, :], ss[:, :]), ["ss"], ["ss"])
                V(lambda e: e.tensor_tensor(ofl[:], ofl[:], bc(ss[:, :]), ALU.mult), ["ofl", "ss"], ["ofl"])
                V(lambda e: e.tensor_tensor(ofl[:], ofl[:], bcp(ng_sb[:, :]), ALU.mult), ["ofl", "ng"], ["ofl"])
                sc.dma("sp", raw[:, 0:TB], zraw[vh, :, t0:t0 + TB], "raw_g", writes=["raw"])
                for p in range(NP):
                    T(lambda e, p=p: e.transpose(psT[:, p, :], raw[:, 128 * p:128 * p + 128], identb[:, :]), ["raw", "identb"], ["psT"])
                A(lambda e, vh=vh: e.activation(out=zt[vh][:], in_=psT[:], func=AF.Silu), ["psT"], [("zt", vh)])
                V(lambda e, vh=vh: e.tensor_tensor(yb[:], ofl[:], zt[vh][:], ALU.mult), ["ofl", ("zt", vh)], ["yb"])
                sc.dma("sp", yg[t0:t0 + TB, 128 * vh:128 * vh + 128].rearrange("(p i) d -> i p d", i=128), yb[:, :, :], "yb", reads=["yb"], writes=["yg_out"])

        for dr in range(2):
            for vh in range(4):
                G(lambda e, vh=vh: e.memset(Sst[vh][:, :], 0.0), [], [("S", vh)])
                G(lambda e, vh=vh: e.memset(Sbf[vh][:, :], 0.0), [], [("Sbf", vh)])
            blocks = list(range(nblk)) if dr == 0 else list(range(nblk))[::-1]
            for bi, blk in enumerate(blocks):
                bb = bi % 2
                prep_block(dr, blk, bb)
                scan_block(dr, blk, bb)
                finish_block(dr, blk)
        sc.final_wait("sp", ["yg_out", "yb"])
        sc.run()


L1_CHUNKS = 27
OFF_QA, OFF_KA, OFF_VA = 0, 6144, 12288
OFF_QG, OFF_KG, OFF_VG, OFF_ZG, OFF_A, OFF_B, OFF_GA, OFF_GB = 14336, 16384, 18432, 22528, 26624, 26688, 26752, 28800


def w1_cols(c):
    cols = []
    r = np.arange(128)
    for s_ in range(2):
        hs = 2 * c + s_
        for g in range(3):
            cols.append(OFF_QA + g * 2048 + hs * 128 + r)
        for g in range(3):
            cols.append(OFF_KA + g * 2048 + hs * 128 + r)
        cols.append(OFF_VA + hs * 128 + r)
    for qh in range(2):
        cols.append(OFF_QG + (2 * c + qh) * 128 + r)
    for qh in range(2):
        cols.append(OFF_KG + (2 * c + qh) * 128 + r)
    for vh in range(4):
        cols.append(OFF_VG + (4 * c + vh) * 128 + r)
    for vh in range(4):
        cols.append(OFF_ZG + (4 * c + vh) * 128 + r)
    ab = np.full(128, -1)
    for dr in range(2):
        for vh in range(4):
            ab[dr * 4 + vh] = OFF_A + dr * 32 + 4 * c + vh
            ab[8 + dr * 4 + vh] = OFF_B + dr * 32 + 4 * c + vh
    cols.append(ab)
    return np.concatenate(cols)


def build_launch1(n_tok):
    nc = bass.Bass("TRN2", target_bir_lowering=False)
    di = lambda name, shape, dt: nc.dram_tensor(name, shape, dt, kind="ExternalInput").ap()
    n_cols = L1_CHUNKS * 128
    xT = di("xT", [D, n_tok], F32); w1 = di("w1", [D, n_cols], F32); gain1 = di("gain1", [128, NCH], F32)
    qg = di("qg", [128, 3], F32); kg = di("kg", [128, 3], F32); bias = di("att_bias", [2, 3, 128, 2, 128], F32)
    aps = {}
    for (name, shape, dt) in GDN_PAR:
        aps[name] = di(name, shape, dt)
    yatt = nc.dram_tensor("yatt", [2, 128, n_tok], BF16, kind="ExternalOutput").ap()
    yg = nc.dram_tensor("yg", [n_tok, 512], BF16, kind="ExternalOutput").ap()
    pT = nc.dram_tensor("pT_scr", [n_cols, n_tok], BF16).ap()
    ofw = nc.dram_tensor("ofw_scr", [4, n_tok, 128], F32).ap()
    emit_stage1(nc, xT, w1, gain1, pT, n_cols, n_tok)
    for s_ in range(2):
        base = 7 * s_ * 128
        emit_att(nc, lambda g, base=base: pT[base + 128 * g:base + 128 * g + 128, :],
                 lambda g, base=base: pT[base + 128 * (3 + g):base + 128 * (4 + g), :],
                 pT[base + 768:base + 896, :], qg, kg, bias[s_], aps["ident_b"], yatt[s_], n_tok, "_s%d" % s_)
    g0 = 14 * 128
    aps["qraw"] = pT[g0:g0 + 256, :].rearrange("(i p) t -> i p t", p=128)
    aps["kraw"] = pT[g0 + 256:g0 + 512, :].rearrange("(i p) t -> i p t", p=128)
    aps["vraw"] = pT[g0 + 512:g0 + 1024, :].rearrange("(i p) t -> i p t", p=128)
    aps["zraw"] = pT[g0 + 1024:g0 + 1536, :].rearrange("(i p) t -> i p t", p=128)
    aps["abr"] = pT[g0 + 1536:g0 + 1552, :]
    emit_gdn(nc, aps, yg, ofw, n_tok)
    return nc


def launch1_inputs(inp, c, n_tok=S):
    import ml_dtypes
    f = np.float32
    cols = w1_cols(c)
    w_in = inp["w_in"][0]
    w1 = np.zeros((D, cols.size), f)
    m = cols >= 0
    w1[:, m] = w_in[:, cols[m]]
    C = gdn_consts()
    cwf = inp["gdn_conv_w"][0]
    chans = [np.arange(128) + (2 * c + qh) * 128 for qh in range(2)] + [2048 + np.arange(128) + (2 * c + qh) * 128 for qh in range(2)] \
        + [4096 + np.arange(128) + (4 * c + vh) * 128 for vh in range(4)]
    cw = np.stack([cwf[:, ch].T for ch in chans], axis=1)
    sel8 = lambda a: np.concatenate([a[0, 4 * c:4 * c + 4], a[1, 4 * c:4 * c + 4]])
    d = {
        "xT": np.ascontiguousarray(inp["x"][0, :n_tok].T), "w1": w1,
        "gain1": np.ascontiguousarray(inp["norm1_gain"][0].reshape(NCH, 128).T),
        "qg": np.ascontiguousarray(inp["q_norm_gain"][0].T), "kg": np.ascontiguousarray(inp["k_norm_gain"][0].T),
        "att_bias": np.stack([att_bias_consts(2 * c), att_bias_consts(2 * c + 1)]),
        "cw": np.ascontiguousarray(cw.astype(f)),
        "alog": np.ascontiguousarray(np.broadcast_to(sel8(inp["gdn_a_log"][0])[None, :], (128, 8)).astype(f)),
        "dtb": np.ascontiguousarray(np.broadcast_to(sel8(inp["gdn_dt_bias"][0])[None, :], (128, 8)).astype(f)),
        "ng": np.ascontiguousarray(np.broadcast_to(inp["gdn_norm_gain"][0][None, :], (128, 128)).astype(f)),
        "ident_f": C["ident_f"], "ident_b": C["ident_f"].astype(ml_dtypes.bfloat16), "ones_f": C["ones_f"],
        "mstrict": C["mstrict"], "mincl": C["mincl"], "cum": C["cum"], "sel": C["sel"], "selh": C["selh"],
    }
    return d


NT = 1024
NEXP = 64


def emit_l2a(nc, a, x2tm, h2b_d, wt_d):
    HT = 512
    xT_v = a["xT"].rearrange("(c p) t -> p c t", p=128)
    yA_v = a["yAT"].rearrange("(c p) t -> p c t", p=128)
    yB_v = a["yBT"].rearrange("(c p) t -> p c t", p=128)
    wgate_v = a["wgate"].rearrange("(c p) n -> p c n", p=128)
    wA_v = a["wA"].rearrange("(c p) n -> p c n", p=128)
    wB_v = a["wB"].rearrange("(c p) n -> p c n", p=128)
    wo_v = a["wo"].rearrange("(c p) n -> p c n", p=128)
    wr_v = a["wr"].rearrange("(c p) n -> p c n", p=128)
    h2b_v = h2b_d.rearrange("(c p) t -> p c t", p=128)
    with contextlib.ExitStack() as st:
        sb = lambda name, shape, dt: st.enter_context(nc.sbuf_tensor(name, shape, dt))
        ps = lambda name, shape, dt: st.enter_context(nc.psum_tensor(name, shape, dt))
        sc = Sched(nc, st)
        V = lambda fn, r, w: sc.op("dve", fn, reads=r, writes=w)
        A = lambda fn, r, w: sc.op("act", fn, reads=r, writes=w)
        G = lambda fn, r, w: sc.op("pool", fn, reads=r, writes=w)
        T = lambda fn, r, w: sc.op("pe", fn, reads=r, writes=w)
        ones = sb("b_ones", [128, 128], BF16); identf = sb("b_identf", [128, 128], F32)
        g1 = sb("b_g1", [128, NCH], F32); g2 = sb("b_g2", [128, NCH], F32)
        wr_sb = sb("b_wr", [128, NCH, 72], F32); br_sb = sb("b_br", [128, 72], F32)
        xf = sb("b_xf", [128, NCH, HT], F32); hb = sb("b_hb", [128, NCH, HT], BF16); xsq = sb("b_xsq", [128, NCH, HT], BF16)
        yA = sb("b_yA", [128, NCH, HT], BF16); bufY = sb("b_bufY", [128, 32 * HT * 2 // 4], F32)
        yB = bufY[:, :].bitcast(BF16).rearrange("p (c t) -> p c t", t=HT)
        stg = bufY[:, :].rearrange("p (k d) -> p k d", d=D)
        h2f = bufY[:, :].rearrange("p (c t) -> p c t", t=HT)
        merged = sb("b_merged", [128, NCH, HT], BF16)
        rstd = sb("b_rstd", [128, HT], F32)
        wgA = [sb("b_wgA%d" % i, [128, NCH, 128], BF16) for i in range(2)]; wgB = [sb("b_wgB%d" % i, [128, NCH, 128], BF16) for i in range(2)]
        wa = [sb("b_wa%d" % i, [128, NCH, 128], BF16) for i in range(2)]; wb_ = [sb("b_wb%d" % i, [128, 32, 128], BF16) for i in range(2)]
        wo = [sb("b_wo%d" % i, [128, NCH, 128], BF16) for i in range(2)]
        tA = sb("b_tA", [128, HT], F32); tB = sb("b_tB", [128, HT], F32); sA = sb("b_sA", [128, HT], F32); sBt = sb("b_sB", [128, HT], F32)
        rt = {n: sb("b_r_" + n, [128, 72], F32) for n in ("lg", "ge", "oh", "tmp", "ig", "m1k", "in2", "m2k", "wl")}
        rs_ = {n: sb("b_s_" + n, [128, 1], F32) for n in ("gmax", "ngmax", "gs", "gw", "m1", "m2", "d12", "w1", "w2")}
        wt_st = sb("b_wtst", [128, 4, 64], F32)
        ps_ss = ps("b_psss", [128, HT], F32)
        psGA = ps("b_psGA", [128, HT], F32); psGB = ps("b_psGB", [128, HT], F32); psMA = ps("b_psMA", [128, HT], F32); psMB = ps("b_psMB", [128, HT], F32)
        psT = ps("b_psT", [128, 4, 128], F32); psR = ps("b_psR", [128, 128], F32)

        G(lambda e: e.memset(ones[:, :], 1.0), [], ["ones"])
        sc.dma("sp", identf[:, :], a["ident_f"][:, :], "c1", writes=["identf"])
        sc.dma("sp", g1[:, :], a["gain1"][:, :], "c2", writes=["g1"])
        sc.dma("sp", g2[:, :], a["gain2"][:, :], "c3", writes=["g2"])
        sc.dma("sp", wr_sb[:, :, :], wr_v, "c4", writes=["wr"])
        sc.dma("sp", br_sb[:, :], a["br"][:, :], "c5", writes=["br"])

        def rms(tag):
            A(lambda e: e.activation(out=xsq[:, :, :], in_=xf[:, :, :], func=AF.Square), ["xf"], ["xsq"])
            for c in range(NCH):
                T(lambda e, c=c: e.matmul(ps_ss[:, :], ones[:, :], xsq[:, c, :], start=(c == 0), stop=(c == NCH - 1)), ["xsq", "ones"], ["ps_ss"])
            V(lambda e: e.tensor_scalar(rstd[:, :], ps_ss[:, :], 1.0 / D, EPS, ALU.mult, ALU.add), ["ps_ss"], ["rstd"])
            A(lambda e: e.activation(out=rstd[:, :], in_=rstd[:, :], func=AF.Sqrt), ["rstd"], ["rstd"])
            V(lambda e: e.reciprocal(rstd[:, :], rstd[:, :]), ["rstd"], ["rstd"])

        for hf in range(NT // HT):
            hs = slice(hf * HT, (hf + 1) * HT)
            sc.dma("sp", xf[:, :, :], xT_v[:, :, hs], "l_xf", writes=["xf"])
            sc.dma("sp", yA[:, :, :], yA_v[:, :, hs], "l_yA", writes=["yA"])
            sc.dma("sp", yB, yB_v[:, :, hs], "l_bufY", writes=["bufY"])
            rms("n1")
            for c in range(NCH):
                V(lambda e, c=c: e.tensor_scalar(hb[:, c, :], xf[:, c, :], g1[:, c:c + 1], None, ALU.mult), ["xf", "g1"], ["hb"])
            for j in range(NCH):
                jb = j % 2
                js = slice(j * 128, (j + 1) * 128)
                j2 = slice(2048 + j * 128, 2048 + (j + 1) * 128)
                sc.dma("pool", wgA[jb][:, :, :], wgate_v[:, :, js], ("wgA", jb), writes=[("wgA", jb)])
                sc.dma("pool", wgB[jb][:, :, :], wgate_v[:, :, j2], ("wgB", jb), writes=[("wgB", jb)])
                sc.dma("pool", wa[jb][:, :, :], wA_v[:, :, js], ("wa", jb), writes=[("wa", jb)])
                sc.dma("pool", wb_[jb][:, :, :], wB_v[:, :, js], ("wb", jb), writes=[("wb", jb)])
                for c in range(NCH):
                    T(lambda e, c=c, jb=jb: e.matmul(psGA[:, :], wgA[jb][:, c, :], hb[:, c, :], start=(c == 0), stop=(c == NCH - 1)), [("wgA", jb), "hb"], ["psGA"])
                for c in range(NCH):
                    T(lambda e, c=c, jb=jb: e.matmul(psGB[:, :], wgB[jb][:, c, :], hb[:, c, :], start=(c == 0), stop=(c == NCH - 1)), [("wgB", jb), "hb"], ["psGB"])
                for c in range(NCH):
                    T(lambda e, c=c, jb=jb: e.matmul(psMA[:, :], wa[jb][:, c, :], yA[:, c, :], start=(c == 0), stop=(c == NCH - 1)), [("wa", jb), "yA"], ["psMA"])
                for c in range(32):
                    T(lambda e, c=c, jb=jb: e.matmul(psMB[:, :], wb_[jb][:, c, :], yB[:, c, :], start=(c == 0), stop=(c == 31)), [("wb", jb), "bufY"], ["psMB"])
                V(lambda e: e.tensor_tensor(tA[:, :], psGA[:, :], rstd[:, :], ALU.mult), ["psGA", "rstd"], ["tA"])
                A(lambda e: e.activation(out=sA[:, :], in_=tA[:, :], func=AF.Sigmoid), ["tA"], ["sA"])
                V(lambda e: e.tensor_tensor(tB[:, :], psGB[:, :], rstd[:, :], ALU.mult), ["psGB", "rstd"], ["tB"])
                A(lambda e: e.activation(out=sBt[:, :], in_=tB[:, :], func=AF.Sigmoid), ["tB"], ["sB"])
                V(lambda e: e.tensor_tensor(tA[:, :], sA[:, :], psMA[:, :], ALU.mult), ["sA", "psMA", "tA"], ["tA"])
                V(lambda e: e.tensor_tensor(tB[:, :], sBt[:, :], psMB[:, :], ALU.mult), ["sB", "psMB", "tB"], ["tB"])
                V(lambda e, j=j: e.tensor_tensor(merged[:, j, :], tA[:, :], tB[:, :], ALU.add), ["tA", "tB"], ["merged"])
            for dch in range(NCH):
                db = dch % 2
                sc.dma("pool", wo[db][:, :, :], wo_v[:, :, dch * 128:(dch + 1) * 128], ("wo", db), writes=[("wo", db)])
                for c in range(NCH):
                    T(lambda e, c=c, db=db: e.matmul(psGA[:, :], wo[db][:, c, :], merged[:, c, :], start=(c == 0), stop=(c == NCH - 1)), [("wo", db), "merged"], ["psGA"])
                V(lambda e, dch=dch: e.tensor_tensor(xf[:, dch, :], xf[:, dch, :], psGA[:, :], ALU.add), ["psGA", "xf"], ["xf"])
            for k in range(4):
                for c in range(NCH):
                    T(lambda e, c=c, k=k: e.transpose(psT[:, c % 4, :], xf[:, c, k * 128:(k + 1) * 128], identf[:, :]), ["xf", "identf"], ["psT"])
                    if c % 4 == 3:
                        V(lambda e, c=c, k=k: e.tensor_copy(stg[:, k, (c - 3) * 128:(c + 1) * 128], psT[:, :, :].rearrange("p a b -> p (a b)")),
                          ["psT", "bufY"], ["bufY"])
            sc.dma("sp", x2tm[hs, :].rearrange("(k p) d -> p k d", p=128), stg, "s_bufY", reads=["bufY"], writes=["x2tm"])
            rms("n2")
            for c in range(NCH):
                V(lambda e, c=c: e.scalar_tensor_tensor(h2f[:, c, :], xf[:, c, :], g2[:, c:c + 1], rstd[:, :], ALU.mult, ALU.mult),
                  ["xf", "g2", "rstd", "bufY"], ["bufY"])
            A(lambda e: e.copy(hb[:, :, :], h2f), ["bufY", "hb"], ["hb"])
            sc.dma("sp", h2b_v[:, :, hs], hb[:, :, :], "s_h2b", reads=["hb"], writes=["h2b_d"])
            for k in range(4):
                for c in range(NCH):
                    T(lambda e, c=c, k=k: e.matmul(psR[:, 0:72], h2f[:, c, k * 128:(k + 1) * 128], wr_sb[:, c, :], start=(c == 0), stop=(c == NCH - 1)),
                      ["bufY", "wr"], ["psR"])
                r = rt; s_ = rs_
                V(lambda e: e.tensor_tensor(r["lg"][:, :], psR[:, 0:72], br_sb[:, :], ALU.add), ["psR", "br"], ["r_lg"])
                V(lambda e: e.reduce_max(s_["gmax"][:, :], r["lg"][:, 0:8], AX.X), ["r_lg"], ["s_gmax"])
                V(lambda e: e.tensor_scalar(s_["ngmax"][:, :], s_["gmax"][:, :], -1.0, None, ALU.mult), ["s_gmax"], ["s_ngmax"])
                A(lambda e: e.activation(out=r["ge"][:, 0:8], in_=r["lg"][:, 0:8], func=AF.Exp, bias=s_["ngmax"][:, 0:1]), ["r_lg", "s_ngmax"], ["r_ge"])
                V(lambda e: e.reduce_sum(s_["gs"][:, :], r["ge"][:, 0:8], AX.X), ["r_ge"], ["s_gs"])
                V(lambda e: e.reciprocal(s_["gw"][:, :], s_["gs"][:, :]), ["s_gs"], ["s_gw"])
                V(lambda e: e.tensor_scalar(r["oh"][:, 0:8], r["lg"][:, 0:8], s_["gmax"][:, 0:1], None, ALU.is_equal), ["r_lg", "s_gmax"], ["r_oh"])
                le = r["lg"][:, 8:72].rearrange("p (g j) -> p g j", j=8)
                V(lambda e, le=le: e.tensor_tensor(r["tmp"][:, 0:64].rearrange("p (g j) -> p g j", j=8), le,
                                                   r["oh"][:, 0:8].unsqueeze(2).to_broadcast([128, 8, 8]), ALU.mult), ["r_lg", "r_oh"], ["r_tmp"])
                V(lambda e: e.reduce_sum(r["ig"][:, 0:8], r["tmp"][:, 0:64].rearrange("p (g j) -> p j g", j=8), AX.X), ["r_tmp"], ["r_ig"])
                V(lambda e: e.reduce_max(s_["m1"][:, :], r["ig"][:, 0:8], AX.X), ["r_ig"], ["s_m1"])
                V(lambda e: e.tensor_scalar(r["m1k"][:, 0:8], r["ig"][:, 0:8], s_["m1"][:, 0:1], None, ALU.is_equal), ["r_ig", "s_m1"], ["r_m1k"])
                V(lambda e: e.scalar_tensor_tensor(r["in2"][:, 0:8], r["m1k"][:, 0:8], -1e30, r["ig"][:, 0:8], ALU.mult, ALU.add), ["r_m1k", "r_ig"], ["r_in2"])
                V(lambda e: e.reduce_max(s_["m2"][:, :], r["in2"][:, 0:8], AX.X), ["r_in2"], ["s_m2"])
                V(lambda e: e.tensor_scalar(r["m2k"][:, 0:8], r["in2"][:, 0:8], s_["m2"][:, 0:1], None, ALU.is_equal), ["r_in2", "s_m2"], ["r_m2k"])
                V(lambda e: e.tensor_tensor(s_["d12"][:, :], s_["m1"][:, :], s_["m2"][:, :], ALU.subtract), ["s_m1", "s_m2"], ["s_d12"])
                A(lambda e: e.activation(out=s_["w1"][:, :], in_=s_["d12"][:, :], func=AF.Sigmoid), ["s_d12"], ["s_w1"])
                A(lambda e: e.activation(out=s_["w2"][:, :], in_=s_["d12"][:, :], func=AF.Sigmoid, scale=-1.0), ["s_d12"], ["s_w2"])
                V(lambda e: e.tensor_scalar(r["wl"][:, 0:8], r["m1k"][:, 0:8], s_["w1"][:, 0:1], None, ALU.mult), ["r_m1k", "s_w1"], ["r_wl"])
                V(lambda e: e.scalar_tensor_tensor(r["wl"][:, 0:8], r["m2k"][:, 0:8], s_["w2"][:, 0:1], r["wl"][:, 0:8], ALU.mult, ALU.add), ["r_m2k", "s_w2", "r_wl"], ["r_wl"])
                V(lambda e: e.tensor_scalar(r["wl"][:, 0:8], r["wl"][:, 0:8], s_["gw"][:, 0:1], None, ALU.mult), ["r_wl", "s_gw"], ["r_wl"])
                V(lambda e, k=k: e.tensor_tensor(wt_st[:, k, :].rearrange("p (g j) -> p g j", j=8), r["oh"][:, 0:8].unsqueeze(2).to_broadcast([128, 8, 8]),
                                                 r["wl"][:, 0:8].unsqueeze(1).to_broadcast([128, 8, 8]), ALU.mult), ["r_oh", "r_wl", "wt_st"], ["wt_st"])
            sc.dma("sp", wt_d[hs, :].rearrange("(k p) e -> p k e", p=128), wt_st[:, :, :], "s_wt", reads=["wt_st"], writes=["wt_d"])
        sc.final_wait("sp", ["x2tm", "h2b_d", "wt_d", "bufY", "hb", "wt_st"])
        sc.run()


def emit_l2b(nc, a, x2tm, h2b_d, wt_d, out, n_exp=NEXP):
    HT = 512
    with contextlib.ExitStack() as st:
        sb = lambda name, shape, dt: st.enter_context(nc.sbuf_tensor(name, shape, dt))
        ps = lambda name, shape, dt: st.enter_context(nc.psum_tensor(name, shape, dt))
        sc = Sched(nc, st)
        V = lambda fn, r, w: sc.op("dve", fn, reads=r, writes=w)
        A = lambda fn, r, w: sc.op("act", fn, reads=r, writes=w)
        T = lambda fn, r, w: sc.op("pe", fn, reads=r, writes=w)
        acc = sb("m_acc", [128, 8, D], F32); h2b = sb("m_h2b", [128, NCH, NT], BF16); wt = sb("m_wt", [128, 8, 64], F32)
        wg = [sb("m_wg%d" % i, [128, NCH, 512], BF16) for i in range(2)]
        wu2 = [sb("m_wu%d" % i, [128, NCH, 512], BF16) for i in range(2)]; wd2 = [sb("m_wd%d" % i, [128, 4, D], BF16) for i in range(2)]
        act = sb("m_act", [128, 4, NT], BF16); slb = [sb("m_slb%d" % i, [128, HT], BF16) for i in range(2)]
        psG = [ps("m_psG%d" % i, [128, HT], F32) for i in range(2)]; psU = [ps("m_psU%d" % i, [128, HT], F32) for i in range(2)]
        psD = [ps("m_psD%d" % i, [128, 512], F32) for i in range(2)]
        sc.dma("sp", acc[:, :, :], x2tm.rearrange("(k p) d -> p k d", p=128), "l_acc", writes=["acc"])
        sc.dma("sp", h2b[:, :, :], h2b_d.rearrange("(c p) t -> p c t", p=128), "l_h2b", writes=["h2b"])
        sc.dma("sp", wt[:, :, :], wt_d.rearrange("(k p) e -> p k e", p=128), "l_wt", writes=["wt"])
        n = 0
        for e_ in range(n_exp):
            eb = e_ % 2
            sc.dma("pool", wg[eb][:, :, :], a["w_gate"][e_].rearrange("(c p) f -> p c f", p=128), ("wg", eb), writes=[("wg", eb)])
            wu, wd = wu2[eb], wd2[eb]
            sc.dma("pool", wu[:, :, :], a["w_up"][e_].rearrange("(c p) f -> p c f", p=128), ("wu", eb), writes=[("wu", eb)])
            sc.dma("pool", wd[:, :, :], a["w_down"][e_].rearrange("(c p) d -> p c d", p=128), ("wd", eb), writes=[("wd", eb)])
            for hf in range(NT // HT):
                hs = slice(hf * HT, (hf + 1) * HT)
                for fch in range(4):
                    pb = n % 2; n += 1
                    fs = slice(fch * 128, (fch + 1) * 128)
                    for c in range(NCH):
                        T(lambda e, c=c, eb=eb, fs=fs, hs=hs, pb=pb: e.matmul(psG[pb][:, :], wg[eb][:, c, fs], h2b[:, c, hs], start=(c == 0), stop=(c == NCH - 1)),
                          [("wg", eb), "h2b"], [("psG", pb)])
                    for c in range(NCH):
                        T(lambda e, wu=wu, c=c, fs=fs, hs=hs, pb=pb: e.matmul(psU[pb][:, :], wu[:, c, fs], h2b[:, c, hs], start=(c == 0), stop=(c == NCH - 1)),
                          [("wu", eb), "h2b"], [("psU", pb)])
                    A(lambda e, pb=pb: e.activation(out=slb[pb][:, :], in_=psG[pb][:, :], func=AF.Silu), [("psG", pb)], [("slb", pb)])
                    V(lambda e, pb=pb, fch=fch, hs=hs: e.tensor_tensor(act[:, fch, hs], slb[pb][:, :], psU[pb][:, :], ALU.mult), [("slb", pb), ("psU", pb)], ["act"])
            for k in range(8):
                for db in range(4):
                    pd = (k * 4 + db) % 2
                    ds_ = slice(db * 512, (db + 1) * 512)
                    for fch in range(4):
                        T(lambda e, wd=wd, k=k, fch=fch, ds_=ds_, pd=pd: e.matmul(psD[pd][:, :], act[:, fch, k * 128:(k + 1) * 128], wd[:, fch, ds_], start=(fch == 0), stop=(fch == 3)),
                          ["act", ("wd", eb)], [("psD", pd)])
                    V(lambda e, k=k, ds_=ds_, pd=pd, e_=e_: e.scalar_tensor_tensor(acc[:, k, ds_], psD[pd][:, :], wt[:, k, e_:e_ + 1], acc[:, k, ds_], ALU.mult, ALU.add),
                      [("psD", pd), "wt", "acc"], ["acc"])
        sc.dma("sp", out.rearrange("(k p) d -> p k d", p=128), acc[:, :, :], "s_out", reads=["acc"], writes=["out"])
        sc.final_wait("sp", ["out"])
        sc.run()


L2_IN = (("xT", [D, NT], F32), ("yAT", [2048, NT], BF16), ("yBT", [4096, NT], BF16), ("gain1", [128, NCH], F32), ("gain2", [128, NCH], F32),
         ("wgate", [D, 4096], F32), ("wA", [2048, D], F32), ("wB", [4096, D], F32), ("wo", [D, D], F32), ("wr", [D, 72], F32), ("br", [128, 72], F32),
         ("ident_f", [128, 128], F32), ("w_gate", [NEXP, D, 512], F32), ("w_up", [NEXP, D, 512], F32), ("w_down", [NEXP, 512, D], F32))


def build_launch2(n_exp=NEXP):
    nc = bass.Bass("TRN2", target_bir_lowering=False)
    a = {name: nc.dram_tensor(name, shape, dt, kind="ExternalInput").ap() for (name, shape, dt) in L2_IN}
    out = nc.dram_tensor("out", [NT, D], F32, kind="ExternalOutput").ap()
    x2tm = nc.dram_tensor("x2tm_scr", [NT, D], F32).ap()
    h2b_d = nc.dram_tensor("h2b_scr", [D, NT], BF16).ap()
    wt_d = nc.dram_tensor("wt_scr", [NT, 64], F32).ap()
    emit_l2a(nc, a, x2tm, h2b_d, wt_d)
    emit_l2b(nc, a, x2tm, h2b_d, wt_d, out, n_exp)
    return nc


def launch2_inputs(inp, c, yAT, yBT, shared):
    ts = slice(c * NT, (c + 1) * NT)
    d = dict(shared)
    d["xT"] = np.ascontiguousarray(inp["x"][0, ts].T)
    d["yAT"] = np.ascontiguousarray(yAT[:, ts]); d["yBT"] = np.ascontiguousarray(yBT[:, ts])
    return d


def launch2_shared(inp):
    f = np.float32
    return {
        "gain1": np.ascontiguousarray(inp["norm1_gain"][0].reshape(NCH, 128).T), "gain2": np.ascontiguousarray(inp["norm2_gain"][0].reshape(NCH, 128).T),
        "wgate": np.ascontiguousarray(inp["w_in"][0][:, OFF_GA:OFF_GA + 4096]), "wA": inp["w_branch_att"][0], "wB": inp["w_branch_gdn"][0], "wo": inp["w_out"][0],
        "wr": np.ascontiguousarray(np.concatenate([inp["w_group_router"][0], inp["w_expert_router"][0]], axis=1)),
        "br": np.ascontiguousarray(np.broadcast_to(np.concatenate([inp["b_group_router"][0], inp["b_expert_router"][0]])[None, :], (128, 72)).astype(f)),
        "ident_f": np.eye(128, dtype=f), "w_gate": inp["w_gate"][0], "w_up": inp["w_up"][0], "w_down": inp["w_down"][0],
    }


def kernel(**inputs):
    inp = {k: np.asarray(v) for k, v in inputs.items()}
    cores = list(range(NCORES))
    nc1 = build_launch1(S)
    res1 = run_bass_kernel_spmd(nc1, [launch1_inputs(inp, c) for c in cores], core_ids=cores).results
    yAT = np.concatenate([np.asarray(res1[c]["yatt"]).reshape(256, S) for c in cores], axis=0)
    yBT = np.concatenate([np.ascontiguousarray(np.asarray(res1[c]["yg"]).T) for c in cores], axis=0)
    del res1
    nc2 = build_launch2()
    shared = launch2_shared(inp)
    res2 = run_bass_kernel_spmd(nc2, [launch2_inputs(inp, c, yAT, yBT, shared) for c in cores], core_ids=cores).results
    out = np.concatenate([np.asarray(res2[c]["out"]) for c in cores], axis=0)
    return out.reshape(1, S, D).astype(np.float32)
```

```python
import contextlib
import numpy as np
import concourse.bass as bass
import concourse.mybir as mybir
from concourse.bass_utils import run_bass_kernel_spmd

F32 = mybir.dt.float32
BF16 = mybir.dt.bfloat16
AF = mybir.ActivationFunctionType
ALU = mybir.AluOpType
AX = mybir.AxisListType

D = 2048
S = 8192
NCORES = 8
NCH = D // 128
EPS = 1e-6

ENG_NAMES = ("pe", "dve", "act", "pool", "sp")


class Sched:
    def __init__(self, nc, stack):
        self.nc = nc
        self.stack = stack
        self.q = {k: [] for k in ENG_NAMES}
        self.sems = {}
        self.count = {}
        self.waited = {k: {} for k in ENG_NAMES}
        self.last_w = {}
        self.readers = {}

    def _sem(self, key):
        if key not in self.sems:
            name = "s%d" % len(self.sems)
            self.sems[key] = self.stack.enter_context(self.nc.semaphore(name))
            self.count[key] = 0
        return self.sems[key]

    def _deps(self, eng, reads, writes):
        deps = []
        for r in reads:
            if r in self.last_w:
                deps.append(self.last_w[r])
        for w in writes:
            if w in self.last_w:
                deps.append(self.last_w[w])
            deps.extend(self.readers.get(w, ()))
        waits = []
        for (key, val, src) in deps:
            if src == "pe" and eng == "pe":
                continue
            if self.waited[eng].get(key, 0) >= val:
                continue
            self.waited[eng][key] = val
            waits.append((key, val))
        return waits

    def _commit(self, tok, reads, writes):
        for w in writes:
            self.last_w[w] = tok
            self.readers[w] = []
        for r in reads:
            self.readers.setdefault(r, []).append(tok)

    def op(self, eng, fn, reads=(), writes=()):
        waits = self._deps(eng, reads, writes)
        key = ("e", eng)
        sem = self._sem(key)
        self.count[key] += 1
        tok = (key, self.count[key], eng)
        wl = [(self._sem(k), v) for (k, v) in waits]

        def emit(e, fn=fn, wl=wl, sem=sem):
            for (s, v) in wl:
                e.wait_ge(s, v)
            fn(e).then_inc(sem, 1)
        self.q[eng].append(emit)
        self._commit(tok, reads, writes)

    def dma(self, eng, out, in_, sem_key, reads=(), writes=(), **kw):
        waits = self._deps(eng, reads, writes)
        key = ("d", sem_key)
        sem = self._sem(key)
        self.count[key] += 16
        tok = (key, self.count[key], "dma")
        wl = [(self._sem(k), v) for (k, v) in waits]

        def emit(e, wl=wl, sem=sem):
            for (s, v) in wl:
                e.wait_ge(s, v)
            e.dma_start(out=out, in_=in_, **kw).then_inc(sem, 16)
        self.q[eng].append(emit)
        self._commit(tok, reads, writes)

    def final_wait(self, eng, resources):
        waits = self._deps(eng, resources, ())
        wl = [(self._sem(k), v) for (k, v) in waits]

        def emit(e, wl=wl):
            for (s, v) in wl:
                e.wait_ge(s, v)
        self.q[eng].append(emit)

    def run(self):
        nc = self.nc
        with nc.Block() as block:
            @block.tensor
            def _(e):
                for f in self.q["pe"]:
                    f(e)

            @block.vector
            def _(e):
                for f in self.q["dve"]:
                    f(e)

            @block.scalar
            def _(e):
                for f in self.q["act"]:
                    f(e)

            @block.gpsimd
            def _(e):
                for f in self.q["pool"]:
                    f(e)

            @block.sync
            def _(e):
                for f in self.q["sp"]:
                    f(e)


def emit_stage1(nc, xT, w, gain, pT, n_cols, n_tok, tok_blk=256):
    assert n_cols % 128 == 0 and n_tok % tok_blk == 0
    ncc = n_cols // 128
    ntt = n_tok // tok_blk
    xT_v = xT.rearrange("(c p) t -> p c t", p=128)
    w_v = w.rearrange("(c p) n -> p c n", p=128)
    with contextlib.ExitStack() as st:
        sb = lambda name, shape, dt: st.enter_context(nc.sbuf_tensor(name, shape, dt))
        ps = lambda name, shape, dt: st.enter_context(nc.psum_tensor(name, shape, dt))
        wb = sb("wb", [128, NCH, n_cols], BF16)
        g_sb = sb("g_sb", [128, NCH], F32)
        ones = sb("ones1", [128, 128], BF16)
        xf = [sb("xf%d" % i, [128, NCH, tok_blk], F32) for i in range(2)]
        xb = [sb("xb%d" % i, [128, NCH, tok_blk], BF16) for i in range(2)]
        xsq = [sb("xsq%d" % i, [128, NCH, tok_blk], BF16) for i in range(1)] * 2
        rstd = [sb("rstd%d" % i, [128, tok_blk], F32) for i in range(2)]
        ob = [sb("ob%d" % i, [128, tok_blk], BF16) for i in range(4)]
        ps_ss = ps("ps_ss", [128, tok_blk], F32)
        ps_o = [ps("ps_o%d" % i, [128, tok_blk], F32) for i in range(4)]
        sc = Sched(nc, st)

        sc.op("pool", lambda e: e.memset(ones[:, :], 1.0), writes=["ones"])
        sc.dma("sp", g_sb[:, :], gain[:, :], "g_sb", writes=["g_sb"])
        for c in range(NCH):
            sc.dma("pool", wb[:, c, :], w_v[:, c, :], "wb", writes=[("wb", c), "wb_all"])
        for c in range(NCH):
            sc.op("dve", lambda e, c=c: e.tensor_scalar(wb[:, c, :], wb[:, c, :], g_sb[:, c:c + 1], None, ALU.mult),
                  reads=[("wb", c), "wb_all", "g_sb"], writes=[("wb", c)])
        for t in range(ntt):
            b = t % 2
            tsl = slice(t * tok_blk, (t + 1) * tok_blk)
            sc.dma("sp", xf[b][:, :, :], xT_v[:, :, tsl], ("xf", b), writes=[("xf", b)])
            sc.op("act", lambda e, b=b: e.activation(out=xsq[b][:, :, :], in_=xf[b][:, :, :], func=AF.Square),
                  reads=[("xf", b)], writes=["xsq"])
            sc.op("dve", lambda e, b=b: e.tensor_copy(xb[b][:, :, :], xf[b][:, :, :]),
                  reads=[("xf", b)], writes=[("xb", b)])
            for c in range(NCH):
                sc.op("pe", lambda e, b=b, c=c: e.matmul(ps_ss[:, :], ones[:, :], xsq[b][:, c, :],
                                                         start=(c == 0), stop=(c == NCH - 1)),
                      reads=["xsq", "ones"], writes=["ps_ss"])
            sc.op("dve", lambda e, b=b: e.tensor_scalar(rstd[b][:, :], ps_ss[:, :], 1.0 / D, EPS, ALU.mult, ALU.add),
                  reads=["ps_ss"], writes=[("rstd", b)])
            sc.op("act", lambda e, b=b: e.activation(out=rstd[b][:, :], in_=rstd[b][:, :], func=AF.Sqrt),
                  reads=[("rstd", b)], writes=[("rstd", b)])
            sc.op("dve", lambda e, b=b: e.reciprocal(rstd[b][:, :], rstd[b][:, :]),
                  reads=[("rstd", b)], writes=[("rstd", b)])
            for j in range(ncc):
                k = (t * ncc + j) % 4
                for c in range(NCH):
                    sc.op("pe", lambda e, b=b, c=c, j=j, k=k: e.matmul(
                        ps_o[k][:, :], wb[:, c, j * 128:(j + 1) * 128], xb[b][:, c, :],
                        start=(c == 0), stop=(c == NCH - 1)),
                        reads=[("xb", b), ("wb", c)], writes=[("ps_o", k)])
                sc.op("dve", lambda e, b=b, k=k: e.tensor_tensor(ob[k][:, :], ps_o[k][:, :], rstd[b][:, :], ALU.mult),
                      reads=[("ps_o", k), ("rstd", b)], writes=[("ob", k)])
                sc.dma("sp", pT[j * 128:(j + 1) * 128, tsl], ob[k][:, :], ("ob", k),
                       reads=[("ob", k)], writes=["pT_out"])
        sc.final_wait("sp", [("ob", k) for k in range(4)] + ["pT_out"])
        for k in range(4):
            key = ("d", ("ob", k))
            if key in sc.sems:
                sc.q["sp"].append(lambda e, s=sc.sems[key], v=sc.count[key]: e.wait_ge(s, v))
        sc.run()


def build_stage1(n_cols, n_tok, tok_blk=256):
    nc = bass.Bass("TRN2", target_bir_lowering=False)
    xT = nc.dram_tensor("xT", [D, n_tok], F32, kind="ExternalInput").ap()
    w = nc.dram_tensor("w", [D, n_cols], F32, kind="ExternalInput").ap()
    gain = nc.dram_tensor("gain", [128, NCH], F32, kind="ExternalInput").ap()
    pT = nc.dram_tensor("pT", [n_cols, n_tok], BF16, kind="ExternalOutput").ap()
    emit_stage1(nc, xT, w, gain, pT, n_cols, n_tok, tok_blk)
    return nc


ATT_PATTERNS = ((128, 1), (512, 4), (2048, 16))
HALF = 64


def alibi_slopes_np():
    n = 48
    return np.exp2(-8.0 * np.arange(1, n + 1, dtype=np.float64) / n).reshape(3, 16)


def att_bias_consts(head_slot):
    out = np.zeros((3, 128, 2, 128), np.float32)
    i = np.arange(128)[:, None]
    j = np.arange(128)[None, :]
    sl = alibi_slopes_np()
    for g, (_, d) in enumerate(ATT_PATTERNS):
        for h, off in enumerate((-64, 64)):
            rel = i - j + off
            b = -sl[g, head_slot] * np.abs(rel) * d
            out[g, :, h, :] = np.where(np.abs(rel) <= HALF, b, -1e30)
    return out


def emit_att(nc, q_of, k_of, vT, qg, kg, bias, ident_d, yT, n_tok, tag):
    QT = 512
    PIECE = min(2048, n_tok)
    DMAX = 16
    with contextlib.ExitStack() as st:
        sb = lambda name, shape, dt: st.enter_context(nc.sbuf_tensor(name + tag, shape, dt))
        ps = lambda name, shape, dt: st.enter_context(nc.psum_tensor(name + tag, shape, dt))
        sc = Sched(nc, st)
        ones = sb("a_ones", [128, 128], BF16)
        ident = sb("a_ident", [128, 128], BF16)
        g_q = sb("a_gq", [128, 3], F32)
        g_k = sb("a_gk", [128, 3], F32)
        gmax = sb("a_gmax", [128, 8], F32)
        gabs = sb("a_gabs", [128, 8], BF16)
        gT = sb("a_gT", [8, 128], BF16)
        gred = sb("a_gred", [8, 2], BF16)
        negB = sb("a_negB", [128, 1], F32)
        bias_sb = sb("a_bias", [128, 3, 2, 128], F32)
        raw = sb("a_raw", [128, PIECE], BF16)
        sq = sb("a_sq", [128, PIECE], BF16)
        rs = sb("a_rs", [128, PIECE], F32)
        vT_sb = sb("a_vT", [128, n_tok], BF16)
        accO = sb("a_accO", [128, n_tok], F32)
        accL = sb("a_accL", [128, n_tok], F32)
        yb = sb("a_yb", [128, PIECE], BF16)
        qn = sb("a_qn", [128, n_tok], BF16)
        kn = sb("a_kn", [128, n_tok + 128 * DMAX], BF16)
        vg = sb("a_vg", [128, n_tok + 128 * DMAX], BF16)
        sS = sb("a_sS", [128, 4, 2, 128], F32)
        pP = sb("a_pP", [128, 4, 2, 128], BF16)
        ps_n = ps("a_psn", [128, 512], F32)
        ps_t = ps("a_pst", [128, 128], BF16)
        ps_s = ps("a_pss", [128, 4, 2, 128], F32)
        ps_O = ps("a_psO", [128, QT], F32)
        ps_L = ps("a_psL", [128, QT], F32)
        ps_g = ps("a_psg", [128, 128], F32)

        sc.op("pool", lambda e: e.memset(ones[:, :], 1.0), writes=["ones"])
        sc.dma("sp", ident[:, :], ident_d[:, :], "c_ident", writes=["ident"])
        sc.dma("sp", g_q[:, :], qg[:, :], "c_gq", writes=["g_q"])
        sc.dma("sp", g_k[:, :], kg[:, :], "c_gk", writes=["g_k"])
        sc.dma("sp", bias_sb[:, :, :, :], bias.rearrange("g p h j -> p g h j"), "c_bias", writes=["bias"])
        sc.dma("sp", vT_sb[:, :], vT, "c_v", writes=["vT"])

        def bcast_absmax(gain_sb, col, gtag):
            sc.op("dve", lambda e: e.tensor_scalar(gmax[:, 0:3], gain_sb[:, :], -1.0, None, ALU.mult),
                  reads=[gtag, "gmax"], writes=["gmax"])
            sc.op("dve", lambda e: e.tensor_tensor(gmax[:, 0:3], gmax[:, 0:3], gain_sb[:, :], ALU.max),
                  reads=[gtag, "gmax"], writes=["gmax"])
            sc.op("dve", lambda e: e.reduce_max(gmax[:, 4:5], gmax[:, 0:3], AX.X), reads=["gmax"], writes=["gmax"])
            sc.op("dve", lambda e: e.tensor_copy(gabs[:, 0:1], gmax[:, 4:5]), reads=["gmax"], writes=["gabs"])
            sc.op("pe", lambda e: e.transpose(ps_t[0:1, :], gabs[:, 0:1], ident[:, :]), reads=["gabs", "ident"], writes=["ps_t"])
            sc.op("dve", lambda e: e.tensor_copy(gT[0:1, :], ps_t[0:1, :]), reads=["ps_t"], writes=["gT"])
            sc.op("dve", lambda e: e.reduce_max(gred[0:1, 0:1], gT[0:1, :], AX.X), reads=["gT"], writes=["gred"])
            sc.op("pe", lambda e: e.matmul(ps_g[:, col:col + 1], ones[0:1, :], gred[0:1, 0:1], start=True, stop=True),
                  reads=["gred", "ones"], writes=["ps_g"])
            sc.op("dve", lambda e: e.tensor_copy(gmax[:, 5 + col:6 + col], ps_g[:, col:col + 1]), reads=["ps_g", "gmax"], writes=["gmax"])
        sc.op("pool", lambda e: e.memset(gmax[:, :], 0.0), writes=["gmax"])
        bcast_absmax(g_q, 0, "g_q")
        bcast_absmax(g_k, 1, "g_k")
        sc.op("dve", lambda e: e.scalar_tensor_tensor(negB[:, :], gmax[:, 5:6], -1.02 * float(np.sqrt(128.0)),
                                                      gmax[:, 6:7], ALU.mult, ALU.mult),
              reads=["gmax"], writes=["negB"])

        def norm_into(src, gain_sb, g, dst, dst_is_k, extra_scale):
            d = ATT_PATTERNS[g][1]
            L = n_tok // d
            name = "kn" if dst_is_k else "qn"
            if dst_is_k:
                sc.op("pool", lambda e: e.memset(dst[:, :], 0.0), writes=[name])
            for T0 in range(0, n_tok, PIECE):
                sc.dma("sp", raw[:, :], src[:, T0:T0 + PIECE], "a_raw", writes=["raw"])
                sc.op("act", lambda e: e.activation(out=sq[:, :], in_=raw[:, :], func=AF.Square), reads=["raw"], writes=["sq"])
                for t0 in range(0, PIECE, 512):
                    sc.op("pe", lambda e, t0=t0: e.matmul(ps_n[:, :], ones[:, :], sq[:, t0:t0 + 512], start=True, stop=True),
                          reads=["sq", "ones"], writes=["ps_n"])
                    sc.op("dve", lambda e, t0=t0: e.tensor_scalar(rs[:, t0:t0 + 512], ps_n[:, :], 1.0 / 128, EPS, ALU.mult, ALU.add),
                          reads=["ps_n"], writes=["rs"])
                sc.op("act", lambda e: e.activation(out=rs[:, :], in_=rs[:, :], func=AF.Sqrt), reads=["rs"], writes=["rs"])
                sc.op("dve", lambda e: e.reciprocal(rs[:, :], rs[:, :]), reads=["rs"], writes=["rs"])
                if extra_scale != 1.0:
                    sc.op("dve", lambda e: e.tensor_scalar(rs[:, :], rs[:, :], extra_scale, None, ALU.mult), reads=["rs"], writes=["rs"])
                l0, l1 = T0 // d, (T0 + PIECE) // d
                if dst_is_k:
                    out_ap = dst[:, 0:d * (L + 128)].rearrange("p (r l) -> p r l", r=d)[:, :, HALF + l0:HALF + l1]
                else:
                    out_ap = dst[:, 0:n_tok].rearrange("p (r l) -> p r l", r=d)[:, :, l0:l1]
                in_raw = raw[:, :].rearrange("p (l r) -> p r l", r=d)
                in_rs = rs[:, :].rearrange("p (l r) -> p r l", r=d)
                sc.op("dve", lambda e, out_ap=out_ap, in_raw=in_raw, in_rs=in_rs: e.scalar_tensor_tensor(
                    out_ap, in_raw, gain_sb[:, g:g + 1], in_rs, ALU.mult, ALU.mult),
                    reads=["raw", "rs", "g_q", "g_k", name], writes=[name])

        first = True
        for g in range(3):
            d = ATT_PATTERNS[g][1]
            L = n_tok // d
            nb = L // 128 + 1
            Lp = L + 128
            norm_into(q_of(g), g_q, g, qn, False, float(128.0 ** -0.5))
            norm_into(k_of(g), g_k, g, kn, True, 1.0)
            sc.op("pool", lambda e: e.memset(vg[:, :], 0.0), writes=["vg"])
            v_res = vT_sb[:, :].rearrange("p (l r) -> p r l", r=d)
            for r in range(d):
                for b in range(nb):
                    lo, hi = max(128 * b - HALF, 0), min(128 * b + HALF, L)
                    p0 = lo - (128 * b - HALF)
                    n = hi - lo
                    col = (r * nb + b) * 128
                    sc.op("pe", lambda e, vr=v_res, r=r, lo=lo, hi=hi, n=n: e.transpose(ps_t[0:n, :], vr[:, r, lo:hi], ident[:, :]),
                          reads=["vT", "ident"], writes=["ps_t"])
                    sc.op("act", lambda e, p0=p0, n=n, col=col: e.copy(vg[p0:p0 + n, col:col + 128], ps_t[0:n, :]),
                          reads=["ps_t", "vg"], writes=["vg"])
            accO_v = accO[:, :].rearrange("p (l r) -> p r l", r=d)
            accL_v = accL[:, :].rearrange("p (l r) -> p r l", r=d)
            for r in range(d):
                for q0 in range(0, L, QT):
                    nt = min(QT, L - q0) // 128
                    for a in range(nt):
                        qa = q0 // 128 + a
                        for h in range(2):
                            kcol = r * Lp + 128 * (qa + h)
                            sc.op("pe", lambda e, a=a, h=h, kcol=kcol, r=r, qa=qa, L=L: e.matmul(
                                ps_s[:, a, h, :], kn[:, kcol:kcol + 128], qn[:, r * L + 128 * qa:r * L + 128 * qa + 128],
                                start=True, stop=True), reads=["kn", "qn"], writes=["ps_s"])
                    sc.op("dve", lambda e, g=g, nt=nt: e.tensor_tensor(
                        sS[:, 0:nt, :, :], ps_s[:, 0:nt, :, :],
                        bias_sb[:, g, :, :].unsqueeze(1).to_broadcast([128, nt, 2, 128]),
                        ALU.add), reads=["ps_s", "bias"], writes=["sS"])
                    sc.op("act", lambda e, nt=nt: e.activation(out=pP[:, 0:nt, :, :], in_=sS[:, 0:nt, :, :], func=AF.Exp, bias=negB[:, 0:1]),
                          reads=["sS", "negB"], writes=["pP"])
                    if q0 == 0:
                        sc.op("pool", lambda e: e.memset(pP[0:64, 0, 0, :], 0.0), reads=["pP"], writes=["pP"])
                    if q0 + 128 * nt == L:
                        sc.op("pool", lambda e, nt=nt: e.memset(pP[64:128, nt - 1, 1, :], 0.0), reads=["pP"], writes=["pP"])
                    for a in range(nt):
                        qa = q0 // 128 + a
                        for h in range(2):
                            col = (r * nb + qa + h) * 128
                            sc.op("pe", lambda e, a=a, h=h, col=col: e.matmul(
                                ps_O[:, 128 * a:128 * a + 128], vg[:, col:col + 128], pP[:, a, h, :],
                                start=(h == 0), stop=(h == 1)), reads=["vg", "pP"], writes=["ps_O"])
                            sc.op("pe", lambda e, a=a, h=h: e.matmul(
                                ps_L[:, 128 * a:128 * a + 128], ones[:, :], pP[:, a, h, :],
                                start=(h == 0), stop=(h == 1)), reads=["ones", "pP"], writes=["ps_L"])
                    n = 128 * nt
                    if first:
                        sc.op("dve", lambda e, av=accO_v, r=r, q0=q0, n=n: e.tensor_copy(av[:, r, q0:q0 + n], ps_O[:, 0:n]),
                              reads=["ps_O"], writes=["accO"])
                        sc.op("act", lambda e, av=accL_v, r=r, q0=q0, n=n: e.copy(av[:, r, q0:q0 + n], ps_L[:, 0:n]),
                              reads=["ps_L"], writes=["accL"])
                    else:
                        sc.op("dve", lambda e, av=accO_v, r=r, q0=q0, n=n: e.tensor_tensor(av[:, r, q0:q0 + n], av[:, r, q0:q0 + n], ps_O[:, 0:n], ALU.add),
                              reads=["ps_O", "accO"], writes=["accO"])
                        sc.op("dve", lambda e, av=accL_v, r=r, q0=q0, n=n: e.tensor_tensor(av[:, r, q0:q0 + n], av[:, r, q0:q0 + n], ps_L[:, 0:n], ALU.add),
                              reads=["ps_L", "accL"], writes=["accL"])
            first = False
        sc.op("dve", lambda e: e.reciprocal(accL[:, :], accL[:, :]), reads=["accL"], writes=["accL"])
        for T0 in range(0, n_tok, PIECE):
            sc.op("dve", lambda e, T0=T0: e.tensor_tensor(yb[:, :], accO[:, T0:T0 + PIECE], accL[:, T0:T0 + PIECE], ALU.mult),
                  reads=["accO", "accL", "yb"], writes=["yb"])
            sc.dma("sp", yT[:, T0:T0 + PIECE], yb[:, :], "a_yb", reads=["yb"], writes=["y_out"])
        sc.final_wait("sp", ["y_out", "yb"])
        sc.run()


def build_att(n_tok):
    nc = bass.Bass("TRN2", target_bir_lowering=False)
    qT = nc.dram_tensor("qT", [3, 128, n_tok], BF16, kind="ExternalInput").ap()
    kT = nc.dram_tensor("kT", [3, 128, n_tok], BF16, kind="ExternalInput").ap()
    vT = nc.dram_tensor("vT", [128, n_tok], BF16, kind="ExternalInput").ap()
    qg = nc.dram_tensor("qg", [128, 3], F32, kind="ExternalInput").ap()
    kg = nc.dram_tensor("kg", [128, 3], F32, kind="ExternalInput").ap()
    bias = nc.dram_tensor("bias", [3, 128, 2, 128], F32, kind="ExternalInput").ap()
    ident_d = nc.dram_tensor("ident", [128, 128], BF16, kind="ExternalInput").ap()
    yT = nc.dram_tensor("yT", [128, n_tok], BF16, kind="ExternalOutput").ap()
    emit_att(nc, lambda g: qT[g], lambda g: kT[g], vT[:, :], qg, kg, bias, ident_d, yT, n_tok, "")
    return nc


def gdn_consts():
    i = np.arange(128)[:, None]; j = np.arange(128)[None, :]
    same = (i // 64) == (j // 64)
    c = {}
    c["ident_f"] = np.eye(128, dtype=np.float32)
    c["ones_f"] = np.ones((128, 128), np.float32)
    c["mstrict"] = np.stack([(same & (i > j)), (same & (i < j))]).astype(np.float32)
    c["mincl"] = np.stack([(same & (i >= j)), (same & (i <= j))]).astype(np.float32)
    r = i; m = j
    c["cum"] = np.stack([(same & (r <= m)), (same & (r >= m))]).astype(np.float32)
    lastf = np.where(np.arange(128) < 64, 63, 127)[None, :]; lastb = np.where(np.arange(128) < 64, 0, 64)[None, :]
    c["sel"] = np.stack([(r == lastf), (r == lastb)]).astype(np.float32)
    selh = np.zeros((2, 2, 128, 128), np.float32)
    for dr in range(2):
        for hf in range(2):
            selh[dr, hf, (63 + 64 * hf) if dr == 0 else 64 * hf, :] = 1.0
    c["selh"] = selh
    return c


GDN_IN = (("qraw", [2, 128, None], BF16), ("kraw", [2, 128, None], BF16), ("vraw", [4, 128, None], BF16), ("zraw", [4, 128, None], BF16),
          ("abr", [16, None], BF16))
GDN_PAR = (("cw", [128, 8, 5], F32), ("alog", [128, 8], F32), ("dtb", [128, 8], F32), ("ng", [128, 128], F32),
           ("ident_f", [128, 128], F32), ("ident_b", [128, 128], BF16), ("ones_f", [128, 128], F32),
           ("mstrict", [2, 128, 128], F32), ("mincl", [2, 128, 128], F32), ("cum", [2, 128, 128], F32), ("sel", [2, 128, 128], F32),
           ("selh", [2, 2, 128, 128], F32))


def build_gdn(n_tok):
    nc = bass.Bass("TRN2", target_bir_lowering=False)
    aps = {}
    for (name, shape, dt) in GDN_IN + GDN_PAR:
        aps[name] = nc.dram_tensor(name, [n_tok if v is None else v for v in shape], dt, kind="ExternalInput").ap()
    yg = nc.dram_tensor("yg", [n_tok, 512], BF16, kind="ExternalOutput").ap()
    ofw = nc.dram_tensor("ofw_scr", [4, n_tok, 128], F32).ap()
    emit_gdn(nc, aps, yg, ofw, n_tok)
    return nc


def emit_gdn(nc, aps, yg, ofw, n_tok):
    TB = 512; NP = 4
    nblk = n_tok // TB
    qraw, kraw, vraw, zraw, abr = (aps[k] for k in ("qraw", "kraw", "vraw", "zraw", "abr"))
    cw, alog, dtb, ngd = (aps[k] for k in ("cw", "alog", "dtb", "ng"))
    c_ident_f, c_ident_b, c_ones_f = aps["ident_f"], aps["ident_b"], aps["ones_f"]
    c_mstrict, c_mincl, c_cum, c_sel, c_selh = (aps[k] for k in ("mstrict", "mincl", "cum", "sel", "selh"))
    with contextlib.ExitStack() as st:
        sb = lambda name, shape, dt: st.enter_context(nc.sbuf_tensor(name, shape, dt))
        ps = lambda name, shape, dt: st.enter_context(nc.psum_tensor(name, shape, dt))
        sc = Sched(nc, st)
        V = lambda fn, r, w: sc.op("dve", fn, reads=r, writes=w)
        A = lambda fn, r, w: sc.op("act", fn, reads=r, writes=w)
        G = lambda fn, r, w: sc.op("pool", fn, reads=r, writes=w)
        T = lambda fn, r, w: sc.op("pe", fn, reads=r, writes=w)
        identf = sb("identf", [128, 128], F32); identb = sb("identb", [128, 128], BF16); onesf = sb("onesf", [128, 128], F32)
        mstr = sb("mstr", [128, 2, 128], F32); minc = sb("minc", [128, 2, 128], F32)
        cum = sb("cum_sb", [128, 2, 128], F32); sel = sb("sel_sb", [128, 2, 128], F32); selh = sb("selh_sb", [128, 4, 128], F32)
        cw_sb = sb("cw_sb", [128, 8, 5], F32); nega = sb("nega", [128, 8], F32); dtb_sb = sb("dtb_sb", [128, 8], F32)
        ng_sb = sb("ng_sb", [128, 128], F32)
        for (t_, d_, nm) in ((identf, c_ident_f, "identf"), (identb, c_ident_b, "identb"), (onesf, c_ones_f, "onesf"),
                             (nega, alog, "nega"), (dtb_sb, dtb, "dtb"), (ng_sb, ngd, "ng"), (cw_sb, cw, "cw")):
            sc.dma("sp", t_[:], d_[:] if len(d_.shape) == 2 else d_[:, :, :], "c_" + nm, writes=[nm])
        sc.dma("sp", mstr[:, :, :], c_mstrict.rearrange("r p j -> p r j"), "c_mstr", writes=["mstr"])
        sc.dma("sp", minc[:, :, :], c_mincl.rearrange("r p j -> p r j"), "c_minc", writes=["minc"])
        sc.dma("sp", cum[:, :, :], c_cum.rearrange("r p j -> p r j"), "c_cum", writes=["cum"])
        sc.dma("sp", sel[:, :, :], c_sel.rearrange("r p j -> p r j"), "c_sel", writes=["sel"])
        sc.dma("sp", selh[:, :, :], c_selh.rearrange("r h p j -> p (r h) j"), "c_selh", writes=["selh"])
        A(lambda e: e.activation(out=nega[:, :], in_=nega[:, :], func=AF.Exp), ["nega"], ["nega"])
        V(lambda e: e.tensor_scalar(nega[:, :], nega[:, :], -1.0, None, ALU.mult), ["nega"], ["nega"])

        raw2 = [sb("raw_g%d" % i, [128, TB + 4], BF16) for i in range(2)]; rawf2 = [sb("rawf%d" % i, [128, TB + 4], F32) for i in range(2)]
        raw = raw2[0]
        acc8 = [sb("acc_g%d" % i, [128, TB], F32) for i in range(8)]; sq4 = [sb("sq_g%d" % i, [128, TB], F32) for i in range(4)]
        fm = [sb("fm%d" % i, [128, TB], BF16) for i in range(8)]
        tm = [sb("tm%d" % i, [128, NP, 128], BF16) for i in range(8)]
        zt = [sb("zt%d" % i, [128, NP, 128], F32) for i in range(4)]
        KK = [sb("KK%d" % i, [128, NP, 128], F32) for i in range(2)]; QK = [sb("QK%d" % i, [128, NP, 128], F32) for i in range(2)]
        ab_fm = sb("ab_fm", [16, TB], BF16); ab_tm = sb("ab_tm", [128, NP, 16], F32)
        gt = {n: sb("gt_" + n, [128, NP, 4], F32) for n in ("x", "nx", "ax", "e", "l", "g", "beta", "nbeta", "G", "eG", "gl", "tail", "bG", "d0", "d1")}
        diagG = sb("diagG", [128, NP, 128], F32); dec = sb("dec", [128, NP, 128], F32); t1 = sb("t1", [128, NP, 128], F32)
        Ab = [sb("Ab%d" % i, [128, NP, 128], F32) for i in range(2)]; Bb = [sb("Bb%d" % i, [128, NP, 128], F32) for i in range(2)]
        intra = sb("intra", [128, NP, 128], BF16); X = sb("X_g", [128, NP, 256], F32); qd = sb("qd", [128, NP, 128], BF16)
        scan = [[{n: sb("sc%d_%d_%s" % (bb, vh, n), [128, NP, 128], BF16) for n in ("u", "wT", "qdT", "inT", "kt")} for vh in range(4)] for bb in range(2)]
        dS = [[sb("dS%d_%d" % (bb, hf), [128, NP, 4], F32) for hf in range(2)] for bb in range(2)]
        oblk = [sb("oblk%d" % vh, [128, NP, 128], F32) for vh in range(4)]
        ofl = sb("ofl", [128, NP, 128], F32); junk = sb("junk", [128, 128], F32); ss = sb("ss_g", [128, NP], F32); yb = sb("yb", [128, NP, 128], BF16)
        Sst = [sb("S%d" % vh, [128, 128], F32) for vh in range(4)]; Sbf = [sb("Sbf%d" % vh, [128, 128], BF16) for vh in range(4)]
        vnew = [sb("vnew%d" % vh, [128, 128], BF16) for vh in range(4)]
        psM = ps("psM", [128, NP, 128], F32); psX = ps("psX", [128, NP, 256], F32); psA = ps("psA", [128, NP, 128], F32); psB = ps("psB", [128, NP, 128], F32)
        psT = ps("psT", [128, NP, 128], BF16)
        psg = psB[:, 0, :]
        pss = [ps("pss%d" % i, [128, 3, 128], F32) for i in range(2)]
        bc = lambda ap: ap.unsqueeze(2).to_broadcast([128, NP, 128])
        bcp = lambda ap: ap.unsqueeze(1).to_broadcast([128, NP, 128])

        def prep_block(dr, blk, bb):
            t0 = blk * TB
            lo, hi = max(t0 - 2, 0), min(t0 + TB + 2, n_tok)
            srcs = [(qraw, 0), (qraw, 1), (kraw, 0), (kraw, 1), (vraw, 0), (vraw, 1), (vraw, 2), (vraw, 3)]
            for ti, (src, idx) in enumerate(srcs):
                rb = ti % 2
                G(lambda e, rb=rb: e.memset(raw2[rb][:, :], 0.0), [], [("raw", rb)])
                sc.dma("sp", raw2[rb][:, lo - (t0 - 2):hi - (t0 - 2)], src[idx, :, lo:hi], ("raw_g", rb), writes=[("raw", rb)])
                V(lambda e, rb=rb: e.tensor_copy(rawf2[rb][:, :], raw2[rb][:, :]), [("raw", rb)], [("rawf", rb)])
                V(lambda e, ti=ti, rb=rb: e.tensor_scalar(acc8[ti][:, :], rawf2[rb][:, 0:TB], cw_sb[:, ti, 0:1], None, ALU.mult), [("rawf", rb), "cw"], [("acc", ti)])
                for j in range(1, 5):
                    V(lambda e, ti=ti, j=j, rb=rb: e.scalar_tensor_tensor(acc8[ti][:, :], rawf2[rb][:, j:j + TB], cw_sb[:, ti, j:j + 1], acc8[ti][:, :], ALU.mult, ALU.add),
                      [("rawf", rb), "cw", ("acc", ti)], [("acc", ti)])
            for ti in range(8):
                if ti >= 4:
                    A(lambda e, ti=ti: e.activation(out=fm[ti][:, :], in_=acc8[ti][:, :], func=AF.Silu), [("acc", ti)], [("fm", ti)])
                else:
                    A(lambda e, ti=ti: e.activation(out=acc8[ti][:, :], in_=acc8[ti][:, :], func=AF.Silu), [("acc", ti)], [("acc", ti)])
            nps = [psX[:, 0:2, :], psX[:, 2:4, :], psA[:, :, :], psB[:, :, :]]
            npk = [[("psX", 0)], [("psX", 1)], [("psA", 0), ("psA", 1)], [("psB", 0), ("psB", 1)]]
            for ti in range(4):
                V(lambda e, ti=ti: e.tensor_tensor(sq4[ti][:, :], acc8[ti][:, :], acc8[ti][:, :], ALU.mult), [("acc", ti)], [("sq", ti)])
            for ti in range(4):
                T(lambda e, ti=ti: e.matmul(nps[ti], onesf[:, :], sq4[ti][:, :], start=True, stop=True), [("sq", ti), "onesf"], npk[ti])
            for ti in range(4):
                V(lambda e, ti=ti: e.tensor_scalar(sq4[ti][:, :], nps[ti], EPS, None, ALU.add), npk[ti] + [("sq", ti)], [("sq", ti)])
            for ti in range(4):
                A(lambda e, ti=ti: e.activation(out=sq4[ti][:, :], in_=sq4[ti][:, :], func=AF.Sqrt), [("sq", ti)], [("sq", ti)])
            for ti in range(4):
                V(lambda e, ti=ti: e.reciprocal(sq4[ti][:, :], sq4[ti][:, :]), [("sq", ti)], [("sq", ti)])
            for ti in range(4):
                scl = float(128.0 ** -0.5) if ti < 2 else 1.0
                V(lambda e, ti=ti, scl=scl: e.scalar_tensor_tensor(fm[ti][:, :], acc8[ti][:, :], scl, sq4[ti][:, :], ALU.mult, ALU.mult), [("acc", ti), ("sq", ti)], [("fm", ti)])
            for ti in range(8):
                for p in range(NP):
                    T(lambda e, ti=ti, p=p: e.transpose(psT[:, p, :], fm[ti][:, 128 * p:128 * p + 128], identb[:, :]), [("fm", ti), "identb"], ["psT"])
                A(lambda e, ti=ti: e.copy(tm[ti][:, :, :], psT[:, :, :]), ["psT"], [("tm", ti)])
            sc.dma("sp", ab_fm[:, :], abr[:, t0:t0 + TB], "ab_fm", writes=["ab_fm"])
            for p in range(NP):
                T(lambda e, p=p: e.transpose(psT[:, p, 0:16], ab_fm[:, 128 * p:128 * p + 128], identb[0:16, 0:16]), ["ab_fm", "identb"], ["psT"])
            V(lambda e: e.tensor_copy(ab_tm[:, :, :], psT[:, :, 0:16]), ["psT"], ["ab_tm"])
            g_ = gt
            row = lambda t_, dr=dr: t_[:, dr * 4:dr * 4 + 4].unsqueeze(1).to_broadcast([128, NP, 4])
            V(lambda e: e.tensor_tensor(g_["x"][:], ab_tm[:, :, dr * 4:dr * 4 + 4], row(dtb_sb), ALU.add), ["ab_tm", "dtb"], ["g_x"])
            V(lambda e: e.tensor_scalar(g_["nx"][:], g_["x"][:], -1.0, None, ALU.mult), ["g_x"], ["g_nx"])
            V(lambda e: e.tensor_tensor(g_["ax"][:], g_["x"][:], g_["nx"][:], ALU.max), ["g_x", "g_nx"], ["g_ax"])
            A(lambda e: e.activation(out=g_["e"][:], in_=g_["ax"][:], func=AF.Exp, scale=-1.0), ["g_ax"], ["g_e"])
            V(lambda e: e.tensor_scalar(g_["e"][:], g_["e"][:], 1.0, None, ALU.add), ["g_e"], ["g_e"])
            A(lambda e: e.activation(out=g_["l"][:], in_=g_["e"][:], func=AF.Ln), ["g_e"], ["g_l"])
            V(lambda e: e.scalar_tensor_tensor(g_["g"][:], g_["x"][:], 0.0, g_["l"][:], ALU.max, ALU.add), ["g_x", "g_l"], ["g_g"])
            V(lambda e: e.tensor_tensor(g_["g"][:], g_["g"][:], row(nega), ALU.mult), ["g_g", "nega"], ["g_g"])
            A(lambda e: e.activation(out=g_["beta"][:], in_=ab_tm[:, :, 8 + dr * 4:12 + dr * 4], func=AF.Sigmoid), ["ab_tm"], ["g_beta"])
            V(lambda e: e.tensor_scalar(g_["nbeta"][:], g_["beta"][:], -1.0, None, ALU.mult), ["g_beta"], ["g_nbeta"])
            T(lambda e: e.matmul(psg[:, 0:16], cum[:, dr, :], g_["g"][:].rearrange("p a b -> p (a b)"), start=True, stop=True), ["g_g", "cum"], [("psB", 0)])
            V(lambda e: e.tensor_copy(g_["G"][:].rearrange("p a b -> p (a b)"), psg[:, 0:16]), [("psB", 0)], ["g_G"])
            A(lambda e: e.activation(out=g_["eG"][:], in_=g_["G"][:], func=AF.Exp), ["g_G"], ["g_eG"])
            T(lambda e: e.matmul(psg[:, 16:32], sel[:, dr, :], g_["G"][:].rearrange("p a b -> p (a b)"), start=True, stop=True), ["g_G", "sel"], [("psB", 0)])
            V(lambda e: e.tensor_tensor(g_["gl"][:].rearrange("p a b -> p (a b)"), psg[:, 16:32], g_["G"][:].rearrange("p a b -> p (a b)"), ALU.subtract), [("psB", 0), "g_G"], ["g_gl"])
            A(lambda e: e.activation(out=g_["tail"][:], in_=g_["gl"][:], func=AF.Exp), ["g_gl"], ["g_tail"])
            V(lambda e: e.tensor_tensor(g_["bG"][:], g_["beta"][:], g_["eG"][:], ALU.mult), ["g_beta", "g_eG"], ["g_bG"])
            for hf in range(2):
                T(lambda e, hf=hf: e.matmul(psg[:, 32 + 16 * hf:48 + 16 * hf], selh[:, dr * 2 + hf, :], g_["G"][:].rearrange("p a b -> p (a b)"), start=True, stop=True),
                  ["g_G", "selh"], [("psB", 0)])
                A(lambda e, hf=hf: e.activation(out=dS[bb][hf][:].rearrange("p a b -> p (a b)"), in_=psg[:, 32 + 16 * hf:48 + 16 * hf], func=AF.Exp), [("psB", 0)], [("dS", bb)])
            for qh in range(2):
                for p in range(NP):
                    cs = slice(128 * p, 128 * p + 128)
                    T(lambda e, qh=qh, p=p, cs=cs: e.matmul(psA[:, p, :], fm[2 + qh][:, cs], fm[2 + qh][:, cs], start=True, stop=True), [("fm", 2 + qh)], [("psA", 0), ("psA", 1)])
                    T(lambda e, qh=qh, p=p, cs=cs: e.matmul(psB[:, p, :], fm[qh][:, cs], fm[2 + qh][:, cs], start=True, stop=True), [("fm", qh), ("fm", 2 + qh)], [("psB", 0), ("psB", 1)])
                V(lambda e, qh=qh: e.tensor_copy(KK[qh][:], psA[:]), [("psA", 0), ("psA", 1)], [("KK", qh)])
                A(lambda e, qh=qh: e.copy(QK[qh][:], psB[:]), [("psB", 0), ("psB", 1)], [("QK", qh)])
            for vh in range(4):
                qh = vh // 2
                sb_ = scan[bb][vh]
                Gc = g_["G"][:, :, vh]
                V(lambda e, Gc=Gc: e.tensor_tensor(diagG[:], bcp(identf[:, :]), bc(Gc), ALU.mult), ["identf", "g_G"], ["diagG"])
                for p in range(NP):
                    T(lambda e, p=p: e.matmul(psM[:, p, :], onesf[:, :], diagG[:, p, :], start=True, stop=True), ["diagG", "onesf"], ["psM"])
                V(lambda e, Gc=Gc: e.tensor_tensor(dec[:], bc(Gc), psM[:], ALU.subtract), ["psM", "g_G"], ["dec"])
                V(lambda e: e.tensor_scalar_min(dec[:], dec[:], 0.0), ["dec"], ["dec"])
                A(lambda e: e.activation(out=dec[:], in_=dec[:], func=AF.Exp), ["dec"], ["dec"])
                V(lambda e, qh=qh: e.tensor_tensor(t1[:], dec[:], KK[qh][:], ALU.mult), ["dec", ("KK", qh)], ["t1"])
                V(lambda e, vh=vh: e.tensor_tensor(t1[:], t1[:], bc(g_["nbeta"][:, :, vh]), ALU.mult), ["t1", "g_nbeta"], ["t1"])
                V(lambda e: e.tensor_tensor(Ab[0][:], t1[:], bcp(mstr[:, dr, :]), ALU.mult), ["t1", "mstr"], [("Ab", 0, 0), ("Ab", 0, 1)])
                G(lambda e, qh=qh: e.tensor_tensor(t1[:], dec[:], QK[qh][:], ALU.mult), ["dec", ("QK", qh), "t1"], ["t1"])
                G(lambda e: e.tensor_tensor(intra[:], t1[:], bcp(minc[:, dr, :]), ALU.mult), ["t1", "minc"], ["intra"])
                for p in range(NP):
                    T(lambda e, p=p: e.transpose(psM[:, p, :], Ab[0][:, p, :], identf[:, :]), [("Ab", 0, 0), ("Ab", 0, 1), "identf"], ["psM"])
                    T(lambda e, p=p: e.transpose(psT[:, p, :], intra[:, p, :], identb[:, :]), ["intra", "identb"], ["psT"])
                V(lambda e: e.tensor_copy(Bb[0][:], psM[:]), ["psM"], [("Bb", 0, 0), ("Bb", 0, 1)])
                A(lambda e, sb_=sb_: e.copy(sb_["inT"][:], psT[:]), ["psT"], [("scan", bb, vh)])
                V(lambda e, vh=vh: e.tensor_tensor(X[:, :, 0:128], tm[4 + vh][:], bc(g_["beta"][:, :, vh]), ALU.mult), [("tm", 4 + vh), "g_beta"], [("X", 0), ("X", 1)])
                V(lambda e, vh=vh, qh=qh: e.tensor_tensor(X[:, :, 128:256], tm[2 + qh][:], bc(g_["bG"][:, :, vh]), ALU.mult), [("tm", 2 + qh), "g_bG", ("X", 0), ("X", 1)], [("X", 0), ("X", 1)])
                for k in range(6):
                    a_, b_ = Ab[k % 2], Bb[k % 2]
                    an, bn = Ab[(k + 1) % 2], Bb[(k + 1) % 2]
                    for hp in range(2):
                        for p in (2 * hp, 2 * hp + 1):
                            T(lambda e, p=p, b_=b_: e.matmul(psX[:, p, :], b_[:, p, :], X[:, p, :], start=True, stop=True), [("Bb", k % 2, hp), ("X", hp)], [("psX", hp)])
                        if k < 5:
                            for p in (2 * hp, 2 * hp + 1):
                                T(lambda e, p=p, a_=a_, b_=b_: e.matmul(psA[:, p, :], b_[:, p, :], a_[:, p, :], start=True, stop=True), [("Ab", k % 2, hp), ("Bb", k % 2, hp)], [("psA", hp)])
                                T(lambda e, p=p, a_=a_, b_=b_: e.matmul(psB[:, p, :], a_[:, p, :], b_[:, p, :], start=True, stop=True), [("Ab", k % 2, hp), ("Bb", k % 2, hp)], [("psB", hp)])
                    for hp in range(2):
                        prs = slice(2 * hp, 2 * hp + 2)
                        V(lambda e, prs=prs: e.tensor_tensor(X[:, prs, :], X[:, prs, :], psX[:, prs, :], ALU.add), [("psX", hp), ("X", hp)], [("X", hp)])
                        if k < 5:
                            A(lambda e, an=an, prs=prs: e.copy(an[:, prs, :], psA[:, prs, :]), [("psA", hp)], [("Ab", (k + 1) % 2, hp)])
                            (A if hp == 0 else V)(lambda e, bn=bn, prs=prs, hp=hp: (e.copy if hp == 0 else e.tensor_copy)(bn[:, prs, :], psB[:, prs, :]), [("psB", hp)], [("Bb", (k + 1) % 2, hp)])
                A(lambda e, sb_=sb_: e.copy(sb_["u"][:], X[:, :, 0:128]), [("X", 0), ("X", 1)], [("scan", bb, vh)])
                for p in range(NP):
                    T(lambda e, p=p: e.transpose(psM[:, p, :], X[:, p, 128:256], identf[:, :]), [("X", p // 2), "identf"], ["psM"])
                V(lambda e, sb_=sb_: e.tensor_copy(sb_["wT"][:], psM[:]), ["psM", ("scan", bb, vh)], [("scan", bb, vh)])
                V(lambda e, vh=vh, qh=qh: e.tensor_tensor(qd[:], tm[qh][:], bc(g_["eG"][:, :, vh]), ALU.mult), [("tm", qh), "g_eG"], ["qd"])
                for p in range(NP):
                    T(lambda e, p=p: e.transpose(psT[:, p, :], qd[:, p, :], identb[:, :]), ["qd", "identb"], ["psT"])
                A(lambda e, sb_=sb_: e.copy(sb_["qdT"][:], psT[:]), ["psT", ("scan", bb, vh)], [("scan", bb, vh)])
                G(lambda e, sb_=sb_, vh=vh, qh=qh: e.tensor_tensor(sb_["kt"][:], tm[2 + qh][:], bc(g_["tail"][:, :, vh]), ALU.mult),
                  [("tm", 2 + qh), "g_tail", ("scan", bb, vh)], [("scan", bb, vh)])

        def scan_block(dr, blk, bb):
            order = [(p, hf) for p in range(NP) for hf in range(2)]
            if dr == 1:
                order = order[::-1]
            for si, (p, hf) in enumerate(order):
                rows = slice(64 * hf, 64 * hf + 64)
                for vh in range(4):
                    sb_ = scan[bb][vh]
                    pp = pss[vh % 2]
                    T(lambda e, sb_=sb_, p=p, vh=vh, pp=pp: e.matmul(pp[:, 0, :], sb_["wT"][:, p, :], Sbf[vh][:, :], start=True, stop=True),
                      [("scan", bb, vh), ("Sbf", vh)], [("pss", vh % 2)])
                    V(lambda e, sb_=sb_, p=p, vh=vh, pp=pp, rows=rows: e.tensor_tensor(vnew[vh][rows, :], sb_["u"][rows, p, :], pp[rows, 0, :], ALU.subtract),
                      [("pss", vh % 2), ("scan", bb, vh)], [("vnew", vh)])
                    T(lambda e, sb_=sb_, p=p, vh=vh, pp=pp: e.matmul(pp[:, 1, :], sb_["qdT"][:, p, :], Sbf[vh][:, :], start=True, stop=False),
                      [("scan", bb, vh), ("Sbf", vh)], [("pss", vh % 2)])
                    T(lambda e, sb_=sb_, p=p, vh=vh, pp=pp, rows=rows: e.matmul(pp[:, 1, :], sb_["inT"][rows, p, :], vnew[vh][rows, :], start=False, stop=True),
                      [("scan", bb, vh), ("vnew", vh)], [("pss", vh % 2)])
                    A(lambda e, p=p, vh=vh, pp=pp, rows=rows: e.copy(oblk[vh][rows, p, :], pp[rows, 1, :]), [("pss", vh % 2)], [("oblk", vh)])
                    T(lambda e, sb_=sb_, p=p, vh=vh, pp=pp, rows=rows: e.matmul(pp[:, 2, :], sb_["kt"][rows, p, :], vnew[vh][rows, :], start=True, stop=True),
                      [("scan", bb, vh), ("vnew", vh)], [("pss", vh % 2)])
                    V(lambda e, p=p, vh=vh, pp=pp, hf=hf: e.scalar_tensor_tensor(Sst[vh][:, :], Sst[vh][:, :], dS[bb][hf][:, p, vh:vh + 1], pp[:, 2, :], ALU.mult, ALU.add),
                      [("pss", vh % 2), ("dS", bb), ("S", vh)], [("S", vh)])
                    A(lambda e, vh=vh: e.copy(Sbf[vh][:, :], Sst[vh][:, :]), [("S", vh)], [("Sbf", vh)])

        def finish_block(dr, blk):
            t0 = blk * TB
            for vh in range(4):
                dst = ofw[vh, t0:t0 + TB, :].rearrange("(p i) d -> i p d", i=128)
                if dr == 0:
                    sc.dma("sp", dst, oblk[vh][:, :, :], ("oblk", vh), reads=[("oblk", vh)], writes=[("ofw", vh, blk)])
                    continue
                sc.dma("sp", ofl[:, :, :], dst, "ofl", reads=[("ofw", vh, blk)], writes=["ofl"])
                V(lambda e, vh=vh: e.tensor_tensor(ofl[:], ofl[:], oblk[vh][:], ALU.add), ["ofl", ("oblk", vh)], ["ofl"])
                for p in range(NP):
                    A(lambda e, p=p: e.activation(out=junk[:, :], in_=ofl[:, p, :], func=AF.Square, accum_out=ss[:, p:p + 1]), ["ofl", "junk", "ss"], ["junk", "ss"])
                V(lambda e: e.tensor_scalar(ss[:, :], ss[:, :], 1.0 / 128, EPS, ALU.mult, ALU.add), ["ss"], ["ss"])
                A(lambda e: e.activation(out=ss[:, :], in_=ss[:, :], func=AF.Sqrt), ["ss"], ["ss"])
                V(lambda e: e.reciprocal(ss[:, :], ss[:, :]), ["ss"], ["ss"])
                V(lambda e: e.tensor_tensor(ofl[:], ofl[:], bc(ss[:, :]), ALU.mult), ["ofl", "ss"], ["ofl"])
                V(lambda e: e.tensor_tensor(ofl[:], ofl[:], bcp(ng_sb[:, :]), ALU.mult), ["ofl", "ng"], ["ofl"])
                sc.dma("sp", raw[:, 0:TB], zraw[vh, :, t0:t0 + TB], ("raw_g", 0), writes=[("raw", 0)])
                for p in range(NP):
                    T(lambda e, p=p: e.transpose(psT[:, p, :], raw[:, 128 * p:128 * p + 128], identb[:, :]), [("raw", 0), "identb"], ["psT"])
                A(lambda e, vh=vh: e.activation(out=zt[vh][:], in_=psT[:], func=AF.Silu), ["psT"], [("zt", vh)])
                V(lambda e, vh=vh: e.tensor_tensor(yb[:], ofl[:], zt[vh][:], ALU.mult), ["ofl", ("zt", vh)], ["yb"])
                sc.dma("sp", yg[t0:t0 + TB, 128 * vh:128 * vh + 128].rearrange("(p i) d -> i p d", i=128), yb[:, :, :], "yb", reads=["yb"], writes=["yg_out"])

        for dr in range(2):
            for vh in range(4):
                G(lambda e, vh=vh: e.memset(Sst[vh][:, :], 0.0), [], [("S", vh)])
                G(lambda e, vh=vh: e.memset(Sbf[vh][:, :], 0.0), [], [("Sbf", vh)])
            blocks = list(range(nblk)) if dr == 0 else list(range(nblk))[::-1]
            for bi, blk in enumerate(blocks):
                bb = bi % 2
                prep_block(dr, blk, bb)
                scan_block(dr, blk, bb)
                finish_block(dr, blk)
        sc.final_wait("sp", ["yg_out", "yb"])
        sc.run()


L1_CHUNKS = 27
OFF_QA, OFF_KA, OFF_VA = 0, 6144, 12288
OFF_QG, OFF_KG, OFF_VG, OFF_ZG, OFF_A, OFF_B, OFF_GA, OFF_GB = 14336, 16384, 18432, 22528, 26624, 26688, 26752, 28800


def w1_cols(c):
    cols = []
    r = np.arange(128)
    for s_ in range(2):
        hs = 2 * c + s_
        for g in range(3):
            cols.append(OFF_QA + g * 2048 + hs * 128 + r)
        for g in range(3):
            cols.append(OFF_KA + g * 2048 + hs * 128 + r)
        cols.append(OFF_VA + hs * 128 + r)
    for qh in range(2):
        cols.append(OFF_QG + (2 * c + qh) * 128 + r)
    for qh in range(2):
        cols.append(OFF_KG + (2 * c + qh) * 128 + r)
    for vh in range(4):
        cols.append(OFF_VG + (4 * c + vh) * 128 + r)
    for vh in range(4):
        cols.append(OFF_ZG + (4 * c + vh) * 128 + r)
    ab = np.full(128, -1)
    for dr in range(2):
        for vh in range(4):
            ab[dr * 4 + vh] = OFF_A + dr * 32 + 4 * c + vh
            ab[8 + dr * 4 + vh] = OFF_B + dr * 32 + 4 * c + vh
    cols.append(ab)
    return np.concatenate(cols)


def build_launch1(n_tok):
    nc = bass.Bass("TRN2", target_bir_lowering=False)
    di = lambda name, shape, dt: nc.dram_tensor(name, shape, dt, kind="ExternalInput").ap()
    n_cols = L1_CHUNKS * 128
    xT = di("xT", [D, n_tok], F32); w1 = di("w1", [D, n_cols], F32); gain1 = di("gain1", [128, NCH], F32)
    qg = di("qg", [128, 3], F32); kg = di("kg", [128, 3], F32); bias = di("att_bias", [2, 3, 128, 2, 128], F32)
    aps = {}
    for (name, shape, dt) in GDN_PAR:
        aps[name] = di(name, shape, dt)
    yatt = nc.dram_tensor("yatt", [2, 128, n_tok], BF16, kind="ExternalOutput").ap()
    yg = nc.dram_tensor("yg", [n_tok, 512], BF16, kind="ExternalOutput").ap()
    pT = nc.dram_tensor("pT_scr", [n_cols, n_tok], BF16).ap()
    ofw = nc.dram_tensor("ofw_scr", [4, n_tok, 128], F32).ap()
    emit_stage1(nc, xT, w1, gain1, pT, n_cols, n_tok)
    for s_ in range(2):
        base = 7 * s_ * 128
        emit_att(nc, lambda g, base=base: pT[base + 128 * g:base + 128 * g + 128, :],
                 lambda g, base=base: pT[base + 128 * (3 + g):base + 128 * (4 + g), :],
                 pT[base + 768:base + 896, :], qg, kg, bias[s_], aps["ident_b"], yatt[s_], n_tok, "_s%d" % s_)
    g0 = 14 * 128
    aps["qraw"] = pT[g0:g0 + 256, :].rearrange("(i p) t -> i p t", p=128)
    aps["kraw"] = pT[g0 + 256:g0 + 512, :].rearrange("(i p) t -> i p t", p=128)
    aps["vraw"] = pT[g0 + 512:g0 + 1024, :].rearrange("(i p) t -> i p t", p=128)
    aps["zraw"] = pT[g0 + 1024:g0 + 1536, :].rearrange("(i p) t -> i p t", p=128)
    aps["abr"] = pT[g0 + 1536:g0 + 1552, :]
    emit_gdn(nc, aps, yg, ofw, n_tok)
    return nc


def launch1_inputs(inp, c, n_tok=S):
    import ml_dtypes
    f = np.float32
    cols = w1_cols(c)
    w_in = inp["w_in"][0]
    w1 = np.zeros((D, cols.size), f)
    m = cols >= 0
    w1[:, m] = w_in[:, cols[m]]
    C = gdn_consts()
    cwf = inp["gdn_conv_w"][0]
    chans = [np.arange(128) + (2 * c + qh) * 128 for qh in range(2)] + [2048 + np.arange(128) + (2 * c + qh) * 128 for qh in range(2)] \
        + [4096 + np.arange(128) + (4 * c + vh) * 128 for vh in range(4)]
    cw = np.stack([cwf[:, ch].T for ch in chans], axis=1)
    sel8 = lambda a: np.concatenate([a[0, 4 * c:4 * c + 4], a[1, 4 * c:4 * c + 4]])
    d = {
        "xT": np.ascontiguousarray(inp["x"][0, :n_tok].T), "w1": w1,
        "gain1": np.ascontiguousarray(inp["norm1_gain"][0].reshape(NCH, 128).T),
        "qg": np.ascontiguousarray(inp["q_norm_gain"][0].T), "kg": np.ascontiguousarray(inp["k_norm_gain"][0].T),
        "att_bias": np.stack([att_bias_consts(2 * c), att_bias_consts(2 * c + 1)]),
        "cw": np.ascontiguousarray(cw.astype(f)),
        "alog": np.ascontiguousarray(np.broadcast_to(sel8(inp["gdn_a_log"][0])[None, :], (128, 8)).astype(f)),
        "dtb": np.ascontiguousarray(np.broadcast_to(sel8(inp["gdn_dt_bias"][0])[None, :], (128, 8)).astype(f)),
        "ng": np.ascontiguousarray(np.broadcast_to(inp["gdn_norm_gain"][0][None, :], (128, 128)).astype(f)),
        "ident_f": C["ident_f"], "ident_b": C["ident_f"].astype(ml_dtypes.bfloat16), "ones_f": C["ones_f"],
        "mstrict": C["mstrict"], "mincl": C["mincl"], "cum": C["cum"], "sel": C["sel"], "selh": C["selh"],
    }
    return d


NT = 1024
NEXP = 64


def emit_l2a(nc, a, x2tm, h2b_d, wt_d):
    HT = 512
    xT_v = a["xT"].rearrange("(c p) t -> p c t", p=128)
    yA_v = a["yAT"].rearrange("(c p) t -> p c t", p=128)
    yB_v = a["yBT"].rearrange("(c p) t -> p c t", p=128)
    wgate_v = a["wgate"].rearrange("(c p) n -> p c n", p=128)
    wA_v = a["wA"].rearrange("(c p) n -> p c n", p=128)
    wB_v = a["wB"].rearrange("(c p) n -> p c n", p=128)
    wo_v = a["wo"].rearrange("(c p) n -> p c n", p=128)
    wr_v = a["wr"].rearrange("(c p) n -> p c n", p=128)
    h2b_v = h2b_d.rearrange("(c p) t -> p c t", p=128)
    with contextlib.ExitStack() as st:
        sb = lambda name, shape, dt: st.enter_context(nc.sbuf_tensor(name, shape, dt))
        ps = lambda name, shape, dt: st.enter_context(nc.psum_tensor(name, shape, dt))
        sc = Sched(nc, st)
        V = lambda fn, r, w: sc.op("dve", fn, reads=r, writes=w)
        A = lambda fn, r, w: sc.op("act", fn, reads=r, writes=w)
        G = lambda fn, r, w: sc.op("pool", fn, reads=r, writes=w)
        T = lambda fn, r, w: sc.op("pe", fn, reads=r, writes=w)
        ones = sb("b_ones", [128, 128], BF16); identf = sb("b_identf", [128, 128], F32)
        g1 = sb("b_g1", [128, NCH], F32); g2 = sb("b_g2", [128, NCH], F32)
        wr_sb = sb("b_wr", [128, NCH, 72], F32); br_sb = sb("b_br", [128, 72], F32)
        xf = sb("b_xf", [128, NCH, HT], F32); hb = sb("b_hb", [128, NCH, HT], BF16); xsq = sb("b_xsq", [128, NCH, HT], BF16)
        yA = sb("b_yA", [128, NCH, HT], BF16); bufY = sb("b_bufY", [128, 32 * HT * 2 // 4], F32)
        yB = bufY[:, :].bitcast(BF16).rearrange("p (c t) -> p c t", t=HT)
        stg = bufY[:, :].rearrange("p (k d) -> p k d", d=D)
        h2f = bufY[:, :].rearrange("p (c t) -> p c t", t=HT)
        merged = sb("b_merged", [128, NCH, HT], BF16)
        rstd = sb("b_rstd", [128, HT], F32)
        wgA = [sb("b_wgA%d" % i, [128, NCH, 128], BF16) for i in range(2)]; wgB = [sb("b_wgB%d" % i, [128, NCH, 128], BF16) for i in range(2)]
        wa = [sb("b_wa%d" % i, [128, NCH, 128], BF16) for i in range(2)]; wb_ = [sb("b_wb%d" % i, [128, 32, 128], BF16) for i in range(2)]
        wo = [sb("b_wo%d" % i, [128, NCH, 128], BF16) for i in range(2)]
        tA = sb("b_tA", [128, HT], F32); tB = sb("b_tB", [128, HT], F32); sA = sb("b_sA", [128, HT], F32); sBt = sb("b_sB", [128, HT], F32)
        rt = {n: sb("b_r_" + n, [128, 72], F32) for n in ("lg", "ge", "oh", "tmp", "ig", "m1k", "in2", "m2k", "wl")}
        rs_ = {n: sb("b_s_" + n, [128, 1], F32) for n in ("gmax", "ngmax", "gs", "gw", "m1", "m2", "d12", "w1", "w2")}
        wt_st = sb("b_wtst", [128, 4, 64], F32)
        ps_ss = ps("b_psss", [128, HT], F32)
        psGA = ps("b_psGA", [128, HT], F32); psGB = ps("b_psGB", [128, HT], F32); psMA = ps("b_psMA", [128, HT], F32); psMB = ps("b_psMB", [128, HT], F32)
        psT = ps("b_psT", [128, 4, 128], F32); psR = ps("b_psR", [128, 128], F32)

        G(lambda e: e.memset(ones[:, :], 1.0), [], ["ones"])
        sc.dma("sp", identf[:, :], a["ident_f"][:, :], "c1", writes=["identf"])
        sc.dma("sp", g1[:, :], a["gain1"][:, :], "c2", writes=["g1"])
        sc.dma("sp", g2[:, :], a["gain2"][:, :], "c3", writes=["g2"])
        sc.dma("sp", wr_sb[:, :, :], wr_v, "c4", writes=["wr"])
        sc.dma("sp", br_sb[:, :], a["br"][:, :], "c5", writes=["br"])

        def rms(tag):
            A(lambda e: e.activation(out=xsq[:, :, :], in_=xf[:, :, :], func=AF.Square), ["xf"], ["xsq"])
            for c in range(NCH):
                T(lambda e, c=c: e.matmul(ps_ss[:, :], ones[:, :], xsq[:, c, :], start=(c == 0), stop=(c == NCH - 1)), ["xsq", "ones"], ["ps_ss"])
            V(lambda e: e.tensor_scalar(rstd[:, :], ps_ss[:, :], 1.0 / D, EPS, ALU.mult, ALU.add), ["ps_ss"], ["rstd"])
            A(lambda e: e.activation(out=rstd[:, :], in_=rstd[:, :], func=AF.Sqrt), ["rstd"], ["rstd"])
            V(lambda e: e.reciprocal(rstd[:, :], rstd[:, :]), ["rstd"], ["rstd"])

        for hf in range(NT // HT):
            hs = slice(hf * HT, (hf + 1) * HT)
            sc.dma("sp", xf[:, :, :], xT_v[:, :, hs], "l_xf", writes=["xf"])
            sc.dma("sp", yA[:, :, :], yA_v[:, :, hs], "l_yA", writes=["yA"])
            sc.dma("sp", yB, yB_v[:, :, hs], "l_bufY", writes=["bufY"])
            rms("n1")
            for c in range(NCH):
                V(lambda e, c=c: e.tensor_scalar(hb[:, c, :], xf[:, c, :], g1[:, c:c + 1], None, ALU.mult), ["xf", "g1"], ["hb"])
            for j in range(NCH):
                jb = j % 2
                js = slice(j * 128, (j + 1) * 128)
                j2 = slice(2048 + j * 128, 2048 + (j + 1) * 128)
                sc.dma("pool", wgA[jb][:, :, :], wgate_v[:, :, js], ("wgA", jb), writes=[("wgA", jb)])
                sc.dma("pool", wgB[jb][:, :, :], wgate_v[:, :, j2], ("wgB", jb), writes=[("wgB", jb)])
                sc.dma("pool", wa[jb][:, :, :], wA_v[:, :, js], ("wa", jb), writes=[("wa", jb)])
                sc.dma("pool", wb_[jb][:, :, :], wB_v[:, :, js], ("wb", jb), writes=[("wb", jb)])
                for c in range(NCH):
                    T(lambda e, c=c, jb=jb: e.matmul(psGA[:, :], wgA[jb][:, c, :], hb[:, c, :], start=(c == 0), stop=(c == NCH - 1)), [("wgA", jb), "hb"], ["psGA"])
                for c in range(NCH):
                    T(lambda e, c=c, jb=jb: e.matmul(psGB[:, :], wgB[jb][:, c, :], hb[:, c, :], start=(c == 0), stop=(c == NCH - 1)), [("wgB", jb), "hb"], ["psGB"])
                for c in range(NCH):
                    T(lambda e, c=c, jb=jb: e.matmul(psMA[:, :], wa[jb][:, c, :], yA[:, c, :], start=(c == 0), stop=(c == NCH - 1)), [("wa", jb), "yA"], ["psMA"])
                for c in range(32):
                    T(lambda e, c=c, jb=jb: e.matmul(psMB[:, :], wb_[jb][:, c, :], yB[:, c, :], start=(c == 0), stop=(c == 31)), [("wb", jb), "bufY"], ["psMB"])
                V(lambda e: e.tensor_tensor(tA[:, :], psGA[:, :], rstd[:, :], ALU.mult), ["psGA", "rstd"], ["tA"])
                A(lambda e: e.activation(out=sA[:, :], in_=tA[:, :], func=AF.Sigmoid), ["tA"], ["sA"])
                V(lambda e: e.tensor_tensor(tB[:, :], psGB[:, :], rstd[:, :], ALU.mult), ["psGB", "rstd"], ["tB"])
                A(lambda e: e.activation(out=sBt[:, :], in_=tB[:, :], func=AF.Sigmoid), ["tB"], ["sB"])
                V(lambda e: e.tensor_tensor(tA[:, :], sA[:, :], psMA[:, :], ALU.mult), ["sA", "psMA", "tA"], ["tA"])
                V(lambda e: e.tensor_tensor(tB[:, :], sBt[:, :], psMB[:, :], ALU.mult), ["sB", "psMB", "tB"], ["tB"])
                V(lambda e, j=j: e.tensor_tensor(merged[:, j, :], tA[:, :], tB[:, :], ALU.add), ["tA", "tB"], ["merged"])
            for dch in range(NCH):
                db = dch % 2
                sc.dma("pool", wo[db][:, :, :], wo_v[:, :, dch * 128:(dch + 1) * 128], ("wo", db), writes=[("wo", db)])
                for c in range(NCH):
                    T(lambda e, c=c, db=db: e.matmul(psGA[:, :], wo[db][:, c, :], merged[:, c, :], start=(c == 0), stop=(c == NCH - 1)), [("wo", db), "merged"], ["psGA"])
                V(lambda e, dch=dch: e.tensor_tensor(xf[:, dch, :], xf[:, dch, :], psGA[:, :], ALU.add), ["psGA", "xf"], ["xf"])
            for k in range(4):
                for c in range(NCH):
                    T(lambda e, c=c, k=k: e.transpose(psT[:, c % 4, :], xf[:, c, k * 128:(k + 1) * 128], identf[:, :]), ["xf", "identf"], ["psT"])
                    if c % 4 == 3:
                        V(lambda e, c=c, k=k: e.tensor_copy(stg[:, k, (c - 3) * 128:(c + 1) * 128], psT[:, :, :].rearrange("p a b -> p (a b)")),
                          ["psT", "bufY"], ["bufY"])
            sc.dma("sp", x2tm[hs, :].rearrange("(k p) d -> p k d", p=128), stg, "s_bufY", reads=["bufY"], writes=["x2tm"])
            rms("n2")
            for c in range(NCH):
                V(lambda e, c=c: e.scalar_tensor_tensor(h2f[:, c, :], xf[:, c, :], g2[:, c:c + 1], rstd[:, :], ALU.mult, ALU.mult),
                  ["xf", "g2", "rstd", "bufY"], ["bufY"])
            A(lambda e: e.copy(hb[:, :, :], h2f), ["bufY", "hb"], ["hb"])
            sc.dma("sp", h2b_v[:, :, hs], hb[:, :, :], "s_h2b", reads=["hb"], writes=["h2b_d"])
            for k in range(4):
                for c in range(NCH):
                    T(lambda e, c=c, k=k: e.matmul(psR[:, 0:72], h2f[:, c, k * 128:(k + 1) * 128], wr_sb[:, c, :], start=(c == 0), stop=(c == NCH - 1)),
                      ["bufY", "wr"], ["psR"])
                r = rt; s_ = rs_
                V(lambda e: e.tensor_tensor(r["lg"][:, :], psR[:, 0:72], br_sb[:, :], ALU.add), ["psR", "br"], ["r_lg"])
                V(lambda e: e.reduce_max(s_["gmax"][:, :], r["lg"][:, 0:8], AX.X), ["r_lg"], ["s_gmax"])
                V(lambda e: e.tensor_scalar(s_["ngmax"][:, :], s_["gmax"][:, :], -1.0, None, ALU.mult), ["s_gmax"], ["s_ngmax"])
                A(lambda e: e.activation(out=r["ge"][:, 0:8], in_=r["lg"][:, 0:8], func=AF.Exp, bias=s_["ngmax"][:, 0:1]), ["r_lg", "s_ngmax"], ["r_ge"])
                V(lambda e: e.reduce_sum(s_["gs"][:, :], r["ge"][:, 0:8], AX.X), ["r_ge"], ["s_gs"])
                V(lambda e: e.reciprocal(s_["gw"][:, :], s_["gs"][:, :]), ["s_gs"], ["s_gw"])
                V(lambda e: e.tensor_scalar(r["oh"][:, 0:8], r["lg"][:, 0:8], s_["gmax"][:, 0:1], None, ALU.is_equal), ["r_lg", "s_gmax"], ["r_oh"])
                le = r["lg"][:, 8:72].rearrange("p (g j) -> p g j", j=8)
                V(lambda e, le=le: e.tensor_tensor(r["tmp"][:, 0:64].rearrange("p (g j) -> p g j", j=8), le,
                                                   r["oh"][:, 0:8].unsqueeze(2).to_broadcast([128, 8, 8]), ALU.mult), ["r_lg", "r_oh"], ["r_tmp"])
                V(lambda e: e.reduce_sum(r["ig"][:, 0:8], r["tmp"][:, 0:64].rearrange("p (g j) -> p j g", j=8), AX.X), ["r_tmp"], ["r_ig"])
                V(lambda e: e.reduce_max(s_["m1"][:, :], r["ig"][:, 0:8], AX.X), ["r_ig"], ["s_m1"])
                V(lambda e: e.tensor_scalar(r["m1k"][:, 0:8], r["ig"][:, 0:8], s_["m1"][:, 0:1], None, ALU.is_equal), ["r_ig", "s_m1"], ["r_m1k"])
                V(lambda e: e.scalar_tensor_tensor(r["in2"][:, 0:8], r["m1k"][:, 0:8], -1e30, r["ig"][:, 0:8], ALU.mult, ALU.add), ["r_m1k", "r_ig"], ["r_in2"])
                V(lambda e: e.reduce_max(s_["m2"][:, :], r["in2"][:, 0:8], AX.X), ["r_in2"], ["s_m2"])
                V(lambda e: e.tensor_scalar(r["m2k"][:, 0:8], r["in2"][:, 0:8], s_["m2"][:, 0:1], None, ALU.is_equal), ["r_in2", "s_m2"], ["r_m2k"])
                V(lambda e: e.tensor_tensor(s_["d12"][:, :], s_["m1"][:, :], s_["m2"][:, :], ALU.subtract), ["s_m1", "s_m2"], ["s_d12"])
                A(lambda e: e.activation(out=s_["w1"][:, :], in_=s_["d12"][:, :], func=AF.Sigmoid), ["s_d12"], ["s_w1"])
                A(lambda e: e.activation(out=s_["w2"][:, :], in_=s_["d12"][:, :], func=AF.Sigmoid, scale=-1.0), ["s_d12"], ["s_w2"])
                V(lambda e: e.tensor_scalar(r["wl"][:, 0:8], r["m1k"][:, 0:8], s_["w1"][:, 0:1], None, ALU.mult), ["r_m1k", "s_w1"], ["r_wl"])
                V(lambda e: e.scalar_tensor_tensor(r["wl"][:, 0:8], r["m2k"][:, 0:8], s_["w2"][:, 0:1], r["wl"][:, 0:8], ALU.mult, ALU.add), ["r_m2k", "s_w2", "r_wl"], ["r_wl"])
                V(lambda e: e.tensor_scalar(r["wl"][:, 0:8], r["wl"][:, 0:8], s_["gw"][:, 0:1], None, ALU.mult), ["r_wl", "s_gw"], ["r_wl"])
                V(lambda e, k=k: e.tensor_tensor(wt_st[:, k, :].rearrange("p (g j) -> p g j", j=8), r["oh"][:, 0:8].unsqueeze(2).to_broadcast([128, 8, 8]),
                                                 r["wl"][:, 0:8].unsqueeze(1).to_broadcast([128, 8, 8]), ALU.mult), ["r_oh", "r_wl", "wt_st"], ["wt_st"])
            sc.dma("sp", wt_d[hs, :].rearrange("(k p) e -> p k e", p=128), wt_st[:, :, :], "s_wt", reads=["wt_st"], writes=["wt_d"])
        sc.final_wait("sp", ["x2tm", "h2b_d", "wt_d", "bufY", "hb", "wt_st"])
        sc.run()


def emit_l2b(nc, a, x2tm, h2b_d, wt_d, out, n_exp=NEXP):
    HT = 512
    with contextlib.ExitStack() as st:
        sb = lambda name, shape, dt: st.enter_context(nc.sbuf_tensor(name, shape, dt))
        ps = lambda name, shape, dt: st.enter_context(nc.psum_tensor(name, shape, dt))
        sc = Sched(nc, st)
        V = lambda fn, r, w: sc.op("dve", fn, reads=r, writes=w)
        A = lambda fn, r, w: sc.op("act", fn, reads=r, writes=w)
        T = lambda fn, r, w: sc.op("pe", fn, reads=r, writes=w)
        acc = sb("m_acc", [128, 8, D], F32); h2b = sb("m_h2b", [128, NCH, NT], BF16); wt = sb("m_wt", [128, 8, 64], F32)
        wg = [sb("m_wg%d" % i, [128, NCH, 512], BF16) for i in range(2)]
        wu2 = [sb("m_wu%d" % i, [128, NCH, 512], BF16) for i in range(2)]; wd2 = [sb("m_wd%d" % i, [128, 4, D], BF16) for i in range(2)]
        act = sb("m_act", [128, 4, NT], BF16); slb = [sb("m_slb%d" % i, [128, HT], BF16) for i in range(2)]
        psG = [ps("m_psG%d" % i, [128, HT], F32) for i in range(2)]; psU = [ps("m_psU%d" % i, [128, HT], F32) for i in range(2)]
        psD = [ps("m_psD%d" % i, [128, 512], F32) for i in range(2)]
        sc.dma("sp", acc[:, :, :], x2tm.rearrange("(k p) d -> p k d", p=128), "l_acc", writes=["acc"])
        sc.dma("sp", h2b[:, :, :], h2b_d.rearrange("(c p) t -> p c t", p=128), "l_h2b", writes=["h2b"])
        sc.dma("sp", wt[:, :, :], wt_d.rearrange("(k p) e -> p k e", p=128), "l_wt", writes=["wt"])
        n = 0
        for e_ in range(n_exp):
            eb = e_ % 2
            sc.dma("pool", wg[eb][:, :, :], a["w_gate"][e_].rearrange("(c p) f -> p c f", p=128), ("wg", eb), writes=[("wg", eb)])
            wu, wd = wu2[eb], wd2[eb]
            sc.dma("pool", wu[:, :, :], a["w_up"][e_].rearrange("(c p) f -> p c f", p=128), ("wu", eb), writes=[("wu", eb)])
            sc.dma("pool", wd[:, :, :], a["w_down"][e_].rearrange("(c p) d -> p c d", p=128), ("wd", eb), writes=[("wd", eb)])
            for hf in range(NT // HT):
                hs = slice(hf * HT, (hf + 1) * HT)
                for fch in range(4):
                    pb = n % 2; n += 1
                    fs = slice(fch * 128, (fch + 1) * 128)
                    for c in range(NCH):
                        T(lambda e, c=c, eb=eb, fs=fs, hs=hs, pb=pb: e.matmul(psG[pb][:, :], wg[eb][:, c, fs], h2b[:, c, hs], start=(c == 0), stop=(c == NCH - 1)),
                          [("wg", eb), "h2b"], [("psG", pb)])
                    for c in range(NCH):
                        T(lambda e, wu=wu, c=c, fs=fs, hs=hs, pb=pb: e.matmul(psU[pb][:, :], wu[:, c, fs], h2b[:, c, hs], start=(c == 0), stop=(c == NCH - 1)),
                          [("wu", eb), "h2b"], [("psU", pb)])
                    A(lambda e, pb=pb: e.activation(out=slb[pb][:, :], in_=psG[pb][:, :], func=AF.Silu), [("psG", pb)], [("slb", pb)])
                    V(lambda e, pb=pb, fch=fch, hs=hs: e.tensor_tensor(act[:, fch, hs], slb[pb][:, :], psU[pb][:, :], ALU.mult), [("slb", pb), ("psU", pb)], ["act"])
            for k in range(8):
                for db in range(4):
                    pd = (k * 4 + db) % 2
                    ds_ = slice(db * 512, (db + 1) * 512)
                    for fch in range(4):
                        T(lambda e, wd=wd, k=k, fch=fch, ds_=ds_, pd=pd: e.matmul(psD[pd][:, :], act[:, fch, k * 128:(k + 1) * 128], wd[:, fch, ds_], start=(fch == 0), stop=(fch == 3)),
                          ["act", ("wd", eb)], [("psD", pd)])
                    V(lambda e, k=k, ds_=ds_, pd=pd, e_=e_: e.scalar_tensor_tensor(acc[:, k, ds_], psD[pd][:, :], wt[:, k, e_:e_ + 1], acc[:, k, ds_], ALU.mult, ALU.add),
                      [("psD", pd), "wt", "acc"], ["acc"])
        sc.dma("sp", out.rearrange("(k p) d -> p k d", p=128), acc[:, :, :], "s_out", reads=["acc"], writes=["out"])
        sc.final_wait("sp", ["out"])
        sc.run()


L2_IN = (("xT", [D, NT], F32), ("yAT", [2048, NT], BF16), ("yBT", [4096, NT], BF16), ("gain1", [128, NCH], F32), ("gain2", [128, NCH], F32),
         ("wgate", [D, 4096], F32), ("wA", [2048, D], F32), ("wB", [4096, D], F32), ("wo", [D, D], F32), ("wr", [D, 72], F32), ("br", [128, 72], F32),
         ("ident_f", [128, 128], F32), ("w_gate", [NEXP, D, 512], F32), ("w_up", [NEXP, D, 512], F32), ("w_down", [NEXP, 512, D], F32))


def build_launch2(n_exp=NEXP):
    nc = bass.Bass("TRN2", target_bir_lowering=False)
    a = {name: nc.dram_tensor(name, shape, dt, kind="ExternalInput").ap() for (name, shape, dt) in L2_IN}
    out = nc.dram_tensor("out", [NT, D], F32, kind="ExternalOutput").ap()
    x2tm = nc.dram_tensor("x2tm_scr", [NT, D], F32).ap()
    h2b_d = nc.dram_tensor("h2b_scr", [D, NT], BF16).ap()
    wt_d = nc.dram_tensor("wt_scr", [NT, 64], F32).ap()
    emit_l2a(nc, a, x2tm, h2b_d, wt_d)
    emit_l2b(nc, a, x2tm, h2b_d, wt_d, out, n_exp)
    return nc


def launch2_inputs(inp, c, yAT, yBT, shared):
    ts = slice(c * NT, (c + 1) * NT)
    d = dict(shared)
    d["xT"] = np.ascontiguousarray(inp["x"][0, ts].T)
    d["yAT"] = np.ascontiguousarray(yAT[:, ts]); d["yBT"] = np.ascontiguousarray(yBT[:, ts])
    return d


def launch2_shared(inp):
    f = np.float32
    return {
        "gain1": np.ascontiguousarray(inp["norm1_gain"][0].reshape(NCH, 128).T), "gain2": np.ascontiguousarray(inp["norm2_gain"][0].reshape(NCH, 128).T),
        "wgate": np.ascontiguousarray(inp["w_in"][0][:, OFF_GA:OFF_GA + 4096]), "wA": inp["w_branch_att"][0], "wB": inp["w_branch_gdn"][0], "wo": inp["w_out"][0],
        "wr": np.ascontiguousarray(np.concatenate([inp["w_group_router"][0], inp["w_expert_router"][0]], axis=1)),
        "br": np.ascontiguousarray(np.broadcast_to(np.concatenate([inp["b_group_router"][0], inp["b_expert_router"][0]])[None, :], (128, 72)).astype(f)),
        "ident_f": np.eye(128, dtype=f), "w_gate": inp["w_gate"][0], "w_up": inp["w_up"][0], "w_down": inp["w_down"][0],
    }


def kernel(**inputs):
    inp = {k: np.asarray(v) for k, v in inputs.items()}
    cores = list(range(NCORES))
    nc1 = build_launch1(S)
    res1 = run_bass_kernel_spmd(nc1, [launch1_inputs(inp, c) for c in cores], core_ids=cores).results
    yAT = np.concatenate([np.asarray(res1[c]["yatt"]).reshape(256, S) for c in cores], axis=0)
    yBT = np.concatenate([np.ascontiguousarray(np.asarray(res1[c]["yg"]).T) for c in cores], axis=0)
    del res1
    nc2 = build_launch2()
    shared = launch2_shared(inp)
    res2 = run_bass_kernel_spmd(nc2, [launch2_inputs(inp, c, yAT, yBT, shared) for c in cores], core_ids=cores).results
    out = np.concatenate([np.asarray(res2[c]["out"]) for c in cores], axis=0)
    return out.reshape(1, S, D).astype(np.float32)
```

```python
import contextlib
import numpy as np
import concourse.bass as bass
import concourse.mybir as mybir
from concourse.bass_utils import run_bass_kernel_spmd

F32 = mybir.dt.float32
BF16 = mybir.dt.bfloat16
AF = mybir.ActivationFunctionType
ALU = mybir.AluOpType
AX = mybir.AxisListType

D = 2048
S = 8192
NCORES = 8
NCH = D // 128
EPS = 1e-6

ENG_NAMES = ("pe", "dve", "act", "pool", "sp")


class Sched:
    def __init__(self, nc, stack):
        self.nc = nc
        self.stack = stack
        self.q = {k: [] for k in ENG_NAMES}
        self.sems = {}
        self.count = {}
        self.waited = {k: {} for k in ENG_NAMES}
        self.last_w = {}
        self.readers = {}

    def _sem(self, key):
        if key not in self.sems:
            name = "s%d" % len(self.sems)
            self.sems[key] = self.stack.enter_context(self.nc.semaphore(name))
            self.count[key] = 0
        return self.sems[key]

    def _deps(self, eng, reads, writes):
        deps = []
        for r in reads:
            if r in self.last_w:
                deps.append(self.last_w[r])
        for w in writes:
            if w in self.last_w:
                deps.append(self.last_w[w])
            deps.extend(self.readers.get(w, ()))
        waits = []
        for (key, val, src) in deps:
            if src == "pe" and eng == "pe":
                continue
            if self.waited[eng].get(key, 0) >= val:
                continue
            self.waited[eng][key] = val
            waits.append((key, val))
        return waits

    def _commit(self, tok, reads, writes):
        for w in writes:
            self.last_w[w] = tok
            self.readers[w] = []
        for r in reads:
            self.readers.setdefault(r, []).append(tok)

    def op(self, eng, fn, reads=(), writes=()):
        waits = self._deps(eng, reads, writes)
        key = ("e", eng)
        sem = self._sem(key)
        self.count[key] += 1
        tok = (key, self.count[key], eng)
        wl = [(self._sem(k), v) for (k, v) in waits]

        def emit(e, fn=fn, wl=wl, sem=sem):
            for (s, v) in wl:
                e.wait_ge(s, v)
            fn(e).then_inc(sem, 1)
        self.q[eng].append(emit)
        self._commit(tok, reads, writes)

    def dma(self, eng, out, in_, sem_key, reads=(), writes=(), **kw):
        waits = self._deps(eng, reads, writes)
        key = ("d", sem_key)
        sem = self._sem(key)
        self.count[key] += 16
        tok = (key, self.count[key], "dma")
        wl = [(self._sem(k), v) for (k, v) in waits]

        def emit(e, wl=wl, sem=sem):
            for (s, v) in wl:
                e.wait_ge(s, v)
            e.dma_start(out=out, in_=in_, **kw).then_inc(sem, 16)
        self.q[eng].append(emit)
        self._commit(tok, reads, writes)

    def final_wait(self, eng, resources):
        waits = self._deps(eng, resources, ())
        wl = [(self._sem(k), v) for (k, v) in waits]

        def emit(e, wl=wl):
            for (s, v) in wl:
                e.wait_ge(s, v)
        self.q[eng].append(emit)

    def run(self):
        nc = self.nc
        with nc.Block() as block:
            @block.tensor
            def _(e):
                for f in self.q["pe"]:
                    f(e)

            @block.vector
            def _(e):
                for f in self.q["dve"]:
                    f(e)

            @block.scalar
            def _(e):
                for f in self.q["act"]:
                    f(e)

            @block.gpsimd
            def _(e):
                for f in self.q["pool"]:
                    f(e)

            @block.sync
            def _(e):
                for f in self.q["sp"]:
                    f(e)


def emit_stage1(nc, xT, w, gain, pT, n_cols, n_tok, tok_blk=256):
    assert n_cols % 128 == 0 and n_tok % tok_blk == 0
    ncc = n_cols // 128
    ntt = n_tok // tok_blk
    xT_v = xT.rearrange("(c p) t -> p c t", p=128)
    w_v = w.rearrange("(c p) n -> p c n", p=128)
    with contextlib.ExitStack() as st:
        sb = lambda name, shape, dt: st.enter_context(nc.sbuf_tensor(name, shape, dt))
        ps = lambda name, shape, dt: st.enter_context(nc.psum_tensor(name, shape, dt))
        wb = sb("wb", [128, NCH, n_cols], BF16)
        g_sb = sb("g_sb", [128, NCH], F32)
        ones = sb("ones1", [128, 128], BF16)
        xf = [sb("xf%d" % i, [128, NCH, tok_blk], F32) for i in range(2)]
        xb = [sb("xb%d" % i, [128, NCH, tok_blk], BF16) for i in range(2)]
        xsq = [sb("xsq%d" % i, [128, NCH, tok_blk], BF16) for i in range(1)] * 2
        rstd = [sb("rstd%d" % i, [128, tok_blk], F32) for i in range(2)]
        ob = [sb("ob%d" % i, [128, tok_blk], BF16) for i in range(4)]
        ps_ss = ps("ps_ss", [128, tok_blk], F32)
        ps_o = [ps("ps_o%d" % i, [128, tok_blk], F32) for i in range(4)]
        sc = Sched(nc, st)

        sc.op("pool", lambda e: e.memset(ones[:, :], 1.0), writes=["ones"])
        sc.dma("sp", g_sb[:, :], gain[:, :], "g_sb", writes=["g_sb"])
        for c in range(NCH):
            sc.dma("pool", wb[:, c, :], w_v[:, c, :], "wb", writes=[("wb", c), "wb_all"])
        for c in range(NCH):
            sc.op("dve", lambda e, c=c: e.tensor_scalar(wb[:, c, :], wb[:, c, :], g_sb[:, c:c + 1], None, ALU.mult),
                  reads=[("wb", c), "wb_all", "g_sb"], writes=[("wb", c)])
        for t in range(ntt):
            b = t % 2
            tsl = slice(t * tok_blk, (t + 1) * tok_blk)
            sc.dma("sp", xf[b][:, :, :], xT_v[:, :, tsl], ("xf", b), writes=[("xf", b)])
            sc.op("act", lambda e, b=b: e.activation(out=xsq[b][:, :, :], in_=xf[b][:, :, :], func=AF.Square),
                  reads=[("xf", b)], writes=["xsq"])
            sc.op("dve", lambda e, b=b: e.tensor_copy(xb[b][:, :, :], xf[b][:, :, :]),
                  reads=[("xf", b)], writes=[("xb", b)])
            for c in range(NCH):
                sc.op("pe", lambda e, b=b, c=c: e.matmul(ps_ss[:, :], ones[:, :], xsq[b][:, c, :],
                                                         start=(c == 0), stop=(c == NCH - 1)),
                      reads=["xsq", "ones"], writes=["ps_ss"])
            sc.op("dve", lambda e, b=b: e.tensor_scalar(rstd[b][:, :], ps_ss[:, :], 1.0 / D, EPS, ALU.mult, ALU.add),
                  reads=["ps_ss"], writes=[("rstd", b)])
            sc.op("act", lambda e, b=b: e.activation(out=rstd[b][:, :], in_=rstd[b][:, :], func=AF.Sqrt),
                  reads=[("rstd", b)], writes=[("rstd", b)])
            sc.op("dve", lambda e, b=b: e.reciprocal(rstd[b][:, :], rstd[b][:, :]),
                  reads=[("rstd", b)], writes=[("rstd", b)])
            for j in range(ncc):
                k = (t * ncc + j) % 4
                for c in range(NCH):
                    sc.op("pe", lambda e, b=b, c=c, j=j, k=k: e.matmul(
                        ps_o[k][:, :], wb[:, c, j * 128:(j + 1) * 128], xb[b][:, c, :],
                        start=(c == 0), stop=(c == NCH - 1)),
                        reads=[("xb", b), ("wb", c)], writes=[("ps_o", k)])
                sc.op("dve", lambda e, b=b, k=k: e.tensor_tensor(ob[k][:, :], ps_o[k][:, :], rstd[b][:, :], ALU.mult),
                      reads=[("ps_o", k), ("rstd", b)], writes=[("ob", k)])
                sc.dma("sp", pT[j * 128:(j + 1) * 128, tsl], ob[k][:, :], ("ob", k),
                       reads=[("ob", k)], writes=["pT_out"])
        sc.final_wait("sp", [("ob", k) for k in range(4)] + ["pT_out"])
        for k in range(4):
            key = ("d", ("ob", k))
            if key in sc.sems:
                sc.q["sp"].append(lambda e, s=sc.sems[key], v=sc.count[key]: e.wait_ge(s, v))
        sc.run()


def build_stage1(n_cols, n_tok, tok_blk=256):
    nc = bass.Bass("TRN2", target_bir_lowering=False)
    xT = nc.dram_tensor("xT", [D, n_tok], F32, kind="ExternalInput").ap()
    w = nc.dram_tensor("w", [D, n_cols], F32, kind="ExternalInput").ap()
    gain = nc.dram_tensor("gain", [128, NCH], F32, kind="ExternalInput").ap()
    pT = nc.dram_tensor("pT", [n_cols, n_tok], BF16, kind="ExternalOutput").ap()
    emit_stage1(nc, xT, w, gain, pT, n_cols, n_tok, tok_blk)
    return nc


ATT_PATTERNS = ((128, 1), (512, 4), (2048, 16))
HALF = 64


def alibi_slopes_np():
    n = 48
    return np.exp2(-8.0 * np.arange(1, n + 1, dtype=np.float64) / n).reshape(3, 16)


def att_bias_consts(head_slot):
    out = np.zeros((3, 128, 2, 128), np.float32)
    i = np.arange(128)[:, None]
    j = np.arange(128)[None, :]
    sl = alibi_slopes_np()
    for g, (_, d) in enumerate(ATT_PATTERNS):
        for h, off in enumerate((-64, 64)):
            rel = i - j + off
            b = -sl[g, head_slot] * np.abs(rel) * d
            out[g, :, h, :] = np.where(np.abs(rel) <= HALF, b, -1e30)
    return out


def emit_att(nc, q_of, k_of, vT, qg, kg, bias, ident_d, yT, n_tok, tag):
    QT = 512
    PIECE = min(2048, n_tok)
    DMAX = 16
    with contextlib.ExitStack() as st:
        sb = lambda name, shape, dt: st.enter_context(nc.sbuf_tensor(name + tag, shape, dt))
        ps = lambda name, shape, dt: st.enter_context(nc.psum_tensor(name + tag, shape, dt))
        sc = Sched(nc, st)
        ones = sb("a_ones", [128, 128], BF16)
        ident = sb("a_ident", [128, 128], BF16)
        g_q = sb("a_gq", [128, 3], F32)
        g_k = sb("a_gk", [128, 3], F32)
        gmax = sb("a_gmax", [128, 8], F32)
        gabs = sb("a_gabs", [128, 8], BF16)
        gT = sb("a_gT", [8, 128], BF16)
        gred = sb("a_gred", [8, 2], BF16)
        negB = sb("a_negB", [128, 1], F32)
        bias_sb = sb("a_bias", [128, 3, 2, 128], F32)
        raw = sb("a_raw", [128, PIECE], BF16)
        sq = sb("a_sq", [128, PIECE], BF16)
        rs = sb("a_rs", [128, PIECE], F32)
        vT_sb = sb("a_vT", [128, n_tok], BF16)
        accO = sb("a_accO", [128, n_tok], F32)
        accL = sb("a_accL", [128, n_tok], F32)
        yb = sb("a_yb", [128, PIECE], BF16)
        qn = sb("a_qn", [128, n_tok], BF16)
        kn = sb("a_kn", [128, n_tok + 128 * DMAX], BF16)
        vg = sb("a_vg", [128, n_tok + 128 * DMAX], BF16)
        sS = sb("a_sS", [128, 4, 2, 128], F32)
        pP = sb("a_pP", [128, 4, 2, 128], BF16)
        ps_n = ps("a_psn", [128, 512], F32)
        ps_t = ps("a_pst", [128, 128], BF16)
        ps_s = ps("a_pss", [128, 4, 2, 128], F32)
        ps_O = ps("a_psO", [128, QT], F32)
        ps_L = ps("a_psL", [128, QT], F32)
        ps_g = ps("a_psg", [128, 128], F32)

        sc.op("pool", lambda e: e.memset(ones[:, :], 1.0), writes=["ones"])
        sc.dma("sp", ident[:, :], ident_d[:, :], "c_ident", writes=["ident"])
        sc.dma("sp", g_q[:, :], qg[:, :], "c_gq", writes=["g_q"])
        sc.dma("sp", g_k[:, :], kg[:, :], "c_gk", writes=["g_k"])
        sc.dma("sp", bias_sb[:, :, :, :], bias.rearrange("g p h j -> p g h j"), "c_bias", writes=["bias"])
        sc.dma("sp", vT_sb[:, :], vT, "c_v", writes=["vT"])

        def bcast_absmax(gain_sb, col, gtag):
            sc.op("dve", lambda e: e.tensor_scalar(gmax[:, 0:3], gain_sb[:, :], -1.0, None, ALU.mult),
                  reads=[gtag, "gmax"], writes=["gmax"])
            sc.op("dve", lambda e: e.tensor_tensor(gmax[:, 0:3], gmax[:, 0:3], gain_sb[:, :], ALU.max),
                  reads=[gtag, "gmax"], writes=["gmax"])
            sc.op("dve", lambda e: e.reduce_max(gmax[:, 4:5], gmax[:, 0:3], AX.X), reads=["gmax"], writes=["gmax"])
            sc.op("dve", lambda e: e.tensor_copy(gabs[:, 0:1], gmax[:, 4:5]), reads=["gmax"], writes=["gabs"])
            sc.op("pe", lambda e: e.transpose(ps_t[0:1, :], gabs[:, 0:1], ident[:, :]), reads=["gabs", "ident"], writes=["ps_t"])
            sc.op("dve", lambda e: e.tensor_copy(gT[0:1, :], ps_t[0:1, :]), reads=["ps_t"], writes=["gT"])
            sc.op("dve", lambda e: e.reduce_max(gred[0:1, 0:1], gT[0:1, :], AX.X), reads=["gT"], writes=["gred"])
            sc.op("pe", lambda e: e.matmul(ps_g[:, col:col + 1], ones[0:1, :], gred[0:1, 0:1], start=True, stop=True),
                  reads=["gred", "ones"], writes=["ps_g"])
            sc.op("dve", lambda e: e.tensor_copy(gmax[:, 5 + col:6 + col], ps_g[:, col:col + 1]), reads=["ps_g", "gmax"], writes=["gmax"])
        sc.op("pool", lambda e: e.memset(gmax[:, :], 0.0), writes=["gmax"])
        bcast_absmax(g_q, 0, "g_q")
        bcast_absmax(g_k, 1, "g_k")
        sc.op("dve", lambda e: e.scalar_tensor_tensor(negB[:, :], gmax[:, 5:6], -1.02 * float(np.sqrt(128.0)),
                                                      gmax[:, 6:7], ALU.mult, ALU.mult),
              reads=["gmax"], writes=["negB"])

        def norm_into(src, gain_sb, g, dst, dst_is_k, extra_scale):
            d = ATT_PATTERNS[g][1]
            L = n_tok // d
            name = "kn" if dst_is_k else "qn"
            if dst_is_k:
                sc.op("pool", lambda e: e.memset(dst[:, :], 0.0), writes=[name])
            for T0 in range(0, n_tok, PIECE):
                sc.dma("sp", raw[:, :], src[:, T0:T0 + PIECE], "a_raw", writes=["raw"])
                sc.op("act", lambda e: e.activation(out=sq[:, :], in_=raw[:, :], func=AF.Square), reads=["raw"], writes=["sq"])
                for t0 in range(0, PIECE, 512):
                    sc.op("pe", lambda e, t0=t0: e.matmul(ps_n[:, :], ones[:, :], sq[:, t0:t0 + 512], start=True, stop=True),
                          reads=["sq", "ones"], writes=["ps_n"])
                    sc.op("dve", lambda e, t0=t0: e.tensor_scalar(rs[:, t0:t0 + 512], ps_n[:, :], 1.0 / 128, EPS, ALU.mult, ALU.add),
                          reads=["ps_n"], writes=["rs"])
                sc.op("act", lambda e: e.activation(out=rs[:, :], in_=rs[:, :], func=AF.Sqrt), reads=["rs"], writes=["rs"])
                sc.op("dve", lambda e: e.reciprocal(rs[:, :], rs[:, :]), reads=["rs"], writes=["rs"])
                if extra_scale != 1.0:
                    sc.op("dve", lambda e: e.tensor_scalar(rs[:, :], rs[:, :], extra_scale, None, ALU.mult), reads=["rs"], writes=["rs"])
                l0, l1 = T0 // d, (T0 + PIECE) // d
                if dst_is_k:
                    out_ap = dst[:, 0:d * (L + 128)].rearrange("p (r l) -> p r l", r=d)[:, :, HALF + l0:HALF + l1]
                else:
                    out_ap = dst[:, 0:n_tok].rearrange("p (r l) -> p r l", r=d)[:, :, l0:l1]
                in_raw = raw[:, :].rearrange("p (l r) -> p r l", r=d)
                in_rs = rs[:, :].rearrange("p (l r) -> p r l", r=d)
                sc.op("dve", lambda e, out_ap=out_ap, in_raw=in_raw, in_rs=in_rs: e.scalar_tensor_tensor(
                    out_ap, in_raw, gain_sb[:, g:g + 1], in_rs, ALU.mult, ALU.mult),
                    reads=["raw", "rs", "g_q", "g_k", name], writes=[name])

        first = True
        for g in range(3):
            d = ATT_PATTERNS[g][1]
            L = n_tok // d
            nb = L // 128 + 1
            Lp = L + 128
            norm_into(q_of(g), g_q, g, qn, False, float(128.0 ** -0.5))
            norm_into(k_of(g), g_k, g, kn, True, 1.0)
            sc.op("pool", lambda e: e.memset(vg[:, :], 0.0), writes=["vg"])
            v_res = vT_sb[:, :].rearrange("p (l r) -> p r l", r=d)
            for r in range(d):
                for b in range(nb):
                    lo, hi = max(128 * b - HALF, 0), min(128 * b + HALF, L)
                    p0 = lo - (128 * b - HALF)
                    n = hi - lo
                    col = (r * nb + b) * 128
                    sc.op("pe", lambda e, vr=v_res, r=r, lo=lo, hi=hi, n=n: e.transpose(ps_t[0:n, :], vr[:, r, lo:hi], ident[:, :]),
                          reads=["vT", "ident"], writes=["ps_t"])
                    sc.op("act", lambda e, p0=p0, n=n, col=col: e.copy(vg[p0:p0 + n, col:col + 128], ps_t[0:n, :]),
                          reads=["ps_t", "vg"], writes=["vg"])
            accO_v = accO[:, :].rearrange("p (l r) -> p r l", r=d)
            accL_v = accL[:, :].rearrange("p (l r) -> p r l", r=d)
            for r in range(d):
                for q0 in range(0, L, QT):
                    nt = min(QT, L - q0) // 128
                    for a in range(nt):
                        qa = q0 // 128 + a
                        for h in range(2):
                            kcol = r * Lp + 128 * (qa + h)
                            sc.op("pe", lambda e, a=a, h=h, kcol=kcol, r=r, qa=qa, L=L: e.matmul(
                                ps_s[:, a, h, :], kn[:, kcol:kcol + 128], qn[:, r * L + 128 * qa:r * L + 128 * qa + 128],
                                start=True, stop=True), reads=["kn", "qn"], writes=["ps_s"])
                    sc.op("dve", lambda e, g=g, nt=nt: e.tensor_tensor(
                        sS[:, 0:nt, :, :], ps_s[:, 0:nt, :, :],
                        bias_sb[:, g, :, :].unsqueeze(1).to_broadcast([128, nt, 2, 128]),
                        ALU.add), reads=["ps_s", "bias"], writes=["sS"])
                    sc.op("act", lambda e, nt=nt: e.activation(out=pP[:, 0:nt, :, :], in_=sS[:, 0:nt, :, :], func=AF.Exp, bias=negB[:, 0:1]),
                          reads=["sS", "negB"], writes=["pP"])
                    if q0 == 0:
                        sc.op("pool", lambda e: e.memset(pP[0:64, 0, 0, :], 0.0), reads=["pP"], writes=["pP"])
                    if q0 + 128 * nt == L:
                        sc.op("pool", lambda e, nt=nt: e.memset(pP[64:128, nt - 1, 1, :], 0.0), reads=["pP"], writes=["pP"])
                    for a in range(nt):
                        qa = q0 // 128 + a
                        for h in range(2):
                            col = (r * nb + qa + h) * 128
                            sc.op("pe", lambda e, a=a, h=h, col=col: e.matmul(
                                ps_O[:, 128 * a:128 * a + 128], vg[:, col:col + 128], pP[:, a, h, :],
                                start=(h == 0), stop=(h == 1)), reads=["vg", "pP"], writes=["ps_O"])
                            sc.op("pe", lambda e, a=a, h=h: e.matmul(
                                ps_L[:, 128 * a:128 * a + 128], ones[:, :], pP[:, a, h, :],
                                start=(h == 0), stop=(h == 1)), reads=["ones", "pP"], writes=["ps_L"])
                    n = 128 * nt
                    if first:
                        sc.op("dve", lambda e, av=accO_v, r=r, q0=q0, n=n: e.tensor_copy(av[:, r, q0:q0 + n], ps_O[:, 0:n]),
                              reads=["ps_O"], writes=["accO"])
                        sc.op("act", lambda e, av=accL_v, r=r, q0=q0, n=n: e.copy(av[:, r, q0:q0 + n], ps_L[:, 0:n]),
                              reads=["ps_L"], writes=["accL"])
                    else:
                        sc.op("dve", lambda e, av=accO_v, r=r, q0=q0, n=n: e.tensor_tensor(av[:, r, q0:q0 + n], av[:, r, q0:q0 + n], ps_O[:, 0:n], ALU.add),
                              reads=["ps_O", "accO"], writes=["accO"])
                        sc.op("dve", lambda e, av=accL_v, r=r, q0=q0, n=n: e.tensor_tensor(av[:, r, q0:q0 + n], av[:, r, q0:q0 + n], ps_L[:, 0:n], ALU.add),
                              reads=["ps_L", "accL"], writes=["accL"])
            first = False
        sc.op("dve", lambda e: e.reciprocal(accL[:, :], accL[:, :]), reads=["accL"], writes=["accL"])
        for T0 in range(0, n_tok, PIECE):
            sc.op("dve", lambda e, T0=T0: e.tensor_tensor(yb[:, :], accO[:, T0:T0 + PIECE], accL[:, T0:T0 + PIECE], ALU.mult),
                  reads=["accO", "accL", "yb"], writes=["yb"])
            sc.dma("sp", yT[:, T0:T0 + PIECE], yb[:, :], "a_yb", reads=["yb"], writes=["y_out"])
        sc.final_wait("sp", ["y_out", "yb"])
        sc.run()


def build_att(n_tok):
    nc = bass.Bass("TRN2", target_bir_lowering=False)
    qT = nc.dram_tensor("qT", [3, 128, n_tok], BF16, kind="ExternalInput").ap()
    kT = nc.dram_tensor("kT", [3, 128, n_tok], BF16, kind="ExternalInput").ap()
    vT = nc.dram_tensor("vT", [128, n_tok], BF16, kind="ExternalInput").ap()
    qg = nc.dram_tensor("qg", [128, 3], F32, kind="ExternalInput").ap()
    kg = nc.dram_tensor("kg", [128, 3], F32, kind="ExternalInput").ap()
    bias = nc.dram_tensor("bias", [3, 128, 2, 128], F32, kind="ExternalInput").ap()
    ident_d = nc.dram_tensor("ident", [128, 128], BF16, kind="ExternalInput").ap()
    yT = nc.dram_tensor("yT", [128, n_tok], BF16, kind="ExternalOutput").ap()
    emit_att(nc, lambda g: qT[g], lambda g: kT[g], vT[:, :], qg, kg, bias, ident_d, yT, n_tok, "")
    return nc


def gdn_consts():
    i = np.arange(128)[:, None]; j = np.arange(128)[None, :]
    same = (i // 64) == (j // 64)
    c = {}
    c["ident_f"] = np.eye(128, dtype=np.float32)
    c["ones_f"] = np.ones((128, 128), np.float32)
    c["mstrict"] = np.stack([(same & (i > j)), (same & (i < j))]).astype(np.float32)
    c["mincl"] = np.stack([(same & (i >= j)), (same & (i <= j))]).astype(np.float32)
    r = i; m = j
    c["cum"] = np.stack([(same & (r <= m)), (same & (r >= m))]).astype(np.float32)
    lastf = np.where(np.arange(128) < 64, 63, 127)[None, :]; lastb = np.where(np.arange(128) < 64, 0, 64)[None, :]
    c["sel"] = np.stack([(r == lastf), (r == lastb)]).astype(np.float32)
    selh = np.zeros((2, 2, 128, 128), np.float32)
    for dr in range(2):
        for hf in range(2):
            selh[dr, hf, (63 + 64 * hf) if dr == 0 else 64 * hf, :] = 1.0
    c["selh"] = selh
    return c


GDN_IN = (("qraw", [2, 128, None], BF16), ("kraw", [2, 128, None], BF16), ("vraw", [4, 128, None], BF16), ("zraw", [4, 128, None], BF16),
          ("abr", [16, None], BF16))
GDN_PAR = (("cw", [128, 8, 5], F32), ("alog", [128, 8], F32), ("dtb", [128, 8], F32), ("ng", [128, 128], F32),
           ("ident_f", [128, 128], F32), ("ident_b", [128, 128], BF16), ("ones_f", [128, 128], F32),
           ("mstrict", [2, 128, 128], F32), ("mincl", [2, 128, 128], F32), ("cum", [2, 128, 128], F32), ("sel", [2, 128, 128], F32),
           ("selh", [2, 2, 128, 128], F32))


def build_gdn(n_tok):
    nc = bass.Bass("TRN2", target_bir_lowering=False)
    aps = {}
    for (name, shape, dt) in GDN_IN + GDN_PAR:
        aps[name] = nc.dram_tensor(name, [n_tok if v is None else v for v in shape], dt, kind="ExternalInput").ap()
    yg = nc.dram_tensor("yg", [n_tok, 512], BF16, kind="ExternalOutput").ap()
    ofw = nc.dram_tensor("ofw_scr", [4, n_tok, 128], F32).ap()
    emit_gdn(nc, aps, yg, ofw, n_tok)
    return nc


def emit_gdn(nc, aps, yg, ofw, n_tok):
    TB = 512; NP = 4
    nblk = n_tok // TB
    qraw, kraw, vraw, zraw, abr = (aps[k] for k in ("qraw", "kraw", "vraw", "zraw", "abr"))
    cw, alog, dtb, ngd = (aps[k] for k in ("cw", "alog", "dtb", "ng"))
    c_ident_f, c_ident_b, c_ones_f = aps["ident_f"], aps["ident_b"], aps["ones_f"]
    c_mstrict, c_mincl, c_cum, c_sel, c_selh = (aps[k] for k in ("mstrict", "mincl", "cum", "sel", "selh"))
    with contextlib.ExitStack() as st:
        sb = lambda name, shape, dt: st.enter_context(nc.sbuf_tensor(name, shape, dt))
        ps = lambda name, shape, dt: st.enter_context(nc.psum_tensor(name, shape, dt))
        sc = Sched(nc, st)
        V = lambda fn, r, w: sc.op("dve", fn, reads=r, writes=w)
        A = lambda fn, r, w: sc.op("act", fn, reads=r, writes=w)
        G = lambda fn, r, w: sc.op("pool", fn, reads=r, writes=w)
        T = lambda fn, r, w: sc.op("pe", fn, reads=r, writes=w)
        identf = sb("identf", [128, 128], F32); identb = sb("identb", [128, 128], BF16); onesf = sb("onesf", [128, 128], F32)
        mstr = sb("mstr", [128, 2, 128], F32); minc = sb("minc", [128, 2, 128], F32)
        cum = sb("cum_sb", [128, 2, 128], F32); sel = sb("sel_sb", [128, 2, 128], F32); selh = sb("selh_sb", [128, 4, 128], F32)
        cw_sb = sb("cw_sb", [128, 8, 5], F32); nega = sb("nega", [128, 8], F32); dtb_sb = sb("dtb_sb", [128, 8], F32)
        ng_sb = sb("ng_sb", [128, 128], F32)
        for (t_, d_, nm) in ((identf, c_ident_f, "identf"), (identb, c_ident_b, "identb"), (onesf, c_ones_f, "onesf"),
                             (nega, alog, "nega"), (dtb_sb, dtb, "dtb"), (ng_sb, ngd, "ng"), (cw_sb, cw, "cw")):
            sc.dma("sp", t_[:], d_[:] if len(d_.shape) == 2 else d_[:, :, :], "c_" + nm, writes=[nm])
        sc.dma("sp", mstr[:, :, :], c_mstrict.rearrange("r p j -> p r j"), "c_mstr", writes=["mstr"])
        sc.dma("sp", minc[:, :, :], c_mincl.rearrange("r p j -> p r j"), "c_minc", writes=["minc"])
        sc.dma("sp", cum[:, :, :], c_cum.rearrange("r p j -> p r j"), "c_cum", writes=["cum"])
        sc.dma("sp", sel[:, :, :], c_sel.rearrange("r p j -> p r j"), "c_sel", writes=["sel"])
        sc.dma("sp", selh[:, :, :], c_selh.rearrange("r h p j -> p (r h) j"), "c_selh", writes=["selh"])
        A(lambda e: e.activation(out=nega[:, :], in_=nega[:, :], func=AF.Exp), ["nega"], ["nega"])
        V(lambda e: e.tensor_scalar(nega[:, :], nega[:, :], -1.0, None, ALU.mult), ["nega"], ["nega"])

        raw2 = [sb("raw_g%d" % i, [128, TB + 4], BF16) for i in range(2)]; rawf2 = [sb("rawf%d" % i, [128, TB + 4], F32) for i in range(2)]
        raw = raw2[0]
        acc8 = [sb("acc_g%d" % i, [128, TB], F32) for i in range(8)]; sq4 = [sb("sq_g%d" % i, [128, TB], F32) for i in range(4)]
        fm = [sb("fm%d" % i, [128, TB], BF16) for i in range(8)]
        tm = [sb("tm%d" % i, [128, NP, 128], BF16) for i in range(8)]
        zt = [sb("zt%d" % i, [128, NP, 128], F32) for i in range(4)]
        KK = [sb("KK%d" % i, [128, NP, 128], F32) for i in range(2)]; QK = [sb("QK%d" % i, [128, NP, 128], F32) for i in range(2)]
        ab_fm = sb("ab_fm", [16, TB], BF16); ab_tm = sb("ab_tm", [128, NP, 16], F32)
        gt = {n: sb("gt_" + n, [128, NP, 4], F32) for n in ("x", "nx", "ax", "e", "l", "g", "beta", "nbeta", "G", "eG", "gl", "tail", "bG", "d0", "d1")}
        diagG = sb("diagG", [128, NP, 128], F32); dec = sb("dec", [128, NP, 128], F32); t1 = sb("t1", [128, NP, 128], F32)
        Ab = [sb("Ab%d" % i, [128, NP, 128], F32) for i in range(2)]; Bb = [sb("Bb%d" % i, [128, NP, 128], F32) for i in range(2)]
        intra = sb("intra", [128, NP, 128], BF16); X = sb("X_g", [128, NP, 256], F32); qd = sb("qd", [128, NP, 128], BF16)
        scan = [[{n: sb("sc%d_%d_%s" % (bb, vh, n), [128, NP, 128], BF16) for n in ("u", "wT", "qdT", "inT", "kt")} for vh in range(4)] for bb in range(2)]
        dS = [[sb("dS%d_%d" % (bb, hf), [128, NP, 4], F32) for hf in range(2)] for bb in range(2)]
        oblk = [sb("oblk%d" % vh, [128, NP, 128], F32) for vh in range(4)]
        ofl = sb("ofl", [128, NP, 128], F32); junk = sb("junk", [128, 128], F32); ss = sb("ss_g", [128, NP], F32); yb = sb("yb", [128, NP, 128], BF16)
        Sst = [sb("S%d" % vh, [128, 128], F32) for vh in range(4)]; Sbf = [sb("Sbf%d" % vh, [128, 128], BF16) for vh in range(4)]
        vnew = [sb("vnew%d" % vh, [128, 128], BF16) for vh in range(4)]
        psM = ps("psM", [128, NP, 128], F32); psX = ps("psX", [128, NP, 256], F32); psA = ps("psA", [128, NP, 128], F32); psB = ps("psB", [128, NP, 128], F32)
        psT = ps("psT", [128, NP, 128], BF16)
        psg = psB[:, 0, :]
        pss = [ps("pss%d" % i, [128, 3, 128], F32) for i in range(2)]
        bc = lambda ap: ap.unsqueeze(2).to_broadcast([128, NP, 128])
        bcp = lambda ap: ap.unsqueeze(1).to_broadcast([128, NP, 128])

        def prep_block(dr, blk, bb):
            t0 = blk * TB
            lo, hi = max(t0 - 2, 0), min(t0 + TB + 2, n_tok)
            g_ = gt
            def gates_gen():
                sc.dma("sp", ab_fm[:, :], abr[:, t0:t0 + TB], "ab_fm", writes=["ab_fm"])
                yield
                for p in range(NP):
                    T(lambda e, p=p: e.transpose(psT[:, p, 0:16], ab_fm[:, 128 * p:128 * p + 128], identb[0:16, 0:16]), ["ab_fm", "identb"], ["psT"])
                    yield
                V(lambda e: e.tensor_copy(ab_tm[:, :, :], psT[:, :, 0:16]), ["psT"], ["ab_tm"])
                yield
                g_ = gt
                row = lambda t_, dr=dr: t_[:, dr * 4:dr * 4 + 4].unsqueeze(1).to_broadcast([128, NP, 4])
                V(lambda e: e.tensor_tensor(g_["x"][:], ab_tm[:, :, dr * 4:dr * 4 + 4], row(dtb_sb), ALU.add), ["ab_tm", "dtb"], ["g_x"])
                yield
                V(lambda e: e.tensor_scalar(g_["nx"][:], g_["x"][:], -1.0, None, ALU.mult), ["g_x"], ["g_nx"])
                yield
                V(lambda e: e.tensor_tensor(g_["ax"][:], g_["x"][:], g_["nx"][:], ALU.max), ["g_x", "g_nx"], ["g_ax"])
                yield
                A(lambda e: e.activation(out=g_["e"][:], in_=g_["ax"][:], func=AF.Exp, scale=-1.0), ["g_ax"], ["g_e"])
                yield
                V(lambda e: e.tensor_scalar(g_["e"][:], g_["e"][:], 1.0, None, ALU.add), ["g_e"], ["g_e"])
                yield
                A(lambda e: e.activation(out=g_["l"][:], in_=g_["e"][:], func=AF.Ln), ["g_e"], ["g_l"])
                yield
                V(lambda e: e.scalar_tensor_tensor(g_["g"][:], g_["x"][:], 0.0, g_["l"][:], ALU.max, ALU.add), ["g_x", "g_l"], ["g_g"])
                yield
                V(lambda e: e.tensor_tensor(g_["g"][:], g_["g"][:], row(nega), ALU.mult), ["g_g", "nega"], ["g_g"])
                yield
                A(lambda e: e.activation(out=g_["beta"][:], in_=ab_tm[:, :, 8 + dr * 4:12 + dr * 4], func=AF.Sigmoid), ["ab_tm"], ["g_beta"])
                yield
                V(lambda e: e.tensor_scalar(g_["nbeta"][:], g_["beta"][:], -1.0, None, ALU.mult), ["g_beta"], ["g_nbeta"])
                yield
                T(lambda e: e.matmul(psg[:, 0:16], cum[:, dr, :], g_["g"][:].rearrange("p a b -> p (a b)"), start=True, stop=True), ["g_g", "cum"], [("psB", 0)])
                yield
                V(lambda e: e.tensor_copy(g_["G"][:].rearrange("p a b -> p (a b)"), psg[:, 0:16]), [("psB", 0)], ["g_G"])
                yield
                A(lambda e: e.activation(out=g_["eG"][:], in_=g_["G"][:], func=AF.Exp), ["g_G"], ["g_eG"])
                yield
                T(lambda e: e.matmul(psg[:, 16:32], sel[:, dr, :], g_["G"][:].rearrange("p a b -> p (a b)"), start=True, stop=True), ["g_G", "sel"], [("psB", 0)])
                yield
                V(lambda e: e.tensor_tensor(g_["gl"][:].rearrange("p a b -> p (a b)"), psg[:, 16:32], g_["G"][:].rearrange("p a b -> p (a b)"), ALU.subtract), [("psB", 0), "g_G"], ["g_gl"])
                yield
                A(lambda e: e.activation(out=g_["tail"][:], in_=g_["gl"][:], func=AF.Exp), ["g_gl"], ["g_tail"])
                yield
                V(lambda e: e.tensor_tensor(g_["bG"][:], g_["beta"][:], g_["eG"][:], ALU.mult), ["g_beta", "g_eG"], ["g_bG"])
                yield
                for hf in range(2):
                    T(lambda e, hf=hf: e.matmul(psg[:, 32 + 16 * hf:48 + 16 * hf], selh[:, dr * 2 + hf, :], g_["G"][:].rearrange("p a b -> p (a b)"), start=True, stop=True),
                      ["g_G", "selh"], [("psB", 0)])
                    A(lambda e, hf=hf: e.activation(out=dS[bb][hf][:].rearrange("p a b -> p (a b)"), in_=psg[:, 32 + 16 * hf:48 + 16 * hf], func=AF.Exp), [("psB", 0)], [("dS", bb)])
                    yield

            gg = gates_gen()
            srcs = [(qraw, 0), (qraw, 1), (kraw, 0), (kraw, 1), (vraw, 0), (vraw, 1), (vraw, 2), (vraw, 3)]
            for ti, (src, idx) in enumerate(srcs):
                rb = ti % 2
                G(lambda e, rb=rb: e.memset(raw2[rb][:, :], 0.0), [], [("raw", rb)])
                sc.dma("sp", raw2[rb][:, lo - (t0 - 2):hi - (t0 - 2)], src[idx, :, lo:hi], ("raw_g", rb), writes=[("raw", rb)])
                V(lambda e, rb=rb: e.tensor_copy(rawf2[rb][:, :], raw2[rb][:, :]), [("raw", rb)], [("rawf", rb)])
                V(lambda e, ti=ti, rb=rb: e.tensor_scalar(acc8[ti][:, :], rawf2[rb][:, 0:TB], cw_sb[:, ti, 0:1], None, ALU.mult), [("rawf", rb), "cw"], [("acc", ti)])
                for j in range(1, 5):
                    V(lambda e, ti=ti, j=j, rb=rb: e.scalar_tensor_tensor(acc8[ti][:, :], rawf2[rb][:, j:j + TB], cw_sb[:, ti, j:j + 1], acc8[ti][:, :], ALU.mult, ALU.add),
                      [("rawf", rb), "cw", ("acc", ti)], [("acc", ti)])
                for _ in range(3):
                    next(gg, None)
            for ti in range(8):
                next(gg, None)
                if ti >= 4:
                    A(lambda e, ti=ti: e.activation(out=fm[ti][:, :], in_=acc8[ti][:, :], func=AF.Silu), [("acc", ti)], [("fm", ti)])
                else:
                    A(lambda e, ti=ti: e.activation(out=acc8[ti][:, :], in_=acc8[ti][:, :], func=AF.Silu), [("acc", ti)], [("acc", ti)])
            assert next(gg, 'end') == 'end', 'gate chain must be fully emitted before the norm phase (both use psB)'
            nps = [psX[:, 0:2, :], psX[:, 2:4, :], psA[:, :, :], psB[:, :, :]]
            npk = [[("psX", 0)], [("psX", 1)], [("psA", 0), ("psA", 1)], [("psB", 0), ("psB", 1)]]
            for ti in range(4):
                V(lambda e, ti=ti: e.tensor_tensor(sq4[ti][:, :], acc8[ti][:, :], acc8[ti][:, :], ALU.mult), [("acc", ti)], [("sq", ti)])
            for ti in range(4):
                T(lambda e, ti=ti: e.matmul(nps[ti], onesf[:, :], sq4[ti][:, :], start=True, stop=True), [("sq", ti), "onesf"], npk[ti])
            for ti in range(4):
                V(lambda e, ti=ti: e.tensor_scalar(sq4[ti][:, :], nps[ti], EPS, None, ALU.add), npk[ti] + [("sq", ti)], [("sq", ti)])
            for ti in range(4):
                A(lambda e, ti=ti: e.activation(out=sq4[ti][:, :], in_=sq4[ti][:, :], func=AF.Sqrt), [("sq", ti)], [("sq", ti)])
            for ti in range(4):
                V(lambda e, ti=ti: e.reciprocal(sq4[ti][:, :], sq4[ti][:, :]), [("sq", ti)], [("sq", ti)])
            for ti in range(4):
                scl = float(128.0 ** -0.5) if ti < 2 else 1.0
                V(lambda e, ti=ti, scl=scl: e.scalar_tensor_tensor(fm[ti][:, :], acc8[ti][:, :], scl, sq4[ti][:, :], ALU.mult, ALU.mult), [("acc", ti), ("sq", ti)], [("fm", ti)])
            for ti in range(8):
                for p in range(NP):
                    T(lambda e, ti=ti, p=p: e.transpose(psT[:, p, :], fm[ti][:, 128 * p:128 * p + 128], identb[:, :]), [("fm", ti), "identb"], ["psT"])
                A(lambda e, ti=ti: e.copy(tm[ti][:, :, :], psT[:, :, :]), ["psT"], [("tm", ti)])
            for _ in gg:
                pass
            for qh in range(2):
                for p in range(NP):
                    cs = slice(128 * p, 128 * p + 128)
                    T(lambda e, qh=qh, p=p, cs=cs: e.matmul(psA[:, p, :], fm[2 + qh][:, cs], fm[2 + qh][:, cs], start=True, stop=True), [("fm", 2 + qh)], [("psA", 0), ("psA", 1)])
                    T(lambda e, qh=qh, p=p, cs=cs: e.matmul(psB[:, p, :], fm[qh][:, cs], fm[2 + qh][:, cs], start=True, stop=True), [("fm", qh), ("fm", 2 + qh)], [("psB", 0), ("psB", 1)])
                V(lambda e, qh=qh: e.tensor_copy(KK[qh][:], psA[:]), [("psA", 0), ("psA", 1)], [("KK", qh)])
                A(lambda e, qh=qh: e.copy(QK[qh][:], psB[:]), [("psB", 0), ("psB", 1)], [("QK", qh)])
            for vh in range(4):
                qh = vh // 2
                sb_ = scan[bb][vh]
                Gc = g_["G"][:, :, vh]
                V(lambda e, Gc=Gc: e.tensor_tensor(diagG[:], bcp(identf[:, :]), bc(Gc), ALU.mult), ["identf", "g_G"], ["diagG"])
                for p in range(NP):
                    T(lambda e, p=p: e.matmul(psM[:, p, :], onesf[:, :], diagG[:, p, :], start=True, stop=True), ["diagG", "onesf"], ["psM"])
                V(lambda e, Gc=Gc: e.tensor_tensor(dec[:], bc(Gc), psM[:], ALU.subtract), ["psM", "g_G"], ["dec"])
                V(lambda e: e.tensor_scalar_min(dec[:], dec[:], 0.0), ["dec"], ["dec"])
                A(lambda e: e.activation(out=dec[:], in_=dec[:], func=AF.Exp), ["dec"], ["dec"])
                V(lambda e, qh=qh: e.tensor_tensor(t1[:], dec[:], KK[qh][:], ALU.mult), ["dec", ("KK", qh)], ["t1"])
                V(lambda e, vh=vh: e.tensor_tensor(t1[:], t1[:], bc(g_["nbeta"][:, :, vh]), ALU.mult), ["t1", "g_nbeta"], ["t1"])
                V(lambda e: e.tensor_tensor(Ab[0][:], t1[:], bcp(mstr[:, dr, :]), ALU.mult), ["t1", "mstr"], [("Ab", 0, 0), ("Ab", 0, 1)])
                G(lambda e, qh=qh: e.tensor_tensor(t1[:], dec[:], QK[qh][:], ALU.mult), ["dec", ("QK", qh), "t1"], ["t1"])
                G(lambda e: e.tensor_tensor(intra[:], t1[:], bcp(minc[:, dr, :]), ALU.mult), ["t1", "minc"], ["intra"])
                for p in range(NP):
                    T(lambda e, p=p: e.transpose(psM[:, p, :], Ab[0][:, p, :], identf[:, :]), [("Ab", 0, 0), ("Ab", 0, 1), "identf"], ["psM"])
                    T(lambda e, p=p: e.transpose(psT[:, p, :], intra[:, p, :], identb[:, :]), ["intra", "identb"], ["psT"])
                V(lambda e: e.tensor_copy(Bb[0][:], psM[:]), ["psM"], [("Bb", 0, 0), ("Bb", 0, 1)])
                A(lambda e, sb_=sb_: e.copy(sb_["inT"][:], psT[:]), ["psT"], [("scan", bb, vh)])
                V(lambda e, vh=vh: e.tensor_tensor(X[:, :, 0:128], tm[4 + vh][:], bc(g_["beta"][:, :, vh]), ALU.mult), [("tm", 4 + vh), "g_beta"], [("X", 0), ("X", 1)])
                V(lambda e, vh=vh, qh=qh: e.tensor_tensor(X[:, :, 128:256], tm[2 + qh][:], bc(g_["bG"][:, :, vh]), ALU.mult), [("tm", 2 + qh), "g_bG", ("X", 0), ("X", 1)], [("X", 0), ("X", 1)])
                for k in range(6):
                    a_, b_ = Ab[k % 2], Bb[k % 2]
                    an, bn = Ab[(k + 1) % 2], Bb[(k + 1) % 2]
                    for hp in range(2):
                        for p in (2 * hp, 2 * hp + 1):
                            T(lambda e, p=p, b_=b_: e.matmul(psX[:, p, :], b_[:, p, :], X[:, p, :], start=True, stop=True), [("Bb", k % 2, hp), ("X", hp)], [("psX", hp)])
                        if k < 5:
                            for p in (2 * hp, 2 * hp + 1):
                                T(lambda e, p=p, a_=a_, b_=b_: e.matmul(psA[:, p, :], b_[:, p, :], a_[:, p, :], start=True, stop=True), [("Ab", k % 2, hp), ("Bb", k % 2, hp)], [("psA", hp)])
                                T(lambda e, p=p, a_=a_, b_=b_: e.matmul(psB[:, p, :], a_[:, p, :], b_[:, p, :], start=True, stop=True), [("Ab", k % 2, hp), ("Bb", k % 2, hp)], [("psB", hp)])
                    for hp in range(2):
                        prs = slice(2 * hp, 2 * hp + 2)
                        V(lambda e, prs=prs: e.tensor_tensor(X[:, prs, :], X[:, prs, :], psX[:, prs, :], ALU.add), [("psX", hp), ("X", hp)], [("X", hp)])
                        if k < 5:
                            A(lambda e, an=an, prs=prs: e.copy(an[:, prs, :], psA[:, prs, :]), [("psA", hp)], [("Ab", (k + 1) % 2, hp)])
                            (A if hp == 0 else V)(lambda e, bn=bn, prs=prs, hp=hp: (e.copy if hp == 0 else e.tensor_copy)(bn[:, prs, :], psB[:, prs, :]), [("psB", hp)], [("Bb", (k + 1) % 2, hp)])
                A(lambda e, sb_=sb_: e.copy(sb_["u"][:], X[:, :, 0:128]), [("X", 0), ("X", 1)], [("scan", bb, vh)])
                for p in range(NP):
                    T(lambda e, p=p: e.transpose(psM[:, p, :], X[:, p, 128:256], identf[:, :]), [("X", p // 2), "identf"], ["psM"])
                V(lambda e, sb_=sb_: e.tensor_copy(sb_["wT"][:], psM[:]), ["psM", ("scan", bb, vh)], [("scan", bb, vh)])
                V(lambda e, vh=vh, qh=qh: e.tensor_tensor(qd[:], tm[qh][:], bc(g_["eG"][:, :, vh]), ALU.mult), [("tm", qh), "g_eG"], ["qd"])
                for p in range(NP):
                    T(lambda e, p=p: e.transpose(psT[:, p, :], qd[:, p, :], identb[:, :]), ["qd", "identb"], ["psT"])
                A(lambda e, sb_=sb_: e.copy(sb_["qdT"][:], psT[:]), ["psT", ("scan", bb, vh)], [("scan", bb, vh)])
                G(lambda e, sb_=sb_, vh=vh, qh=qh: e.tensor_tensor(sb_["kt"][:], tm[2 + qh][:], bc(g_["tail"][:, :, vh]), ALU.mult),
                  [("tm", 2 + qh), "g_tail", ("scan", bb, vh)], [("scan", bb, vh)])

        def scan_block(dr, blk, bb):
            order = [(p, hf) for p in range(NP) for hf in range(2)]
            if dr == 1:
                order = order[::-1]
            for si, (p, hf) in enumerate(order):
                rows = slice(64 * hf, 64 * hf + 64)
                for vh in range(4):
                    sb_ = scan[bb][vh]
                    pp = pss[vh % 2]
                    T(lambda e, sb_=sb_, p=p, vh=vh, pp=pp: e.matmul(pp[:, 0, :], sb_["wT"][:, p, :], Sbf[vh][:, :], start=True, stop=True),
                      [("scan", bb, vh), ("Sbf", vh)], [("pss", vh % 2)])
                    V(lambda e, sb_=sb_, p=p, vh=vh, pp=pp, rows=rows: e.tensor_tensor(vnew[vh][rows, :], sb_["u"][rows, p, :], pp[rows, 0, :], ALU.subtract),
                      [("pss", vh % 2), ("scan", bb, vh)], [("vnew", vh)])
                    T(lambda e, sb_=sb_, p=p, vh=vh, pp=pp: e.matmul(pp[:, 1, :], sb_["qdT"][:, p, :], Sbf[vh][:, :], start=True, stop=False),
                      [("scan", bb, vh), ("Sbf", vh)], [("pss", vh % 2)])
                    T(lambda e, sb_=sb_, p=p, vh=vh, pp=pp, rows=rows: e.matmul(pp[:, 1, :], sb_["inT"][rows, p, :], vnew[vh][rows, :], start=False, stop=True),
                      [("scan", bb, vh), ("vnew", vh)], [("pss", vh % 2)])
                    A(lambda e, p=p, vh=vh, pp=pp, rows=rows: e.copy(oblk[vh][rows, p, :], pp[rows, 1, :]), [("pss", vh % 2)], [("oblk", vh)])
                    T(lambda e, sb_=sb_, p=p, vh=vh, pp=pp, rows=rows: e.matmul(pp[:, 2, :], sb_["kt"][rows, p, :], vnew[vh][rows, :], start=True, stop=True),
                      [("scan", bb, vh), ("vnew", vh)], [("pss", vh % 2)])
                    V(lambda e, p=p, vh=vh, pp=pp, hf=hf: e.scalar_tensor_tensor(Sst[vh][:, :], Sst[vh][:, :], dS[bb][hf][:, p, vh:vh + 1], pp[:, 2, :], ALU.mult, ALU.add),
                      [("pss", vh % 2), ("dS", bb), ("S", vh)], [("S", vh)])
                    A(lambda e, vh=vh: e.copy(Sbf[vh][:, :], Sst[vh][:, :]), [("S", vh)], [("Sbf", vh)])

        def finish_block(dr, blk):
            t0 = blk * TB
            for vh in range(4):
                dst = ofw[vh, t0:t0 + TB, :].rearrange("(p i) d -> i p d", i=128)
                if dr == 0:
                    sc.dma("sp", dst, oblk[vh][:, :, :], ("oblk", vh), reads=[("oblk", vh)], writes=[("ofw", vh, blk)])
                    continue
                sc.dma("sp", ofl[:, :, :], dst, "ofl", reads=[("ofw", vh, blk)], writes=["ofl"])
                V(lambda e, vh=vh: e.tensor_tensor(ofl[:], ofl[:], oblk[vh][:], ALU.add), ["ofl", ("oblk", vh)], ["ofl"])
                for p in range(NP):
                    A(lambda e, p=p: e.activation(out=junk[:, :], in_=ofl[:, p, :], func=AF.Square, accum_out=ss[:, p:p + 1]), ["ofl", "junk", "ss"], ["junk", "ss"])
                V(lambda e: e.tensor_scalar(ss[:, :], ss[:, :], 1.0 / 128, EPS, ALU.mult, ALU.add), ["ss"], ["ss"])
                A(lambda e: e.activation(out=ss[:, :], in_=ss[:, :], func=AF.Sqrt), ["ss"], ["ss"])
                V(lambda e: e.reciprocal(ss[:, :], ss[:, :]), ["ss"], ["ss"])
                V(lambda e: e.tensor_tensor(ofl[:], ofl[:], bc(ss[:, :]), ALU.mult), ["ofl", "ss"], ["ofl"])
                V(lambda e: e.tensor_tensor(ofl[:], ofl[:], bcp(ng_sb[:, :]), ALU.mult), ["ofl", "ng"], ["ofl"])
                sc.dma("sp", raw[:, 0:TB], zraw[vh, :, t0:t0 + TB], ("raw_g", 0), writes=[("raw", 0)])
                for p in range(NP):
                    T(lambda e, p=p: e.transpose(psT[:, p, :], raw[:, 128 * p:128 * p + 128], identb[:, :]), [("raw", 0), "identb"], ["psT"])
                A(lambda e, vh=vh: e.activation(out=zt[vh][:], in_=psT[:], func=AF.Silu), ["psT"], [("zt", vh)])
                V(lambda e, vh=vh: e.tensor_tensor(yb[:], ofl[:], zt[vh][:], ALU.mult), ["ofl", ("zt", vh)], ["yb"])
                sc.dma("sp", yg[t0:t0 + TB, 128 * vh:128 * vh + 128].rearrange("(p i) d -> i p d", i=128), yb[:, :, :], "yb", reads=["yb"], writes=["yg_out"])

        for dr in range(2):
            for vh in range(4):
                G(lambda e, vh=vh: e.memset(Sst[vh][:, :], 0.0), [], [("S", vh)])
                G(lambda e, vh=vh: e.memset(Sbf[vh][:, :], 0.0), [], [("Sbf", vh)])
            blocks = list(range(nblk)) if dr == 0 else list(range(nblk))[::-1]
            for bi, blk in enumerate(blocks):
                bb = bi % 2
                prep_block(dr, blk, bb)
                scan_block(dr, blk, bb)
                finish_block(dr, blk)
        sc.final_wait("sp", ["yg_out", "yb"])
        sc.run()


L1_CHUNKS = 27
OFF_QA, OFF_KA, OFF_VA = 0, 6144, 12288
OFF_QG, OFF_KG, OFF_VG, OFF_ZG, OFF_A, OFF_B, OFF_GA, OFF_GB = 14336, 16384, 18432, 22528, 26624, 26688, 26752, 28800


def w1_cols(c):
    cols = []
    r = np.arange(128)
    for s_ in range(2):
        hs = 2 * c + s_
        for g in range(3):
            cols.append(OFF_QA + g * 2048 + hs * 128 + r)
        for g in range(3):
            cols.append(OFF_KA + g * 2048 + hs * 128 + r)
        cols.append(OFF_VA + hs * 128 + r)
    for qh in range(2):
        cols.append(OFF_QG + (2 * c + qh) * 128 + r)
    for qh in range(2):
        cols.append(OFF_KG + (2 * c + qh) * 128 + r)
    for vh in range(4):
        cols.append(OFF_VG + (4 * c + vh) * 128 + r)
    for vh in range(4):
        cols.append(OFF_ZG + (4 * c + vh) * 128 + r)
    ab = np.full(128, -1)
    for dr in range(2):
        for vh in range(4):
            ab[dr * 4 + vh] = OFF_A + dr * 32 + 4 * c + vh
            ab[8 + dr * 4 + vh] = OFF_B + dr * 32 + 4 * c + vh
    cols.append(ab)
    return np.concatenate(cols)


def build_launch1(n_tok):
    nc = bass.Bass("TRN2", target_bir_lowering=False)
    di = lambda name, shape, dt: nc.dram_tensor(name, shape, dt, kind="ExternalInput").ap()
    n_cols = L1_CHUNKS * 128
    xT = di("xT", [D, n_tok], F32); w1 = di("w1", [D, n_cols], F32); gain1 = di("gain1", [128, NCH], F32)
    qg = di("qg", [128, 3], F32); kg = di("kg", [128, 3], F32); bias = di("att_bias", [2, 3, 128, 2, 128], F32)
    aps = {}
    for (name, shape, dt) in GDN_PAR:
        aps[name] = di(name, shape, dt)
    yatt = nc.dram_tensor("yatt", [2, 128, n_tok], BF16, kind="ExternalOutput").ap()
    yg = nc.dram_tensor("yg", [n_tok, 512], BF16, kind="ExternalOutput").ap()
    pT = nc.dram_tensor("pT_scr", [n_cols, n_tok], BF16).ap()
    ofw = nc.dram_tensor("ofw_scr", [4, n_tok, 128], F32).ap()
    emit_stage1(nc, xT, w1, gain1, pT, n_cols, n_tok)
    for s_ in range(2):
        base = 7 * s_ * 128
        emit_att(nc, lambda g, base=base: pT[base + 128 * g:base + 128 * g + 128, :],
                 lambda g, base=base: pT[base + 128 * (3 + g):base + 128 * (4 + g), :],
                 pT[base + 768:base + 896, :], qg, kg, bias[s_], aps["ident_b"], yatt[s_], n_tok, "_s%d" % s_)
    g0 = 14 * 128
    aps["qraw"] = pT[g0:g0 + 256, :].rearrange("(i p) t -> i p t", p=128)
    aps["kraw"] = pT[g0 + 256:g0 + 512, :].rearrange("(i p) t -> i p t", p=128)
    aps["vraw"] = pT[g0 + 512:g0 + 1024, :].rearrange("(i p) t -> i p t", p=128)
    aps["zraw"] = pT[g0 + 1024:g0 + 1536, :].rearrange("(i p) t -> i p t", p=128)
    aps["abr"] = pT[g0 + 1536:g0 + 1552, :]
    emit_gdn(nc, aps, yg, ofw, n_tok)
    return nc


def launch1_inputs(inp, c, n_tok=S):
    import ml_dtypes
    f = np.float32
    cols = w1_cols(c)
    w_in = inp["w_in"][0]
    w1 = np.zeros((D, cols.size), f)
    m = cols >= 0
    w1[:, m] = w_in[:, cols[m]]
    C = gdn_consts()
    cwf = inp["gdn_conv_w"][0]
    chans = [np.arange(128) + (2 * c + qh) * 128 for qh in range(2)] + [2048 + np.arange(128) + (2 * c + qh) * 128 for qh in range(2)] \
        + [4096 + np.arange(128) + (4 * c + vh) * 128 for vh in range(4)]
    cw = np.stack([cwf[:, ch].T for ch in chans], axis=1)
    sel8 = lambda a: np.concatenate([a[0, 4 * c:4 * c + 4], a[1, 4 * c:4 * c + 4]])
    d = {
        "xT": np.ascontiguousarray(inp["x"][0, :n_tok].T), "w1": w1,
        "gain1": np.ascontiguousarray(inp["norm1_gain"][0].reshape(NCH, 128).T),
        "qg": np.ascontiguousarray(inp["q_norm_gain"][0].T), "kg": np.ascontiguousarray(inp["k_norm_gain"][0].T),
        "att_bias": np.stack([att_bias_consts(2 * c), att_bias_consts(2 * c + 1)]),
        "cw": np.ascontiguousarray(cw.astype(f)),
        "alog": np.ascontiguousarray(np.broadcast_to(sel8(inp["gdn_a_log"][0])[None, :], (128, 8)).astype(f)),
        "dtb": np.ascontiguousarray(np.broadcast_to(sel8(inp["gdn_dt_bias"][0])[None, :], (128, 8)).astype(f)),
        "ng": np.ascontiguousarray(np.broadcast_to(inp["gdn_norm_gain"][0][None, :], (128, 128)).astype(f)),
        "ident_f": C["ident_f"], "ident_b": C["ident_f"].astype(ml_dtypes.bfloat16), "ones_f": C["ones_f"],
        "mstrict": C["mstrict"], "mincl": C["mincl"], "cum": C["cum"], "sel": C["sel"], "selh": C["selh"],
    }
    return d


NT = 1024
NEXP = 64


def emit_l2a(nc, a, x2tm, h2b_d, wt_d):
    HT = 512
    xT_v = a["xT"].rearrange("(c p) t -> p c t", p=128)
    yA_v = a["yAT"].rearrange("(c p) t -> p c t", p=128)
    yB_v = a["yBT"].rearrange("(c p) t -> p c t", p=128)
    wgate_v = a["wgate"].rearrange("(c p) n -> p c n", p=128)
    wA_v = a["wA"].rearrange("(c p) n -> p c n", p=128)
    wB_v = a["wB"].rearrange("(c p) n -> p c n", p=128)
    wo_v = a["wo"].rearrange("(c p) n -> p c n", p=128)
    wr_v = a["wr"].rearrange("(c p) n -> p c n", p=128)
    h2b_v = h2b_d.rearrange("(c p) t -> p c t", p=128)
    with contextlib.ExitStack() as st:
        sb = lambda name, shape, dt: st.enter_context(nc.sbuf_tensor(name, shape, dt))
        ps = lambda name, shape, dt: st.enter_context(nc.psum_tensor(name, shape, dt))
        sc = Sched(nc, st)
        V = lambda fn, r, w: sc.op("dve", fn, reads=r, writes=w)
        A = lambda fn, r, w: sc.op("act", fn, reads=r, writes=w)
        G = lambda fn, r, w: sc.op("pool", fn, reads=r, writes=w)
        T = lambda fn, r, w: sc.op("pe", fn, reads=r, writes=w)
        ones = sb("b_ones", [128, 128], BF16); identf = sb("b_identf", [128, 128], F32)
        g1 = sb("b_g1", [128, NCH], F32); g2 = sb("b_g2", [128, NCH], F32)
        wr_sb = sb("b_wr", [128, NCH, 72], F32); br_sb = sb("b_br", [128, 72], F32)
        xf = sb("b_xf", [128, NCH, HT], F32); hb = sb("b_hb", [128, NCH, HT], BF16); xsq = sb("b_xsq", [128, NCH, HT], BF16)
        yA = sb("b_yA", [128, NCH, HT], BF16); bufY = sb("b_bufY", [128, 32 * HT * 2 // 4], F32)
        yB = bufY[:, :].bitcast(BF16).rearrange("p (c t) -> p c t", t=HT)
        stg = bufY[:, :].rearrange("p (k d) -> p k d", d=D)
        h2f = bufY[:, :].rearrange("p (c t) -> p c t", t=HT)
        merged = sb("b_merged", [128, NCH, HT], BF16)
        rstd = sb("b_rstd", [128, HT], F32)
        wgA = [sb("b_wgA%d" % i, [128, NCH, 128], BF16) for i in range(2)]; wgB = [sb("b_wgB%d" % i, [128, NCH, 128], BF16) for i in range(2)]
        wa = [sb("b_wa%d" % i, [128, NCH, 128], BF16) for i in range(2)]; wb_ = [sb("b_wb%d" % i, [128, 32, 128], BF16) for i in range(2)]
        wo = [sb("b_wo%d" % i, [128, NCH, 128], BF16) for i in range(2)]
        tA = sb("b_tA", [128, HT], F32); tB = sb("b_tB", [128, HT], F32); sA = sb("b_sA", [128, HT], F32); sBt = sb("b_sB", [128, HT], F32)
        rt = {n: sb("b_r_" + n, [128, 72], F32) for n in ("lg", "ge", "oh", "tmp", "ig", "m1k", "in2", "m2k", "wl")}
        rs_ = {n: sb("b_s_" + n, [128, 1], F32) for n in ("gmax", "ngmax", "gs", "gw", "m1", "m2", "d12", "w1", "w2")}
        wt_st = sb("b_wtst", [128, 4, 64], F32)
        ps_ss = ps("b_psss", [128, HT], F32)
        psGA = ps("b_psGA", [128, HT], F32); psGB = ps("b_psGB", [128, HT], F32); psMA = ps("b_psMA", [128, HT], F32); psMB = ps("b_psMB", [128, HT], F32)
        psT = ps("b_psT", [128, 4, 128], F32); psR = ps("b_psR", [128, 128], F32)

        G(lambda e: e.memset(ones[:, :], 1.0), [], ["ones"])
        sc.dma("sp", identf[:, :], a["ident_f"][:, :], "c1", writes=["identf"])
        sc.dma("sp", g1[:, :], a["gain1"][:, :], "c2", writes=["g1"])
        sc.dma("sp", g2[:, :], a["gain2"][:, :], "c3", writes=["g2"])
        sc.dma("sp", wr_sb[:, :, :], wr_v, "c4", writes=["wr"])
        sc.dma("sp", br_sb[:, :], a["br"][:, :], "c5", writes=["br"])

        def rms(tag):
            A(lambda e: e.activation(out=xsq[:, :, :], in_=xf[:, :, :], func=AF.Square), ["xf"], ["xsq"])
            for c in range(NCH):
                T(lambda e, c=c: e.matmul(ps_ss[:, :], ones[:, :], xsq[:, c, :], start=(c == 0), stop=(c == NCH - 1)), ["xsq", "ones"], ["ps_ss"])
            V(lambda e: e.tensor_scalar(rstd[:, :], ps_ss[:, :], 1.0 / D, EPS, ALU.mult, ALU.add), ["ps_ss"], ["rstd"])
            A(lambda e: e.activation(out=rstd[:, :], in_=rstd[:, :], func=AF.Sqrt), ["rstd"], ["rstd"])
            V(lambda e: e.reciprocal(rstd[:, :], rstd[:, :]), ["rstd"], ["rstd"])

        for hf in range(NT // HT):
            hs = slice(hf * HT, (hf + 1) * HT)
            sc.dma("sp", xf[:, :, :], xT_v[:, :, hs], "l_xf", writes=["xf"])
            sc.dma("sp", yA[:, :, :], yA_v[:, :, hs], "l_yA", writes=["yA"])
            sc.dma("sp", yB, yB_v[:, :, hs], "l_bufY", writes=["bufY"])
            rms("n1")
            for c in range(NCH):
                V(lambda e, c=c: e.tensor_scalar(hb[:, c, :], xf[:, c, :], g1[:, c:c + 1], None, ALU.mult), ["xf", "g1"], ["hb"])
            for j in range(NCH):
                jb = j % 2
                js = slice(j * 128, (j + 1) * 128)
                j2 = slice(2048 + j * 128, 2048 + (j + 1) * 128)
                sc.dma("pool", wgA[jb][:, :, :], wgate_v[:, :, js], ("wgA", jb), writes=[("wgA", jb)])
                sc.dma("pool", wgB[jb][:, :, :], wgate_v[:, :, j2], ("wgB", jb), writes=[("wgB", jb)])
                sc.dma("pool", wa[jb][:, :, :], wA_v[:, :, js], ("wa", jb), writes=[("wa", jb)])
                sc.dma("pool", wb_[jb][:, :, :], wB_v[:, :, js], ("wb", jb), writes=[("wb", jb)])
                for c in range(NCH):
                    T(lambda e, c=c, jb=jb: e.matmul(psGA[:, :], wgA[jb][:, c, :], hb[:, c, :], start=(c == 0), stop=(c == NCH - 1)), [("wgA", jb), "hb"], ["psGA"])
                for c in range(NCH):
                    T(lambda e, c=c, jb=jb: e.matmul(psGB[:, :], wgB[jb][:, c, :], hb[:, c, :], start=(c == 0), stop=(c == NCH - 1)), [("wgB", jb), "hb"], ["psGB"])
                for c in range(NCH):
                    T(lambda e, c=c, jb=jb: e.matmul(psMA[:, :], wa[jb][:, c, :], yA[:, c, :], start=(c == 0), stop=(c == NCH - 1)), [("wa", jb), "yA"], ["psMA"])
                for c in range(32):
                    T(lambda e, c=c, jb=jb: e.matmul(psMB[:, :], wb_[jb][:, c, :], yB[:, c, :], start=(c == 0), stop=(c == 31)), [("wb", jb), "bufY"], ["psMB"])
                V(lambda e: e.tensor_tensor(tA[:, :], psGA[:, :], rstd[:, :], ALU.mult), ["psGA", "rstd"], ["tA"])
                A(lambda e: e.activation(out=sA[:, :], in_=tA[:, :], func=AF.Sigmoid), ["tA"], ["sA"])
                V(lambda e: e.tensor_tensor(tB[:, :], psGB[:, :], rstd[:, :], ALU.mult), ["psGB", "rstd"], ["tB"])
                A(lambda e: e.activation(out=sBt[:, :], in_=tB[:, :], func=AF.Sigmoid), ["tB"], ["sB"])
                V(lambda e: e.tensor_tensor(tA[:, :], sA[:, :], psMA[:, :], ALU.mult), ["sA", "psMA", "tA"], ["tA"])
                V(lambda e: e.tensor_tensor(tB[:, :], sBt[:, :], psMB[:, :], ALU.mult), ["sB", "psMB", "tB"], ["tB"])
                V(lambda e, j=j: e.tensor_tensor(merged[:, j, :], tA[:, :], tB[:, :], ALU.add), ["tA", "tB"], ["merged"])
            for dch in range(NCH):
                db = dch % 2
                sc.dma("pool", wo[db][:, :, :], wo_v[:, :, dch * 128:(dch + 1) * 128], ("wo", db), writes=[("wo", db)])
                for c in range(NCH):
                    T(lambda e, c=c, db=db: e.matmul(psGA[:, :], wo[db][:, c, :], merged[:, c, :], start=(c == 0), stop=(c == NCH - 1)), [("wo", db), "merged"], ["psGA"])
                V(lambda e, dch=dch: e.tensor_tensor(xf[:, dch, :], xf[:, dch, :], psGA[:, :], ALU.add), ["psGA", "xf"], ["xf"])
            for k in range(4):
                for c in range(NCH):
                    T(lambda e, c=c, k=k: e.transpose(psT[:, c % 4, :], xf[:, c, k * 128:(k + 1) * 128], identf[:, :]), ["xf", "identf"], ["psT"])
                    if c % 4 == 3:
                        V(lambda e, c=c, k=k: e.tensor_copy(stg[:, k, (c - 3) * 128:(c + 1) * 128], psT[:, :, :].rearrange("p a b -> p (a b)")),
                          ["psT", "bufY"], ["bufY"])
            sc.dma("sp", x2tm[hs, :].rearrange("(k p) d -> p k d", p=128), stg, "s_bufY", reads=["bufY"], writes=["x2tm"])
            rms("n2")
            for c in range(NCH):
                V(lambda e, c=c: e.scalar_tensor_tensor(h2f[:, c, :], xf[:, c, :], g2[:, c:c + 1], rstd[:, :], ALU.mult, ALU.mult),
                  ["xf", "g2", "rstd", "bufY"], ["bufY"])
            A(lambda e: e.copy(hb[:, :, :], h2f), ["bufY", "hb"], ["hb"])
            sc.dma("sp", h2b_v[:, :, hs], hb[:, :, :], "s_h2b", reads=["hb"], writes=["h2b_d"])
            for k in range(4):
                for c in range(NCH):
                    T(lambda e, c=c, k=k: e.matmul(psR[:, 0:72], h2f[:, c, k * 128:(k + 1) * 128], wr_sb[:, c, :], start=(c == 0), stop=(c == NCH - 1)),
                      ["bufY", "wr"], ["psR"])
                r = rt; s_ = rs_
                V(lambda e: e.tensor_tensor(r["lg"][:, :], psR[:, 0:72], br_sb[:, :], ALU.add), ["psR", "br"], ["r_lg"])
                V(lambda e: e.reduce_max(s_["gmax"][:, :], r["lg"][:, 0:8], AX.X), ["r_lg"], ["s_gmax"])
                V(lambda e: e.tensor_scalar(s_["ngmax"][:, :], s_["gmax"][:, :], -1.0, None, ALU.mult), ["s_gmax"], ["s_ngmax"])
                A(lambda e: e.activation(out=r["ge"][:, 0:8], in_=r["lg"][:, 0:8], func=AF.Exp, bias=s_["ngmax"][:, 0:1]), ["r_lg", "s_ngmax"], ["r_ge"])
                V(lambda e: e.reduce_sum(s_["gs"][:, :], r["ge"][:, 0:8], AX.X), ["r_ge"], ["s_gs"])
                V(lambda e: e.reciprocal(s_["gw"][:, :], s_["gs"][:, :]), ["s_gs"], ["s_gw"])
                V(lambda e: e.tensor_scalar(r["oh"][:, 0:8], r["lg"][:, 0:8], s_["gmax"][:, 0:1], None, ALU.is_equal), ["r_lg", "s_gmax"], ["r_oh"])
                le = r["lg"][:, 8:72].rearrange("p (g j) -> p g j", j=8)
                V(lambda e, le=le: e.tensor_tensor(r["tmp"][:, 0:64].rearrange("p (g j) -> p g j", j=8), le,
                                                   r["oh"][:, 0:8].unsqueeze(2).to_broadcast([128, 8, 8]), ALU.mult), ["r_lg", "r_oh"], ["r_tmp"])
                V(lambda e: e.reduce_sum(r["ig"][:, 0:8], r["tmp"][:, 0:64].rearrange("p (g j) -> p j g", j=8), AX.X), ["r_tmp"], ["r_ig"])
                V(lambda e: e.reduce_max(s_["m1"][:, :], r["ig"][:, 0:8], AX.X), ["r_ig"], ["s_m1"])
                V(lambda e: e.tensor_scalar(r["m1k"][:, 0:8], r["ig"][:, 0:8], s_["m1"][:, 0:1], None, ALU.is_equal), ["r_ig", "s_m1"], ["r_m1k"])
                V(lambda e: e.scalar_tensor_tensor(r["in2"][:, 0:8], r["m1k"][:, 0:8], -1e30, r["ig"][:, 0:8], ALU.mult, ALU.add), ["r_m1k", "r_ig"], ["r_in2"])
                V(lambda e: e.reduce_max(s_["m2"][:, :], r["in2"][:, 0:8], AX.X), ["r_in2"], ["s_m2"])
                V(lambda e: e.tensor_scalar(r["m2k"][:, 0:8], r["in2"][:, 0:8], s_["m2"][:, 0:1], None, ALU.is_equal), ["r_in2", "s_m2"], ["r_m2k"])
                V(lambda e: e.tensor_tensor(s_["d12"][:, :], s_["m1"][:, :], s_["m2"][:, :], ALU.subtract), ["s_m1", "s_m2"], ["s_d12"])
                A(lambda e: e.activation(out=s_["w1"][:, :], in_=s_["d12"][:, :], func=AF.Sigmoid), ["s_d12"], ["s_w1"])
                A(lambda e: e.activation(out=s_["w2"][:, :], in_=s_["d12"][:, :], func=AF.Sigmoid, scale=-1.0), ["s_d12"], ["s_w2"])
                V(lambda e: e.tensor_scalar(r["wl"][:, 0:8], r["m1k"][:, 0:8], s_["w1"][:, 0:1], None, ALU.mult), ["r_m1k", "s_w1"], ["r_wl"])
                V(lambda e: e.scalar_tensor_tensor(r["wl"][:, 0:8], r["m2k"][:, 0:8], s_["w2"][:, 0:1], r["wl"][:, 0:8], ALU.mult, ALU.add), ["r_m2k", "s_w2", "r_wl"], ["r_wl"])
                V(lambda e: e.tensor_scalar(r["wl"][:, 0:8], r["wl"][:, 0:8], s_["gw"][:, 0:1], None, ALU.mult), ["r_wl", "s_gw"], ["r_wl"])
                V(lambda e, k=k: e.tensor_tensor(wt_st[:, k, :].rearrange("p (g j) -> p g j", j=8), r["oh"][:, 0:8].unsqueeze(2).to_broadcast([128, 8, 8]),
                                                 r["wl"][:, 0:8].unsqueeze(1).to_broadcast([128, 8, 8]), ALU.mult), ["r_oh", "r_wl", "wt_st"], ["wt_st"])
            sc.dma("sp", wt_d[hs, :].rearrange("(k p) e -> p k e", p=128), wt_st[:, :, :], "s_wt", reads=["wt_st"], writes=["wt_d"])
        sc.final_wait("sp", ["x2tm", "h2b_d", "wt_d", "bufY", "hb", "wt_st"])
        sc.run()


def emit_l2b(nc, a, x2tm, h2b_d, wt_d, out, n_exp=NEXP):
    HT = 512
    with contextlib.ExitStack() as st:
        sb = lambda name, shape, dt: st.enter_context(nc.sbuf_tensor(name, shape, dt))
        ps = lambda name, shape, dt: st.enter_context(nc.psum_tensor(name, shape, dt))
        sc = Sched(nc, st)
        V = lambda fn, r, w: sc.op("dve", fn, reads=r, writes=w)
        A = lambda fn, r, w: sc.op("act", fn, reads=r, writes=w)
        T = lambda fn, r, w: sc.op("pe", fn, reads=r, writes=w)
        acc = sb("m_acc", [128, 8, D], F32); h2b = sb("m_h2b", [128, NCH, NT], BF16); wt = sb("m_wt", [128, 8, 64], F32)
        wg = [sb("m_wg%d" % i, [128, NCH, 512], BF16) for i in range(2)]
        wu2 = [sb("m_wu%d" % i, [128, NCH, 512], BF16) for i in range(2)]; wd2 = [sb("m_wd%d" % i, [128, 4, D], BF16) for i in range(2)]
        act = sb("m_act", [128, 4, NT], BF16); slb = [sb("m_slb%d" % i, [128, HT], BF16) for i in range(2)]
        psG = [ps("m_psG%d" % i, [128, HT], F32) for i in range(2)]; psU = [ps("m_psU%d" % i, [128, HT], F32) for i in range(2)]
        psD = [ps("m_psD%d" % i, [128, 512], F32) for i in range(2)]
        sc.dma("sp", acc[:, :, :], x2tm.rearrange("(k p) d -> p k d", p=128), "l_acc", writes=["acc"])
        sc.dma("sp", h2b[:, :, :], h2b_d.rearrange("(c p) t -> p c t", p=128), "l_h2b", writes=["h2b"])
        sc.dma("sp", wt[:, :, :], wt_d.rearrange("(k p) e -> p k e", p=128), "l_wt", writes=["wt"])
        n = 0
        for e_ in range(n_exp):
            eb = e_ % 2
            sc.dma("pool", wg[eb][:, :, :], a["w_gate"][e_].rearrange("(c p) f -> p c f", p=128), ("wg", eb), writes=[("wg", eb)])
            wu, wd = wu2[eb], wd2[eb]
            sc.dma("pool", wu[:, :, :], a["w_up"][e_].rearrange("(c p) f -> p c f", p=128), ("wu", eb), writes=[("wu", eb)])
            sc.dma("pool", wd[:, :, :], a["w_down"][e_].rearrange("(c p) d -> p c d", p=128), ("wd", eb), writes=[("wd", eb)])
            for hf in range(NT // HT):
                hs = slice(hf * HT, (hf + 1) * HT)
                for fch in range(4):
                    pb = n % 2; n += 1
                    fs = slice(fch * 128, (fch + 1) * 128)
                    for c in range(NCH):
                        T(lambda e, c=c, eb=eb, fs=fs, hs=hs, pb=pb: e.matmul(psG[pb][:, :], wg[eb][:, c, fs], h2b[:, c, hs], start=(c == 0), stop=(c == NCH - 1)),
                          [("wg", eb), "h2b"], [("psG", pb)])
                    for c in range(NCH):
                        T(lambda e, wu=wu, c=c, fs=fs, hs=hs, pb=pb: e.matmul(psU[pb][:, :], wu[:, c, fs], h2b[:, c, hs], start=(c == 0), stop=(c == NCH - 1)),
                          [("wu", eb), "h2b"], [("psU", pb)])
                    A(lambda e, pb=pb: e.activation(out=slb[pb][:, :], in_=psG[pb][:, :], func=AF.Silu), [("psG", pb)], [("slb", pb)])
                    V(lambda e, pb=pb, fch=fch, hs=hs: e.tensor_tensor(act[:, fch, hs], slb[pb][:, :], psU[pb][:, :], ALU.mult), [("slb", pb), ("psU", pb)], ["act"])
            for k in range(8):
                for db in range(4):
                    pd = (k * 4 + db) % 2
                    ds_ = slice(db * 512, (db + 1) * 512)
                    for fch in range(4):
                        T(lambda e, wd=wd, k=k, fch=fch, ds_=ds_, pd=pd: e.matmul(psD[pd][:, :], act[:, fch, k * 128:(k + 1) * 128], wd[:, fch, ds_], start=(fch == 0), stop=(fch == 3)),
                          ["act", ("wd", eb)], [("psD", pd)])
                    V(lambda e, k=k, ds_=ds_, pd=pd, e_=e_: e.scalar_tensor_tensor(acc[:, k, ds_], psD[pd][:, :], wt[:, k, e_:e_ + 1], acc[:, k, ds_], ALU.mult, ALU.add),
                      [("psD", pd), "wt", "acc"], ["acc"])
        sc.dma("sp", out.rearrange("(k p) d -> p k d", p=128), acc[:, :, :], "s_out", reads=["acc"], writes=["out"])
        sc.final_wait("sp", ["out"])
        sc.run()


L2_IN = (("xT", [D, NT], F32), ("yAT", [2048, NT], BF16), ("yBT", [4096, NT], BF16), ("gain1", [128, NCH], F32), ("gain2", [128, NCH], F32),
         ("wgate", [D, 4096], F32), ("wA", [2048, D], F32), ("wB", [4096, D], F32), ("wo", [D, D], F32), ("wr", [D, 72], F32), ("br", [128, 72], F32),
         ("ident_f", [128, 128], F32), ("w_gate", [NEXP, D, 512], F32), ("w_up", [NEXP, D, 512], F32), ("w_down", [NEXP, 512, D], F32))


def build_launch2(n_exp=NEXP):
    nc = bass.Bass("TRN2", target_bir_lowering=False)
    a = {name: nc.dram_tensor(name, shape, dt, kind="ExternalInput").ap() for (name, shape, dt) in L2_IN}
    out = nc.dram_tensor("out", [NT, D], F32, kind="ExternalOutput").ap()
    x2tm = nc.dram_tensor("x2tm_scr", [NT, D], F32).ap()
    h2b_d = nc.dram_tensor("h2b_scr", [D, NT], BF16).ap()
    wt_d = nc.dram_tensor("wt_scr", [NT, 64], F32).ap()
    emit_l2a(nc, a, x2tm, h2b_d, wt_d)
    emit_l2b(nc, a, x2tm, h2b_d, wt_d, out, n_exp)
    return nc


def launch2_inputs(inp, c, yAT, yBT, shared):
    ts = slice(c * NT, (c + 1) * NT)
    d = dict(shared)
    d["xT"] = np.ascontiguousarray(inp["x"][0, ts].T)
    d["yAT"] = np.ascontiguousarray(yAT[:, ts]); d["yBT"] = np.ascontiguousarray(yBT[:, ts])
    return d


def launch2_shared(inp):
    f = np.float32
    return {
        "gain1": np.ascontiguousarray(inp["norm1_gain"][0].reshape(NCH, 128).T), "gain2": np.ascontiguousarray(inp["norm2_gain"][0].reshape(NCH, 128).T),
        "wgate": np.ascontiguousarray(inp["w_in"][0][:, OFF_GA:OFF_GA + 4096]), "wA": inp["w_branch_att"][0], "wB": inp["w_branch_gdn"][0], "wo": inp["w_out"][0],
        "wr": np.ascontiguousarray(np.concatenate([inp["w_group_router"][0], inp["w_expert_router"][0]], axis=1)),
        "br": np.ascontiguousarray(np.broadcast_to(np.concatenate([inp["b_group_router"][0], inp["b_expert_router"][0]])[None, :], (128, 72)).astype(f)),
        "ident_f": np.eye(128, dtype=f), "w_gate": inp["w_gate"][0], "w_up": inp["w_up"][0], "w_down": inp["w_down"][0],
    }


def kernel(**inputs):
    inp = {k: np.asarray(v) for k, v in inputs.items()}
    cores = list(range(NCORES))
    nc1 = build_launch1(S)
    res1 = run_bass_kernel_spmd(nc1, [launch1_inputs(inp, c) for c in cores], core_ids=cores).results
    yAT = np.concatenate([np.asarray(res1[c]["yatt"]).reshape(256, S) for c in cores], axis=0)
    yBT = np.concatenate([np.ascontiguousarray(np.asarray(res1[c]["yg"]).T) for c in cores], axis=0)
    del res1
    nc2 = build_launch2()
    shared = launch2_shared(inp)
    res2 = run_bass_kernel_spmd(nc2, [launch2_inputs(inp, c, yAT, yBT, shared) for c in cores], core_ids=cores).results
    out = np.concatenate([np.asarray(res2[c]["out"]) for c in cores], axis=0)
    return out.reshape(1, S, D).astype(np.float32)
```

```python
import contextlib
import numpy as np
import concourse.bass as bass
import concourse.mybir as mybir
from concourse.bass_utils import run_bass_kernel_spmd

F32 = mybir.dt.float32
BF16 = mybir.dt.bfloat16
AF = mybir.ActivationFunctionType
ALU = mybir.AluOpType
AX = mybir.AxisListType

D = 2048
S = 8192
NCORES = 8
NCH = D // 128
EPS = 1e-6

ENG_NAMES = ("pe", "dve", "act", "pool", "sp")


class Sched:
    def __init__(self, nc, stack):
        self.nc = nc
        self.stack = stack
        self.q = {k: [] for k in ENG_NAMES}
        self.sems = {}
        self.count = {}
        self.waited = {k: {} for k in ENG_NAMES}
        self.last_w = {}
        self.readers = {}

    def _sem(self, key):
        if key not in self.sems:
            name = "s%d" % len(self.sems)
            self.sems[key] = self.stack.enter_context(self.nc.semaphore(name))
            self.count[key] = 0
        return self.sems[key]

    def _deps(self, eng, reads, writes):
        deps = []
        for r in reads:
            if r in self.last_w:
                deps.append(self.last_w[r])
        for w in writes:
            if w in self.last_w:
                deps.append(self.last_w[w])
            deps.extend(self.readers.get(w, ()))
        waits = []
        for (key, val, src) in deps:
            if src == "pe" and eng == "pe":
                continue
            if self.waited[eng].get(key, 0) >= val:
                continue
            self.waited[eng][key] = val
            waits.append((key, val))
        return waits

    def _commit(self, tok, reads, writes):
        for w in writes:
            self.last_w[w] = tok
            self.readers[w] = []
        for r in reads:
            self.readers.setdefault(r, []).append(tok)

    def op(self, eng, fn, reads=(), writes=()):
        waits = self._deps(eng, reads, writes)
        key = ("e", eng)
        sem = self._sem(key)
        self.count[key] += 1
        tok = (key, self.count[key], eng)
        wl = [(self._sem(k), v) for (k, v) in waits]

        def emit(e, fn=fn, wl=wl, sem=sem):
            for (s, v) in wl:
                e.wait_ge(s, v)
            fn(e).then_inc(sem, 1)
        self.q[eng].append(emit)
        self._commit(tok, reads, writes)

    def dma(self, eng, out, in_, sem_key, reads=(), writes=(), **kw):
        waits = self._deps(eng, reads, writes)
        key = ("d", sem_key)
        sem = self._sem(key)
        self.count[key] += 16
        tok = (key, self.count[key], "dma")
        wl = [(self._sem(k), v) for (k, v) in waits]

        def emit(e, wl=wl, sem=sem):
            for (s, v) in wl:
                e.wait_ge(s, v)
            e.dma_start(out=out, in_=in_, **kw).then_inc(sem, 16)
        self.q[eng].append(emit)
        self._commit(tok, reads, writes)

    def final_wait(self, eng, resources):
        waits = self._deps(eng, resources, ())
        wl = [(self._sem(k), v) for (k, v) in waits]

        def emit(e, wl=wl):
            for (s, v) in wl:
                e.wait_ge(s, v)
        self.q[eng].append(emit)

    def run(self):
        nc = self.nc
        with nc.Block() as block:
            @block.tensor
            def _(e):
                for f in self.q["pe"]:
                    f(e)

            @block.vector
            def _(e):
                for f in self.q["dve"]:
                    f(e)

            @block.scalar
            def _(e):
                for f in self.q["act"]:
                    f(e)

            @block.gpsimd
            def _(e):
                for f in self.q["pool"]:
                    f(e)

            @block.sync
            def _(e):
                for f in self.q["sp"]:
                    f(e)


def emit_stage1(nc, xT, w, gain, pT, n_cols, n_tok, tok_blk=512):
    assert n_cols % 128 == 0 and n_tok % tok_blk == 0
    ncc = n_cols // 128
    ntt = n_tok // tok_blk
    xT_v = xT.rearrange("(c p) t -> p c t", p=128)
    w_v = w.rearrange("(c p) n -> p c n", p=128)
    with contextlib.ExitStack() as st:
        sb = lambda name, shape, dt: st.enter_context(nc.sbuf_tensor(name, shape, dt))
        ps = lambda name, shape, dt: st.enter_context(nc.psum_tensor(name, shape, dt))
        wb = sb("wb", [128, NCH, n_cols], BF16)
        g_sb = sb("g_sb", [128, NCH], F32)
        ones = sb("ones1", [128, 128], BF16)
        xf = [sb("xf0", [128, NCH, tok_blk], F32)] * 2
        xb = [sb("xb%d" % i, [128, NCH, tok_blk], BF16) for i in range(2)]
        xsq = [sb("xsq%d" % i, [128, NCH, tok_blk], BF16) for i in range(1)] * 2
        rstd = [sb("rstd%d" % i, [128, tok_blk], F32) for i in range(2)]
        ob = [sb("ob%d" % i, [128, tok_blk], BF16) for i in range(4)]
        ps_ss = ps("ps_ss", [128, tok_blk], F32)
        ps_o = [ps("ps_o%d" % i, [128, tok_blk], F32) for i in range(4)]
        sc = Sched(nc, st)

        sc.op("pool", lambda e: e.memset(ones[:, :], 1.0), writes=["ones"])
        sc.dma("sp", g_sb[:, :], gain[:, :], "g_sb", writes=["g_sb"])
        for c in range(NCH):
            sc.dma("pool", wb[:, c, :], w_v[:, c, :], "wb", writes=[("wb", c), "wb_all"])
        for c in range(NCH):
            sc.op("dve", lambda e, c=c: e.tensor_scalar(wb[:, c, :], wb[:, c, :], g_sb[:, c:c + 1], None, ALU.mult),
                  reads=[("wb", c), "wb_all", "g_sb"], writes=[("wb", c)])
        for t in range(ntt):
            b = t % 2
            tsl = slice(t * tok_blk, (t + 1) * tok_blk)
            sc.dma("sp", xf[b][:, :, :], xT_v[:, :, tsl], "xf", writes=["xf"])
            sc.op("act", lambda e, b=b: e.activation(out=xsq[b][:, :, :], in_=xf[b][:, :, :], func=AF.Square),
                  reads=["xf"], writes=["xsq"])
            sc.op("dve", lambda e, b=b: e.tensor_copy(xb[b][:, :, :], xf[b][:, :, :]),
                  reads=["xf"], writes=[("xb", b)])
            for c in range(NCH):
                sc.op("pe", lambda e, b=b, c=c: e.matmul(ps_ss[:, :], ones[:, :], xsq[b][:, c, :],
                                                         start=(c == 0), stop=(c == NCH - 1)),
                      reads=["xsq", "ones"], writes=["ps_ss"])
            sc.op("dve", lambda e, b=b: e.tensor_scalar(rstd[b][:, :], ps_ss[:, :], 1.0 / D, EPS, ALU.mult, ALU.add),
                  reads=["ps_ss"], writes=[("rstd", b)])
            sc.op("act", lambda e, b=b: e.activation(out=rstd[b][:, :], in_=rstd[b][:, :], func=AF.Sqrt),
                  reads=[("rstd", b)], writes=[("rstd", b)])
            sc.op("dve", lambda e, b=b: e.reciprocal(rstd[b][:, :], rstd[b][:, :]),
                  reads=[("rstd", b)], writes=[("rstd", b)])
            for j in range(ncc):
                k = (t * ncc + j) % 4
                for c in range(NCH):
                    sc.op("pe", lambda e, b=b, c=c, j=j, k=k: e.matmul(
                        ps_o[k][:, :], wb[:, c, j * 128:(j + 1) * 128], xb[b][:, c, :],
                        start=(c == 0), stop=(c == NCH - 1)),
                        reads=[("xb", b), ("wb", c)], writes=[("ps_o", k)])
                sc.op("dve", lambda e, b=b, k=k: e.tensor_tensor(ob[k][:, :], ps_o[k][:, :], rstd[b][:, :], ALU.mult),
                      reads=[("ps_o", k), ("rstd", b)], writes=[("ob", k)])
                sc.dma("sp", pT[j * 128:(j + 1) * 128, tsl], ob[k][:, :], ("ob", k),
                       reads=[("ob", k)], writes=["pT_out"])
        sc.final_wait("sp", [("ob", k) for k in range(4)] + ["pT_out"])
        for k in range(4):
            key = ("d", ("ob", k))
            if key in sc.sems:
                sc.q["sp"].append(lambda e, s=sc.sems[key], v=sc.count[key]: e.wait_ge(s, v))
        sc.run()


def build_stage1(n_cols, n_tok, tok_blk=512):
    nc = bass.Bass("TRN2", target_bir_lowering=False)
    xT = nc.dram_tensor("xT", [D, n_tok], F32, kind="ExternalInput").ap()
    w = nc.dram_tensor("w", [D, n_cols], F32, kind="ExternalInput").ap()
    gain = nc.dram_tensor("gain", [128, NCH], F32, kind="ExternalInput").ap()
    pT = nc.dram_tensor("pT", [n_cols, n_tok], BF16, kind="ExternalOutput").ap()
    emit_stage1(nc, xT, w, gain, pT, n_cols, n_tok, tok_blk)
    return nc


ATT_PATTERNS = ((128, 1), (512, 4), (2048, 16))
HALF = 64


def alibi_slopes_np():
    n = 48
    return np.exp2(-8.0 * np.arange(1, n + 1, dtype=np.float64) / n).reshape(3, 16)


def att_bias_consts(head_slot):
    out = np.zeros((3, 128, 2, 128), np.float32)
    i = np.arange(128)[:, None]
    j = np.arange(128)[None, :]
    sl = alibi_slopes_np()
    for g, (_, d) in enumerate(ATT_PATTERNS):
        for h, off in enumerate((-64, 64)):
            rel = i - j + off
            b = -sl[g, head_slot] * np.abs(rel) * d
            out[g, :, h, :] = np.where(np.abs(rel) <= HALF, b, -1e30)
    return out


def emit_att(nc, q_of, k_of, vT, qg, kg, bias, ident_d, yT, n_tok, tag):
    QT = 512
    PIECE = min(2048, n_tok)
    DMAX = 16
    with contextlib.ExitStack() as st:
        sb = lambda name, shape, dt: st.enter_context(nc.sbuf_tensor(name + tag, shape, dt))
        ps = lambda name, shape, dt: st.enter_context(nc.psum_tensor(name + tag, shape, dt))
        sc = Sched(nc, st)
        ones = sb("a_ones", [128, 128], BF16)
        ident = sb("a_ident", [128, 128], BF16)
        g_q = sb("a_gq", [128, 3], F32)
        g_k = sb("a_gk", [128, 3], F32)
        gmax = sb("a_gmax", [128, 8], F32)
        gabs = sb("a_gabs", [128, 8], BF16)
        gT = sb("a_gT", [8, 128], BF16)
        gred = sb("a_gred", [8, 2], BF16)
        negB = sb("a_negB", [128, 1], F32)
        bias_sb = sb("a_bias", [128, 3, 2, 128], F32)
        raw = sb("a_raw", [128, PIECE], BF16)
        sq = sb("a_sq", [128, PIECE], BF16)
        rs = sb("a_rs", [128, PIECE], F32)
        vT_sb = sb("a_vT", [128, n_tok], BF16)
        accO = sb("a_accO", [128, n_tok], F32)
        accL = sb("a_accL", [128, n_tok], F32)
        yb = sb("a_yb", [128, PIECE], BF16)
        qn = sb("a_qn", [128, n_tok], BF16)
        kn = sb("a_kn", [128, n_tok + 128 * DMAX], BF16)
        vg = sb("a_vg", [128, n_tok + 128 * DMAX], BF16)
        sS2 = [sb("a_sS%d" % i, [128, 4, 2, 128], F32) for i in range(2)]
        pP2 = [sb("a_pP%d" % i, [128, 4, 2, 128], BF16) for i in range(2)]
        ps_n = ps("a_psn", [128, 512], F32)
        ps_t = ps("a_pst", [128, 128], BF16)
        ps_s2 = [ps("a_pss%d" % i, [128, 4, 2, 128], F32) for i in range(2)]
        ps_O = ps("a_psO", [128, QT], F32)
        ps_L = ps("a_psL", [128, QT], F32)
        ps_g = ps_n[:, 0:128]

        sc.op("pool", lambda e: e.memset(ones[:, :], 1.0), writes=["ones"])
        sc.dma("sp", ident[:, :], ident_d[:, :], "c_ident", writes=["ident"])
        sc.dma("sp", g_q[:, :], qg[:, :], "c_gq", writes=["g_q"])
        sc.dma("sp", g_k[:, :], kg[:, :], "c_gk", writes=["g_k"])
        sc.dma("sp", bias_sb[:, :, :, :], bias.rearrange("g p h j -> p g h j"), "c_bias", writes=["bias"])
        sc.dma("sp", vT_sb[:, :], vT, "c_v", writes=["vT"])

        def bcast_absmax(gain_sb, col, gtag):
            sc.op("dve", lambda e: e.tensor_scalar(gmax[:, 0:3], gain_sb[:, :], -1.0, None, ALU.mult),
                  reads=[gtag, "gmax"], writes=["gmax"])
            sc.op("dve", lambda e: e.tensor_tensor(gmax[:, 0:3], gmax[:, 0:3], gain_sb[:, :], ALU.max),
                  reads=[gtag, "gmax"], writes=["gmax"])
            sc.op("dve", lambda e: e.reduce_max(gmax[:, 4:5], gmax[:, 0:3], AX.X), reads=["gmax"], writes=["gmax"])
            sc.op("dve", lambda e: e.tensor_copy(gabs[:, 0:1], gmax[:, 4:5]), reads=["gmax"], writes=["gabs"])
            sc.op("pe", lambda e: e.transpose(ps_t[0:1, :], gabs[:, 0:1], ident[:, :]), reads=["gabs", "ident"], writes=["ps_t"])
            sc.op("dve", lambda e: e.tensor_copy(gT[0:1, :], ps_t[0:1, :]), reads=["ps_t"], writes=["gT"])
            sc.op("dve", lambda e: e.reduce_max(gred[0:1, 0:1], gT[0:1, :], AX.X), reads=["gT"], writes=["gred"])
            sc.op("pe", lambda e: e.matmul(ps_g[:, col:col + 1], ones[0:1, :], gred[0:1, 0:1], start=True, stop=True),
                  reads=["gred", "ones"], writes=["ps_n"])
            sc.op("dve", lambda e: e.tensor_copy(gmax[:, 5 + col:6 + col], ps_g[:, col:col + 1]), reads=["ps_n", "gmax"], writes=["gmax"])
        sc.op("pool", lambda e: e.memset(gmax[:, :], 0.0), writes=["gmax"])
        bcast_absmax(g_q, 0, "g_q")
        bcast_absmax(g_k, 1, "g_k")
        sc.op("dve", lambda e: e.scalar_tensor_tensor(negB[:, :], gmax[:, 5:6], -1.02 * float(np.sqrt(128.0)),
                                                      gmax[:, 6:7], ALU.mult, ALU.mult),
              reads=["gmax"], writes=["negB"])

        def norm_into(src, gain_sb, g, dst, dst_is_k, extra_scale):
            d = ATT_PATTERNS[g][1]
            L = n_tok // d
            name = "kn" if dst_is_k else "qn"
            if dst_is_k:
                sc.op("pool", lambda e: e.memset(dst[:, :], 0.0), writes=[name])
            for T0 in range(0, n_tok, PIECE):
                sc.dma("sp", raw[:, :], src[:, T0:T0 + PIECE], "a_raw", writes=["raw"])
                sc.op("act", lambda e: e.activation(out=sq[:, :], in_=raw[:, :], func=AF.Square), reads=["raw"], writes=["sq"])
                for t0 in range(0, PIECE, 512):
                    sc.op("pe", lambda e, t0=t0: e.matmul(ps_n[:, :], ones[:, :], sq[:, t0:t0 + 512], start=True, stop=True),
                          reads=["sq", "ones"], writes=["ps_n"])
                    sc.op("dve", lambda e, t0=t0: e.tensor_scalar(rs[:, t0:t0 + 512], ps_n[:, :], 1.0 / 128, EPS, ALU.mult, ALU.add),
                          reads=["ps_n"], writes=["rs"])
                sc.op("act", lambda e: e.activation(out=rs[:, :], in_=rs[:, :], func=AF.Sqrt), reads=["rs"], writes=["rs"])
                sc.op("dve", lambda e: e.reciprocal(rs[:, :], rs[:, :]), reads=["rs"], writes=["rs"])
                if extra_scale != 1.0:
                    sc.op("dve", lambda e: e.tensor_scalar(rs[:, :], rs[:, :], extra_scale, None, ALU.mult), reads=["rs"], writes=["rs"])
                l0, l1 = T0 // d, (T0 + PIECE) // d
                if dst_is_k:
                    out_ap = dst[:, 0:d * (L + 128)].rearrange("p (r l) -> p r l", r=d)[:, :, HALF + l0:HALF + l1]
                else:
                    out_ap = dst[:, 0:n_tok].rearrange("p (r l) -> p r l", r=d)[:, :, l0:l1]
                in_raw = raw[:, :].rearrange("p (l r) -> p r l", r=d)
                in_rs = rs[:, :].rearrange("p (l r) -> p r l", r=d)
                sc.op("dve", lambda e, out_ap=out_ap, in_raw=in_raw, in_rs=in_rs: e.scalar_tensor_tensor(
                    out_ap, in_raw, gain_sb[:, g:g + 1], in_rs, ALU.mult, ALU.mult),
                    reads=["raw", "rs", "g_q", "g_k", name], writes=[name])

        first = True
        for g in range(3):
            d = ATT_PATTERNS[g][1]
            L = n_tok // d
            nb = L // 128 + 1
            Lp = L + 128
            norm_into(q_of(g), g_q, g, qn, False, float(128.0 ** -0.5))
            norm_into(k_of(g), g_k, g, kn, True, 1.0)
            sc.op("pool", lambda e: e.memset(vg[:, :], 0.0), writes=["vg"])
            v_res = vT_sb[:, :].rearrange("p (l r) -> p r l", r=d)
            for r in range(d):
                for b in range(nb):
                    lo, hi = max(128 * b - HALF, 0), min(128 * b + HALF, L)
                    p0 = lo - (128 * b - HALF)
                    n = hi - lo
                    col = (r * nb + b) * 128
                    sc.op("pe", lambda e, vr=v_res, r=r, lo=lo, hi=hi, n=n: e.transpose(ps_t[0:n, :], vr[:, r, lo:hi], ident[:, :]),
                          reads=["vT", "ident"], writes=["ps_t"])
                    sc.op("act", lambda e, p0=p0, n=n, col=col: e.copy(vg[p0:p0 + n, col:col + 128], ps_t[0:n, :]),
                          reads=["ps_t", "vg"], writes=["vg"])
            accO_v = accO[:, :].rearrange("p (l r) -> p r l", r=d)
            accL_v = accL[:, :].rearrange("p (l r) -> p r l", r=d)
            def front(r, q0, si):
                nt = min(QT, L - q0) // 128
                ps_s, sS, pP = ps_s2[si], sS2[si], pP2[si]
                for a in range(nt):
                    qa = q0 // 128 + a
                    for h in range(2):
                        kcol = r * Lp + 128 * (qa + h)
                        sc.op("pe", lambda e, ps_s=ps_s, a=a, h=h, kcol=kcol, r=r, qa=qa, L=L: e.matmul(
                            ps_s[:, a, h, :], kn[:, kcol:kcol + 128], qn[:, r * L + 128 * qa:r * L + 128 * qa + 128],
                            start=True, stop=True), reads=["kn", "qn"], writes=[("ps_s", si)])
                sc.op("dve", lambda e, ps_s=ps_s, sS=sS, g=g, nt=nt: e.tensor_tensor(
                    sS[:, 0:nt, :, :], ps_s[:, 0:nt, :, :],
                    bias_sb[:, g, :, :].unsqueeze(1).to_broadcast([128, nt, 2, 128]),
                    ALU.add), reads=[("ps_s", si), "bias"], writes=[("sS", si)])
                sc.op("act", lambda e, sS=sS, pP=pP, nt=nt: e.activation(out=pP[:, 0:nt, :, :], in_=sS[:, 0:nt, :, :], func=AF.Exp, bias=negB[:, 0:1]),
                      reads=[("sS", si), "negB"], writes=[("pP", si)])
                if q0 == 0:
                    sc.op("pool", lambda e, pP=pP: e.memset(pP[0:64, 0, 0, :], 0.0), reads=[("pP", si)], writes=[("pP", si)])
                if q0 + 128 * nt == L:
                    sc.op("pool", lambda e, pP=pP, nt=nt: e.memset(pP[64:128, nt - 1, 1, :], 0.0), reads=[("pP", si)], writes=[("pP", si)])

            def back(r, q0, si, first=first):
                nt = min(QT, L - q0) // 128
                pP = pP2[si]
                for a in range(nt):
                    qa = q0 // 128 + a
                    for h in range(2):
                        col = (r * nb + qa + h) * 128
                        sc.op("pe", lambda e, pP=pP, a=a, h=h, col=col: e.matmul(
                            ps_O[:, 128 * a:128 * a + 128], vg[:, col:col + 128], pP[:, a, h, :],
                            start=(h == 0), stop=(h == 1)), reads=["vg", ("pP", si)], writes=["ps_O"])
                        sc.op("pe", lambda e, pP=pP, a=a, h=h: e.matmul(
                            ps_L[:, 128 * a:128 * a + 128], ones[:, :], pP[:, a, h, :],
                            start=(h == 0), stop=(h == 1)), reads=["ones", ("pP", si)], writes=["ps_L"])
                n = 128 * nt
                if first:
                    sc.op("dve", lambda e, av=accO_v, r=r, q0=q0, n=n: e.tensor_copy(av[:, r, q0:q0 + n], ps_O[:, 0:n]),
                          reads=["ps_O"], writes=["accO"])
                    sc.op("act", lambda e, av=accL_v, r=r, q0=q0, n=n: e.copy(av[:, r, q0:q0 + n], ps_L[:, 0:n]),
                          reads=["ps_L"], writes=["accL"])
                else:
                    sc.op("dve", lambda e, av=accO_v, r=r, q0=q0, n=n: e.tensor_tensor(av[:, r, q0:q0 + n], av[:, r, q0:q0 + n], ps_O[:, 0:n], ALU.add),
                          reads=["ps_O", "accO"], writes=["accO"])
                    sc.op("dve", lambda e, av=accL_v, r=r, q0=q0, n=n: e.tensor_tensor(av[:, r, q0:q0 + n], av[:, r, q0:q0 + n], ps_L[:, 0:n], ALU.add),
                          reads=["ps_L", "accL"], writes=["accL"])

            sts = [(r, q0) for r in range(d) for q0 in range(0, L, QT)]
            for i, (r, q0) in enumerate(sts):
                front(r, q0, i % 2)
                if i > 0:
                    back(sts[i - 1][0], sts[i - 1][1], (i - 1) % 2)
            back(sts[-1][0], sts[-1][1], (len(sts) - 1) % 2)
            first = False
        sc.op("dve", lambda e: e.reciprocal(accL[:, :], accL[:, :]), reads=["accL"], writes=["accL"])
        for T0 in range(0, n_tok, PIECE):
            sc.op("dve", lambda e, T0=T0: e.tensor_tensor(yb[:, :], accO[:, T0:T0 + PIECE], accL[:, T0:T0 + PIECE], ALU.mult),
                  reads=["accO", "accL", "yb"], writes=["yb"])
            sc.dma("sp", yT[:, T0:T0 + PIECE], yb[:, :], "a_yb", reads=["yb"], writes=["y_out"])
        sc.final_wait("sp", ["y_out", "yb"])
        sc.run()


def build_att(n_tok):
    nc = bass.Bass("TRN2", target_bir_lowering=False)
    qT = nc.dram_tensor("qT", [3, 128, n_tok], BF16, kind="ExternalInput").ap()
    kT = nc.dram_tensor("kT", [3, 128, n_tok], BF16, kind="ExternalInput").ap()
    vT = nc.dram_tensor("vT", [128, n_tok], BF16, kind="ExternalInput").ap()
    qg = nc.dram_tensor("qg", [128, 3], F32, kind="ExternalInput").ap()
    kg = nc.dram_tensor("kg", [128, 3], F32, kind="ExternalInput").ap()
    bias = nc.dram_tensor("bias", [3, 128, 2, 128], F32, kind="ExternalInput").ap()
    ident_d = nc.dram_tensor("ident", [128, 128], BF16, kind="ExternalInput").ap()
    yT = nc.dram_tensor("yT", [128, n_tok], BF16, kind="ExternalOutput").ap()
    emit_att(nc, lambda g: qT[g], lambda g: kT[g], vT[:, :], qg, kg, bias, ident_d, yT, n_tok, "")
    return nc


def gdn_consts():
    i = np.arange(128)[:, None]; j = np.arange(128)[None, :]
    same = (i // 64) == (j // 64)
    c = {}
    c["ident_f"] = np.eye(128, dtype=np.float32)
    c["ones_f"] = np.ones((128, 128), np.float32)
    c["mstrict"] = np.stack([(same & (i > j)), (same & (i < j))]).astype(np.float32)
    c["mincl"] = np.stack([(same & (i >= j)), (same & (i <= j))]).astype(np.float32)
    r = i; m = j
    c["cum"] = np.stack([(same & (r <= m)), (same & (r >= m))]).astype(np.float32)
    lastf = np.where(np.arange(128) < 64, 63, 127)[None, :]; lastb = np.where(np.arange(128) < 64, 0, 64)[None, :]
    c["sel"] = np.stack([(r == lastf), (r == lastb)]).astype(np.float32)
    selh = np.zeros((2, 2, 128, 128), np.float32)
    for dr in range(2):
        for hf in range(2):
            selh[dr, hf, (63 + 64 * hf) if dr == 0 else 64 * hf, :] = 1.0
    c["selh"] = selh
    return c


GDN_IN = (("qraw", [2, 128, None], BF16), ("kraw", [2, 128, None], BF16), ("vraw", [4, 128, None], BF16), ("zraw", [4, 128, None], BF16),
          ("abr", [16, None], BF16))
GDN_PAR = (("cw", [128, 8, 5], F32), ("alog", [128, 8], F32), ("dtb", [128, 8], F32), ("ng", [128, 128], F32),
           ("ident_f", [128, 128], F32), ("ident_b", [128, 128], BF16), ("ones_f", [128, 128], F32),
           ("mstrict", [2, 128, 128], F32), ("mincl", [2, 128, 128], F32), ("cum", [2, 128, 128], F32), ("sel", [2, 128, 128], F32),
           ("selh", [2, 2, 128, 128], F32))


def build_gdn(n_tok):
    nc = bass.Bass("TRN2", target_bir_lowering=False)
    aps = {}
    for (name, shape, dt) in GDN_IN + GDN_PAR:
        aps[name] = nc.dram_tensor(name, [n_tok if v is None else v for v in shape], dt, kind="ExternalInput").ap()
    yg = nc.dram_tensor("yg", [n_tok, 512], BF16, kind="ExternalOutput").ap()
    ofw = nc.dram_tensor("ofw_scr", [4, n_tok, 128], F32).ap()
    emit_gdn(nc, aps, yg, ofw, n_tok)
    return nc


def emit_gdn(nc, aps, yg, ofw, n_tok):
    TB = 512; NP = 4
    nblk = n_tok // TB
    qraw, kraw, vraw, zraw, abr = (aps[k] for k in ("qraw", "kraw", "vraw", "zraw", "abr"))
    cw, alog, dtb, ngd = (aps[k] for k in ("cw", "alog", "dtb", "ng"))
    c_ident_f, c_ident_b, c_ones_f = aps["ident_f"], aps["ident_b"], aps["ones_f"]
    c_mstrict, c_mincl, c_cum, c_sel, c_selh = (aps[k] for k in ("mstrict", "mincl", "cum", "sel", "selh"))
    with contextlib.ExitStack() as st:
        sb = lambda name, shape, dt: st.enter_context(nc.sbuf_tensor(name, shape, dt))
        ps = lambda name, shape, dt: st.enter_context(nc.psum_tensor(name, shape, dt))
        sc = Sched(nc, st)
        V = lambda fn, r, w: sc.op("dve", fn, reads=r, writes=w)
        A = lambda fn, r, w: sc.op("act", fn, reads=r, writes=w)
        G = lambda fn, r, w: sc.op("pool", fn, reads=r, writes=w)
        T = lambda fn, r, w: sc.op("pe", fn, reads=r, writes=w)
        identf = sb("identf", [128, 128], F32); identb = sb("identb", [128, 128], BF16); onesf = sb("onesf", [128, 128], F32)
        mstr = sb("mstr", [128, 2, 128], F32); minc = sb("minc", [128, 2, 128], F32)
        cum = sb("cum_sb", [128, 2, 128], F32); sel = sb("sel_sb", [128, 2, 128], F32); selh = sb("selh_sb", [128, 4, 128], F32)
        cw_sb = sb("cw_sb", [128, 8, 5], F32); nega = sb("nega", [128, 8], F32); dtb_sb = sb("dtb_sb", [128, 8], F32)
        ng_sb = sb("ng_sb", [128, 128], F32)
        for (t_, d_, nm) in ((identf, c_ident_f, "identf"), (identb, c_ident_b, "identb"), (onesf, c_ones_f, "onesf"),
                             (nega, alog, "nega"), (dtb_sb, dtb, "dtb"), (ng_sb, ngd, "ng"), (cw_sb, cw, "cw")):
            sc.dma("sp", t_[:], d_[:] if len(d_.shape) == 2 else d_[:, :, :], "c_" + nm, writes=[nm])
        sc.dma("sp", mstr[:, :, :], c_mstrict.rearrange("r p j -> p r j"), "c_mstr", writes=["mstr"])
        sc.dma("sp", minc[:, :, :], c_mincl.rearrange("r p j -> p r j"), "c_minc", writes=["minc"])
        sc.dma("sp", cum[:, :, :], c_cum.rearrange("r p j -> p r j"), "c_cum", writes=["cum"])
        sc.dma("sp", sel[:, :, :], c_sel.rearrange("r p j -> p r j"), "c_sel", writes=["sel"])
        sc.dma("sp", selh[:, :, :], c_selh.rearrange("r h p j -> p (r h) j"), "c_selh", writes=["selh"])
        A(lambda e: e.activation(out=nega[:, :], in_=nega[:, :], func=AF.Exp), ["nega"], ["nega"])
        V(lambda e: e.tensor_scalar(nega[:, :], nega[:, :], -1.0, None, ALU.mult), ["nega"], ["nega"])

        raw2 = [sb("raw_g%d" % i, [128, TB + 4], BF16) for i in range(2)]; rawf2 = [sb("rawf%d" % i, [128, TB + 4], F32) for i in range(2)]
        raw = raw2[0]
        acc8 = [sb("acc_g%d" % i, [128, TB], F32) for i in range(8)]; sq4 = [sb("sq_g%d" % i, [128, TB], F32) for i in range(4)]
        fm = [sb("fm%d" % i, [128, TB], BF16) for i in range(8)]
        tm = [sb("tm%d" % i, [128, NP, 128], BF16) for i in range(8)]
        zt = [sb("zt%d" % i, [128, NP, 128], F32) for i in range(4)]
        KK = [sb("KK%d" % i, [128, NP, 128], F32) for i in range(2)]; QK = [sb("QK%d" % i, [128, NP, 128], F32) for i in range(2)]
        ab_fm = sb("ab_fm", [16, TB], BF16); ab_tm = sb("ab_tm", [128, NP, 16], F32)
        gt = {n: sb("gt_" + n, [128, NP, 4], F32) for n in ("x", "nx", "ax", "e", "l", "g", "beta", "nbeta", "G", "eG", "gl", "tail", "bG", "d0", "d1")}
        diagG = sb("diagG", [128, NP, 128], F32); dec = sb("dec", [128, NP, 128], F32); t1 = sb("t1", [128, NP, 128], F32)
        Ab = [sb("Ab%d" % i, [128, NP, 128], F32) for i in range(2)]; Bb = [sb("Bb%d" % i, [128, NP, 128], F32) for i in range(2)]
        intra = sb("intra", [128, NP, 128], BF16); X = sb("X_g", [128, NP, 256], F32); qd = sb("qd", [128, NP, 128], BF16)
        scan = [[{n: sb("sc%d_%d_%s" % (bb, vh, n), [128, NP, 128], BF16) for n in ("u", "wT", "qdT", "inT", "kt")} for vh in range(4)] for bb in range(2)]
        dS = [[sb("dS%d_%d" % (bb, hf), [128, NP, 4], F32) for hf in range(2)] for bb in range(2)]
        oblk = [sb("oblk%d" % vh, [128, NP, 128], F32) for vh in range(4)]
        ofl = sb("ofl", [128, NP, 128], F32); junk = sb("junk", [128, 128], F32); ss = sb("ss_g", [128, NP], F32); yb = sb("yb", [128, NP, 128], BF16)
        Sst = [sb("S%d" % vh, [128, 128], F32) for vh in range(4)]; Sbf = [sb("Sbf%d" % vh, [128, 128], BF16) for vh in range(4)]
        vnew = [sb("vnew%d" % vh, [128, 128], BF16) for vh in range(4)]
        psM = ps("psM", [128, NP, 128], F32); psX = ps("psX", [128, NP, 256], F32); psA = ps("psA", [128, NP, 128], F32); psB = ps("psB", [128, NP, 128], F32)
        psT = ps("psT", [128, NP, 128], BF16)
        psg = psB[:, 0, :]
        pss = [ps("pss%d" % i, [128, 3, 128], F32) for i in range(2)]
        bc = lambda ap: ap.unsqueeze(2).to_broadcast([128, NP, 128])
        bcp = lambda ap: ap.unsqueeze(1).to_broadcast([128, NP, 128])

        def prep_block(dr, blk, bb):
            t0 = blk * TB
            lo, hi = max(t0 - 2, 0), min(t0 + TB + 2, n_tok)
            srcs = [(qraw, 0), (qraw, 1), (kraw, 0), (kraw, 1), (vraw, 0), (vraw, 1), (vraw, 2), (vraw, 3)]
            for ti, (src, idx) in enumerate(srcs):
                rb = ti % 2
                G(lambda e, rb=rb: e.memset(raw2[rb][:, :], 0.0), [], [("raw", rb)])
                sc.dma("sp", raw2[rb][:, lo - (t0 - 2):hi - (t0 - 2)], src[idx, :, lo:hi], ("raw_g", rb), writes=[("raw", rb)])
                V(lambda e, rb=rb: e.tensor_copy(rawf2[rb][:, :], raw2[rb][:, :]), [("raw", rb)], [("rawf", rb)])
                V(lambda e, ti=ti, rb=rb: e.tensor_scalar(acc8[ti][:, :], rawf2[rb][:, 0:TB], cw_sb[:, ti, 0:1], None, ALU.mult), [("rawf", rb), "cw"], [("acc", ti)])
                for j in range(1, 5):
                    V(lambda e, ti=ti, j=j, rb=rb: e.scalar_tensor_tensor(acc8[ti][:, :], rawf2[rb][:, j:j + TB], cw_sb[:, ti, j:j + 1], acc8[ti][:, :], ALU.mult, ALU.add),
                      [("rawf", rb), "cw", ("acc", ti)], [("acc", ti)])
            for ti in range(8):
                if ti >= 4:
                    A(lambda e, ti=ti: e.activation(out=fm[ti][:, :], in_=acc8[ti][:, :], func=AF.Silu), [("acc", ti)], [("fm", ti)])
                else:
                    A(lambda e, ti=ti: e.activation(out=acc8[ti][:, :], in_=acc8[ti][:, :], func=AF.Silu), [("acc", ti)], [("acc", ti)])
            nps = [psX[:, 0:2, :], psX[:, 2:4, :], psA[:, :, :], psB[:, :, :]]
            npk = [[("psX", 0)], [("psX", 1)], [("psA", 0), ("psA", 1)], [("psB", 0), ("psB", 1)]]
            for ti in range(4):
                V(lambda e, ti=ti: e.tensor_tensor(sq4[ti][:, :], acc8[ti][:, :], acc8[ti][:, :], ALU.mult), [("acc", ti)], [("sq", ti)])
            for ti in range(4):
                T(lambda e, ti=ti: e.matmul(nps[ti], onesf[:, :], sq4[ti][:, :], start=True, stop=True), [("sq", ti), "onesf"], npk[ti])
            for ti in range(4):
                V(lambda e, ti=ti: e.tensor_scalar(sq4[ti][:, :], nps[ti], EPS, None, ALU.add), npk[ti] + [("sq", ti)], [("sq", ti)])
            for ti in range(4):
                A(lambda e, ti=ti: e.activation(out=sq4[ti][:, :], in_=sq4[ti][:, :], func=AF.Sqrt), [("sq", ti)], [("sq", ti)])
            for ti in range(4):
                V(lambda e, ti=ti: e.reciprocal(sq4[ti][:, :], sq4[ti][:, :]), [("sq", ti)], [("sq", ti)])
            for ti in range(4):
                scl = float(128.0 ** -0.5) if ti < 2 else 1.0
                V(lambda e, ti=ti, scl=scl: e.scalar_tensor_tensor(fm[ti][:, :], acc8[ti][:, :], scl, sq4[ti][:, :], ALU.mult, ALU.mult), [("acc", ti), ("sq", ti)], [("fm", ti)])
            for ti in range(8):
                for p in range(NP):
                    T(lambda e, ti=ti, p=p: e.transpose(psT[:, p, :], fm[ti][:, 128 * p:128 * p + 128], identb[:, :]), [("fm", ti), "identb"], ["psT"])
                A(lambda e, ti=ti: e.copy(tm[ti][:, :, :], psT[:, :, :]), ["psT"], [("tm", ti)])
            sc.dma("sp", ab_fm[:, :], abr[:, t0:t0 + TB], "ab_fm", writes=["ab_fm"])
            for p in range(NP):
                T(lambda e, p=p: e.transpose(psT[:, p, 0:16], ab_fm[:, 128 * p:128 * p + 128], identb[0:16, 0:16]), ["ab_fm", "identb"], ["psT"])
            V(lambda e: e.tensor_copy(ab_tm[:, :, :], psT[:, :, 0:16]), ["psT"], ["ab_tm"])
            g_ = gt
            row = lambda t_, dr=dr: t_[:, dr * 4:dr * 4 + 4].unsqueeze(1).to_broadcast([128, NP, 4])
            V(lambda e: e.tensor_tensor(g_["x"][:], ab_tm[:, :, dr * 4:dr * 4 + 4], row(dtb_sb), ALU.add), ["ab_tm", "dtb"], ["g_x"])
            V(lambda e: e.tensor_scalar(g_["nx"][:], g_["x"][:], -1.0, None, ALU.mult), ["g_x"], ["g_nx"])
            V(lambda e: e.tensor_tensor(g_["ax"][:], g_["x"][:], g_["nx"][:], ALU.max), ["g_x", "g_nx"], ["g_ax"])
            A(lambda e: e.activation(out=g_["e"][:], in_=g_["ax"][:], func=AF.Exp, scale=-1.0), ["g_ax"], ["g_e"])
            V(lambda e: e.tensor_scalar(g_["e"][:], g_["e"][:], 1.0, None, ALU.add), ["g_e"], ["g_e"])
            A(lambda e: e.activation(out=g_["l"][:], in_=g_["e"][:], func=AF.Ln), ["g_e"], ["g_l"])
            V(lambda e: e.scalar_tensor_tensor(g_["g"][:], g_["x"][:], 0.0, g_["l"][:], ALU.max, ALU.add), ["g_x", "g_l"], ["g_g"])
            V(lambda e: e.tensor_tensor(g_["g"][:], g_["g"][:], row(nega), ALU.mult), ["g_g", "nega"], ["g_g"])
            A(lambda e: e.activation(out=g_["beta"][:], in_=ab_tm[:, :, 8 + dr * 4:12 + dr * 4], func=AF.Sigmoid), ["ab_tm"], ["g_beta"])
            V(lambda e: e.tensor_scalar(g_["nbeta"][:], g_["beta"][:], -1.0, None, ALU.mult), ["g_beta"], ["g_nbeta"])
            T(lambda e: e.matmul(psg[:, 0:16], cum[:, dr, :], g_["g"][:].rearrange("p a b -> p (a b)"), start=True, stop=True), ["g_g", "cum"], [("psB", 0)])
            V(lambda e: e.tensor_copy(g_["G"][:].rearrange("p a b -> p (a b)"), psg[:, 0:16]), [("psB", 0)], ["g_G"])
            A(lambda e: e.activation(out=g_["eG"][:], in_=g_["G"][:], func=AF.Exp), ["g_G"], ["g_eG"])
            T(lambda e: e.matmul(psg[:, 16:32], sel[:, dr, :], g_["G"][:].rearrange("p a b -> p (a b)"), start=True, stop=True), ["g_G", "sel"], [("psB", 0)])
            V(lambda e: e.tensor_tensor(g_["gl"][:].rearrange("p a b -> p (a b)"), psg[:, 16:32], g_["G"][:].rearrange("p a b -> p (a b)"), ALU.subtract), [("psB", 0), "g_G"], ["g_gl"])
            A(lambda e: e.activation(out=g_["tail"][:], in_=g_["gl"][:], func=AF.Exp), ["g_gl"], ["g_tail"])
            V(lambda e: e.tensor_tensor(g_["bG"][:], g_["beta"][:], g_["eG"][:], ALU.mult), ["g_beta", "g_eG"], ["g_bG"])
            for hf in range(2):
                T(lambda e, hf=hf: e.matmul(psg[:, 32 + 16 * hf:48 + 16 * hf], selh[:, dr * 2 + hf, :], g_["G"][:].rearrange("p a b -> p (a b)"), start=True, stop=True),
                  ["g_G", "selh"], [("psB", 0)])
                A(lambda e, hf=hf: e.activation(out=dS[bb][hf][:].rearrange("p a b -> p (a b)"), in_=psg[:, 32 + 16 * hf:48 + 16 * hf], func=AF.Exp), [("psB", 0)], [("dS", bb)])
            for qh in range(2):
                for p in range(NP):
                    cs = slice(128 * p, 128 * p + 128)
                    T(lambda e, qh=qh, p=p, cs=cs: e.matmul(psA[:, p, :], fm[2 + qh][:, cs], fm[2 + qh][:, cs], start=True, stop=True), [("fm", 2 + qh)], [("psA", 0), ("psA", 1)])
                    T(lambda e, qh=qh, p=p, cs=cs: e.matmul(psB[:, p, :], fm[qh][:, cs], fm[2 + qh][:, cs], start=True, stop=True), [("fm", qh), ("fm", 2 + qh)], [("psB", 0), ("psB", 1)])
                V(lambda e, qh=qh: e.tensor_copy(KK[qh][:], psA[:]), [("psA", 0), ("psA", 1)], [("KK", qh)])
                A(lambda e, qh=qh: e.copy(QK[qh][:], psB[:]), [("psB", 0), ("psB", 1)], [("QK", qh)])
            for vh in range(4):
                qh = vh // 2
                sb_ = scan[bb][vh]
                Gc = g_["G"][:, :, vh]
                V(lambda e, Gc=Gc: e.tensor_tensor(diagG[:], bcp(identf[:, :]), bc(Gc), ALU.mult), ["identf", "g_G"], ["diagG"])
                for p in range(NP):
                    T(lambda e, p=p: e.matmul(psM[:, p, :], onesf[:, :], diagG[:, p, :], start=True, stop=True), ["diagG", "onesf"], ["psM"])
                V(lambda e, Gc=Gc: e.tensor_tensor(dec[:], bc(Gc), psM[:], ALU.subtract), ["psM", "g_G"], ["dec"])
                V(lambda e: e.tensor_scalar_min(dec[:], dec[:], 0.0), ["dec"], ["dec"])
                A(lambda e: e.activation(out=dec[:], in_=dec[:], func=AF.Exp), ["dec"], ["dec"])
                V(lambda e, qh=qh: e.tensor_tensor(t1[:], dec[:], KK[qh][:], ALU.mult), ["dec", ("KK", qh)], ["t1"])
                V(lambda e, vh=vh: e.tensor_tensor(t1[:], t1[:], bc(g_["nbeta"][:, :, vh]), ALU.mult), ["t1", "g_nbeta"], ["t1"])
                V(lambda e: e.tensor_tensor(Ab[0][:], t1[:], bcp(mstr[:, dr, :]), ALU.mult), ["t1", "mstr"], [("Ab", 0, 0), ("Ab", 0, 1)])
                G(lambda e, qh=qh: e.tensor_tensor(t1[:], dec[:], QK[qh][:], ALU.mult), ["dec", ("QK", qh), "t1"], ["t1"])
                G(lambda e: e.tensor_tensor(intra[:], t1[:], bcp(minc[:, dr, :]), ALU.mult), ["t1", "minc"], ["intra"])
                for p in range(NP):
                    T(lambda e, p=p: e.transpose(psM[:, p, :], Ab[0][:, p, :], identf[:, :]), [("Ab", 0, 0), ("Ab", 0, 1), "identf"], ["psM"])
                    T(lambda e, p=p: e.transpose(psT[:, p, :], intra[:, p, :], identb[:, :]), ["intra", "identb"], ["psT"])
                V(lambda e: e.tensor_copy(Bb[0][:], psM[:]), ["psM"], [("Bb", 0, 0), ("Bb", 0, 1)])
                A(lambda e, sb_=sb_: e.copy(sb_["inT"][:], psT[:]), ["psT"], [("scan", bb, vh)])
                V(lambda e, vh=vh: e.tensor_tensor(X[:, :, 0:128], tm[4 + vh][:], bc(g_["beta"][:, :, vh]), ALU.mult), [("tm", 4 + vh), "g_beta"], [("X", 0), ("X", 1)])
                V(lambda e, vh=vh, qh=qh: e.tensor_tensor(X[:, :, 128:256], tm[2 + qh][:], bc(g_["bG"][:, :, vh]), ALU.mult), [("tm", 2 + qh), "g_bG", ("X", 0), ("X", 1)], [("X", 0), ("X", 1)])
                for k in range(6):
                    a_, b_ = Ab[k % 2], Bb[k % 2]
                    an, bn = Ab[(k + 1) % 2], Bb[(k + 1) % 2]
                    for hp in range(2):
                        for p in (2 * hp, 2 * hp + 1):
                            T(lambda e, p=p, b_=b_: e.matmul(psX[:, p, :], b_[:, p, :], X[:, p, :], start=True, stop=True), [("Bb", k % 2, hp), ("X", hp)], [("psX", hp)])
                        if k < 5:
                            for p in (2 * hp, 2 * hp + 1):
                                T(lambda e, p=p, a_=a_, b_=b_: e.matmul(psA[:, p, :], b_[:, p, :], a_[:, p, :], start=True, stop=True), [("Ab", k % 2, hp), ("Bb", k % 2, hp)], [("psA", hp)])
                                T(lambda e, p=p, a_=a_, b_=b_: e.matmul(psB[:, p, :], a_[:, p, :], b_[:, p, :], start=True, stop=True), [("Ab", k % 2, hp), ("Bb", k % 2, hp)], [("psB", hp)])
                    for hp in range(2):
                        prs = slice(2 * hp, 2 * hp + 2)
                        V(lambda e, prs=prs: e.tensor_tensor(X[:, prs, :], X[:, prs, :], psX[:, prs, :], ALU.add), [("psX", hp), ("X", hp)], [("X", hp)])
                        if k < 5:
                            A(lambda e, an=an, prs=prs: e.copy(an[:, prs, :], psA[:, prs, :]), [("psA", hp)], [("Ab", (k + 1) % 2, hp)])
                            (A if hp == 0 else V)(lambda e, bn=bn, prs=prs, hp=hp: (e.copy if hp == 0 else e.tensor_copy)(bn[:, prs, :], psB[:, prs, :]), [("psB", hp)], [("Bb", (k + 1) % 2, hp)])
                A(lambda e, sb_=sb_: e.copy(sb_["u"][:], X[:, :, 0:128]), [("X", 0), ("X", 1)], [("scan", bb, vh)])
                for p in range(NP):
                    T(lambda e, p=p: e.transpose(psM[:, p, :], X[:, p, 128:256], identf[:, :]), [("X", p // 2), "identf"], ["psM"])
                V(lambda e, sb_=sb_: e.tensor_copy(sb_["wT"][:], psM[:]), ["psM", ("scan", bb, vh)], [("scan", bb, vh)])
                V(lambda e, vh=vh, qh=qh: e.tensor_tensor(qd[:], tm[qh][:], bc(g_["eG"][:, :, vh]), ALU.mult), [("tm", qh), "g_eG"], ["qd"])
                for p in range(NP):
                    T(lambda e, p=p: e.transpose(psT[:, p, :], qd[:, p, :], identb[:, :]), ["qd", "identb"], ["psT"])
                A(lambda e, sb_=sb_: e.copy(sb_["qdT"][:], psT[:]), ["psT", ("scan", bb, vh)], [("scan", bb, vh)])
                G(lambda e, sb_=sb_, vh=vh, qh=qh: e.tensor_tensor(sb_["kt"][:], tm[2 + qh][:], bc(g_["tail"][:, :, vh]), ALU.mult),
                  [("tm", 2 + qh), "g_tail", ("scan", bb, vh)], [("scan", bb, vh)])

        def scan_block(dr, blk, bb):
            order = [(p, hf) for p in range(NP) for hf in range(2)]
            if dr == 1:
                order = order[::-1]
            for si, (p, hf) in enumerate(order):
                rows = slice(64 * hf, 64 * hf + 64)
                for vh in range(4):
                    sb_ = scan[bb][vh]
                    pp = pss[vh % 2]
                    T(lambda e, sb_=sb_, p=p, vh=vh, pp=pp: e.matmul(pp[:, 0, :], sb_["wT"][:, p, :], Sbf[vh][:, :], start=True, stop=True),
                      [("scan", bb, vh), ("Sbf", vh)], [("pss", vh % 2)])
                    V(lambda e, sb_=sb_, p=p, vh=vh, pp=pp, rows=rows: e.tensor_tensor(vnew[vh][rows, :], sb_["u"][rows, p, :], pp[rows, 0, :], ALU.subtract),
                      [("pss", vh % 2), ("scan", bb, vh)], [("vnew", vh)])
                    T(lambda e, sb_=sb_, p=p, vh=vh, pp=pp: e.matmul(pp[:, 1, :], sb_["qdT"][:, p, :], Sbf[vh][:, :], start=True, stop=False),
                      [("scan", bb, vh), ("Sbf", vh)], [("pss", vh % 2)])
                    T(lambda e, sb_=sb_, p=p, vh=vh, pp=pp, rows=rows: e.matmul(pp[:, 1, :], sb_["inT"][rows, p, :], vnew[vh][rows, :], start=False, stop=True),
                      [("scan", bb, vh), ("vnew", vh)], [("pss", vh % 2)])
                    A(lambda e, p=p, vh=vh, pp=pp, rows=rows: e.copy(oblk[vh][rows, p, :], pp[rows, 1, :]), [("pss", vh % 2)], [("oblk", vh)])
                    T(lambda e, sb_=sb_, p=p, vh=vh, pp=pp, rows=rows: e.matmul(pp[:, 2, :], sb_["kt"][rows, p, :], vnew[vh][rows, :], start=True, stop=True),
                      [("scan", bb, vh), ("vnew", vh)], [("pss", vh % 2)])
                    V(lambda e, p=p, vh=vh, pp=pp, hf=hf: e.scalar_tensor_tensor(Sst[vh][:, :], Sst[vh][:, :], dS[bb][hf][:, p, vh:vh + 1], pp[:, 2, :], ALU.mult, ALU.add),
                      [("pss", vh % 2), ("dS", bb), ("S", vh)], [("S", vh)])
                    A(lambda e, vh=vh: e.copy(Sbf[vh][:, :], Sst[vh][:, :]), [("S", vh)], [("Sbf", vh)])

        def finish_block(dr, blk):
            t0 = blk * TB
            for vh in range(4):
                dst = ofw[vh, t0:t0 + TB, :].rearrange("(p i) d -> i p d", i=128)
                if dr == 0:
                    sc.dma("sp", dst, oblk[vh][:, :, :], ("oblk", vh), reads=[("oblk", vh)], writes=[("ofw", vh, blk)])
                    continue
                sc.dma("sp", ofl[:, :, :], dst, "ofl", reads=[("ofw", vh, blk)], writes=["ofl"])
                V(lambda e, vh=vh: e.tensor_tensor(ofl[:], ofl[:], oblk[vh][:], ALU.add), ["ofl", ("oblk", vh)], ["ofl"])
                for p in range(NP):
                    A(lambda e, p=p: e.activation(out=junk[:, :], in_=ofl[:, p, :], func=AF.Square, accum_out=ss[:, p:p + 1]), ["ofl", "junk", "ss"], ["junk", "ss"])
                V(lambda e: e.tensor_scalar(ss[:, :], ss[:, :], 1.0 / 128, EPS, ALU.mult, ALU.add), ["ss"], ["ss"])
                A(lambda e: e.activation(out=ss[:, :], in_=ss[:, :], func=AF.Sqrt), ["ss"], ["ss"])
                V(lambda e: e.reciprocal(ss[:, :], ss[:, :]), ["ss"], ["ss"])
                V(lambda e: e.tensor_tensor(ofl[:], ofl[:], bc(ss[:, :]), ALU.mult), ["ofl", "ss"], ["ofl"])
                V(lambda e: e.tensor_tensor(ofl[:], ofl[:], bcp(ng_sb[:, :]), ALU.mult), ["ofl", "ng"], ["ofl"])
                sc.dma("sp", raw[:, 0:TB], zraw[vh, :, t0:t0 + TB], ("raw_g", 0), writes=[("raw", 0)])
                for p in range(NP):
                    T(lambda e, p=p: e.transpose(psT[:, p, :], raw[:, 128 * p:128 * p + 128], identb[:, :]), [("raw", 0), "identb"], ["psT"])
                A(lambda e, vh=vh: e.activation(out=zt[vh][:], in_=psT[:], func=AF.Silu), ["psT"], [("zt", vh)])
                V(lambda e, vh=vh: e.tensor_tensor(yb[:], ofl[:], zt[vh][:], ALU.mult), ["ofl", ("zt", vh)], ["yb"])
                sc.dma("sp", yg[t0:t0 + TB, 128 * vh:128 * vh + 128].rearrange("(p i) d -> i p d", i=128), yb[:, :, :], "yb", reads=["yb"], writes=["yg_out"])

        for dr in range(2):
            for vh in range(4):
                G(lambda e, vh=vh: e.memset(Sst[vh][:, :], 0.0), [], [("S", vh)])
                G(lambda e, vh=vh: e.memset(Sbf[vh][:, :], 0.0), [], [("Sbf", vh)])
            blocks = list(range(nblk)) if dr == 0 else list(range(nblk))[::-1]
            for bi, blk in enumerate(blocks):
                bb = bi % 2
                prep_block(dr, blk, bb)
                scan_block(dr, blk, bb)
                finish_block(dr, blk)
        sc.final_wait("sp", ["yg_out", "yb"])
        sc.run()


L1_CHUNKS = 27
OFF_QA, OFF_KA, OFF_VA = 0, 6144, 12288
OFF_QG, OFF_KG, OFF_VG, OFF_ZG, OFF_A, OFF_B, OFF_GA, OFF_GB = 14336, 16384, 18432, 22528, 26624, 26688, 26752, 28800


def w1_cols(c):
    cols = []
    r = np.arange(128)
    for s_ in range(2):
        hs = 2 * c + s_
        for g in range(3):
            cols.append(OFF_QA + g * 2048 + hs * 128 + r)
        for g in range(3):
            cols.append(OFF_KA + g * 2048 + hs * 128 + r)
        cols.append(OFF_VA + hs * 128 + r)
    for qh in range(2):
        cols.append(OFF_QG + (2 * c + qh) * 128 + r)
    for qh in range(2):
        cols.append(OFF_KG + (2 * c + qh) * 128 + r)
    for vh in range(4):
        cols.append(OFF_VG + (4 * c + vh) * 128 + r)
    for vh in range(4):
        cols.append(OFF_ZG + (4 * c + vh) * 128 + r)
    ab = np.full(128, -1)
    for dr in range(2):
        for vh in range(4):
            ab[dr * 4 + vh] = OFF_A + dr * 32 + 4 * c + vh
            ab[8 + dr * 4 + vh] = OFF_B + dr * 32 + 4 * c + vh
    cols.append(ab)
    return np.concatenate(cols)


def build_launch1(n_tok):
    nc = bass.Bass("TRN2", target_bir_lowering=False)
    di = lambda name, shape, dt: nc.dram_tensor(name, shape, dt, kind="ExternalInput").ap()
    n_cols = L1_CHUNKS * 128
    xT = di("xT", [D, n_tok], F32); w1 = di("w1", [D, n_cols], F32); gain1 = di("gain1", [128, NCH], F32)
    qg = di("qg", [128, 3], F32); kg = di("kg", [128, 3], F32); bias = di("att_bias", [2, 3, 128, 2, 128], F32)
    aps = {}
    for (name, shape, dt) in GDN_PAR:
        aps[name] = di(name, shape, dt)
    yatt = nc.dram_tensor("yatt", [2, 128, n_tok], BF16, kind="ExternalOutput").ap()
    yg = nc.dram_tensor("yg", [n_tok, 512], BF16, kind="ExternalOutput").ap()
    pT = nc.dram_tensor("pT_scr", [n_cols, n_tok], BF16).ap()
    ofw = nc.dram_tensor("ofw_scr", [4, n_tok, 128], F32).ap()
    emit_stage1(nc, xT, w1, gain1, pT, n_cols, n_tok)
    for s_ in range(2):
        base = 7 * s_ * 128
        emit_att(nc, lambda g, base=base: pT[base + 128 * g:base + 128 * g + 128, :],
                 lambda g, base=base: pT[base + 128 * (3 + g):base + 128 * (4 + g), :],
                 pT[base + 768:base + 896, :], qg, kg, bias[s_], aps["ident_b"], yatt[s_], n_tok, "_s%d" % s_)
    g0 = 14 * 128
    aps["qraw"] = pT[g0:g0 + 256, :].rearrange("(i p) t -> i p t", p=128)
    aps["kraw"] = pT[g0 + 256:g0 + 512, :].rearrange("(i p) t -> i p t", p=128)
    aps["vraw"] = pT[g0 + 512:g0 + 1024, :].rearrange("(i p) t -> i p t", p=128)
    aps["zraw"] = pT[g0 + 1024:g0 + 1536, :].rearrange("(i p) t -> i p t", p=128)
    aps["abr"] = pT[g0 + 1536:g0 + 1552, :]
    emit_gdn(nc, aps, yg, ofw, n_tok)
    return nc


def launch1_inputs(inp, c, n_tok=S):
    import ml_dtypes
    f = np.float32
    cols = w1_cols(c)
    w_in = inp["w_in"][0]
    w1 = np.zeros((D, cols.size), f)
    m = cols >= 0
    w1[:, m] = w_in[:, cols[m]]
    C = gdn_consts()
    cwf = inp["gdn_conv_w"][0]
    chans = [np.arange(128) + (2 * c + qh) * 128 for qh in range(2)] + [2048 + np.arange(128) + (2 * c + qh) * 128 for qh in range(2)] \
        + [4096 + np.arange(128) + (4 * c + vh) * 128 for vh in range(4)]
    cw = np.stack([cwf[:, ch].T for ch in chans], axis=1)
    sel8 = lambda a: np.concatenate([a[0, 4 * c:4 * c + 4], a[1, 4 * c:4 * c + 4]])
    d = {
        "xT": np.ascontiguousarray(inp["x"][0, :n_tok].T), "w1": w1,
        "gain1": np.ascontiguousarray(inp["norm1_gain"][0].reshape(NCH, 128).T),
        "qg": np.ascontiguousarray(inp["q_norm_gain"][0].T), "kg": np.ascontiguousarray(inp["k_norm_gain"][0].T),
        "att_bias": np.stack([att_bias_consts(2 * c), att_bias_consts(2 * c + 1)]),
        "cw": np.ascontiguousarray(cw.astype(f)),
        "alog": np.ascontiguousarray(np.broadcast_to(sel8(inp["gdn_a_log"][0])[None, :], (128, 8)).astype(f)),
        "dtb": np.ascontiguousarray(np.broadcast_to(sel8(inp["gdn_dt_bias"][0])[None, :], (128, 8)).astype(f)),
        "ng": np.ascontiguousarray(np.broadcast_to(inp["gdn_norm_gain"][0][None, :], (128, 128)).astype(f)),
        "ident_f": C["ident_f"], "ident_b": C["ident_f"].astype(ml_dtypes.bfloat16), "ones_f": C["ones_f"],
        "mstrict": C["mstrict"], "mincl": C["mincl"], "cum": C["cum"], "sel": C["sel"], "selh": C["selh"],
    }
    return d


NT = 1024
NEXP = 64


def emit_l2a(nc, a, x2tm, h2b_d, wt_d):
    HT = 512
    xT_v = a["xT"].rearrange("(c p) t -> p c t", p=128)
    yA_v = a["yAT"].rearrange("(c p) t -> p c t", p=128)
    yB_v = a["yBT"].rearrange("(c p) t -> p c t", p=128)
    wgate_v = a["wgate"].rearrange("(c p) n -> p c n", p=128)
    wA_v = a["wA"].rearrange("(c p) n -> p c n", p=128)
    wB_v = a["wB"].rearrange("(c p) n -> p c n", p=128)
    wo_v = a["wo"].rearrange("(c p) n -> p c n", p=128)
    wr_v = a["wr"].rearrange("(c p) n -> p c n", p=128)
    h2b_v = h2b_d.rearrange("(c p) t -> p c t", p=128)
    with contextlib.ExitStack() as st:
        sb = lambda name, shape, dt: st.enter_context(nc.sbuf_tensor(name, shape, dt))
        ps = lambda name, shape, dt: st.enter_context(nc.psum_tensor(name, shape, dt))
        sc = Sched(nc, st)
        V = lambda fn, r, w: sc.op("dve", fn, reads=r, writes=w)
        A = lambda fn, r, w: sc.op("act", fn, reads=r, writes=w)
        G = lambda fn, r, w: sc.op("pool", fn, reads=r, writes=w)
        T = lambda fn, r, w: sc.op("pe", fn, reads=r, writes=w)
        ones = sb("b_ones", [128, 128], BF16); identf = sb("b_identf", [128, 128], F32)
        g1 = sb("b_g1", [128, NCH], F32); g2 = sb("b_g2", [128, NCH], F32)
        wr_sb = sb("b_wr", [128, NCH, 72], F32); br_sb = sb("b_br", [128, 72], F32)
        xf = sb("b_xf", [128, NCH, HT], F32); hb = sb("b_hb", [128, NCH, HT], BF16); xsq = sb("b_xsq", [128, NCH, HT], BF16)
        yA = sb("b_yA", [128, NCH, HT], BF16); bufY = sb("b_bufY", [128, 32 * HT * 2 // 4], F32)
        yB = bufY[:, :].bitcast(BF16).rearrange("p (c t) -> p c t", t=HT)
        stg = bufY[:, :].rearrange("p (k d) -> p k d", d=D)
        h2f = bufY[:, :].rearrange("p (c t) -> p c t", t=HT)
        merged = sb("b_merged", [128, NCH, HT], BF16)
        rstd = sb("b_rstd", [128, HT], F32)
        wgA = [sb("b_wgA%d" % i, [128, NCH, 128], BF16) for i in range(2)]; wgB = [sb("b_wgB%d" % i, [128, NCH, 128], BF16) for i in range(2)]
        wa = [sb("b_wa%d" % i, [128, NCH, 128], BF16) for i in range(2)]; wb_ = [sb("b_wb%d" % i, [128, 32, 128], BF16) for i in range(2)]
        wo = [sb("b_wo%d" % i, [128, NCH, 128], BF16) for i in range(2)]
        tA = sb("b_tA", [128, HT], F32); tB = sb("b_tB", [128, HT], F32); sA = sb("b_sA", [128, HT], F32); sBt = sb("b_sB", [128, HT], F32)
        rt = {n: sb("b_r_" + n, [128, 72], F32) for n in ("lg", "ge", "oh", "tmp", "ig", "m1k", "in2", "m2k", "wl")}
        rs_ = {n: sb("b_s_" + n, [128, 1], F32) for n in ("gmax", "ngmax", "gs", "gw", "m1", "m2", "d12", "w1", "w2")}
        wt_st = sb("b_wtst", [128, 4, 64], F32)
        ps_ss = ps("b_psss", [128, HT], F32)
        psGA = ps("b_psGA", [128, HT], F32); psGB = ps("b_psGB", [128, HT], F32); psMA = ps("b_psMA", [128, HT], F32); psMB = ps("b_psMB", [128, HT], F32)
        psT = ps("b_psT", [128, 4, 128], F32); psR = ps("b_psR", [128, 128], F32)

        G(lambda e: e.memset(ones[:, :], 1.0), [], ["ones"])
        sc.dma("sp", identf[:, :], a["ident_f"][:, :], "c1", writes=["identf"])
        sc.dma("sp", g1[:, :], a["gain1"][:, :], "c2", writes=["g1"])
        sc.dma("sp", g2[:, :], a["gain2"][:, :], "c3", writes=["g2"])
        sc.dma("sp", wr_sb[:, :, :], wr_v, "c4", writes=["wr"])
        sc.dma("sp", br_sb[:, :], a["br"][:, :], "c5", writes=["br"])

        def rms(tag):
            A(lambda e: e.activation(out=xsq[:, :, :], in_=xf[:, :, :], func=AF.Square), ["xf"], ["xsq"])
            for c in range(NCH):
                T(lambda e, c=c: e.matmul(ps_ss[:, :], ones[:, :], xsq[:, c, :], start=(c == 0), stop=(c == NCH - 1)), ["xsq", "ones"], ["ps_ss"])
            V(lambda e: e.tensor_scalar(rstd[:, :], ps_ss[:, :], 1.0 / D, EPS, ALU.mult, ALU.add), ["ps_ss"], ["rstd"])
            A(lambda e: e.activation(out=rstd[:, :], in_=rstd[:, :], func=AF.Sqrt), ["rstd"], ["rstd"])
            V(lambda e: e.reciprocal(rstd[:, :], rstd[:, :]), ["rstd"], ["rstd"])

        for hf in range(NT // HT):
            hs = slice(hf * HT, (hf + 1) * HT)
            sc.dma("sp", xf[:, :, :], xT_v[:, :, hs], "l_xf", writes=["xf"])
            sc.dma("sp", yA[:, :, :], yA_v[:, :, hs], "l_yA", writes=["yA"])
            sc.dma("sp", yB, yB_v[:, :, hs], "l_bufY", writes=["bufY"])
            rms("n1")
            for c in range(NCH):
                V(lambda e, c=c: e.tensor_scalar(hb[:, c, :], xf[:, c, :], g1[:, c:c + 1], None, ALU.mult), ["xf", "g1"], ["hb"])
            for j in range(NCH):
                jb = j % 2
                js = slice(j * 128, (j + 1) * 128)
                j2 = slice(2048 + j * 128, 2048 + (j + 1) * 128)
                sc.dma("pool", wgA[jb][:, :, :], wgate_v[:, :, js], ("wgA", jb), writes=[("wgA", jb)])
                sc.dma("pool", wgB[jb][:, :, :], wgate_v[:, :, j2], ("wgB", jb), writes=[("wgB", jb)])
                sc.dma("pool", wa[jb][:, :, :], wA_v[:, :, js], ("wa", jb), writes=[("wa", jb)])
                sc.dma("pool", wb_[jb][:, :, :], wB_v[:, :, js], ("wb", jb), writes=[("wb", jb)])
                for c in range(NCH):
                    T(lambda e, c=c, jb=jb: e.matmul(psGA[:, :], wgA[jb][:, c, :], hb[:, c, :], start=(c == 0), stop=(c == NCH - 1)), [("wgA", jb), "hb"], ["psGA"])
                for c in range(NCH):
                    T(lambda e, c=c, jb=jb: e.matmul(psGB[:, :], wgB[jb][:, c, :], hb[:, c, :], start=(c == 0), stop=(c == NCH - 1)), [("wgB", jb), "hb"], ["psGB"])
                for c in range(NCH):
                    T(lambda e, c=c, jb=jb: e.matmul(psMA[:, :], wa[jb][:, c, :], yA[:, c, :], start=(c == 0), stop=(c == NCH - 1)), [("wa", jb), "yA"], ["psMA"])
                for c in range(32):
                    T(lambda e, c=c, jb=jb: e.matmul(psMB[:, :], wb_[jb][:, c, :], yB[:, c, :], start=(c == 0), stop=(c == 31)), [("wb", jb), "bufY"], ["psMB"])
                V(lambda e: e.tensor_tensor(tA[:, :], psGA[:, :], rstd[:, :], ALU.mult), ["psGA", "rstd"], ["tA"])
                A(lambda e: e.activation(out=sA[:, :], in_=tA[:, :], func=AF.Sigmoid), ["tA"], ["sA"])
                V(lambda e: e.tensor_tensor(tB[:, :], psGB[:, :], rstd[:, :], ALU.mult), ["psGB", "rstd"], ["tB"])
                A(lambda e: e.activation(out=sBt[:, :], in_=tB[:, :], func=AF.Sigmoid), ["tB"], ["sB"])
                V(lambda e: e.tensor_tensor(tA[:, :], sA[:, :], psMA[:, :], ALU.mult), ["sA", "psMA", "tA"], ["tA"])
                V(lambda e: e.tensor_tensor(tB[:, :], sBt[:, :], psMB[:, :], ALU.mult), ["sB", "psMB", "tB"], ["tB"])
                V(lambda e, j=j: e.tensor_tensor(merged[:, j, :], tA[:, :], tB[:, :], ALU.add), ["tA", "tB"], ["merged"])
            for dch in range(NCH):
                db = dch % 2
                sc.dma("pool", wo[db][:, :, :], wo_v[:, :, dch * 128:(dch + 1) * 128], ("wo", db), writes=[("wo", db)])
                for c in range(NCH):
                    T(lambda e, c=c, db=db: e.matmul(psGA[:, :], wo[db][:, c, :], merged[:, c, :], start=(c == 0), stop=(c == NCH - 1)), [("wo", db), "merged"], ["psGA"])
                V(lambda e, dch=dch: e.tensor_tensor(xf[:, dch, :], xf[:, dch, :], psGA[:, :], ALU.add), ["psGA", "xf"], ["xf"])
            for k in range(4):
                for c in range(NCH):
                    T(lambda e, c=c, k=k: e.transpose(psT[:, c % 4, :], xf[:, c, k * 128:(k + 1) * 128], identf[:, :]), ["xf", "identf"], ["psT"])
                    if c % 4 == 3:
                        V(lambda e, c=c, k=k: e.tensor_copy(stg[:, k, (c - 3) * 128:(c + 1) * 128], psT[:, :, :].rearrange("p a b -> p (a b)")),
                          ["psT", "bufY"], ["bufY"])
            sc.dma("sp", x2tm[hs, :].rearrange("(k p) d -> p k d", p=128), stg, "s_bufY", reads=["bufY"], writes=["x2tm"])
            rms("n2")
            for c in range(NCH):
                V(lambda e, c=c: e.scalar_tensor_tensor(h2f[:, c, :], xf[:, c, :], g2[:, c:c + 1], rstd[:, :], ALU.mult, ALU.mult),
                  ["xf", "g2", "rstd", "bufY"], ["bufY"])
            A(lambda e: e.copy(hb[:, :, :], h2f), ["bufY", "hb"], ["hb"])
            sc.dma("sp", h2b_v[:, :, hs], hb[:, :, :], "s_h2b", reads=["hb"], writes=["h2b_d"])
            for k in range(4):
                for c in range(NCH):
                    T(lambda e, c=c, k=k: e.matmul(psR[:, 0:72], h2f[:, c, k * 128:(k + 1) * 128], wr_sb[:, c, :], start=(c == 0), stop=(c == NCH - 1)),
                      ["bufY", "wr"], ["psR"])
                r = rt; s_ = rs_
                V(lambda e: e.tensor_tensor(r["lg"][:, :], psR[:, 0:72], br_sb[:, :], ALU.add), ["psR", "br"], ["r_lg"])
                V(lambda e: e.reduce_max(s_["gmax"][:, :], r["lg"][:, 0:8], AX.X), ["r_lg"], ["s_gmax"])
                V(lambda e: e.tensor_scalar(s_["ngmax"][:, :], s_["gmax"][:, :], -1.0, None, ALU.mult), ["s_gmax"], ["s_ngmax"])
                A(lambda e: e.activation(out=r["ge"][:, 0:8], in_=r["lg"][:, 0:8], func=AF.Exp, bias=s_["ngmax"][:, 0:1]), ["r_lg", "s_ngmax"], ["r_ge"])
                V(lambda e: e.reduce_sum(s_["gs"][:, :], r["ge"][:, 0:8], AX.X), ["r_ge"], ["s_gs"])
                V(lambda e: e.reciprocal(s_["gw"][:, :], s_["gs"][:, :]), ["s_gs"], ["s_gw"])
                V(lambda e: e.tensor_scalar(r["oh"][:, 0:8], r["lg"][:, 0:8], s_["gmax"][:, 0:1], None, ALU.is_equal), ["r_lg", "s_gmax"], ["r_oh"])
                le = r["lg"][:, 8:72].rearrange("p (g j) -> p g j", j=8)
                V(lambda e, le=le: e.tensor_tensor(r["tmp"][:, 0:64].rearrange("p (g j) -> p g j", j=8), le,
                                                   r["oh"][:, 0:8].unsqueeze(2).to_broadcast([128, 8, 8]), ALU.mult), ["r_lg", "r_oh"], ["r_tmp"])
                V(lambda e: e.reduce_sum(r["ig"][:, 0:8], r["tmp"][:, 0:64].rearrange("p (g j) -> p j g", j=8), AX.X), ["r_tmp"], ["r_ig"])
                V(lambda e: e.reduce_max(s_["m1"][:, :], r["ig"][:, 0:8], AX.X), ["r_ig"], ["s_m1"])
                V(lambda e: e.tensor_scalar(r["m1k"][:, 0:8], r["ig"][:, 0:8], s_["m1"][:, 0:1], None, ALU.is_equal), ["r_ig", "s_m1"], ["r_m1k"])
                V(lambda e: e.scalar_tensor_tensor(r["in2"][:, 0:8], r["m1k"][:, 0:8], -1e30, r["ig"][:, 0:8], ALU.mult, ALU.add), ["r_m1k", "r_ig"], ["r_in2"])
                V(lambda e: e.reduce_max(s_["m2"][:, :], r["in2"][:, 0:8], AX.X), ["r_in2"], ["s_m2"])
                V(lambda e: e.tensor_scalar(r["m2k"][:, 0:8], r["in2"][:, 0:8], s_["m2"][:, 0:1], None, ALU.is_equal), ["r_in2", "s_m2"], ["r_m2k"])
                V(lambda e: e.tensor_tensor(s_["d12"][:, :], s_["m1"][:, :], s_["m2"][:, :], ALU.subtract), ["s_m1", "s_m2"], ["s_d12"])
                A(lambda e: e.activation(out=s_["w1"][:, :], in_=s_["d12"][:, :], func=AF.Sigmoid), ["s_d12"], ["s_w1"])
                A(lambda e: e.activation(out=s_["w2"][:, :], in_=s_["d12"][:, :], func=AF.Sigmoid, scale=-1.0), ["s_d12"], ["s_w2"])
                V(lambda e: e.tensor_scalar(r["wl"][:, 0:8], r["m1k"][:, 0:8], s_["w1"][:, 0:1], None, ALU.mult), ["r_m1k", "s_w1"], ["r_wl"])
                V(lambda e: e.scalar_tensor_tensor(r["wl"][:, 0:8], r["m2k"][:, 0:8], s_["w2"][:, 0:1], r["wl"][:, 0:8], ALU.mult, ALU.add), ["r_m2k", "s_w2", "r_wl"], ["r_wl"])
                V(lambda e: e.tensor_scalar(r["wl"][:, 0:8], r["wl"][:, 0:8], s_["gw"][:, 0:1], None, ALU.mult), ["r_wl", "s_gw"], ["r_wl"])
                V(lambda e, k=k: e.tensor_tensor(wt_st[:, k, :].rearrange("p (g j) -> p g j", j=8), r["oh"][:, 0:8].unsqueeze(2).to_broadcast([128, 8, 8]),
                                                 r["wl"][:, 0:8].unsqueeze(1).to_broadcast([128, 8, 8]), ALU.mult), ["r_oh", "r_wl", "wt_st"], ["wt_st"])
            sc.dma("sp", wt_d[hs, :].rearrange("(k p) e -> p k e", p=128), wt_st[:, :, :], "s_wt", reads=["wt_st"], writes=["wt_d"])
        sc.final_wait("sp", ["x2tm", "h2b_d", "wt_d", "bufY", "hb", "wt_st"])
        sc.run()


def emit_l2b(nc, a, x2tm, h2b_d, wt_d, out, n_exp=NEXP):
    HT = 512
    with contextlib.ExitStack() as st:
        sb = lambda name, shape, dt: st.enter_context(nc.sbuf_tensor(name, shape, dt))
        ps = lambda name, shape, dt: st.enter_context(nc.psum_tensor(name, shape, dt))
        sc = Sched(nc, st)
        V = lambda fn, r, w: sc.op("dve", fn, reads=r, writes=w)
        A = lambda fn, r, w: sc.op("act", fn, reads=r, writes=w)
        T = lambda fn, r, w: sc.op("pe", fn, reads=r, writes=w)
        acc = sb("m_acc", [128, 8, D], F32); h2b = sb("m_h2b", [128, NCH, NT], BF16); wt = sb("m_wt", [128, 8, 64], F32)
        wg = [sb("m_wg%d" % i, [128, NCH, 512], BF16) for i in range(2)]
        wu2 = [sb("m_wu%d" % i, [128, NCH, 512], BF16) for i in range(2)]; wd2 = [sb("m_wd%d" % i, [128, 4, D], BF16) for i in range(2)]
        act = sb("m_act", [128, 4, NT], BF16); slb = [sb("m_slb%d" % i, [128, HT], BF16) for i in range(2)]
        psG = [ps("m_psG%d" % i, [128, HT], F32) for i in range(2)]; psU = [ps("m_psU%d" % i, [128, HT], F32) for i in range(2)]
        psD = [ps("m_psD%d" % i, [128, 512], F32) for i in range(2)]
        sc.dma("sp", acc[:, :, :], x2tm.rearrange("(k p) d -> p k d", p=128), "l_acc", writes=["acc"])
        sc.dma("sp", h2b[:, :, :], h2b_d.rearrange("(c p) t -> p c t", p=128), "l_h2b", writes=["h2b"])
        sc.dma("sp", wt[:, :, :], wt_d.rearrange("(k p) e -> p k e", p=128), "l_wt", writes=["wt"])
        n = 0
        for e_ in range(n_exp):
            eb = e_ % 2
            sc.dma("pool", wg[eb][:, :, :], a["w_gate"][e_].rearrange("(c p) f -> p c f", p=128), ("wg", eb), writes=[("wg", eb)])
            wu, wd = wu2[eb], wd2[eb]
            sc.dma("pool", wu[:, :, :], a["w_up"][e_].rearrange("(c p) f -> p c f", p=128), ("wu", eb), writes=[("wu", eb)])
            sc.dma("pool", wd[:, :, :], a["w_down"][e_].rearrange("(c p) d -> p c d", p=128), ("wd", eb), writes=[("wd", eb)])
            for hf in range(NT // HT):
                hs = slice(hf * HT, (hf + 1) * HT)
                for fch in range(4):
                    pb = n % 2; n += 1
                    fs = slice(fch * 128, (fch + 1) * 128)
                    for c in range(NCH):
                        T(lambda e, c=c, eb=eb, fs=fs, hs=hs, pb=pb: e.matmul(psG[pb][:, :], wg[eb][:, c, fs], h2b[:, c, hs], start=(c == 0), stop=(c == NCH - 1)),
                          [("wg", eb), "h2b"], [("psG", pb)])
                    for c in range(NCH):
                        T(lambda e, wu=wu, c=c, fs=fs, hs=hs, pb=pb: e.matmul(psU[pb][:, :], wu[:, c, fs], h2b[:, c, hs], start=(c == 0), stop=(c == NCH - 1)),
                          [("wu", eb), "h2b"], [("psU", pb)])
                    A(lambda e, pb=pb: e.activation(out=slb[pb][:, :], in_=psG[pb][:, :], func=AF.Silu), [("psG", pb)], [("slb", pb)])
                    V(lambda e, pb=pb, fch=fch, hs=hs: e.tensor_tensor(act[:, fch, hs], slb[pb][:, :], psU[pb][:, :], ALU.mult), [("slb", pb), ("psU", pb)], ["act"])
            for k in range(8):
                for db in range(4):
                    pd = (k * 4 + db) % 2
                    ds_ = slice(db * 512, (db + 1) * 512)
                    for fch in range(4):
                        T(lambda e, wd=wd, k=k, fch=fch, ds_=ds_, pd=pd: e.matmul(psD[pd][:, :], act[:, fch, k * 128:(k + 1) * 128], wd[:, fch, ds_], start=(fch == 0), stop=(fch == 3)),
                          ["act", ("wd", eb)], [("psD", pd)])
                    V(lambda e, k=k, ds_=ds_, pd=pd, e_=e_: e.scalar_tensor_tensor(acc[:, k, ds_], psD[pd][:, :], wt[:, k, e_:e_ + 1], acc[:, k, ds_], ALU.mult, ALU.add),
                      [("psD", pd), "wt", "acc"], ["acc"])
        sc.dma("sp", out.rearrange("(k p) d -> p k d", p=128), acc[:, :, :], "s_out", reads=["acc"], writes=["out"])
        sc.final_wait("sp", ["out"])
        sc.run()


L2_IN = (("xT", [D, NT], F32), ("yAT", [2048, NT], BF16), ("yBT", [4096, NT], BF16), ("gain1", [128, NCH], F32), ("gain2", [128, NCH], F32),
         ("wgate", [D, 4096], F32), ("wA", [2048, D], F32), ("wB", [4096, D], F32), ("wo", [D, D], F32), ("wr", [D, 72], F32), ("br", [128, 72], F32),
         ("ident_f", [128, 128], F32), ("w_gate", [NEXP, D, 512], F32), ("w_up", [NEXP, D, 512], F32), ("w_down", [NEXP, 512, D], F32))


def build_launch2(n_exp=NEXP):
    nc = bass.Bass("TRN2", target_bir_lowering=False)
    a = {name: nc.dram_tensor(name, shape, dt, kind="ExternalInput").ap() for (name, shape, dt) in L2_IN}
    out = nc.dram_tensor("out", [NT, D], F32, kind="ExternalOutput").ap()
    x2tm = nc.dram_tensor("x2tm_scr", [NT, D], F32).ap()
    h2b_d = nc.dram_tensor("h2b_scr", [D, NT], BF16).ap()
    wt_d = nc.dram_tensor("wt_scr", [NT, 64], F32).ap()
    emit_l2a(nc, a, x2tm, h2b_d, wt_d)
    emit_l2b(nc, a, x2tm, h2b_d, wt_d, out, n_exp)
    return nc


def launch2_inputs(inp, c, yAT, yBT, shared):
    ts = slice(c * NT, (c + 1) * NT)
    d = dict(shared)
    d["xT"] = np.ascontiguousarray(inp["x"][0, ts].T)
    d["yAT"] = np.ascontiguousarray(yAT[:, ts]); d["yBT"] = np.ascontiguousarray(yBT[:, ts])
    return d


def launch2_shared(inp):
    f = np.float32
    return {
        "gain1": np.ascontiguousarray(inp["norm1_gain"][0].reshape(NCH, 128).T), "gain2": np.ascontiguousarray(inp["norm2_gain"][0].reshape(NCH, 128).T),
        "wgate": np.ascontiguousarray(inp["w_in"][0][:, OFF_GA:OFF_GA + 4096]), "wA": inp["w_branch_att"][0], "wB": inp["w_branch_gdn"][0], "wo": inp["w_out"][0],
        "wr": np.ascontiguousarray(np.concatenate([inp["w_group_router"][0], inp["w_expert_router"][0]], axis=1)),
        "br": np.ascontiguousarray(np.broadcast_to(np.concatenate([inp["b_group_router"][0], inp["b_expert_router"][0]])[None, :], (128, 72)).astype(f)),
        "ident_f": np.eye(128, dtype=f), "w_gate": inp["w_gate"][0], "w_up": inp["w_up"][0], "w_down": inp["w_down"][0],
    }


def kernel(**inputs):
    inp = {k: np.asarray(v) for k, v in inputs.items()}
    cores = list(range(NCORES))
    nc1 = build_launch1(S)
    res1 = run_bass_kernel_spmd(nc1, [launch1_inputs(inp, c) for c in cores], core_ids=cores).results
    yAT = np.concatenate([np.asarray(res1[c]["yatt"]).reshape(256, S) for c in cores], axis=0)
    yBT = np.concatenate([np.ascontiguousarray(np.asarray(res1[c]["yg"]).T) for c in cores], axis=0)
    del res1
    nc2 = build_launch2()
    shared = launch2_shared(inp)
    res2 = run_bass_kernel_spmd(nc2, [launch2_inputs(inp, c, yAT, yBT, shared) for c in cores], core_ids=cores).results
    out = np.concatenate([np.asarray(res2[c]["out"]) for c in cores], axis=0)
    return out.reshape(1, S, D).astype(np.float32)
```
